# Optimizing a Trainium2 kernel written in Bass

```python
import math
import jax, jax.numpy as jnp
from jax import lax
import numpy as np

D_MODEL = 1024
BATCH = 8
SEQ = 4096
DEPTH = 1

DA_HEADS = 8
DA_DH = 64
DA_QK = DA_HEADS * 2 * DA_DH
DA_V = DA_HEADS * 2 * DA_DH
DA_QBLOCK = 128
ROPE_THETA = 10000.0

GDN_HEADS = 8
GDN_DK = 128
GDN_DV = 128
GDN_QK = GDN_HEADS * GDN_DK
GDN_VW = GDN_HEADS * GDN_DV
GDN_CONV_DIM = 2 * GDN_QK + GDN_VW
CONV_K = 5
GDN_CHUNK = 64

N_EXPERTS = 16
EXPERT_FF = 2048
CAP_FACTOR = 2

NORM_EPS = 1e-6
IN_WIDTH = 2 * DA_QK + DA_V + GDN_CONV_DIM + GDN_VW + 4 * GDN_HEADS + 2 * D_MODEL

kernel_name = "hybrid_diffattn_gdn_ecmoe_encoder"


def rmsnorm(t, w, eps=NORM_EPS):
    t32 = t.astype(jnp.float32)
    y = t32 * lax.rsqrt(jnp.mean(t32 * t32, axis=-1, keepdims=True) + eps)
    return (y * w.astype(jnp.float32)).astype(t.dtype)


def l2norm(t, eps=1e-6):
    t32 = t.astype(jnp.float32)
    return t32 * lax.rsqrt(jnp.sum(t32 * t32, axis=-1, keepdims=True) + eps)


def rope_tables(seq, dim):
    inv = ROPE_THETA ** (-jnp.arange(0, dim, 2, dtype=jnp.float32) / dim)
    ang = jnp.arange(seq, dtype=jnp.float32)[:, None] * inv[None, :]
    ang = jnp.concatenate([ang, ang], axis=-1)
    return jnp.cos(ang), jnp.sin(ang)


def apply_rope(t, cos, sin):
    half = t.shape[-1] // 2
    t32 = t.astype(jnp.float32)
    rot = jnp.concatenate([-t32[..., half:], t32[..., :half]], axis=-1)
    shp = (1, t.shape[1]) + (1,) * (t.ndim - 3) + (t.shape[-1],)
    return (t32 * cos.reshape(shp) + rot * sin.reshape(shp)).astype(t.dtype)


def diff_attention(q, k, v, lam):
    b, s, h, _, dh = q.shape
    nb = s // DA_QBLOCK
    scale = dh ** -0.5
    qb = jnp.moveaxis(q.reshape(b, nb, DA_QBLOCK, h, 2, dh), 1, 0)

    def block(qblk):
        sc = jnp.einsum('bqhtd,bkhtd->bhtqk', qblk, k).astype(jnp.float32) * scale
        pr = jax.nn.softmax(sc, axis=-1)
        pd = pr[:, :, 0] - lam * pr[:, :, 1]
        return jnp.einsum('bhqk,bkhe->bqhe', pd.astype(v.dtype), v)

    o = lax.map(block, qb)
    return jnp.moveaxis(o, 0, 1).reshape(b, s, h, v.shape[-1])


def gated_delta_rule_chunked(q, k, v, g, beta):
    b, s, h, dk = q.shape
    dv = v.shape[-1]
    n = s // GDN_CHUNK

    def chunks(t):
        t = t.astype(jnp.float32).reshape((b, n, GDN_CHUNK, h) + t.shape[3:])
        return jnp.moveaxis(t, 3, 1)

    q = chunks(q) * dk ** -0.5
    k = chunks(k)
    v = chunks(v)
    g = jnp.cumsum(chunks(g), axis=-1)
    beta = chunks(beta)

    incl = jnp.tril(jnp.ones((GDN_CHUNK, GDN_CHUNK), dtype=bool))
    strict = jnp.tril(jnp.ones((GDN_CHUNK, GDN_CHUNK), dtype=bool), k=-1)
    gdiff = g[..., :, None] - g[..., None, :]
    decay = jnp.where(incl, jnp.exp(jnp.where(incl, gdiff, 0.0)), 0.0)

    k_beta = k * beta[..., None]
    a_mat = jnp.where(strict, jnp.einsum('bhnid,bhnjd->bhnij', k_beta, k) * decay, 0.0)
    t_mat = a_mat + jnp.eye(GDN_CHUNK, dtype=jnp.float32)
    rhs = jnp.concatenate([v * beta[..., None], k_beta * jnp.exp(g)[..., None]], axis=-1)
    sol = lax.linalg.triangular_solve(t_mat, rhs, left_side=True, lower=True,
                                      unit_diagonal=True)
    u, w = sol[..., :dv], sol[..., dv:]

    qk = jnp.where(incl, jnp.einsum('bhnid,bhnjd->bhnij', q, k) * decay, 0.0)
    q_dec = q * jnp.exp(g)[..., None]
    k_dec = k * jnp.exp(g[..., -1:] - g)[..., None]
    g_last = jnp.exp(g[..., -1])

    def step(state, inp):
        qk_c, qd_c, kd_c, u_c, w_c, gl_c = inp
        v_new = u_c - jnp.einsum('bhcd,bhde->bhce', w_c, state)
        o_c = (jnp.einsum('bhcd,bhde->bhce', qd_c, state)
               + jnp.einsum('bhij,bhje->bhie', qk_c, v_new))
        state = state * gl_c[..., None, None] + jnp.einsum('bhcd,bhce->bhde', kd_c, v_new)
        return state, o_c

    xs = tuple(jnp.moveaxis(t, 2, 0) for t in (qk, q_dec, k_dec, u, w, g_last))
    state0 = jnp.zeros((b, h, dk, dv), jnp.float32)
    _, o = lax.scan(step, state0, xs)
    o = jnp.moveaxis(o, 0, 2)
    return jnp.moveaxis(o, 1, 3).reshape(b, s, h, dv)


def centred_depthwise_conv(t, w):
    c = t.shape[-1]
    pad = CONV_K // 2
    return lax.conv_general_dilated(
        t, w[:, None, :].astype(t.dtype), window_strides=(1,), padding=[(pad, pad)],
        dimension_numbers=('NWC', 'WIO', 'NWC'), feature_group_count=c)


def expert_choice_ffn(h, w_router, w_gate, w_up, w_down):
    b, s, d = h.shape
    cap = CAP_FACTOR * s // N_EXPERTS
    logits = jnp.einsum('bsd,de->bse', h, w_router).astype(jnp.float32)
    aff = jax.nn.softmax(logits, axis=-1)
    top_val, top_idx = lax.top_k(jnp.swapaxes(aff, 1, 2), cap)
    xs = jax.vmap(lambda hb, ib: hb[ib])(h, top_idx)
    gt = jnp.einsum('becd,edf->becf', xs, w_gate)
    up = jnp.einsum('becd,edf->becf', xs, w_up)
    ye = jnp.einsum('becf,efd->becd', jax.nn.silu(gt) * up, w_down)
    ye = ye * top_val[..., None].astype(ye.dtype)

    def scatter(yb, ib):
        return jnp.zeros((s, d), yb.dtype).at[ib.reshape(-1)].add(yb.reshape(-1, d))

    return jax.vmap(scatter)(ye, top_idx)


def setup_inputs(seed: int = 0) -> dict:
    key = jax.random.key(seed)
    ks = jax.random.split(key, 32)
    f32 = jnp.float32

    def nrm(k, shape, scale):
        return jax.random.normal(k, shape, f32) * scale

    def gain(k, n):
        return 1.0 + 0.02 * jax.random.normal(k, (DEPTH, n), f32)

    def a_log(k):
        return jnp.log(jax.random.uniform(k, (DEPTH, GDN_HEADS), f32, 1.0, 16.0))

    def dt_bias(k):
        dt = jnp.exp(jax.random.uniform(k, (DEPTH, GDN_HEADS), f32,
                                        math.log(1e-3), math.log(1e-1)))
        return dt + jnp.log(-jnp.expm1(-dt))

    return {
        "x": nrm(ks[0], (BATCH, SEQ, D_MODEL), 1.0),
        "norm1_w": gain(ks[1], D_MODEL),
        "w_in": nrm(ks[2], (DEPTH, D_MODEL, IN_WIDTH), D_MODEL ** -0.5),
        "conv_w": nrm(ks[3], (DEPTH, CONV_K, GDN_CONV_DIM), CONV_K ** -0.5),
        "a_log_fwd": a_log(ks[4]),
        "dt_bias_fwd": dt_bias(ks[5]),
        "a_log_bwd": a_log(ks[6]),
        "dt_bias_bwd": dt_bias(ks[7]),
        "gdn_norm_w": gain(ks[8], GDN_DV),
        "lambda_q1": nrm(ks[9], (DEPTH, DA_DH), 0.1),
        "lambda_k1": nrm(ks[10], (DEPTH, DA_DH), 0.1),
        "lambda_q2": nrm(ks[11], (DEPTH, DA_DH), 0.1),
        "lambda_k2": nrm(ks[12], (DEPTH, DA_DH), 0.1),
        "subln_w": gain(ks[13], 2 * DA_DH),
        "w_proj_attn": nrm(ks[14], (DEPTH, DA_V, D_MODEL), DA_V ** -0.5),
        "w_proj_gdn": nrm(ks[15], (DEPTH, GDN_VW, D_MODEL), GDN_VW ** -0.5),
        "w_out": nrm(ks[16], (DEPTH, D_MODEL, D_MODEL), D_MODEL ** -0.5),
        "norm2_w": gain(ks[17], D_MODEL),
        "w_router": nrm(ks[18], (DEPTH, D_MODEL, N_EXPERTS), D_MODEL ** -0.5),
        "w_gate": nrm(ks[19], (DEPTH, N_EXPERTS, D_MODEL, EXPERT_FF), D_MODEL ** -0.5),
        "w_up": nrm(ks[20], (DEPTH, N_EXPERTS, D_MODEL, EXPERT_FF), D_MODEL ** -0.5),
        "w_down": nrm(ks[21], (DEPTH, N_EXPERTS, EXPERT_FF, D_MODEL), EXPERT_FF ** -0.5),
        "norm_f_w": 1.0 + 0.02 * jax.random.normal(ks[22], (D_MODEL,), f32),
    }


def reference(x, norm1_w, w_in, conv_w, a_log_fwd, dt_bias_fwd, a_log_bwd, dt_bias_bwd,
              gdn_norm_w, lambda_q1, lambda_k1, lambda_q2, lambda_k2, subln_w,
              w_proj_attn, w_proj_gdn, w_out, norm2_w, w_router, w_gate, w_up, w_down,
              norm_f_w):
    b, s, _ = x.shape
    cos, sin = rope_tables(s, DA_DH)
    sizes = [DA_QK, DA_QK, DA_V, GDN_CONV_DIM, GDN_VW,
             GDN_HEADS, GDN_HEADS, GDN_HEADS, GDN_HEADS, D_MODEL, D_MODEL]
    split_at = [int(v) for v in np.cumsum(sizes)[:-1]]

    for l in range(DEPTH):
        lambda_init = 0.8 - 0.6 * math.exp(-0.3 * l)
        h = rmsnorm(x, norm1_w[l])
        p = jnp.einsum('bsd,dn->bsn', h, w_in[l])
        (qa, ka, va, qkv_g, z, a_f, a_b, b_f, b_b, gate_a, gate_g) = jnp.split(p, split_at, axis=-1)

        qa = apply_rope(qa.reshape(b, s, DA_HEADS, 2, DA_DH), cos, sin)
        ka = apply_rope(ka.reshape(b, s, DA_HEADS, 2, DA_DH), cos, sin)
        va = va.reshape(b, s, DA_HEADS, 2 * DA_DH)
        lam = (jnp.exp(jnp.sum(lambda_q1[l].astype(jnp.float32) * lambda_k1[l].astype(jnp.float32)))
               - jnp.exp(jnp.sum(lambda_q2[l].astype(jnp.float32) * lambda_k2[l].astype(jnp.float32)))
               + lambda_init)
        oa = diff_attention(qa, ka, va, lam)
        oa = rmsnorm(oa, subln_w[l]) * (1.0 - lambda_init)
        ya = jnp.einsum('bse,ed->bsd', oa.reshape(b, s, DA_V).astype(x.dtype), w_proj_attn[l])

        qkv_g = jax.nn.silu(centred_depthwise_conv(qkv_g, conv_w[l]))
        qg, kg, vg = jnp.split(qkv_g, [GDN_QK, 2 * GDN_QK], axis=-1)
        qg = l2norm(qg.reshape(b, s, GDN_HEADS, GDN_DK))
        kg = l2norm(kg.reshape(b, s, GDN_HEADS, GDN_DK))
        vg = vg.reshape(b, s, GDN_HEADS, GDN_DV).astype(jnp.float32)
        g_f = -jnp.exp(a_log_fwd[l].astype(jnp.float32)) * jax.nn.softplus(
            (a_f + dt_bias_fwd[l]).astype(jnp.float32))
        g_b = -jnp.exp(a_log_bwd[l].astype(jnp.float32)) * jax.nn.softplus(
            (a_b + dt_bias_bwd[l]).astype(jnp.float32))
        beta_f = jax.nn.sigmoid(b_f.astype(jnp.float32))
        beta_b = jax.nn.sigmoid(b_b.astype(jnp.float32))
        o_fwd = gated_delta_rule_chunked(qg, kg, vg, g_f, beta_f)
        flip = lambda t: jnp.flip(t, axis=1)
        o_bwd = flip(gated_delta_rule_chunked(flip(qg), flip(kg), flip(vg), flip(g_b), flip(beta_b)))
        og = rmsnorm(o_fwd + o_bwd, gdn_norm_w[l])
        og = og * jax.nn.silu(z.reshape(b, s, GDN_HEADS, GDN_DV).astype(jnp.float32))
        yg = jnp.einsum('bse,ed->bsd', og.reshape(b, s, GDN_VW).astype(x.dtype), w_proj_gdn[l])

        merged = jax.nn.sigmoid(gate_a) * ya + jax.nn.sigmoid(gate_g) * yg
        x = x + jnp.einsum('bsd,de->bse', merged, w_out[l])

        h2 = rmsnorm(x, norm2_w[l])
        x = x + expert_choice_ffn(h2, w_router[l], w_gate[l], w_up[l], w_down[l])

    return rmsnorm(x, norm_f_w)
```

```python
import math
from contextlib import ExitStack
import numpy as np
import concourse.bass as bass
import concourse.mybir as mybir
from concourse.bass_utils import run_bass_kernel_spmd

F32 = mybir.dt.float32
BF16 = mybir.dt.bfloat16
AF = mybir.ActivationFunctionType
ALU = mybir.AluOpType

S_LEN = 4096
D = 1024
NT = S_LEN // 128
NG = S_LEN // 512
H = 8
IN_W = 9248
C_QA, C_KA, C_VA, C_GQ, C_GK, C_GV, C_Z, C_SM, C_GA, C_GG = 0, 1024, 2048, 3072, 4096, 5120, 6144, 7168, 7200, 8224
NE = 16
FF = 2048
CAP = 512
EPS = 1e-6
LAMBDA_INIT = 0.8 - 0.6 * math.exp(-0.3 * 0)

ENGS = ("pe", "act", "dve", "pool", "sp")


class Tok:
    __slots__ = ("w", "r", "excl")

    def __init__(self, excl=False):
        self.w = None
        self.r = {}
        self.excl = excl


class Sched:
    NDMA = 32

    def __init__(self, nc):
        self.nc = nc
        self.sems = {}
        for e in ENGS:
            self.sems[e] = nc.alloc_semaphore(name="sem_" + e)
        for i in range(self.NDMA):
            self.sems[("d", i)] = nc.alloc_semaphore(name="sem_dma%d" % i)
        self.cnt = {k: 0 for k in self.sems}
        self.seen = {e: {} for e in ENGS}
        self.prog = {e: [] for e in ENGS}
        self.ndma = 0
        self.ninstr = 0

    def _deps(self, reads, writes):
        deps = {}
        for t in reads:
            if t.w is not None:
                k, v = t.w
                if deps.get(k, 0) < v:
                    deps[k] = v
        for t in writes:
            if t.w is not None:
                k, v = t.w
                if deps.get(k, 0) < v:
                    deps[k] = v
            for k, v in t.r.items():
                if deps.get(k, 0) < v:
                    deps[k] = v
        return deps

    def _emit_waits(self, e, deps):
        seen = self.seen[e]
        for k, v in deps.items():
            if seen.get(k, 0) >= v:
                continue
            seen[k] = v
            sem = self.sems[k]
            self.prog[e].append(lambda eng, sem=sem, v=v: eng.wait_ge(sem, v))

    def op(self, e, fn, reads=(), writes=()):
        if any(t.excl for t in reads):
            writes = list(writes) + [t for t in reads if t.excl]
            reads = [t for t in reads if not t.excl]
        deps = self._deps(reads, writes)
        if e == "pe":
            deps.pop("pe", None)
        self._emit_waits(e, deps)
        sem = self.sems[e]
        self.cnt[e] += 1
        n = self.cnt[e]
        self.prog[e].append(lambda eng, fn=fn, sem=sem: fn(eng).then_inc(sem, 1))
        for t in reads:
            if t.r.get(e, 0) < n:
                t.r[e] = n
        for t in writes:
            t.w = (e, n)
            t.r = {}
        self.ninstr += 1

    def dma(self, e, fn, reads=(), writes=()):
        i = self.ndma % self.NDMA
        self.ndma += 1
        k = ("d", i)
        deps = self._deps(reads, writes)
        if self.cnt[k] > 0:
            deps[k] = max(deps.get(k, 0), self.cnt[k])
        self._emit_waits(e, deps)
        self.cnt[k] += 16
        v = self.cnt[k]
        sem = self.sems[k]
        self.prog[e].append(lambda eng, fn=fn, sem=sem: fn(eng).then_inc(sem, 16))
        for t in reads:
            if t.r.get(k, 0) < v:
                t.r[k] = v
        for t in writes:
            t.w = (k, v)
            t.r = {}
        self.ninstr += 1

    def barrier(self):
        deps = {k: v for k, v in self.cnt.items() if v > 0}
        for e in ENGS:
            self._emit_waits(e, dict(deps))

    def emit(self):
        nc = self.nc
        prog = self.prog
        with nc.Block() as block:
            @block.tensor
            def _(eng):
                for f in prog["pe"]:
                    f(eng)

            @block.scalar
            def _(eng):
                for f in prog["act"]:
                    f(eng)

            @block.vector
            def _(eng):
                for f in prog["dve"]:
                    f(eng)

            @block.gpsimd
            def _(eng):
                for f in prog["pool"]:
                    f(eng)

            @block.sync
            def _(eng):
                for f in prog["sp"]:
                    f(eng)


class Ring:
    def __init__(self, items):
        self.items = items
        self.i = 0

    def next(self):
        it = self.items[self.i % len(self.items)]
        self.i += 1
        return it


class Ctx:
    pass


def _mk_ring(stack, nc, name, n, shape, dt, psum=False):
    items = []
    for i in range(n):
        if psum:
            t = stack.enter_context(nc.psum_tensor("%s%d" % (name, i), shape, dt))
        else:
            t = stack.enter_context(nc.sbuf_tensor("%s%d" % (name, i), shape, dt))
        items.append((t, Tok()))
    return Ring(items)


def _consts(c, stack):
    nc, S = c.nc, c.S
    c.ident_f = nc.alloc_sbuf_tensor("ident_f", [128, 128], F32)
    c.ident_b = nc.alloc_sbuf_tensor("ident_b", [128, 128], BF16)
    c.ones_b = nc.alloc_sbuf_tensor("ones_b", [128, 128], BF16)
    c.ones_f = nc.alloc_sbuf_tensor("ones_f", [128, 128], F32)
    c.t_const = Tok()
    tc = c.t_const
    S.op("pool", lambda e: e.memset(c.ident_f[:], 0.0), writes=[tc])
    S.op("pool", lambda e: e.affine_select(out=c.ident_f[:], in_=c.ident_f[:], compare_op=ALU.not_equal, fill=1.0,
                                           base=0, pattern=[[-1, 128]], channel_multiplier=1),
         reads=[tc], writes=[tc])
    S.op("pool", lambda e: e.tensor_copy(c.ident_b[:], c.ident_f[:]), reads=[tc], writes=[tc])
    S.op("pool", lambda e: e.memset(c.ones_b[:], 1.0), writes=[tc])
    S.op("pool", lambda e: e.memset(c.ones_f[:], 1.0), writes=[tc])


def phase1(c):
    nc, S = c.nc, c.S
    TC = c.t_const
    with ExitStack() as st:
        sb = lambda name, shape, dt: st.enter_context(nc.sbuf_tensor(name, shape, dt))
        hT = sb("hT", [128, 8, S_LEN], BF16)
        t_hT = [Tok() for _ in range(NT)]
        stA = ExitStack()
        sbA = lambda name, shape, dt: stA.enter_context(nc.sbuf_tensor(name, shape, dt))
        n1w = sbA("n1w", [128, D], F32)
        t_n1w = Tok()
        S.dma("sp", lambda e: e.dma_start(out=n1w[:], in_=c.norm1_w.broadcast_to([128, D])), writes=[t_n1w])
        xring = _mk_ring(stA, nc, "p1x", 3, [128, D], F32)
        sqring = _mk_ring(stA, nc, "p1sq", 2, [128, D], BF16)
        hring = _mk_ring(stA, nc, "p1h", 2, [128, D], BF16)
        ssring = _mk_ring(stA, nc, "p1ss", 4, [128, 2], F32)
        pst = _mk_ring(st, nc, "p1pst", 2, [128, 8, 128], BF16, psum=True)
        psm = _mk_ring(st, nc, "p1psm", 5, [128, 512], F32, psum=True)

        for i in range(NT):
            xt, t_x = xring.next()
            S.dma("sp", lambda e, xt=xt, i=i: e.dma_start(out=xt[:], in_=c.x[i * 128:(i + 1) * 128, :]), writes=[t_x])
            sq, t_sq = sqring.next()
            ss, t_ss = ssring.next()
            S.op("act", lambda e, xt=xt, sq=sq, ss=ss: e.activation(sq[:], xt[:], AF.Square, accum_out=ss[:, 0:1]),
                 reads=[t_x], writes=[t_sq, t_ss])
            S.op("act", lambda e, ss=ss: e.activation(ss[:, 1:2], ss[:, 0:1], AF.Sqrt, bias=EPS, scale=1.0 / D),
                 reads=[t_ss], writes=[t_ss])
            S.op("dve", lambda e, ss=ss: e.reciprocal(ss[:, 1:2], ss[:, 1:2]), reads=[t_ss], writes=[t_ss])
            ht, t_h = hring.next()
            S.op("dve", lambda e, ht=ht, xt=xt, ss=ss: e.scalar_tensor_tensor(
                out=ht[:], in0=xt[:], scalar=ss[:, 1:2], in1=n1w[:], op0=ALU.mult, op1=ALU.mult),
                reads=[t_x, t_ss, t_n1w], writes=[t_h])
            pt, t_pt = pst.next()
            for dc in range(8):
                S.op("pe", lambda e, pt=pt, ht=ht, dc=dc: e.transpose(pt[:, dc, :], ht[:, dc * 128:(dc + 1) * 128], c.ident_b[:]),
                     reads=[t_h, TC], writes=[t_pt])
            S.op("act", lambda e, pt=pt, i=i: e.copy(hT[:, :, i * 128:(i + 1) * 128], pt[:]),
                 reads=[t_pt], writes=[t_hT[i]])

        S.barrier()
        stA.close()
        wfm = _mk_ring(st, nc, "p1wfm", 4, [128, 8, 128], BF16)
        wtm = _mk_ring(st, nc, "p1wtm", 2, [128, 8, 512], BF16)

        def load_w(ring, src, col0, ncols):
            wt, t_w = ring.next()
            S.dma("pool", lambda e: e.dma_start(
                out=wt[:, :, 0:ncols], in_=src[:, col0:col0 + ncols].rearrange("(kc p) n -> p kc n", p=128)),
                writes=[t_w])
            return wt, t_w

        def proj_fm(wt, t_w, g):
            ps, t_ps = psm.next()
            for kc in range(8):
                S.op("pe", lambda e, ps=ps, kc=kc: e.matmul(ps[:], wt[:, kc, :], hT[:, kc, g * 512:(g + 1) * 512],
                                                          start=(kc == 0), stop=(kc == 7)),
                     reads=[t_w] + t_hT[g * 4:(g + 1) * 4], writes=[t_ps])
            return ps, t_ps

        def proj_tm(wt, t_w, i, ncols):
            ps, t_ps = psm.next()
            for kc in range(8):
                S.op("pe", lambda e, ps=ps, kc=kc: e.matmul(ps[:, 0:ncols], hT[:, kc, i * 128:(i + 1) * 128], wt[:, kc, 0:ncols],
                                                          start=(kc == 0), stop=(kc == 7)),
                     reads=[t_w, t_hT[i]], writes=[t_ps])
            return ps, t_ps

        stage = _mk_ring(st, nc, "p1stage", 2, [128, S_LEN], BF16)
        f32a = _mk_ring(st, nc, "p1f32a", 3, [128, 512], F32)
        f32b = _mk_ring(st, nc, "p1f32b", 3, [128, 512], F32)

        stB = ExitStack()
        cosT = stB.enter_context(nc.sbuf_tensor("cosT_sb", [128, S_LEN], F32))
        sinT = stB.enter_context(nc.sbuf_tensor("sinT_sb", [128, S_LEN], F32))
        t_rope = Tok()
        S.dma("sp", lambda e: e.dma_start(out=cosT[:], in_=c.cosT), writes=[t_rope])
        S.dma("sp", lambda e: e.dma_start(out=sinT[:], in_=c.sinT), writes=[t_rope])

        for (col0, dst) in ((C_QA, c.qT_d), (C_KA, c.kT_d)):
            for h in range(H):
                w1, t_w1 = load_w(wfm, c.w_in, col0 + h * 128, 128)
                w2, t_w2 = load_w(wfm, c.w_qkp, (col0 // 1024) * 1024 + h * 128, 128)
                stg, t_stg = stage.next()
                for g in range(NG):
                    ps1, t_ps1 = proj_fm(w1, t_w1, g)
                    ps2, t_ps2 = proj_fm(w2, t_w2, g)
                    a, t_a = f32a.next()
                    b, t_b = f32b.next()
                    sl = slice(g * 512, (g + 1) * 512)
                    S.op("dve", lambda e, a=a, ps1=ps1, sl=sl: e.tensor_tensor(out=a[:], in0=ps1[:], in1=cosT[:, sl], op=ALU.mult),
                         reads=[t_ps1, t_rope], writes=[t_a])
                    S.op("dve", lambda e, b=b, ps2=ps2, sl=sl: e.tensor_tensor(out=b[:], in0=ps2[:], in1=sinT[:, sl], op=ALU.mult),
                         reads=[t_ps2, t_rope], writes=[t_b])
                    S.op("pool", lambda e, a=a, b=b, stg=stg, sl=sl: e.tensor_tensor(out=stg[:, sl], in0=a[:], in1=b[:], op=ALU.add),
                         reads=[t_a, t_b], writes=[t_stg])
                S.dma("sp", lambda e, stg=stg, dst=dst, h=h: e.dma_start(out=dst[h], in_=stg[:]), reads=[t_stg], writes=[c.t_qk])

        S.barrier()
        stB.close()
        for (col0, dst) in ((C_GA, c.sga_d), (C_GG, c.sgg_d)):
            for h in range(8):
                w1, t_w1 = load_w(wfm, c.w_in, col0 + h * 128, 128)
                stg, t_stg = stage.next()
                for g in range(NG):
                    ps1, t_ps1 = proj_fm(w1, t_w1, g)
                    sl = slice(g * 512, (g + 1) * 512)
                    S.op("act", lambda e, ps1=ps1, stg=stg, sl=sl: e.activation(stg[:, sl], ps1[:], AF.Sigmoid),
                         reads=[t_ps1], writes=[t_stg])
                S.dma("sp", lambda e, stg=stg, dst=dst, h=h: e.dma_start(out=dst[h], in_=stg[:]), reads=[t_stg], writes=[c.t_gates])

        tmst = _mk_ring(st, nc, "p1tmst", 3, [128, 512], BF16)
        for (col0, dst, fn) in ((C_VA, c.v_d, None), (C_Z, c.zs_d, AF.Silu)):
            for half in range(2):
                wt, t_w = load_w(wtm, c.w_in, col0 + half * 512, 512)
                for i in range(NT):
                    ps, t_ps = proj_tm(wt, t_w, i, 512)
                    o, t_o = tmst.next()
                    if fn is None:
                        S.op("act", lambda e, o=o, ps=ps: e.copy(o[:], ps[:]), reads=[t_ps], writes=[t_o])
                    else:
                        S.op("act", lambda e, o=o, ps=ps, fn=fn: e.activation(o[:], ps[:], fn), reads=[t_ps], writes=[t_o])
                    S.dma("sp", lambda e, o=o, dst=dst, i=i, half=half: e.dma_start(
                        out=dst[i * 128:(i + 1) * 128, half * 512:(half + 1) * 512], in_=o[:]), reads=[t_o], writes=[c.t_vz])

        sm = sb("p1sm", [128, NT, 32], F32)
        t_sm = Tok()
        wt, t_w = load_w(wtm, c.w_in, C_SM, 32)
        for i in range(NT):
            ps, t_ps = proj_tm(wt, t_w, i, 32)
            S.op("dve", lambda e, ps=ps, i=i: e.tensor_copy(sm[:, i, :], ps[:, 0:32]), reads=[t_ps], writes=[t_sm])
        prm = sb("p1prm", [128, 32], F32)
        t_prm = Tok()
        for j, src in enumerate((c.dt_bias_fwd, c.dt_bias_bwd, c.a_log_fwd, c.a_log_bwd)):
            S.dma("sp", lambda e, j=j, src=src: e.dma_start(out=prm[:, j * 8:(j + 1) * 8], in_=src.broadcast_to([128, 8])),
                  writes=[t_prm])
        tmpa = sb("p1tmpa", [128, NT, 16], F32)
        tmpb = sb("p1tmpb", [128, NT, 16], F32)
        t_ta, t_tb = Tok(), Tok()
        dtb = prm[:, 0:16].unsqueeze(1).broadcast_to([128, NT, 16])
        S.op("act", lambda e: e.activation(prm[:, 16:32], prm[:, 16:32], AF.Exp), reads=[t_prm], writes=[t_prm])
        nA = prm[:, 16:32].unsqueeze(1).broadcast_to([128, NT, 16])
        S.op("dve", lambda e: e.tensor_tensor(out=sm[:, :, 0:16], in0=sm[:, :, 0:16], in1=dtb, op=ALU.add),
             reads=[t_sm, t_prm], writes=[t_sm])
        S.op("act", lambda e: e.activation(tmpa[:], sm[:, :, 0:16], AF.Abs), reads=[t_sm], writes=[t_ta])
        S.op("act", lambda e: e.activation(tmpa[:], tmpa[:], AF.Exp, scale=-1.0), reads=[t_ta], writes=[t_ta])
        S.op("act", lambda e: e.activation(tmpa[:], tmpa[:], AF.Ln, bias=1.0), reads=[t_ta], writes=[t_ta])
        S.op("dve", lambda e: e.scalar_tensor_tensor(out=tmpb[:], in0=sm[:, :, 0:16], scalar=0.0, in1=tmpa[:],
                                                     op0=ALU.max, op1=ALU.add), reads=[t_sm, t_ta], writes=[t_tb])
        S.op("dve", lambda e: e.scalar_tensor_tensor(out=sm[:, :, 0:16], in0=tmpb[:], scalar=-1.0, in1=nA,
                                                     op0=ALU.mult, op1=ALU.mult), reads=[t_tb, t_prm], writes=[t_sm])
        S.op("act", lambda e: e.activation(sm[:, :, 16:32], sm[:, :, 16:32], AF.Sigmoid), reads=[t_sm], writes=[t_sm])
        S.dma("sp", lambda e: e.dma_start(out=c.small_d, in_=sm[:]), reads=[t_sm], writes=[c.t_small])

        cw5 = sb("p1cw5", [5, 3072], F32)
        t_cw5 = Tok()
        S.dma("sp", lambda e: e.dma_start(out=cw5[:], in_=c.conv_w), writes=[t_cw5])
        cwT = sb("p1cwT", [128, 24, 8], F32)
        t_cwT = Tok()
        for ct in range(24):
            ps, t_ps = psm.next()
            S.op("pe", lambda e, ps=ps, ct=ct: e.transpose(ps[:, 0:5], cw5[0:5, ct * 128:(ct + 1) * 128], c.ident_f[0:5, 0:5]),
                 reads=[t_cw5, TC], writes=[t_ps])
            S.op("dve", lambda e, ps=ps, ct=ct: e.tensor_copy(cwT[:, ct, 0:5], ps[:, 0:5]), reads=[t_ps], writes=[t_cwT])
        dgs = _mk_ring(st, nc, "p1dg", 2, [128, 5, 128], BF16)
        xpre = _mk_ring(st, nc, "p1xpre", 2, [128, S_LEN + 4], BF16)
        tmstage = _mk_ring(st, nc, "p1tmstage", 2, [128, NT, 128], BF16)
        for ct in range(24):
            kind = ct // 8
            h = ct % 8
            w1, t_w1 = load_w(wfm, c.w_in, C_GQ + ct * 128, 128)
            dg, t_dg = dgs.next()
            for j in range(5):
                S.op("pool", lambda e, dg=dg, j=j, ct=ct: e.tensor_scalar(
                    out=dg[:, j, :], in0=c.ident_f[:], scalar1=cwT[:, ct, j:j + 1], scalar2=None, op0=ALU.mult),
                    reads=[TC, t_cwT], writes=[t_dg])
            xp, t_xp = xpre.next()
            S.op("pool", lambda e, xp=xp: e.memset(xp[:, 0:2], 0.0), writes=[t_xp])
            S.op("pool", lambda e, xp=xp: e.memset(xp[:, S_LEN + 2:S_LEN + 4], 0.0), writes=[t_xp])
            for g in range(NG):
                ps1, t_ps1 = proj_fm(w1, t_w1, g)
                S.op("act", lambda e, xp=xp, ps1=ps1, g=g: e.copy(xp[:, 2 + g * 512:2 + (g + 1) * 512], ps1[:]),
                     reads=[t_ps1], writes=[t_xp])
            stg, t_stg = stage.next()
            for g in range(NG):
                ps, t_ps = psm.next()
                for j in range(5):
                    S.op("pe", lambda e, ps=ps, dg=dg, xp=xp, g=g, j=j: e.matmul(
                        ps[:], dg[:, j, :], xp[:, g * 512 + j:g * 512 + j + 512], start=(j == 0), stop=(j == 4)),
                        reads=[t_dg, t_xp], writes=[t_ps])
                sl = slice(g * 512, (g + 1) * 512)
                if kind == 2:
                    S.op("act", lambda e, ps=ps, stg=stg, sl=sl: e.activation(stg[:, sl], ps[:], AF.Silu),
                         reads=[t_ps], writes=[t_stg])
                else:
                    a, t_a = f32a.next()
                    S.op("act", lambda e, ps=ps, a=a: e.activation(a[:], ps[:], AF.Silu), reads=[t_ps], writes=[t_a])
                    sqb, t_sqb = tmst.next()
                    S.op("pool", lambda e, sqb=sqb, a=a: e.tensor_tensor(out=sqb[:], in0=a[:], in1=a[:], op=ALU.mult),
                         reads=[t_a], writes=[t_sqb])
                    ps2, t_ps2 = psm.next()
                    S.op("pe", lambda e, ps2=ps2, sqb=sqb: e.matmul(ps2[:], c.ones_b[:], sqb[:], start=True, stop=True),
                         reads=[t_sqb, TC], writes=[t_ps2])
                    b, t_b = f32b.next()
                    S.op("act", lambda e, b=b, ps2=ps2: e.activation(b[:], ps2[:], AF.Sqrt, bias=EPS), reads=[t_ps2], writes=[t_b])
                    S.op("dve", lambda e, b=b: e.reciprocal(b[:], b[:]), reads=[t_b], writes=[t_b])
                    scl = (128.0 ** -0.5) if kind == 0 else 1.0
                    S.op("dve", lambda e, a=a, b=b, stg=stg, sl=sl, scl=scl: e.scalar_tensor_tensor(
                        out=stg[:, sl], in0=a[:], scalar=scl, in1=b[:], op0=ALU.mult, op1=ALU.mult),
                        reads=[t_a, t_b], writes=[t_stg])
            if kind < 2:
                dst = c.gq_d if kind == 0 else c.gk_d
                S.dma("sp", lambda e, stg=stg, dst=dst, h=h: e.dma_start(out=dst[h], in_=stg[:]), reads=[t_stg], writes=[c.t_gqk])
            if kind >= 1:
                dst = c.gktok_d if kind == 1 else c.gvtok_d
                tms, t_tms = tmstage.next()
                for i8 in range(4):
                    pt, t_pt = pst.next()
                    for ii in range(8):
                        i = i8 * 8 + ii
                        S.op("pe", lambda e, pt=pt, stg=stg, ii=ii, i=i: e.transpose(pt[:, ii, :], stg[:, i * 128:(i + 1) * 128], c.ident_b[:]),
                             reads=[t_stg, TC], writes=[t_pt])
                    S.op("dve", lambda e, pt=pt, tms=tms, i8=i8: e.tensor_copy(tms[:, i8 * 8:(i8 + 1) * 8, :], pt[:]),
                         reads=[t_pt], writes=[t_tms])
                S.dma("sp", lambda e, tms=tms, dst=dst, h=h: e.dma_start(
                    out=dst[:, h * 128:(h + 1) * 128].rearrange("(t p) c -> p t c", p=128), in_=tms[:]),
                    reads=[t_tms], writes=[c.t_gtok])
    S.barrier()


def build_program(debug=False, phases=("p1",), inject=(), opts=None):
    nc = bass.Bass("TRN2", target_bir_lowering=False)
    c = Ctx()
    c.inject = set(inject)
    c.opts = opts or {}
    c.nc = nc
    c.S = Sched(nc)

    c.input_names = []

    def din(name, shape):
        c.input_names.append(name)
        return nc.dram_tensor(name, list(shape), F32, kind="ExternalInput").ap()

    c.x = din("x", [S_LEN, D])
    c.norm1_w = din("norm1_w", [1, D])
    c.w_in = din("w_in", [D, IN_W])
    c.w_qkp = din("w_qkp", [D, 2048])
    c.cosT = din("cosT", [128, S_LEN])
    c.sinT = din("sinT", [128, S_LEN])
    c.conv_w = din("conv_w", [5, 3072])
    for n in ("a_log_fwd", "dt_bias_fwd", "a_log_bwd", "dt_bias_bwd"):
        setattr(c, n, din(n, [1, 8]))

    c.debug_names = []

    def scratch(name, shape, dt):
        kind = "ExternalOutput" if debug else "Internal"
        if name in c.inject:
            kind = "ExternalInput"
            c.input_names.append(name)
        elif debug:
            c.debug_names.append(name)
        return nc.dram_tensor(name, list(shape), dt, kind=kind).ap()

    c.qT_d = scratch("qT_d", [H, 128, S_LEN], BF16)
    c.kT_d = scratch("kT_d", [H, 128, S_LEN], BF16)
    c.v_d = scratch("v_d", [S_LEN, D], BF16)
    c.zs_d = scratch("zs_d", [S_LEN, D], BF16)
    c.sga_d = scratch("sga_d", [H, 128, S_LEN], BF16)
    c.sgg_d = scratch("sgg_d", [H, 128, S_LEN], BF16)
    c.gq_d = scratch("gq_d", [H, 128, S_LEN], BF16)
    c.gk_d = scratch("gk_d", [H, 128, S_LEN], BF16)
    c.gktok_d = scratch("gktok_d", [S_LEN, D], BF16)
    c.gvtok_d = scratch("gvtok_d", [S_LEN, D], BF16)
    c.small_d = scratch("small_d", [128, NT, 32], F32)
    for n in ("lambda_q1", "lambda_k1", "lambda_q2", "lambda_k2"):
        setattr(c, n, din(n, [1, 64]))
    c.subln_w = din("subln_w", [1, 128])
    c.gdn_norm_w = din("gdn_norm_w", [1, 128])
    c.w_proj_attn = din("w_proj_attn", [D, D])
    c.w_proj_gdn = din("w_proj_gdn", [D, D])
    c.w_out = din("w_out", [D, D])
    c.w_router = din("w_router", [D, NE])
    c.norm2_w = din("norm2_w", [1, D])
    c.norm_f_w = din("norm_f_w", [1, D])
    c.w_gate = din("w_gate", [NE, D, FF])
    c.w_up = din("w_up", [NE, D, FF])
    c.w_down = din("w_down", [NE, FF, D])
    c.out = nc.dram_tensor("out", [S_LEN, D], F32, kind="ExternalOutput").ap()
    c.x1_d = scratch("x1_d", [S_LEN, D], F32)
    c.h2tok_d = scratch("h2tok_d", [S_LEN, D], BF16)
    c.aff_d = scratch("aff_d", [128, NT, NE], F32)
    c.pos_d = scratch("pos_d", [NE, S_LEN], F32)
    c.ptok_d = scratch("ptok_d", [128, NT, NE], F32)
    c.ye_d = scratch("ye_d", [NE, CAP, D], BF16)
    for n in ("t_x1", "t_h2", "t_aff", "t_pos", "t_ye", "t_out"):
        setattr(c, n, Tok())
    c.ogT_d = scratch("ogT_d", [H, 128, S_LEN], BF16)
    c.t_og = Tok()
    c.oaT_d = scratch("oaT_d", [H, 128, S_LEN], BF16)
    for n in ("t_qk", "t_gates", "t_vz", "t_small", "t_gqk", "t_gtok", "t_oa"):
        setattr(c, n, Tok())

    with ExitStack() as st:
        _consts(c, st)
        if "p1" in phases:
            phase1(c)
        if "p2" in phases:
            phase2(c)
        if "p3" in phases:
            phase3(c)
        if "p4" in phases:
            phase4(c)
        if "p5" in phases:
            phase5(c)
        if "p6a" in phases:
            phase6a(c)
        if "p6b" in phases:
            phase6b(c)
        c.S.barrier()
        c.S.emit()
    return nc, c


def host_prep(inputs):
    f = lambda a: np.ascontiguousarray(np.asarray(a, dtype=np.float32))
    w_in = f(inputs["w_in"][0])
    perm = np.arange(2048).reshape(16, 2, 2, 32)[:, :, ::-1, :].reshape(-1)
    w_qkp = np.ascontiguousarray(w_in[:, :2048][:, perm])
    inv = 10000.0 ** (-np.arange(0, 64, 2, dtype=np.float32) / 64)
    ang = np.arange(S_LEN, dtype=np.float32)[:, None] * inv[None, :]
    ang = np.concatenate([ang, ang], axis=-1)
    cos = np.cos(ang).T.astype(np.float32)
    sin = np.sin(ang).T.astype(np.float32)
    sin[:32] *= -1.0
    shared = {
        "norm1_w": f(inputs["norm1_w"]).reshape(1, D),
        "w_in": w_in,
        "w_qkp": w_qkp,
        "cosT": np.ascontiguousarray(np.concatenate([cos, cos], 0)),
        "sinT": np.ascontiguousarray(np.concatenate([sin, sin], 0)),
        "conv_w": f(inputs["conv_w"][0]),
    }
    for n in ("a_log_fwd", "dt_bias_fwd", "a_log_bwd", "dt_bias_bwd"):
        shared[n] = f(inputs[n]).reshape(1, 8)
    for n in ("lambda_q1", "lambda_k1", "lambda_q2", "lambda_k2"):
        shared[n] = f(inputs[n]).reshape(1, 64)
    shared["subln_w"] = f(inputs["subln_w"]).reshape(1, 128)
    shared["gdn_norm_w"] = f(inputs["gdn_norm_w"]).reshape(1, 128)
    shared["w_proj_attn"] = f(inputs["w_proj_attn"][0])
    shared["w_proj_gdn"] = f(inputs["w_proj_gdn"][0])
    shared["w_out"] = f(inputs["w_out"][0])
    shared["w_router"] = f(inputs["w_router"][0])
    shared["norm2_w"] = f(inputs["norm2_w"]).reshape(1, D)
    shared["norm_f_w"] = f(inputs["norm_f_w"]).reshape(1, D)
    shared["w_gate"] = f(inputs["w_gate"][0])
    shared["w_up"] = f(inputs["w_up"][0])
    shared["w_down"] = f(inputs["w_down"][0])
    return shared


def phase2(c):
    nc, S = c.nc, c.S
    TC = c.t_const
    with ExitStack() as st:
        sb = lambda name, shape, dt: st.enter_context(nc.sbuf_tensor(name, shape, dt))
        lam = sb("p2lam", [128, 4, 64], F32)
        lsc = sb("p2lsc", [128, 8], F32)
        t_lam = Tok()
        for j, src in enumerate((c.lambda_q1, c.lambda_k1, c.lambda_q2, c.lambda_k2)):
            S.dma("sp", lambda e, j=j, src=src: e.dma_start(out=lam[:, j, :], in_=src.broadcast_to([128, 64])), writes=[t_lam])
        S.op("dve", lambda e: e.tensor_tensor(out=lam[:, 0, :], in0=lam[:, 0, :], in1=lam[:, 1, :], op=ALU.mult), reads=[t_lam], writes=[t_lam])
        S.op("dve", lambda e: e.tensor_tensor(out=lam[:, 2, :], in0=lam[:, 2, :], in1=lam[:, 3, :], op=ALU.mult), reads=[t_lam], writes=[t_lam])
        S.op("act", lambda e: e.activation(lam[:, 1, :], lam[:, 0, :], AF.Identity, accum_out=lsc[:, 0:1]), reads=[t_lam], writes=[t_lam])
        S.op("act", lambda e: e.activation(lam[:, 3, :], lam[:, 2, :], AF.Identity, accum_out=lsc[:, 1:2]), reads=[t_lam], writes=[t_lam])
        S.op("act", lambda e: e.activation(lsc[:, 2:4], lsc[:, 0:2], AF.Exp), reads=[t_lam], writes=[t_lam])
        S.op("dve", lambda e: e.scalar_tensor_tensor(out=lsc[:, 4:5], in0=lsc[:, 3:4], scalar=-LAMBDA_INIT, in1=lsc[:, 2:3],
                                                     op0=ALU.add, op1=ALU.subtract), reads=[t_lam], writes=[t_lam])
        wsub = sb("p2wsub", [128, 2], F32)
        t_wsub = Tok()
        S.dma("sp", lambda e: e.dma_start(out=wsub[:, 0:1], in_=c.subln_w.rearrange("o e -> e o")), writes=[t_wsub])
        S.op("dve", lambda e: e.tensor_scalar(out=wsub[:, 1:2], in0=wsub[:, 0:1], scalar1=(1.0 - LAMBDA_INIT), scalar2=None, op0=ALU.mult),
             reads=[t_wsub], writes=[t_wsub])

        qr = _mk_ring(st, nc, "p2q", 2, [128, S_LEN], BF16)
        kr = _mk_ring(st, nc, "p2k", 2, [128, S_LEN], BF16)
        vr = _mk_ring(st, nc, "p2v", 2, [128, NT, 128], BF16)
        pr = _mk_ring(st, nc, "p2p", 4, [128, 512], BF16)
        fr = _mk_ring(st, nc, "p2f", 6, [128, 512], F32)
        sqr = _mk_ring(st, nc, "p2sq", 2, [128, 512], BF16)
        stage = _mk_ring(st, nc, "p2stage", 2, [128, S_LEN], BF16)
        ps_s = _mk_ring(st, nc, "p2pss", 3, [128, 512], F32, psum=True)
        ps_o = [_mk_ring(st, nc, "p2pso%d" % t, 1, [128, 512], F32, psum=True) for t in range(2)]
        ps_l = [_mk_ring(st, nc, "p2psl%d" % t, 1, [128, 512], F32, psum=True) for t in range(2)]
        ps_x = _mk_ring(st, nc, "p2psx", 1, [128, 512], F32, psum=True)

        for h in range(H):
            q, t_q = qr.next()
            k, t_k = kr.next()
            v, t_v = vr.next()
            S.dma("sp", lambda e, q=q, h=h: e.dma_start(out=q[:], in_=c.qT_d[h]), reads=[c.t_qk], writes=[t_q])
            S.dma("sp", lambda e, k=k, h=h: e.dma_start(out=k[:], in_=c.kT_d[h]), reads=[c.t_qk], writes=[t_k])
            S.dma("sp", lambda e, v=v, h=h: e.dma_start(
                out=v[:], in_=c.v_d[:, h * 128:(h + 1) * 128].rearrange("(t p) e -> p t e", p=128)), reads=[c.t_vz], writes=[t_v])
            stg, t_stg = stage.next()
            for g in range(NG):
                qs = slice(g * 512, (g + 1) * 512)
                accs = []
                for t in range(2):
                    po, t_po = ps_o[t].next()
                    pl, t_pl = ps_l[t].next()
                    ts = slice(t * 64, (t + 1) * 64)
                    for j in range(NT):
                        pss, t_pss = ps_s.next()
                        S.op("pe", lambda e, pss=pss, k=k, q=q, ts=ts, j=j, qs=qs: e.matmul(
                            pss[:], k[ts, j * 128:(j + 1) * 128], q[ts, qs], start=True, stop=True),
                            reads=[t_k, t_q], writes=[t_pss])
                        p, t_p = pr.next()
                        S.op("act", lambda e, p=p, pss=pss: e.activation(p[:], pss[:], AF.Exp, scale=0.125),
                             reads=[t_pss], writes=[t_p])
                        S.op("pe", lambda e, po=po, v=v, p=p, j=j: e.matmul(po[:], v[:, j, :], p[:], start=(j == 0), stop=(j == NT - 1)),
                             reads=[t_v, t_p], writes=[t_po])
                        S.op("pe", lambda e, pl=pl, p=p, j=j: e.matmul(pl[:], c.ones_b[:], p[:], start=(j == 0), stop=(j == NT - 1)),
                             reads=[TC, t_p], writes=[t_pl])
                    accs.append((po, t_po, pl, t_pl))
                outs = []
                for t in range(2):
                    po, t_po, pl, t_pl = accs[t]
                    r, t_r = fr.next()
                    S.op("dve", lambda e, r=r, pl=pl: e.reciprocal(r[:], pl[:]), reads=[t_pl], writes=[t_r])
                    a, t_a = fr.next()
                    S.op("dve", lambda e, a=a, po=po, r=r: e.tensor_tensor(out=a[:], in0=po[:], in1=r[:], op=ALU.mult),
                         reads=[t_po, t_r], writes=[t_a])
                    outs.append((a, t_a))
                (a, t_a), (b, t_b) = outs
                oa, t_oa = fr.next()
                S.op("dve", lambda e, oa=oa, a=a, b=b: e.scalar_tensor_tensor(
                    out=oa[:], in0=b[:], scalar=lsc[:, 4:5], in1=a[:], op0=ALU.mult, op1=ALU.add),
                    reads=[t_a, t_b, t_lam], writes=[t_oa])
                sq, t_sq = sqr.next()
                S.op("pool", lambda e, sq=sq, oa=oa: e.tensor_tensor(out=sq[:], in0=oa[:], in1=oa[:], op=ALU.mult),
                     reads=[t_oa], writes=[t_sq])
                px, t_px = ps_x.next()
                S.op("pe", lambda e, px=px, sq=sq: e.matmul(px[:], c.ones_b[:], sq[:], start=True, stop=True),
                     reads=[TC, t_sq], writes=[t_px])
                rs, t_rs = fr.next()
                S.op("act", lambda e, rs=rs, px=px: e.activation(rs[:], px[:], AF.Sqrt, bias=EPS, scale=1.0 / 128), reads=[t_px], writes=[t_rs])
                S.op("dve", lambda e, rs=rs: e.reciprocal(rs[:], rs[:]), reads=[t_rs], writes=[t_rs])
                S.op("dve", lambda e, stg=stg, oa=oa, rs=rs, qs=qs: e.scalar_tensor_tensor(
                    out=stg[:, qs], in0=oa[:], scalar=wsub[:, 1:2], in1=rs[:], op0=ALU.mult, op1=ALU.mult),
                    reads=[t_oa, t_rs, t_wsub], writes=[t_stg])
            S.dma("sp", lambda e, stg=stg, h=h: e.dma_start(out=c.oaT_d[h], in_=stg[:]), reads=[t_stg], writes=[c.t_oa])
    S.barrier()


def phase3(c):
    nc, S = c.nc, c.S
    TC = c.t_const
    NB = NT
    with ExitStack() as st:
        sb = lambda name, shape, dt: st.enter_context(nc.sbuf_tensor(name, shape, dt))
        inc = [sb("p3inc%d" % d, [128, 128], F32) for d in range(2)]
        strm = [sb("p3str%d" % d, [128, 128], F32) for d in range(2)]
        mbias = [sb("p3mb%d" % d, [128, 128], F32) for d in range(2)]
        esel = [sb("p3es%d" % d, [128, 128], F32) for d in range(2)]
        t_m = Tok()
        for d in range(2):
            sgn = 1 if d == 0 else -1
            S.op("pool", lambda e, d=d: e.memset(inc[d][:], 1.0), writes=[t_m])
            S.op("pool", lambda e, d=d, sgn=sgn: e.affine_select(out=inc[d][:], in_=inc[d][:], compare_op=ALU.is_ge, fill=0.0,
                                                                base=0, pattern=[[sgn, 128]], channel_multiplier=-sgn),
                 reads=[t_m], writes=[t_m])
            S.op("pool", lambda e, d=d: e.memset(strm[d][:], 1.0), writes=[t_m])
            S.op("pool", lambda e, d=d, sgn=sgn: e.affine_select(out=strm[d][:], in_=strm[d][:], compare_op=ALU.is_ge, fill=0.0,
                                                                base=-1, pattern=[[sgn, 128]], channel_multiplier=-sgn),
                 reads=[t_m], writes=[t_m])
            S.op("pool", lambda e, d=d: e.tensor_scalar(out=mbias[d][:], in0=inc[d][:], scalar1=30000.0, scalar2=-30000.0,
                                                        op0=ALU.mult, op1=ALU.add), reads=[t_m], writes=[t_m])
            lastp = 127 if d == 0 else 0
            S.op("pool", lambda e, d=d: e.memset(esel[d][:], 0.0), writes=[t_m])
            S.op("pool", lambda e, d=d, lastp=lastp: e.affine_select(out=esel[d][:], in_=esel[d][:], compare_op=ALU.not_equal, fill=1.0,
                                                                    base=-lastp, pattern=[[0, 128]], channel_multiplier=1),
                 reads=[t_m], writes=[t_m])
        sm = sb("p3sm", [128, NT, 32], F32)
        t_sm = Tok()
        S.dma("sp", lambda e: e.dma_start(out=sm[:], in_=c.small_d), reads=[c.t_small], writes=[t_sm])
        gw = sb("p3gw", [128, 128], F32)
        t_gw = Tok()
        S.dma("sp", lambda e: e.dma_start(out=gw[:], in_=c.gdn_norm_w.broadcast_to([128, 128])), writes=[t_gw])

        def bank(name, dt=F32, n=512):
            return st.enter_context(nc.psum_tensor(name, [128, n], dt))
        b0, b2, b3, b4, b5, b6, b7 = [bank("p3b" + x) for x in "0234567"]
        b1 = bank("p3b1", BF16, 1024)
        btok = {id(b): Tok(excl=True) for b in (b0, b1, b2, b3, b4, b5, b6, b7)}
        slot = lambda b, i, w=128: (b[:, i * w:(i + 1) * w], btok[id(b)])
        s_kk, s_kq, s_gd = slot(b0, 0), slot(b0, 1), slot(b0, 2)
        s_misc = [slot(b0, 3)] * 4
        s_tr = [slot(b1, i) for i in range(8)]
        s_sqd = [(slot(b2, 0), slot(b2, 1)), (slot(b4, 0), slot(b4, 1))]
        s_apd = [slot(b3, 0, 256), slot(b5, 0, 256)]
        s_scan = [[slot(b6, i) for i in range(4)], [slot(b7, i) for i in range(4)]]

        kT = sb("p3kT", [128, S_LEN], BF16)
        qT = sb("p3qT", [128, S_LEN], BF16)
        ktok = sb("p3ktok", [128, NB, 128], BF16)
        vtok = sb("p3vtok", [128, NB, 128], BF16)
        zs = sb("p3zs", [128, NB, 128], BF16)
        t_in = Tok()
        ub = [sb("p3ub%d" % d, [128, NB, 128], F32) for d in range(2)]
        nwT = [sb("p3nwT%d" % d, [128, NB, 128], BF16) for d in range(2)]
        kdec = [sb("p3kdec%d" % d, [128, NB, 128], BF16) for d in range(2)]
        qkT = [sb("p3qkT%d" % d, [128, NB, 128], BF16) for d in range(2)]
        t_blk = [[Tok() for _ in range(NB)] for _ in range(2)]
        sc = [sb("p3sc%d" % d, [128, 8, NB], F32) for d in range(2)]
        t_sc = [Tok(), Tok()]
        oacc = sb("p3oacc", [128, NB, 128], F32)
        t_oacc = [Tok() for _ in range(NB)]
        S32 = [sb("p3S32_%d" % d, [128, 128], F32) for d in range(2)]
        Sbf = [sb("p3Sbf_%d" % d, [128, 128], BF16) for d in range(2)]
        t_S = [Tok(), Tok()]
        dtr = _mk_ring(st, nc, "p3dt", 2, [128, 128], F32)
        tmpr = _mk_ring(st, nc, "p3tmp", 2, [128, 128], F32)
        ntr = _mk_ring(st, nc, "p3nt", 4, [128, 128], F32)
        nnr = _mk_ring(st, nc, "p3nn", 4, [128, 128], F32)
        xr = _mk_ring(st, nc, "p3x", 2, [128, 256], F32)
        vnr = _mk_ring(st, nc, "p3vn", 4, [128, 128], BF16)
        o2r = _mk_ring(st, nc, "p3o2", 4, [128, 128], F32)
        big = sb("p3big", [128, NB, 128], F32)
        t_big = Tok()
        red = sb("p3red", [128, NB], F32)
        ogtok = sb("p3ogtok", [128, NB, 128], BF16)
        stage = sb("p3stage", [128, S_LEN], BF16)
        t_stage = Tok()

        stop = c.opts.get("p3_stop", 9)
        for h in range(c.opts.get("p3_heads", H)):
            S.dma("sp", lambda e, h=h: e.dma_start(out=kT[:], in_=c.gk_d[h]), reads=[c.t_gqk], writes=[t_in])
            S.dma("sp", lambda e, h=h: e.dma_start(out=qT[:], in_=c.gq_d[h]), reads=[c.t_gqk], writes=[t_in])
            for (buf, src, tk) in ((ktok, c.gktok_d, c.t_gtok), (vtok, c.gvtok_d, c.t_gtok), (zs, c.zs_d, c.t_vz)):
                S.dma("sp", lambda e, buf=buf, src=src, h=h: e.dma_start(
                    out=buf[:], in_=src[:, h * 128:(h + 1) * 128].rearrange("(t p) e -> p t e", p=128)), reads=[tk], writes=[t_in])
            for d in range(2):
                s_ = sc[d]
                g = sm[:, :, d * 8 + h]
                beta = sm[:, :, 16 + d * 8 + h]
                (pm, t_pm) = s_misc[d * 2]
                S.op("pe", lambda e, pm=pm, d=d, g=g: e.matmul(pm[:, 0:NB], inc[d][:], g, start=True, stop=True),
                     reads=[t_m, t_sm], writes=[t_pm])
                S.op("dve", lambda e, pm=pm, s_=s_: e.tensor_copy(s_[:, 0, :], pm[:, 0:NB]), reads=[t_pm], writes=[t_sc[d]])
                S.op("dve", lambda e, s_=s_: e.tensor_scalar(out=s_[:, 1, :], in0=s_[:, 0, :], scalar1=-1.0, scalar2=None, op0=ALU.mult),
                     reads=[t_sc[d]], writes=[t_sc[d]])
                (pm2, t_pm2) = s_misc[d * 2 + 1]
                S.op("pe", lambda e, pm2=pm2, d=d, s_=s_: e.matmul(pm2[:, 0:NB], esel[d][:], s_[:, 0, :], start=True, stop=True),
                     reads=[t_m, t_sc[d]], writes=[t_pm2])
                S.op("dve", lambda e, pm2=pm2, s_=s_: e.tensor_copy(s_[:, 2, :], pm2[:, 0:NB]), reads=[t_pm2], writes=[t_sc[d]])
                S.op("act", lambda e, s_=s_: e.activation(s_[:, 3, :], s_[:, 0, :], AF.Exp), reads=[t_sc[d]], writes=[t_sc[d]])
                S.op("dve", lambda e, s_=s_: e.tensor_tensor(out=s_[:, 4, :], in0=s_[:, 2, :], in1=s_[:, 0, :], op=ALU.subtract),
                     reads=[t_sc[d]], writes=[t_sc[d]])
                S.op("act", lambda e, s_=s_: e.activation(s_[:, 4, :], s_[:, 4, :], AF.Exp), reads=[t_sc[d]], writes=[t_sc[d]])
                S.op("act", lambda e, s_=s_: e.activation(s_[:, 5, :], s_[:, 2, :], AF.Exp), reads=[t_sc[d]], writes=[t_sc[d]])
                S.op("dve", lambda e, s_=s_, beta=beta: e.tensor_scalar(out=s_[:, 6, :], in0=beta, scalar1=-1.0, scalar2=None, op0=ALU.mult),
                     reads=[t_sm, t_sc[d]], writes=[t_sc[d]])
                S.op("dve", lambda e, s_=s_, beta=beta: e.tensor_copy(s_[:, 7, :], beta), reads=[t_sm, t_sc[d]], writes=[t_sc[d]])

            for b in range(NB if stop >= 2 else 0):
                bs = slice(b * 128, (b + 1) * 128)
                (pkk, t_pkk), (pkq, t_pkq) = s_kk, s_kq
                S.op("pe", lambda e, bs=bs, pkk=pkk: e.matmul(pkk, kT[:, bs], kT[:, bs], start=True, stop=True),
                     reads=[t_in], writes=[t_pkk])
                S.op("pe", lambda e, bs=bs, pkq=pkq: e.matmul(pkq, kT[:, bs], qT[:, bs], start=True, stop=True),
                     reads=[t_in], writes=[t_pkq])
                for d in range(2):
                    s_ = sc[d]
                    (pgd, t_pgd) = s_gd
                    S.op("pe", lambda e, pgd=pgd, s_=s_, b=b: e.matmul(pgd, s_[:, 0, b:b + 1].broadcast_to([128, 128]), c.ident_f[:],
                                                                       start=True, stop=False), reads=[t_sc[d], TC], writes=[t_pgd])
                    S.op("pe", lambda e, pgd=pgd, s_=s_, b=b: e.matmul(pgd, c.ident_f[:], s_[:, 1, b:b + 1].broadcast_to([128, 128]),
                                                                       start=False, stop=False), reads=[t_sc[d], TC], writes=[t_pgd])
                    S.op("pe", lambda e, pgd=pgd, d=d: e.matmul(pgd, c.ident_f[:], mbias[d][:], start=False, stop=True),
                         reads=[t_m, TC], writes=[t_pgd])
                    dt_, t_dt = dtr.next()
                    S.op("act", lambda e, dt_=dt_, pgd=pgd: e.activation(dt_[:], pgd, AF.Exp), reads=[t_pgd], writes=[t_dt])
                    tmp, t_tmp = tmpr.next()
                    S.op("dve", lambda e, tmp=tmp, pkk=pkk, s_=s_, b=b, dt_=dt_: e.scalar_tensor_tensor(
                        out=tmp[:], in0=pkk, scalar=s_[:, 6, b:b + 1], in1=dt_[:], op0=ALU.mult, op1=ALU.mult),
                        reads=[t_pkk, t_sc[d], t_dt], writes=[t_tmp])
                    nt, t_nt = ntr.next()
                    S.op("pool", lambda e, nt=nt, tmp=tmp, d=d: e.tensor_tensor(out=nt[:], in0=tmp[:], in1=strm[d][:], op=ALU.mult),
                         reads=[t_tmp, t_m], writes=[t_nt])
                    S.op("dve", lambda e, d=d, b=b, pkq=pkq, dt_=dt_: e.tensor_tensor(out=qkT[d][:, b, :], in0=pkq, in1=dt_[:], op=ALU.mult),
                         reads=[t_pkq, t_dt], writes=[t_blk[d][b]])
                    (ptr, t_ptr) = s_sqd[d][0]
                    S.op("pe", lambda e, ptr=ptr, nt=nt: e.transpose(ptr, nt[:], c.ident_f[:]), reads=[t_nt, TC], writes=[t_ptr])
                    nn, t_nn = nnr.next()
                    S.op("act", lambda e, nn=nn, ptr=ptr: e.copy(nn[:], ptr), reads=[t_ptr], writes=[t_nn])
                    x, t_x = xr.next()
                    S.op("pool", lambda e, x=x, b=b: e.tensor_copy(x[:, 0:128], vtok[:, b, :]), reads=[t_in], writes=[t_x])
                    S.op("pool", lambda e, x=x, b=b, s_=s_: e.tensor_scalar(out=x[:, 128:256], in0=ktok[:, b, :], scalar1=s_[:, 3, b:b + 1],
                                                                          scalar2=None, op0=ALU.mult), reads=[t_in, t_sc[d]], writes=[t_x])
                    for l in range(7):
                        (pap, t_pap) = s_apd[d]
                        S.op("pe", lambda e, pap=pap, nt=nt, x=x: e.matmul(pap, nt[:], x[:], start=True, stop=True),
                             reads=[t_nt, t_x], writes=[t_pap])
                        if l < 6:
                            (pn2, t_pn2), (pnt2, t_pnt2) = s_sqd[d]
                            S.op("pe", lambda e, pn2=pn2, nt=nt, nn=nn: e.matmul(pn2, nt[:], nn[:], start=True, stop=True),
                                 reads=[t_nt, t_nn], writes=[t_pn2])
                            S.op("pe", lambda e, pnt2=pnt2, nt=nt, nn=nn: e.matmul(pnt2, nn[:], nt[:], start=True, stop=True),
                                 reads=[t_nt, t_nn], writes=[t_pnt2])
                        S.op("dve", lambda e, x=x, pap=pap: e.tensor_tensor(out=x[:], in0=pap, in1=x[:], op=ALU.add),
                             reads=[t_pap, t_x], writes=[t_x])
                        if l < 6:
                            nn2, t_nn2 = nnr.next()
                            nt2, t_nt2 = ntr.next()
                            S.op("act", lambda e, nn2=nn2, pn2=pn2: e.copy(nn2[:], pn2), reads=[t_pn2], writes=[t_nn2])
                            S.op("act", lambda e, nt2=nt2, pnt2=pnt2: e.copy(nt2[:], pnt2), reads=[t_pnt2], writes=[t_nt2])
                            nn, t_nn, nt, t_nt = nn2, t_nn2, nt2, t_nt2
                    S.op("dve", lambda e, d=d, b=b, x=x, s_=s_: e.tensor_scalar(out=ub[d][:, b, :], in0=x[:, 0:128], scalar1=s_[:, 7, b:b + 1],
                                                                              scalar2=None, op0=ALU.mult), reads=[t_x, t_sc[d]], writes=[t_blk[d][b]])
                    (ptw, t_ptw) = s_sqd[d][1]
                    S.op("pe", lambda e, ptw=ptw, x=x: e.transpose(ptw, x[:, 128:256], c.ident_f[:]), reads=[t_x, TC], writes=[t_ptw])
                    S.op("act", lambda e, d=d, b=b, ptw=ptw: e.activation(nwT[d][:, b, :], ptw, AF.Copy, scale=-1.0),
                         reads=[t_ptw], writes=[t_blk[d][b]])
                    S.op("pool", lambda e, d=d, b=b, s_=s_: e.tensor_scalar(out=kdec[d][:, b, :], in0=ktok[:, b, :], scalar1=s_[:, 4, b:b + 1],
                                                                          scalar2=None, op0=ALU.mult), reads=[t_in, t_sc[d]], writes=[t_blk[d][b]])

            for d in range(2):
                S.op("pool", lambda e, d=d: e.memset(S32[d][:], 0.0), writes=[t_S[d]])
                S.op("pool", lambda e, d=d: e.memset(Sbf[d][:], 0.0), writes=[t_S[d]])
            for step in range(NB if stop >= 3 else 0):
                for d in range(2):
                    b = step if d == 0 else NB - 1 - step
                    bs = slice(b * 128, (b + 1) * 128)
                    s_ = sc[d]
                    (pv, t_pv), (po1, t_po1), (po2, t_po2), (pS, t_pS) = s_scan[d]
                    S.op("pe", lambda e, pv=pv, d=d, b=b: e.matmul(pv, nwT[d][:, b, :], Sbf[d][:], start=True, stop=True),
                         reads=[t_blk[d][b], t_S[d]], writes=[t_pv])
                    S.op("pe", lambda e, po1=po1, d=d, bs=bs: e.matmul(po1, qT[:, bs], Sbf[d][:], start=True, stop=True),
                         reads=[t_in, t_S[d]], writes=[t_po1])
                    vn, t_vn = vnr.next()
                    S.op("dve", lambda e, vn=vn, pv=pv, s_=s_, b=b, d=d: e.scalar_tensor_tensor(
                        out=vn[:], in0=pv, scalar=s_[:, 7, b:b + 1], in1=ub[d][:, b, :], op0=ALU.mult, op1=ALU.add),
                        reads=[t_pv, t_sc[d], t_blk[d][b]], writes=[t_vn])
                    S.op("pe", lambda e, pS=pS, d=d, b=b, vn=vn: e.matmul(pS, kdec[d][:, b, :], vn[:], start=True, stop=True),
                         reads=[t_blk[d][b], t_vn], writes=[t_pS])
                    S.op("pe", lambda e, po2=po2, d=d, b=b, vn=vn: e.matmul(po2, qkT[d][:, b, :], vn[:], start=True, stop=True),
                         reads=[t_blk[d][b], t_vn], writes=[t_po2])
                    S.op("dve", lambda e, d=d, b=b, s_=s_, pS=pS: e.scalar_tensor_tensor(
                        out=S32[d][:], in0=S32[d][:], scalar=s_[:, 5, b:b + 1], in1=pS, op0=ALU.mult, op1=ALU.add),
                        reads=[t_pS, t_sc[d], t_S[d]], writes=[t_S[d]])
                    S.op("pool", lambda e, d=d: e.tensor_copy(Sbf[d][:], S32[d][:]), reads=[t_S[d]], writes=[t_S[d]])
                    o2, t_o2 = o2r.next()
                    S.op("act", lambda e, o2=o2, po2=po2: e.copy(o2[:], po2), reads=[t_po2], writes=[t_o2])
                    first = (d == 0 and step < NB // 2) or (d == 1 and step < NB // 2)
                    first = (b < NB // 2) if d == 0 else (b >= NB // 2)
                    if not first:
                        S.op("pool", lambda e, o2=o2, b=b: e.tensor_tensor(out=o2[:], in0=o2[:], in1=oacc[:, b, :], op=ALU.add),
                             reads=[t_o2, t_oacc[b]], writes=[t_o2])
                    S.op("dve", lambda e, b=b, po1=po1, s_=s_, o2=o2: e.scalar_tensor_tensor(
                        out=oacc[:, b, :], in0=po1, scalar=s_[:, 3, b:b + 1], in1=o2[:], op0=ALU.mult, op1=ALU.add),
                        reads=[t_po1, t_sc[d], t_o2], writes=[t_oacc[b]])

            S.op("dve", lambda e: e.tensor_tensor(out=big[:], in0=oacc[:], in1=oacc[:], op=ALU.mult), reads=t_oacc, writes=[t_big])
            S.op("dve", lambda e: e.tensor_reduce(out=red[:], in_=big[:], axis=mybir.AxisListType.X, op=ALU.add), reads=[t_big], writes=[t_big])
            S.op("act", lambda e: e.activation(red[:], red[:], AF.Sqrt, bias=EPS, scale=1.0 / 128), reads=[t_big], writes=[t_big])
            S.op("dve", lambda e: e.reciprocal(red[:], red[:]), reads=[t_big], writes=[t_big])
            S.op("dve", lambda e: e.tensor_tensor(out=big[:], in0=oacc[:], in1=red[:].unsqueeze(2).broadcast_to([128, NB, 128]), op=ALU.mult),
                 reads=t_oacc + [t_big], writes=[t_big])
            S.op("pool", lambda e: e.tensor_tensor(out=big[:], in0=big[:], in1=gw[:].unsqueeze(1).broadcast_to([128, NB, 128]), op=ALU.mult),
                 reads=[t_big, t_gw], writes=[t_big])
            S.op("dve", lambda e: e.tensor_tensor(out=ogtok[:], in0=big[:], in1=zs[:], op=ALU.mult), reads=[t_big, t_in], writes=[t_big])
            for i8 in range(4):
                for ii in range(8):
                    b = i8 * 8 + ii
                    (ptr, t_ptr) = s_tr[ii]
                    S.op("pe", lambda e, ptr=ptr, b=b: e.transpose(ptr, ogtok[:, b, :], c.ident_b[:]), reads=[t_big, TC], writes=[t_ptr])
                    S.op("act", lambda e, ptr=ptr, b=b: e.copy(stage[:, b * 128:(b + 1) * 128], ptr), reads=[t_ptr], writes=[t_stage])
            S.dma("sp", lambda e, h=h: e.dma_start(out=c.ogT_d[h], in_=stage[:]), reads=[t_stage], writes=[c.t_og])
    S.barrier()


def phase4(c):
    nc, S = c.nc, c.S
    TC = c.t_const
    with ExitStack() as st:
        sb = lambda name, shape, dt: st.enter_context(nc.sbuf_tensor(name, shape, dt))
        wpa = sb("p4wpa", [128, 8, D], BF16)
        wpg = sb("p4wpg", [128, 8, D], BF16)
        wout = sb("p4wout", [128, 8, D], BF16)
        wr = sb("p4wr", [128, 8, NE], BF16)
        t_w = Tok()
        for (dst, src) in ((wpa, c.w_proj_attn), (wpg, c.w_proj_gdn), (wout, c.w_out)):
            for kc in range(8):
                S.dma("pool", lambda e, dst=dst, src=src, kc=kc: e.dma_start(out=dst[:, kc, :], in_=src[kc * 128:(kc + 1) * 128, :]),
                      writes=[Tok()])
        S.dma("pool", lambda e: e.dma_start(out=wr[:], in_=c.w_router.rearrange("(kc p) n -> p kc n", p=128)), writes=[t_w])
        S.barrier()
        n2w = sb("p4n2w", [128, D], F32)
        t_n2w = Tok()
        S.dma("sp", lambda e: e.dma_start(out=n2w[:], in_=c.norm2_w.broadcast_to([128, D])), writes=[t_n2w])
        aff = sb("p4aff", [128, NT, NE], F32)
        t_aff = Tok()
        oar = _mk_ring(st, nc, "p4oa", 2, [128, 8, 512], BF16)
        ogr = _mk_ring(st, nc, "p4og", 2, [128, 8, 512], BF16)
        sgar = _mk_ring(st, nc, "p4sga", 2, [128, 8, 512], BF16)
        sggr = _mk_ring(st, nc, "p4sgg", 2, [128, 8, 512], BF16)
        mgr = _mk_ring(st, nc, "p4mg", 2, [128, 8, 512], BF16)
        f1 = _mk_ring(st, nc, "p4f1", 2, [128, 512], F32)
        f2 = _mk_ring(st, nc, "p4f2", 2, [128, 512], F32)
        xr = _mk_ring(st, nc, "p4x", 2, [128, D], F32)
        sqr = _mk_ring(st, nc, "p4sq", 2, [128, D], BF16)
        hr = _mk_ring(st, nc, "p4h", 2, [128, D], BF16)
        hTr = _mk_ring(st, nc, "p4hT", 2, [128, 8, 128], BF16)
        ssr = _mk_ring(st, nc, "p4ss", 4, [128, 4], F32)
        lgr = _mk_ring(st, nc, "p4lg", 2, [128, NE], F32)
        psm = _mk_ring(st, nc, "p4psm", 6, [128, 512], F32, psum=True)
        pst = _mk_ring(st, nc, "p4pst", 2, [128, 8, 128], BF16, psum=True)

        for g in range(NG):
            gs = slice(g * 512, (g + 1) * 512)
            oa, t_oa = oar.next()
            og, t_og = ogr.next()
            sga, t_sga = sgar.next()
            sgg, t_sgg = sggr.next()
            for (buf, tk, src, dep) in ((oa, t_oa, c.oaT_d, c.t_oa), (og, t_og, c.ogT_d, c.t_og),
                                        (sga, t_sga, c.sga_d, c.t_gates), (sgg, t_sgg, c.sgg_d, c.t_gates)):
                S.dma("sp", lambda e, buf=buf, src=src, gs=gs: e.dma_start(out=buf[:], in_=src[:, :, gs].rearrange("h p t -> p h t")),
                      reads=[dep], writes=[tk])
            mg, t_mg = mgr.next()
            for dc in range(8):
                ds = slice(dc * 128, (dc + 1) * 128)
                pa, t_pa = psm.next()
                pg, t_pg = psm.next()
                for ec in range(8):
                    S.op("pe", lambda e, pa=pa, ec=ec, ds=ds, oa=oa: e.matmul(pa[:], wpa[:, ec, ds], oa[:, ec, :], start=(ec == 0), stop=(ec == 7)),
                         reads=[t_oa], writes=[t_pa])
                for ec in range(8):
                    S.op("pe", lambda e, pg=pg, ec=ec, ds=ds, og=og: e.matmul(pg[:], wpg[:, ec, ds], og[:, ec, :], start=(ec == 0), stop=(ec == 7)),
                         reads=[t_og], writes=[t_pg])
                a, t_a = f1.next()
                b, t_b = f2.next()
                S.op("dve", lambda e, a=a, pa=pa, sga=sga, dc=dc: e.tensor_tensor(out=a[:], in0=pa[:], in1=sga[:, dc, :], op=ALU.mult),
                     reads=[t_pa, t_sga], writes=[t_a])
                S.op("dve", lambda e, b=b, pg=pg, sgg=sgg, dc=dc: e.tensor_tensor(out=b[:], in0=pg[:], in1=sgg[:, dc, :], op=ALU.mult),
                     reads=[t_pg, t_sgg], writes=[t_b])
                S.op("pool", lambda e, mg=mg, a=a, b=b, dc=dc: e.tensor_tensor(out=mg[:, dc, :], in0=a[:], in1=b[:], op=ALU.add),
                     reads=[t_a, t_b], writes=[t_mg])
            for tl in range(4):
                i = g * 4 + tl
                xt, t_x = xr.next()
                S.dma("sp", lambda e, xt=xt, i=i: e.dma_start(out=xt[:], in_=c.x[i * 128:(i + 1) * 128, :]), writes=[t_x])
                for dh in range(2):
                    po, t_po = psm.next()
                    for dc in range(8):
                        S.op("pe", lambda e, po=po, mg=mg, dc=dc, tl=tl, dh=dh: e.matmul(
                            po[:], mg[:, dc, tl * 128:(tl + 1) * 128], wout[:, dc, dh * 512:(dh + 1) * 512], start=(dc == 0), stop=(dc == 7)),
                            reads=[t_mg], writes=[t_po])
                    S.op("dve", lambda e, xt=xt, po=po, dh=dh: e.tensor_tensor(out=xt[:, dh * 512:(dh + 1) * 512], in0=po[:],
                                                                              in1=xt[:, dh * 512:(dh + 1) * 512], op=ALU.add),
                         reads=[t_po, t_x], writes=[t_x])
                S.dma("sp", lambda e, xt=xt, i=i: e.dma_start(out=c.x1_d[i * 128:(i + 1) * 128, :], in_=xt[:]), reads=[t_x], writes=[c.t_x1])
                sq, t_sq = sqr.next()
                ss, t_ss = ssr.next()
                S.op("act", lambda e, xt=xt, sq=sq, ss=ss: e.activation(sq[:], xt[:], AF.Square, accum_out=ss[:, 0:1]),
                     reads=[t_x], writes=[t_sq, t_ss])
                S.op("act", lambda e, ss=ss: e.activation(ss[:, 1:2], ss[:, 0:1], AF.Sqrt, bias=EPS, scale=1.0 / D), reads=[t_ss], writes=[t_ss])
                S.op("dve", lambda e, ss=ss: e.reciprocal(ss[:, 1:2], ss[:, 1:2]), reads=[t_ss], writes=[t_ss])
                ht, t_h = hr.next()
                S.op("dve", lambda e, ht=ht, xt=xt, ss=ss: e.scalar_tensor_tensor(
                    out=ht[:], in0=xt[:], scalar=ss[:, 1:2], in1=n2w[:], op0=ALU.mult, op1=ALU.mult),
                    reads=[t_x, t_ss, t_n2w], writes=[t_h])
                S.dma("sp", lambda e, ht=ht, i=i: e.dma_start(out=c.h2tok_d[i * 128:(i + 1) * 128, :], in_=ht[:]), reads=[t_h], writes=[c.t_h2])
                pt, t_pt = pst.next()
                for dc in range(8):
                    S.op("pe", lambda e, pt=pt, ht=ht, dc=dc: e.transpose(pt[:, dc, :], ht[:, dc * 128:(dc + 1) * 128], c.ident_b[:]),
                         reads=[t_h, TC], writes=[t_pt])
                hT, t_hT = hTr.next()
                S.op("act", lambda e, hT=hT, pt=pt: e.copy(hT[:], pt[:]), reads=[t_pt], writes=[t_hT])
                pl, t_pl = psm.next()
                for dc in range(8):
                    S.op("pe", lambda e, pl=pl, hT=hT, dc=dc: e.matmul(pl[:, 0:NE], hT[:, dc, :], wr[:, dc, :], start=(dc == 0), stop=(dc == 7)),
                         reads=[t_hT, t_w], writes=[t_pl])
                lg, t_lg = lgr.next()
                S.op("dve", lambda e, pl=pl, ss=ss: e.tensor_reduce(out=ss[:, 2:3], in_=pl[:, 0:NE], axis=mybir.AxisListType.X, op=ALU.max),
                     reads=[t_pl, t_ss], writes=[t_ss])
                S.op("dve", lambda e, ss=ss: e.tensor_scalar(out=ss[:, 2:3], in0=ss[:, 2:3], scalar1=-1.0, scalar2=None, op0=ALU.mult),
                     reads=[t_ss], writes=[t_ss])
                S.op("act", lambda e, lg=lg, pl=pl, ss=ss: e.activation(lg[:], pl[:, 0:NE], AF.Exp, bias=ss[:, 2:3], accum_out=ss[:, 3:4]),
                     reads=[t_pl, t_ss], writes=[t_lg, t_ss])
                S.op("dve", lambda e, ss=ss: e.reciprocal(ss[:, 3:4], ss[:, 3:4]), reads=[t_ss], writes=[t_ss])
                S.op("dve", lambda e, lg=lg, ss=ss, i=i: e.tensor_scalar(out=aff[:, i, :], in0=lg[:], scalar1=ss[:, 3:4], scalar2=None, op0=ALU.mult),
                     reads=[t_lg, t_ss], writes=[t_aff])
        S.dma("sp", lambda e: e.dma_start(out=c.aff_d, in_=aff[:]), reads=[t_aff], writes=[c.t_aff])
    S.barrier()


def phase5(c):
    nc, S = c.nc, c.S
    TC = c.t_const
    with ExitStack() as st:
        sb = lambda name, shape, dt: st.enter_context(nc.sbuf_tensor(name, shape, dt))
        aff = sb("p5aff", [128, NT, NE], F32)
        t_aff = Tok()
        S.dma("sp", lambda e: e.dma_start(out=aff[:], in_=c.aff_d), reads=[c.t_aff], writes=[t_aff])
        affT = sb("p5affT", [NE, S_LEN], F32)
        work = sb("p5work", [NE, S_LEN], F32)
        ones = sb("p5ones", [NE, S_LEN], F32)
        mx = sb("p5mx", [NE, 8], F32)
        t_affT, t_work, t_mx, t_ones = Tok(), Tok(), Tok(), Tok()
        psm = _mk_ring(st, nc, "p5psm", 4, [128, 512], F32, psum=True)
        for i4 in range(NT // 4):
            ps, t_ps = psm.next()
            for ii in range(4):
                i = i4 * 4 + ii
                S.op("pe", lambda e, ps=ps, ii=ii, i=i: e.transpose(ps[0:NE, ii * 128:(ii + 1) * 128], aff[:, i, :], c.ident_f[:]),
                     reads=[t_aff, TC], writes=[t_ps])
            S.op("act", lambda e, ps=ps, i4=i4: e.copy(affT[:, i4 * 512:(i4 + 1) * 512], ps[0:NE, :]), reads=[t_ps], writes=[t_affT])
        S.op("pool", lambda e: e.memset(ones[:], 1.0), writes=[t_ones])
        src = affT
        for it in range(CAP // 8):
            S.op("dve", lambda e, src=src: e.max(out=mx[:], in_=src[:]), reads=[t_affT, t_work], writes=[t_mx])
            if it < CAP // 8 - 1:
                S.op("dve", lambda e, src=src: e.match_replace(out=work[:], in_to_replace=mx[:], in_values=src[:], imm_value=-1.0),
                     reads=[t_mx, t_affT, t_work], writes=[t_work])
            src = work
        S.op("dve", lambda e: e.tensor_scalar(out=work[:], in0=affT[:], scalar1=mx[:, 7:8], scalar2=None, op0=ALU.is_ge),
             reads=[t_affT, t_mx, t_work], writes=[t_work])
        S.op("dve", lambda e: e.tensor_tensor_scan(out=affT[:], data0=ones[:], data1=work[:], initial=0.0, op0=ALU.mult, op1=ALU.add),
             reads=[t_work, t_ones, t_affT], writes=[t_affT])
        S.op("dve", lambda e: e.tensor_tensor(out=affT[:], in0=affT[:], in1=work[:], op=ALU.mult), reads=[t_work, t_affT], writes=[t_affT])
        S.op("dve", lambda e: e.tensor_scalar(out=affT[:], in0=affT[:], scalar1=-1.0, scalar2=None, op0=ALU.add), reads=[t_affT], writes=[t_affT])
        S.dma("sp", lambda e: e.dma_start(out=c.pos_d, in_=affT[:]), reads=[t_affT], writes=[c.t_pos])
        ptok = sb("p5ptok", [128, NT, NE], F32)
        t_ptok = Tok()
        for i4 in range(NT // 4):
            ps, t_ps = psm.next()
            for ii in range(4):
                i = i4 * 4 + ii
                S.op("pe", lambda e, ps=ps, ii=ii, i=i: e.transpose(ps[:, ii * NE:(ii + 1) * NE], affT[:, i * 128:(i + 1) * 128], c.ident_f[0:NE, 0:NE]),
                     reads=[t_affT, TC], writes=[t_ps])
            S.op("act", lambda e, ps=ps, i4=i4: e.copy(ptok[:, i4 * 4:(i4 + 1) * 4, :], ps[:, 0:4 * NE].rearrange("p (a b) -> p a b", b=NE)),
                 reads=[t_ps], writes=[t_ptok])
        S.dma("sp", lambda e: e.dma_start(out=c.ptok_d, in_=ptok[:]), reads=[t_ptok], writes=[c.t_pos])
    S.barrier()


def phase6a(c):
    nc, S = c.nc, c.S
    TC = c.t_const
    with ExitStack() as st:
        sb = lambda name, shape, dt: st.enter_context(nc.sbuf_tensor(name, shape, dt))
        ptok = sb("p6ptok", [128, NT, NE], F32)
        t_ptok = Tok()
        S.dma("sp", lambda e: e.dma_start(out=ptok[:], in_=c.ptok_d), reads=[c.t_pos], writes=[t_ptok])
        iota = sb("p6iota", [128, CAP], F32)
        t_iota = Tok()
        S.op("pool", lambda e: e.iota(iota[:], pattern=[[1, CAP]], base=0, channel_multiplier=0, allow_small_or_imprecise_dtypes=True),
             writes=[t_iota])
        sel = sb("p6sel", [128, NT, CAP], BF16)
        t_sel = Tok()
        xsT = sb("p6xsT", [128, 8, CAP], BF16)
        t_xsT = Tok()
        actT = sb("p6actT", [128, 16, CAP], BF16)
        t_actT = Tok()
        yes = sb("p6ye", [128, 4, D], BF16)
        t_yes = Tok()
        h2r = _mk_ring(st, nc, "p6h2", 4, [128, D], BF16)
        wgr = _mk_ring(st, nc, "p6wg", 2, [128, 8, 1024], BF16)
        wur = _mk_ring(st, nc, "p6wu", 2, [128, 8, 1024], BF16)
        wdr = _mk_ring(st, nc, "p6wd", 2, [128, 8, D], BF16)
        gfr = _mk_ring(st, nc, "p6gf", 2, [128, CAP], F32)
        psm = _mk_ring(st, nc, "p6psm", 8, [128, 512], F32, psum=True)

        for ex in range(NE):
            for i in range(NT):
                eng = "dve" if i % 2 == 0 else "pool"
                S.op(eng, lambda e, i=i, ex=ex: e.tensor_scalar(out=sel[:, i, :], in0=iota[:], scalar1=ptok[:, i, ex:ex + 1], scalar2=None,
                                                                 op0=ALU.is_equal), reads=[t_iota, t_ptok], writes=[t_sel])
            for half in range(2):
                accs = [psm.next() for _ in range(4)]
                for i in range(NT):
                    ht, t_h = h2r.next()
                    S.dma("sp", lambda e, ht=ht, i=i: e.dma_start(out=ht[:], in_=c.h2tok_d[i * 128:(i + 1) * 128, :]), reads=[c.t_h2], writes=[t_h])
                    for dl in range(4):
                        dc = half * 4 + dl
                        ps, t_ps = accs[dl]
                        S.op("pe", lambda e, ps=ps, ht=ht, dc=dc, i=i: e.matmul(ps[:], ht[:, dc * 128:(dc + 1) * 128], sel[:, i, :],
                                                                               start=(i == 0), stop=(i == NT - 1)),
                             reads=[t_h, t_sel], writes=[t_ps])
                for dl in range(4):
                    dc = half * 4 + dl
                    ps, t_ps = accs[dl]
                    S.op("act", lambda e, ps=ps, dc=dc: e.copy(xsT[:, dc, :], ps[:]), reads=[t_ps], writes=[t_xsT])
            for fh in range(2):
                wg, t_wg = wgr.next()
                wu, t_wu = wur.next()
                for kc in range(8):
                    S.dma("pool", lambda e, wg=wg, kc=kc, ex=ex, fh=fh: e.dma_start(
                        out=wg[:, kc, :], in_=c.w_gate[ex, kc * 128:(kc + 1) * 128, fh * 1024:(fh + 1) * 1024]), writes=[t_wg])
                    S.dma("pool", lambda e, wu=wu, kc=kc, ex=ex, fh=fh: e.dma_start(
                        out=wu[:, kc, :], in_=c.w_up[ex, kc * 128:(kc + 1) * 128, fh * 1024:(fh + 1) * 1024]), writes=[t_wu])
                for fl in range(8):
                    fc = fh * 8 + fl
                    fs = slice(fl * 128, (fl + 1) * 128)
                    pg, t_pg = psm.next()
                    pu, t_pu = psm.next()
                    for kc in range(8):
                        S.op("pe", lambda e, pg=pg, wg=wg, kc=kc, fs=fs: e.matmul(pg[:], wg[:, kc, fs], xsT[:, kc, :], start=(kc == 0), stop=(kc == 7)),
                             reads=[t_wg, t_xsT], writes=[t_pg])
                    for kc in range(8):
                        S.op("pe", lambda e, pu=pu, wu=wu, kc=kc, fs=fs: e.matmul(pu[:], wu[:, kc, fs], xsT[:, kc, :], start=(kc == 0), stop=(kc == 7)),
                             reads=[t_wu, t_xsT], writes=[t_pu])
                    gf, t_gf = gfr.next()
                    S.op("act", lambda e, gf=gf, pg=pg: e.activation(gf[:], pg[:], AF.Silu), reads=[t_pg], writes=[t_gf])
                    S.op("dve", lambda e, gf=gf, pu=pu, fc=fc: e.tensor_tensor(out=actT[:, fc, :], in0=pu[:], in1=gf[:], op=ALU.mult),
                         reads=[t_pu, t_gf], writes=[t_actT])
            accs = [psm.next() for _ in range(8)]
            for fh in range(2):
                wd, t_wd = wdr.next()
                for fl in range(8):
                    fc = fh * 8 + fl
                    S.dma("pool", lambda e, wd=wd, fl=fl, fc=fc, ex=ex: e.dma_start(
                        out=wd[:, fl, :], in_=c.w_down[ex, fc * 128:(fc + 1) * 128, :]), writes=[t_wd])
                for fl in range(8):
                    fc = fh * 8 + fl
                    for sc in range(4):
                        for dh in range(2):
                            ps, t_ps = accs[sc * 2 + dh]
                            S.op("pe", lambda e, ps=ps, fc=fc, sc=sc, wd=wd, fl=fl, dh=dh: e.matmul(
                                ps[:], actT[:, fc, sc * 128:(sc + 1) * 128], wd[:, fl, dh * 512:(dh + 1) * 512], start=(fc == 0), stop=(fc == 15)),
                                reads=[t_actT, t_wd], writes=[t_ps])
            for sc in range(4):
                for dh in range(2):
                    ps, t_ps = accs[sc * 2 + dh]
                    eng = "act" if dh == 0 else "dve"
                    if eng == "act":
                        S.op("act", lambda e, ps=ps, sc=sc, dh=dh: e.copy(yes[:, sc, dh * 512:(dh + 1) * 512], ps[:]), reads=[t_ps], writes=[t_yes])
                    else:
                        S.op("dve", lambda e, ps=ps, sc=sc, dh=dh: e.tensor_copy(yes[:, sc, dh * 512:(dh + 1) * 512], ps[:]), reads=[t_ps], writes=[t_yes])
            S.dma("sp", lambda e, ex=ex: e.dma_start(out=c.ye_d[ex].rearrange("(sc p) d -> p sc d", p=128), in_=yes[:]),
                  reads=[t_yes], writes=[c.t_ye])
    S.barrier()


def phase6b(c):
    nc, S = c.nc, c.S
    TC = c.t_const
    with ExitStack() as st:
        sb = lambda name, shape, dt: st.enter_context(nc.sbuf_tensor(name, shape, dt))
        yall = sb("p7ye", [128, NE, 4, D], BF16)
        t_yall = Tok()
        for ex in range(NE):
            S.dma("sp", lambda e, ex=ex: e.dma_start(out=yall[:, ex, :, :], in_=c.ye_d[ex].rearrange("(sc p) d -> p sc d", p=128)),
                  reads=[c.t_ye], writes=[Tok()])
        S.barrier()
        aff = sb("p7aff", [128, NT, NE], F32)
        t_aff = Tok()
        S.dma("sp", lambda e: e.dma_start(out=aff[:], in_=c.aff_d), reads=[c.t_aff], writes=[t_aff])
        nfw = sb("p7nfw", [128, D], F32)
        t_nfw = Tok()
        S.dma("sp", lambda e: e.dma_start(out=nfw[:], in_=c.norm_f_w.broadcast_to([128, D])), writes=[t_nfw])
        pidx = sb("p7pidx", [128, 4], F32)
        t_pidx = Tok()
        S.op("pool", lambda e: e.iota(pidx[:], pattern=[[128, 4]], base=0, channel_multiplier=1, allow_small_or_imprecise_dtypes=True),
             writes=[t_pidx])
        posbc = sb("p7posbc", [128, NE, 512], F32)
        t_posbc = Tok()
        selT = _mk_ring(st, nc, "p7selT", 8, [128, 512], BF16)
        accr = _mk_ring(st, nc, "p7acc", 4, [128, D], F32)
        sqr = _mk_ring(st, nc, "p7sq", 2, [128, D], BF16)
        ssr = _mk_ring(st, nc, "p7ss", 4, [128, 2], F32)
        psm = _mk_ring(st, nc, "p7psm", 6, [128, 512], F32, psum=True)
        for g in range(NG):
            gs = slice(g * 512, (g + 1) * 512)
            S.dma("sp", lambda e, gs=gs: e.dma_start(out=posbc[:], in_=c.pos_d[:, gs].unsqueeze(0).broadcast_to([128, NE, 512])),
                  reads=[c.t_pos], writes=[t_posbc])
            accs = []
            for tl in range(4):
                i = g * 4 + tl
                acc, t_acc = accr.next()
                S.dma("sp", lambda e, acc=acc, i=i: e.dma_start(out=acc[:], in_=c.x1_d[i * 128:(i + 1) * 128, :]), reads=[c.t_x1], writes=[t_acc])
                accs.append((acc, t_acc))
            for ex in range(NE):
                sts = []
                for sc in range(4):
                    sT, t_sT = selT.next()
                    eng = "dve" if sc % 2 == 0 else "pool"
                    S.op(eng, lambda e, sT=sT, ex=ex, sc=sc: e.tensor_scalar(out=sT[:], in0=posbc[:, ex, :], scalar1=pidx[:, sc:sc + 1], scalar2=None,
                                                                            op0=ALU.is_equal), reads=[t_posbc, t_pidx], writes=[t_sT])
                    sts.append((sT, t_sT))
                for tl in range(4):
                    i = g * 4 + tl
                    acc, t_acc = accs[tl]
                    for dh in range(2):
                        ps, t_ps = psm.next()
                        for sc in range(4):
                            sT, t_sT = sts[sc]
                            S.op("pe", lambda e, ps=ps, sT=sT, tl=tl, ex=ex, sc=sc, dh=dh: e.matmul(
                                ps[:], sT[:, tl * 128:(tl + 1) * 128], yall[:, ex, sc, dh * 512:(dh + 1) * 512], start=(sc == 0), stop=(sc == 3)),
                                reads=[t_sT], writes=[t_ps])
                        S.op("dve", lambda e, acc=acc, ps=ps, i=i, ex=ex, dh=dh: e.scalar_tensor_tensor(
                            out=acc[:, dh * 512:(dh + 1) * 512], in0=ps[:], scalar=aff[:, i, ex:ex + 1], in1=acc[:, dh * 512:(dh + 1) * 512],
                            op0=ALU.mult, op1=ALU.add), reads=[t_ps, t_aff, t_acc], writes=[t_acc])
            for tl in range(4):
                i = g * 4 + tl
                acc, t_acc = accs[tl]
                sq, t_sq = sqr.next()
                ss, t_ss = ssr.next()
                S.op("act", lambda e, acc=acc, sq=sq, ss=ss: e.activation(sq[:], acc[:], AF.Square, accum_out=ss[:, 0:1]),
                     reads=[t_acc], writes=[t_sq, t_ss])
                S.op("act", lambda e, ss=ss: e.activation(ss[:, 1:2], ss[:, 0:1], AF.Sqrt, bias=EPS, scale=1.0 / D), reads=[t_ss], writes=[t_ss])
                S.op("dve", lambda e, ss=ss: e.reciprocal(ss[:, 1:2], ss[:, 1:2]), reads=[t_ss], writes=[t_ss])
                S.op("dve", lambda e, acc=acc, ss=ss: e.scalar_tensor_tensor(
                    out=acc[:], in0=acc[:], scalar=ss[:, 1:2], in1=nfw[:], op0=ALU.mult, op1=ALU.mult),
                    reads=[t_acc, t_ss, t_nfw], writes=[t_acc])
                S.dma("sp", lambda e, acc=acc, i=i: e.dma_start(out=c.out[i * 128:(i + 1) * 128, :], in_=acc[:]), reads=[t_acc], writes=[c.t_out])
    S.barrier()


ALL_PHASES = ("p1", "p2", "p3", "p4", "p5", "p6a", "p6b")


def kernel(**inputs):
    n_cores = 8
    shared = host_prep(inputs)
    nc, c = build_program(debug=False, phases=ALL_PHASES)
    x = np.asarray(inputs["x"], dtype=np.float32)
    in_maps = []
    for b in range(n_cores):
        m = dict(shared)
        m["x"] = np.ascontiguousarray(x[b])
        in_maps.append({k: m[k] for k in c.input_names})
    res = run_bass_kernel_spmd(nc, in_maps, core_ids=list(range(n_cores)))
    out = np.stack([np.asarray(r["out"], dtype=np.float32) for r in res.results], axis=0)
    return out
```

```python
import math
from contextlib import ExitStack
import numpy as np
import concourse.bass as bass
import concourse.mybir as mybir
from concourse.bass_utils import run_bass_kernel_spmd

F32 = mybir.dt.float32
BF16 = mybir.dt.bfloat16
AF = mybir.ActivationFunctionType
ALU = mybir.AluOpType

S_LEN = 4096
D = 1024
NT = S_LEN // 128
NG = S_LEN // 512
H = 8
IN_W = 9248
C_QA, C_KA, C_VA, C_GQ, C_GK, C_GV, C_Z, C_SM, C_GA, C_GG = 0, 1024, 2048, 3072, 4096, 5120, 6144, 7168, 7200, 8224
NE = 16
FF = 2048
CAP = 512
EPS = 1e-6
LAMBDA_INIT = 0.8 - 0.6 * math.exp(-0.3 * 0)

ENGS = ("pe", "act", "dve", "pool", "sp")


class Tok:
    __slots__ = ("w", "r", "excl")

    def __init__(self, excl=False):
        self.w = None
        self.r = {}
        self.excl = excl


class Sched:
    NDMA = 32

    def __init__(self, nc):
        self.nc = nc
        self.sems = {}
        for e in ENGS:
            self.sems[e] = nc.alloc_semaphore(name="sem_" + e)
        for i in range(self.NDMA):
            self.sems[("d", i)] = nc.alloc_semaphore(name="sem_dma%d" % i)
        self.cnt = {k: 0 for k in self.sems}
        self.seen = {e: {} for e in ENGS}
        self.prog = {e: [] for e in ENGS}
        self.ndma = 0
        self.ninstr = 0

    def _deps(self, reads, writes):
        deps = {}
        for t in reads:
            if t.w is not None:
                k, v = t.w
                if deps.get(k, 0) < v:
                    deps[k] = v
        for t in writes:
            if t.w is not None:
                k, v = t.w
                if deps.get(k, 0) < v:
                    deps[k] = v
            for k, v in t.r.items():
                if deps.get(k, 0) < v:
                    deps[k] = v
        return deps

    def _emit_waits(self, e, deps):
        seen = self.seen[e]
        for k, v in deps.items():
            if seen.get(k, 0) >= v:
                continue
            seen[k] = v
            sem = self.sems[k]
            self.prog[e].append(lambda eng, sem=sem, v=v: eng.wait_ge(sem, v))

    def op(self, e, fn, reads=(), writes=()):
        if any(t.excl for t in reads):
            writes = list(writes) + [t for t in reads if t.excl]
            reads = [t for t in reads if not t.excl]
        deps = self._deps(reads, writes)
        if e == "pe":
            deps.pop("pe", None)
        self._emit_waits(e, deps)
        sem = self.sems[e]
        self.cnt[e] += 1
        n = self.cnt[e]
        self.prog[e].append(lambda eng, fn=fn, sem=sem: fn(eng).then_inc(sem, 1))
        for t in reads:
            if t.r.get(e, 0) < n:
                t.r[e] = n
        for t in writes:
            t.w = (e, n)
            t.r = {}
        self.ninstr += 1

    def dma(self, e, fn, reads=(), writes=()):
        i = self.ndma % self.NDMA
        self.ndma += 1
        k = ("d", i)
        deps = self._deps(reads, writes)
        if self.cnt[k] > 0:
            deps[k] = max(deps.get(k, 0), self.cnt[k])
        self._emit_waits(e, deps)
        self.cnt[k] += 16
        v = self.cnt[k]
        sem = self.sems[k]
        self.prog[e].append(lambda eng, fn=fn, sem=sem: fn(eng).then_inc(sem, 16))
        for t in reads:
            if t.r.get(k, 0) < v:
                t.r[k] = v
        for t in writes:
            t.w = (k, v)
            t.r = {}
        self.ninstr += 1

    def barrier(self):
        deps = {k: v for k, v in self.cnt.items() if v > 0}
        for e in ENGS:
            self._emit_waits(e, dict(deps))

    def emit(self):
        nc = self.nc
        prog = self.prog
        with nc.Block() as block:
            @block.tensor
            def _(eng):
                for f in prog["pe"]:
                    f(eng)

            @block.scalar
            def _(eng):
                for f in prog["act"]:
                    f(eng)

            @block.vector
            def _(eng):
                for f in prog["dve"]:
                    f(eng)

            @block.gpsimd
            def _(eng):
                for f in prog["pool"]:
                    f(eng)

            @block.sync
            def _(eng):
                for f in prog["sp"]:
                    f(eng)


class Ring:
    def __init__(self, items):
        self.items = items
        self.i = 0

    def next(self):
        it = self.items[self.i % len(self.items)]
        self.i += 1
        return it


class Ctx:
    pass


def _interleave(gens, width):
    it = iter(gens)
    active = []
    exhausted = False
    while True:
        while len(active) < width and not exhausted:
            try:
                active.append(next(it))
            except StopIteration:
                exhausted = True
        if not active:
            break
        for g in list(active):
            try:
                next(g)
            except StopIteration:
                active.remove(g)


def _mk_ring(stack, nc, name, n, shape, dt, psum=False):
    items = []
    for i in range(n):
        if psum:
            t = stack.enter_context(nc.psum_tensor("%s%d" % (name, i), shape, dt))
        else:
            t = stack.enter_context(nc.sbuf_tensor("%s%d" % (name, i), shape, dt))
        items.append((t, Tok()))
    return Ring(items)


def _consts(c, stack):
    nc, S = c.nc, c.S
    c.ident_f = nc.alloc_sbuf_tensor("ident_f", [128, 128], F32)
    c.ident_b = nc.alloc_sbuf_tensor("ident_b", [128, 128], BF16)
    c.ones_b = nc.alloc_sbuf_tensor("ones_b", [128, 128], BF16)
    c.ones_f = nc.alloc_sbuf_tensor("ones_f", [128, 128], F32)
    c.t_const = Tok()
    tc = c.t_const
    S.op("pool", lambda e: e.memset(c.ident_f[:], 0.0), writes=[tc])
    S.op("pool", lambda e: e.affine_select(out=c.ident_f[:], in_=c.ident_f[:], compare_op=ALU.not_equal, fill=1.0,
                                           base=0, pattern=[[-1, 128]], channel_multiplier=1),
         reads=[tc], writes=[tc])
    S.op("pool", lambda e: e.tensor_copy(c.ident_b[:], c.ident_f[:]), reads=[tc], writes=[tc])
    S.op("pool", lambda e: e.memset(c.ones_b[:], 1.0), writes=[tc])
    S.op("pool", lambda e: e.memset(c.ones_f[:], 1.0), writes=[tc])


def phase1(c):
    nc, S = c.nc, c.S
    TC = c.t_const
    with ExitStack() as st:
        sb = lambda name, shape, dt: st.enter_context(nc.sbuf_tensor(name, shape, dt))
        hT = sb("hT", [128, 8, S_LEN], BF16)
        t_hT = [Tok() for _ in range(NT)]
        stA = ExitStack()
        sbA = lambda name, shape, dt: stA.enter_context(nc.sbuf_tensor(name, shape, dt))
        n1w = sbA("n1w", [128, D], F32)
        t_n1w = Tok()
        S.dma("sp", lambda e: e.dma_start(out=n1w[:], in_=c.norm1_w.broadcast_to([128, D])), writes=[t_n1w])
        xring = _mk_ring(stA, nc, "p1x", 3, [128, D], F32)
        sqring = _mk_ring(stA, nc, "p1sq", 2, [128, D], BF16)
        hring = _mk_ring(stA, nc, "p1h", 2, [128, D], BF16)
        ssring = _mk_ring(stA, nc, "p1ss", 4, [128, 2], F32)
        pst = _mk_ring(st, nc, "p1pst", 2, [128, 8, 128], BF16, psum=True)
        psm = _mk_ring(st, nc, "p1psm", 5, [128, 512], F32, psum=True)

        for i in range(NT):
            xt, t_x = xring.next()
            S.dma("sp", lambda e, xt=xt, i=i: e.dma_start(out=xt[:], in_=c.x[i * 128:(i + 1) * 128, :]), writes=[t_x])
            sq, t_sq = sqring.next()
            ss, t_ss = ssring.next()
            S.op("act", lambda e, xt=xt, sq=sq, ss=ss: e.activation(sq[:], xt[:], AF.Square, accum_out=ss[:, 0:1]),
                 reads=[t_x], writes=[t_sq, t_ss])
            S.op("act", lambda e, ss=ss: e.activation(ss[:, 1:2], ss[:, 0:1], AF.Sqrt, bias=EPS, scale=1.0 / D),
                 reads=[t_ss], writes=[t_ss])
            S.op("dve", lambda e, ss=ss: e.reciprocal(ss[:, 1:2], ss[:, 1:2]), reads=[t_ss], writes=[t_ss])
            ht, t_h = hring.next()
            S.op("dve", lambda e, ht=ht, xt=xt, ss=ss: e.scalar_tensor_tensor(
                out=ht[:], in0=xt[:], scalar=ss[:, 1:2], in1=n1w[:], op0=ALU.mult, op1=ALU.mult),
                reads=[t_x, t_ss, t_n1w], writes=[t_h])
            pt, t_pt = pst.next()
            for dc in range(8):
                S.op("pe", lambda e, pt=pt, ht=ht, dc=dc: e.transpose(pt[:, dc, :], ht[:, dc * 128:(dc + 1) * 128], c.ident_b[:]),
                     reads=[t_h, TC], writes=[t_pt])
            S.op("act", lambda e, pt=pt, i=i: e.copy(hT[:, :, i * 128:(i + 1) * 128], pt[:]),
                 reads=[t_pt], writes=[t_hT[i]])

        S.barrier()
        stA.close()
        wfm = _mk_ring(st, nc, "p1wfm", 4, [128, 8, 128], BF16)
        wtm = _mk_ring(st, nc, "p1wtm", 2, [128, 8, 512], BF16)

        def load_w(ring, src, col0, ncols):
            wt, t_w = ring.next()
            S.dma("pool", lambda e: e.dma_start(
                out=wt[:, :, 0:ncols], in_=src[:, col0:col0 + ncols].rearrange("(kc p) n -> p kc n", p=128)),
                writes=[t_w])
            return wt, t_w

        def proj_fm(wt, t_w, g):
            ps, t_ps = psm.next()
            for kc in range(8):
                S.op("pe", lambda e, ps=ps, kc=kc: e.matmul(ps[:], wt[:, kc, :], hT[:, kc, g * 512:(g + 1) * 512],
                                                          start=(kc == 0), stop=(kc == 7)),
                     reads=[t_w] + t_hT[g * 4:(g + 1) * 4], writes=[t_ps])
            return ps, t_ps

        def proj_tm(wt, t_w, i, ncols):
            ps, t_ps = psm.next()
            for kc in range(8):
                S.op("pe", lambda e, ps=ps, kc=kc: e.matmul(ps[:, 0:ncols], hT[:, kc, i * 128:(i + 1) * 128], wt[:, kc, 0:ncols],
                                                          start=(kc == 0), stop=(kc == 7)),
                     reads=[t_w, t_hT[i]], writes=[t_ps])
            return ps, t_ps

        stage = _mk_ring(st, nc, "p1stage", 2, [128, S_LEN], BF16)
        f32a = _mk_ring(st, nc, "p1f32a", 3, [128, 512], F32)
        f32b = _mk_ring(st, nc, "p1f32b", 3, [128, 512], F32)

        stB = ExitStack()
        cosT = stB.enter_context(nc.sbuf_tensor("cosT_sb", [128, S_LEN], F32))
        sinT = stB.enter_context(nc.sbuf_tensor("sinT_sb", [128, S_LEN], F32))
        t_rope = Tok()
        S.dma("sp", lambda e: e.dma_start(out=cosT[:], in_=c.cosT), writes=[t_rope])
        S.dma("sp", lambda e: e.dma_start(out=sinT[:], in_=c.sinT), writes=[t_rope])

        for (col0, dst) in ((C_QA, c.qT_d), (C_KA, c.kT_d)):
            for h in range(H):
                w1, t_w1 = load_w(wfm, c.w_in, col0 + h * 128, 128)
                w2, t_w2 = load_w(wfm, c.w_qkp, (col0 // 1024) * 1024 + h * 128, 128)
                stg, t_stg = stage.next()
                for g in range(NG):
                    ps1, t_ps1 = proj_fm(w1, t_w1, g)
                    ps2, t_ps2 = proj_fm(w2, t_w2, g)
                    a, t_a = f32a.next()
                    b, t_b = f32b.next()
                    sl = slice(g * 512, (g + 1) * 512)
                    S.op("dve", lambda e, a=a, ps1=ps1, sl=sl: e.tensor_tensor(out=a[:], in0=ps1[:], in1=cosT[:, sl], op=ALU.mult),
                         reads=[t_ps1, t_rope], writes=[t_a])
                    S.op("dve", lambda e, b=b, ps2=ps2, sl=sl: e.tensor_tensor(out=b[:], in0=ps2[:], in1=sinT[:, sl], op=ALU.mult),
                         reads=[t_ps2, t_rope], writes=[t_b])
                    S.op("pool", lambda e, a=a, b=b, stg=stg, sl=sl: e.tensor_tensor(out=stg[:, sl], in0=a[:], in1=b[:], op=ALU.add),
                         reads=[t_a, t_b], writes=[t_stg])
                S.dma("sp", lambda e, stg=stg, dst=dst, h=h: e.dma_start(out=dst[h], in_=stg[:]), reads=[t_stg], writes=[c.t_qk])

        S.barrier()
        stB.close()
        for (col0, dst) in ((C_GA, c.sga_d), (C_GG, c.sgg_d)):
            for h in range(8):
                w1, t_w1 = load_w(wfm, c.w_in, col0 + h * 128, 128)
                stg, t_stg = stage.next()
                for g in range(NG):
                    ps1, t_ps1 = proj_fm(w1, t_w1, g)
                    sl = slice(g * 512, (g + 1) * 512)
                    S.op("act", lambda e, ps1=ps1, stg=stg, sl=sl: e.activation(stg[:, sl], ps1[:], AF.Sigmoid),
                         reads=[t_ps1], writes=[t_stg])
                S.dma("sp", lambda e, stg=stg, dst=dst, h=h: e.dma_start(out=dst[h], in_=stg[:]), reads=[t_stg], writes=[c.t_gates])

        tmst = _mk_ring(st, nc, "p1tmst", 3, [128, 512], BF16)
        for (col0, dst, fn) in ((C_VA, c.v_d, None), (C_Z, c.zs_d, AF.Silu)):
            for half in range(2):
                wt, t_w = load_w(wtm, c.w_in, col0 + half * 512, 512)
                for i in range(NT):
                    ps, t_ps = proj_tm(wt, t_w, i, 512)
                    o, t_o = tmst.next()
                    if fn is None:
                        S.op("act", lambda e, o=o, ps=ps: e.copy(o[:], ps[:]), reads=[t_ps], writes=[t_o])
                    else:
                        S.op("act", lambda e, o=o, ps=ps, fn=fn: e.activation(o[:], ps[:], fn), reads=[t_ps], writes=[t_o])
                    S.dma("sp", lambda e, o=o, dst=dst, i=i, half=half: e.dma_start(
                        out=dst[i * 128:(i + 1) * 128, half * 512:(half + 1) * 512], in_=o[:]), reads=[t_o], writes=[c.t_vz])

        sm = sb("p1sm", [128, NT, 32], F32)
        t_sm = Tok()
        wt, t_w = load_w(wtm, c.w_in, C_SM, 32)
        for i in range(NT):
            ps, t_ps = proj_tm(wt, t_w, i, 32)
            S.op("dve", lambda e, ps=ps, i=i: e.tensor_copy(sm[:, i, :], ps[:, 0:32]), reads=[t_ps], writes=[t_sm])
        prm = sb("p1prm", [128, 32], F32)
        t_prm = Tok()
        for j, src in enumerate((c.dt_bias_fwd, c.dt_bias_bwd, c.a_log_fwd, c.a_log_bwd)):
            S.dma("sp", lambda e, j=j, src=src: e.dma_start(out=prm[:, j * 8:(j + 1) * 8], in_=src.broadcast_to([128, 8])),
                  writes=[t_prm])
        tmpa = sb("p1tmpa", [128, NT, 16], F32)
        tmpb = sb("p1tmpb", [128, NT, 16], F32)
        t_ta, t_tb = Tok(), Tok()
        dtb = prm[:, 0:16].unsqueeze(1).broadcast_to([128, NT, 16])
        S.op("act", lambda e: e.activation(prm[:, 16:32], prm[:, 16:32], AF.Exp), reads=[t_prm], writes=[t_prm])
        nA = prm[:, 16:32].unsqueeze(1).broadcast_to([128, NT, 16])
        S.op("dve", lambda e: e.tensor_tensor(out=sm[:, :, 0:16], in0=sm[:, :, 0:16], in1=dtb, op=ALU.add),
             reads=[t_sm, t_prm], writes=[t_sm])
        S.op("act", lambda e: e.activation(tmpa[:], sm[:, :, 0:16], AF.Abs), reads=[t_sm], writes=[t_ta])
        S.op("act", lambda e: e.activation(tmpa[:], tmpa[:], AF.Exp, scale=-1.0), reads=[t_ta], writes=[t_ta])
        S.op("act", lambda e: e.activation(tmpa[:], tmpa[:], AF.Ln, bias=1.0), reads=[t_ta], writes=[t_ta])
        S.op("dve", lambda e: e.scalar_tensor_tensor(out=tmpb[:], in0=sm[:, :, 0:16], scalar=0.0, in1=tmpa[:],
                                                     op0=ALU.max, op1=ALU.add), reads=[t_sm, t_ta], writes=[t_tb])
        S.op("dve", lambda e: e.scalar_tensor_tensor(out=sm[:, :, 0:16], in0=tmpb[:], scalar=-1.0, in1=nA,
                                                     op0=ALU.mult, op1=ALU.mult), reads=[t_tb, t_prm], writes=[t_sm])
        S.op("act", lambda e: e.activation(sm[:, :, 16:32], sm[:, :, 16:32], AF.Sigmoid), reads=[t_sm], writes=[t_sm])
        S.dma("sp", lambda e: e.dma_start(out=c.small_d, in_=sm[:]), reads=[t_sm], writes=[c.t_small])

        cw5 = sb("p1cw5", [5, 3072], F32)
        t_cw5 = Tok()
        S.dma("sp", lambda e: e.dma_start(out=cw5[:], in_=c.conv_w), writes=[t_cw5])
        cwT = sb("p1cwT", [128, 24, 8], F32)
        t_cwT = Tok()
        for ct in range(24):
            ps, t_ps = psm.next()
            S.op("pe", lambda e, ps=ps, ct=ct: e.transpose(ps[:, 0:5], cw5[0:5, ct * 128:(ct + 1) * 128], c.ident_f[0:5, 0:5]),
                 reads=[t_cw5, TC], writes=[t_ps])
            S.op("dve", lambda e, ps=ps, ct=ct: e.tensor_copy(cwT[:, ct, 0:5], ps[:, 0:5]), reads=[t_ps], writes=[t_cwT])
        dgs = _mk_ring(st, nc, "p1dg", 2, [128, 5, 128], BF16)
        xpre = _mk_ring(st, nc, "p1xpre", 2, [128, S_LEN + 4], BF16)
        tmstage = _mk_ring(st, nc, "p1tmstage", 2, [128, NT, 128], BF16)
        for ct in range(24):
            kind = ct // 8
            h = ct % 8
            w1, t_w1 = load_w(wfm, c.w_in, C_GQ + ct * 128, 128)
            dg, t_dg = dgs.next()
            for j in range(5):
                S.op("pool", lambda e, dg=dg, j=j, ct=ct: e.tensor_scalar(
                    out=dg[:, j, :], in0=c.ident_f[:], scalar1=cwT[:, ct, j:j + 1], scalar2=None, op0=ALU.mult),
                    reads=[TC, t_cwT], writes=[t_dg])
            xp, t_xp = xpre.next()
            S.op("pool", lambda e, xp=xp: e.memset(xp[:, 0:2], 0.0), writes=[t_xp])
            S.op("pool", lambda e, xp=xp: e.memset(xp[:, S_LEN + 2:S_LEN + 4], 0.0), writes=[t_xp])
            for g in range(NG):
                ps1, t_ps1 = proj_fm(w1, t_w1, g)
                S.op("act", lambda e, xp=xp, ps1=ps1, g=g: e.copy(xp[:, 2 + g * 512:2 + (g + 1) * 512], ps1[:]),
                     reads=[t_ps1], writes=[t_xp])
            stg, t_stg = stage.next()
            for g in range(NG):
                ps, t_ps = psm.next()
                for j in range(5):
                    S.op("pe", lambda e, ps=ps, dg=dg, xp=xp, g=g, j=j: e.matmul(
                        ps[:], dg[:, j, :], xp[:, g * 512 + j:g * 512 + j + 512], start=(j == 0), stop=(j == 4)),
                        reads=[t_dg, t_xp], writes=[t_ps])
                sl = slice(g * 512, (g + 1) * 512)
                if kind == 2:
                    S.op("act", lambda e, ps=ps, stg=stg, sl=sl: e.activation(stg[:, sl], ps[:], AF.Silu),
                         reads=[t_ps], writes=[t_stg])
                else:
                    a, t_a = f32a.next()
                    S.op("act", lambda e, ps=ps, a=a: e.activation(a[:], ps[:], AF.Silu), reads=[t_ps], writes=[t_a])
                    sqb, t_sqb = tmst.next()
                    S.op("pool", lambda e, sqb=sqb, a=a: e.tensor_tensor(out=sqb[:], in0=a[:], in1=a[:], op=ALU.mult),
                         reads=[t_a], writes=[t_sqb])
                    ps2, t_ps2 = psm.next()
                    S.op("pe", lambda e, ps2=ps2, sqb=sqb: e.matmul(ps2[:], c.ones_b[:], sqb[:], start=True, stop=True),
                         reads=[t_sqb, TC], writes=[t_ps2])
                    b, t_b = f32b.next()
                    S.op("act", lambda e, b=b, ps2=ps2: e.activation(b[:], ps2[:], AF.Sqrt, bias=EPS), reads=[t_ps2], writes=[t_b])
                    S.op("dve", lambda e, b=b: e.reciprocal(b[:], b[:]), reads=[t_b], writes=[t_b])
                    scl = (128.0 ** -0.5) if kind == 0 else 1.0
                    S.op("dve", lambda e, a=a, b=b, stg=stg, sl=sl, scl=scl: e.scalar_tensor_tensor(
                        out=stg[:, sl], in0=a[:], scalar=scl, in1=b[:], op0=ALU.mult, op1=ALU.mult),
                        reads=[t_a, t_b], writes=[t_stg])
            if kind < 2:
                dst = c.gq_d if kind == 0 else c.gk_d
                S.dma("sp", lambda e, stg=stg, dst=dst, h=h: e.dma_start(out=dst[h], in_=stg[:]), reads=[t_stg], writes=[c.t_gqk])
            if kind >= 1:
                dst = c.gktok_d if kind == 1 else c.gvtok_d
                tms, t_tms = tmstage.next()
                for i8 in range(4):
                    pt, t_pt = pst.next()
                    for ii in range(8):
                        i = i8 * 8 + ii
                        S.op("pe", lambda e, pt=pt, stg=stg, ii=ii, i=i: e.transpose(pt[:, ii, :], stg[:, i * 128:(i + 1) * 128], c.ident_b[:]),
                             reads=[t_stg, TC], writes=[t_pt])
                    S.op("dve", lambda e, pt=pt, tms=tms, i8=i8: e.tensor_copy(tms[:, i8 * 8:(i8 + 1) * 8, :], pt[:]),
                         reads=[t_pt], writes=[t_tms])
                S.dma("sp", lambda e, tms=tms, dst=dst, h=h: e.dma_start(
                    out=dst[:, h * 128:(h + 1) * 128].rearrange("(t p) c -> p t c", p=128), in_=tms[:]),
                    reads=[t_tms], writes=[c.t_gtok])
    S.barrier()


def build_program(debug=False, phases=("p1",), inject=(), opts=None):
    nc = bass.Bass("TRN2", target_bir_lowering=False)
    c = Ctx()
    c.inject = set(inject)
    c.opts = opts or {}
    c.nc = nc
    c.S = Sched(nc)

    c.input_names = []

    def din(name, shape):
        c.input_names.append(name)
        return nc.dram_tensor(name, list(shape), F32, kind="ExternalInput").ap()

    c.x = din("x", [S_LEN, D])
    c.norm1_w = din("norm1_w", [1, D])
    c.w_in = din("w_in", [D, IN_W])
    c.w_qkp = din("w_qkp", [D, 2048])
    c.cosT = din("cosT", [128, S_LEN])
    c.sinT = din("sinT", [128, S_LEN])
    c.conv_w = din("conv_w", [5, 3072])
    for n in ("a_log_fwd", "dt_bias_fwd", "a_log_bwd", "dt_bias_bwd"):
        setattr(c, n, din(n, [1, 8]))

    c.debug_names = []

    def scratch(name, shape, dt):
        kind = "ExternalOutput" if debug else "Internal"
        if name in c.inject:
            kind = "ExternalInput"
            c.input_names.append(name)
        elif debug:
            c.debug_names.append(name)
        return nc.dram_tensor(name, list(shape), dt, kind=kind).ap()

    c.qT_d = scratch("qT_d", [H, 128, S_LEN], BF16)
    c.kT_d = scratch("kT_d", [H, 128, S_LEN], BF16)
    c.v_d = scratch("v_d", [S_LEN, D], BF16)
    c.zs_d = scratch("zs_d", [S_LEN, D], BF16)
    c.sga_d = scratch("sga_d", [H, 128, S_LEN], BF16)
    c.sgg_d = scratch("sgg_d", [H, 128, S_LEN], BF16)
    c.gq_d = scratch("gq_d", [H, 128, S_LEN], BF16)
    c.gk_d = scratch("gk_d", [H, 128, S_LEN], BF16)
    c.gktok_d = scratch("gktok_d", [S_LEN, D], BF16)
    c.gvtok_d = scratch("gvtok_d", [S_LEN, D], BF16)
    c.small_d = scratch("small_d", [128, NT, 32], F32)
    for n in ("lambda_q1", "lambda_k1", "lambda_q2", "lambda_k2"):
        setattr(c, n, din(n, [1, 64]))
    c.subln_w = din("subln_w", [1, 128])
    c.gdn_norm_w = din("gdn_norm_w", [1, 128])
    c.w_proj_attn = din("w_proj_attn", [D, D])
    c.w_proj_gdn = din("w_proj_gdn", [D, D])
    c.w_out = din("w_out", [D, D])
    c.w_router = din("w_router", [D, NE])
    c.norm2_w = din("norm2_w", [1, D])
    c.norm_f_w = din("norm_f_w", [1, D])
    c.w_gate = din("w_gate", [NE, D, FF])
    c.w_up = din("w_up", [NE, D, FF])
    c.w_down = din("w_down", [NE, FF, D])
    c.out = nc.dram_tensor("out", [S_LEN, D], F32, kind="ExternalOutput").ap()
    c.x1_d = scratch("x1_d", [S_LEN, D], F32)
    c.h2tok_d = scratch("h2tok_d", [S_LEN, D], BF16)
    c.aff_d = scratch("aff_d", [128, NT, NE], F32)
    c.pos_d = scratch("pos_d", [NE, S_LEN], F32)
    c.ptok_d = scratch("ptok_d", [128, NT, NE], F32)
    c.ye_d = scratch("ye_d", [NE, CAP, D], BF16)
    for n in ("t_x1", "t_h2", "t_aff", "t_pos", "t_ye", "t_out"):
        setattr(c, n, Tok())
    c.ogT_d = scratch("ogT_d", [H, 128, S_LEN], BF16)
    c.t_og = Tok()
    c.oaT_d = scratch("oaT_d", [H, 128, S_LEN], BF16)
    for n in ("t_qk", "t_gates", "t_vz", "t_small", "t_gqk", "t_gtok", "t_oa"):
        setattr(c, n, Tok())

    with ExitStack() as st:
        _consts(c, st)
        if "p1" in phases:
            phase1(c)
        if "p2" in phases:
            phase2(c)
        if "p3" in phases:
            phase3(c)
        if "p4" in phases:
            phase4(c)
        if "p5" in phases:
            phase5(c)
        if "p6a" in phases:
            phase6a(c)
        if "p6b" in phases:
            phase6b(c)
        c.S.barrier()
        c.S.emit()
    return nc, c


def host_prep(inputs):
    f = lambda a: np.ascontiguousarray(np.asarray(a, dtype=np.float32))
    w_in = f(inputs["w_in"][0])
    perm = np.arange(2048).reshape(16, 2, 2, 32)[:, :, ::-1, :].reshape(-1)
    w_qkp = np.ascontiguousarray(w_in[:, :2048][:, perm])
    inv = 10000.0 ** (-np.arange(0, 64, 2, dtype=np.float32) / 64)
    ang = np.arange(S_LEN, dtype=np.float32)[:, None] * inv[None, :]
    ang = np.concatenate([ang, ang], axis=-1)
    cos = np.cos(ang).T.astype(np.float32)
    sin = np.sin(ang).T.astype(np.float32)
    sin[:32] *= -1.0
    shared = {
        "norm1_w": f(inputs["norm1_w"]).reshape(1, D),
        "w_in": w_in,
        "w_qkp": w_qkp,
        "cosT": np.ascontiguousarray(np.concatenate([cos, cos], 0)),
        "sinT": np.ascontiguousarray(np.concatenate([sin, sin], 0)),
        "conv_w": f(inputs["conv_w"][0]),
    }
    for n in ("a_log_fwd", "dt_bias_fwd", "a_log_bwd", "dt_bias_bwd"):
        shared[n] = f(inputs[n]).reshape(1, 8)
    for n in ("lambda_q1", "lambda_k1", "lambda_q2", "lambda_k2"):
        shared[n] = f(inputs[n]).reshape(1, 64)
    shared["subln_w"] = f(inputs["subln_w"]).reshape(1, 128)
    shared["gdn_norm_w"] = f(inputs["gdn_norm_w"]).reshape(1, 128)
    shared["w_proj_attn"] = f(inputs["w_proj_attn"][0])
    shared["w_proj_gdn"] = f(inputs["w_proj_gdn"][0])
    shared["w_out"] = f(inputs["w_out"][0])
    shared["w_router"] = f(inputs["w_router"][0])
    shared["norm2_w"] = f(inputs["norm2_w"]).reshape(1, D)
    shared["norm_f_w"] = f(inputs["norm_f_w"]).reshape(1, D)
    shared["w_gate"] = f(inputs["w_gate"][0])
    shared["w_up"] = f(inputs["w_up"][0])
    shared["w_down"] = f(inputs["w_down"][0])
    return shared


def phase2(c):
    nc, S = c.nc, c.S
    TC = c.t_const
    with ExitStack() as st:
        sb = lambda name, shape, dt: st.enter_context(nc.sbuf_tensor(name, shape, dt))
        lam = sb("p2lam", [128, 4, 64], F32)
        lsc = sb("p2lsc", [128, 8], F32)
        t_lam = Tok()
        for j, src in enumerate((c.lambda_q1, c.lambda_k1, c.lambda_q2, c.lambda_k2)):
            S.dma("sp", lambda e, j=j, src=src: e.dma_start(out=lam[:, j, :], in_=src.broadcast_to([128, 64])), writes=[t_lam])
        S.op("dve", lambda e: e.tensor_tensor(out=lam[:, 0, :], in0=lam[:, 0, :], in1=lam[:, 1, :], op=ALU.mult), reads=[t_lam], writes=[t_lam])
        S.op("dve", lambda e: e.tensor_tensor(out=lam[:, 2, :], in0=lam[:, 2, :], in1=lam[:, 3, :], op=ALU.mult), reads=[t_lam], writes=[t_lam])
        S.op("act", lambda e: e.activation(lam[:, 1, :], lam[:, 0, :], AF.Identity, accum_out=lsc[:, 0:1]), reads=[t_lam], writes=[t_lam])
        S.op("act", lambda e: e.activation(lam[:, 3, :], lam[:, 2, :], AF.Identity, accum_out=lsc[:, 1:2]), reads=[t_lam], writes=[t_lam])
        S.op("act", lambda e: e.activation(lsc[:, 2:4], lsc[:, 0:2], AF.Exp), reads=[t_lam], writes=[t_lam])
        S.op("dve", lambda e: e.scalar_tensor_tensor(out=lsc[:, 4:5], in0=lsc[:, 3:4], scalar=-LAMBDA_INIT, in1=lsc[:, 2:3],
                                                     op0=ALU.add, op1=ALU.subtract), reads=[t_lam], writes=[t_lam])
        wsub = sb("p2wsub", [128, 2], F32)
        t_wsub = Tok()
        S.dma("sp", lambda e: e.dma_start(out=wsub[:, 0:1], in_=c.subln_w.rearrange("o e -> e o")), writes=[t_wsub])
        S.op("dve", lambda e: e.tensor_scalar(out=wsub[:, 1:2], in0=wsub[:, 0:1], scalar1=(1.0 - LAMBDA_INIT), scalar2=None, op0=ALU.mult),
             reads=[t_wsub], writes=[t_wsub])

        qr = _mk_ring(st, nc, "p2q", 2, [128, S_LEN], BF16)
        kr = _mk_ring(st, nc, "p2k", 2, [128, S_LEN], BF16)
        vr = _mk_ring(st, nc, "p2v", 2, [128, NT, 128], BF16)
        pr = _mk_ring(st, nc, "p2p", 4, [128, 512], BF16)
        fr = _mk_ring(st, nc, "p2f", 6, [128, 512], F32)
        sqr = _mk_ring(st, nc, "p2sq", 2, [128, 512], BF16)
        stage = _mk_ring(st, nc, "p2stage", 2, [128, S_LEN], BF16)
        ps_s = _mk_ring(st, nc, "p2pss", 3, [128, 512], F32, psum=True)
        ps_o = [_mk_ring(st, nc, "p2pso%d" % t, 1, [128, 512], F32, psum=True) for t in range(2)]
        ps_l = [_mk_ring(st, nc, "p2psl%d" % t, 1, [128, 512], F32, psum=True) for t in range(2)]
        ps_x = _mk_ring(st, nc, "p2psx", 1, [128, 512], F32, psum=True)

        for h in range(H):
            q, t_q = qr.next()
            k, t_k = kr.next()
            v, t_v = vr.next()
            S.dma("sp", lambda e, q=q, h=h: e.dma_start(out=q[:], in_=c.qT_d[h]), reads=[c.t_qk], writes=[t_q])
            S.dma("sp", lambda e, k=k, h=h: e.dma_start(out=k[:], in_=c.kT_d[h]), reads=[c.t_qk], writes=[t_k])
            S.dma("sp", lambda e, v=v, h=h: e.dma_start(
                out=v[:], in_=c.v_d[:, h * 128:(h + 1) * 128].rearrange("(t p) e -> p t e", p=128)), reads=[c.t_vz], writes=[t_v])
            stg, t_stg = stage.next()
            for g in range(NG):
                qs = slice(g * 512, (g + 1) * 512)
                accs = []
                for t in range(2):
                    po, t_po = ps_o[t].next()
                    pl, t_pl = ps_l[t].next()
                    ts = slice(t * 64, (t + 1) * 64)
                    def emit_s(j, ts=ts, qs=qs, k=k, q=q, t_k=t_k, t_q=t_q):
                        pss, t_pss = ps_s.next()
                        S.op("pe", lambda e, pss=pss, j=j: e.matmul(
                            pss[:], k[ts, j * 128:(j + 1) * 128], q[ts, qs], start=True, stop=True),
                            reads=[t_k, t_q], writes=[t_pss])
                        return pss, t_pss
                    cur = emit_s(0)
                    for j in range(NT):
                        nxt = emit_s(j + 1) if j + 1 < NT else None
                        pss, t_pss = cur
                        p, t_p = pr.next()
                        S.op("act", lambda e, p=p, pss=pss: e.activation(p[:], pss[:], AF.Exp, scale=0.125),
                             reads=[t_pss], writes=[t_p])
                        S.op("pe", lambda e, po=po, v=v, p=p, j=j: e.matmul(po[:], v[:, j, :], p[:], start=(j == 0), stop=(j == NT - 1)),
                             reads=[t_v, t_p], writes=[t_po])
                        S.op("pe", lambda e, pl=pl, p=p, j=j: e.matmul(pl[:], c.ones_b[:], p[:], start=(j == 0), stop=(j == NT - 1)),
                             reads=[TC, t_p], writes=[t_pl])
                        cur = nxt
                    accs.append((po, t_po, pl, t_pl))
                outs = []
                for t in range(2):
                    po, t_po, pl, t_pl = accs[t]
                    r, t_r = fr.next()
                    S.op("dve", lambda e, r=r, pl=pl: e.reciprocal(r[:], pl[:]), reads=[t_pl], writes=[t_r])
                    a, t_a = fr.next()
                    S.op("dve", lambda e, a=a, po=po, r=r: e.tensor_tensor(out=a[:], in0=po[:], in1=r[:], op=ALU.mult),
                         reads=[t_po, t_r], writes=[t_a])
                    outs.append((a, t_a))
                (a, t_a), (b, t_b) = outs
                oa, t_oa = fr.next()
                S.op("dve", lambda e, oa=oa, a=a, b=b: e.scalar_tensor_tensor(
                    out=oa[:], in0=b[:], scalar=lsc[:, 4:5], in1=a[:], op0=ALU.mult, op1=ALU.add),
                    reads=[t_a, t_b, t_lam], writes=[t_oa])
                sq, t_sq = sqr.next()
                S.op("pool", lambda e, sq=sq, oa=oa: e.tensor_tensor(out=sq[:], in0=oa[:], in1=oa[:], op=ALU.mult),
                     reads=[t_oa], writes=[t_sq])
                px, t_px = ps_x.next()
                S.op("pe", lambda e, px=px, sq=sq: e.matmul(px[:], c.ones_b[:], sq[:], start=True, stop=True),
                     reads=[TC, t_sq], writes=[t_px])
                rs, t_rs = fr.next()
                S.op("act", lambda e, rs=rs, px=px: e.activation(rs[:], px[:], AF.Sqrt, bias=EPS, scale=1.0 / 128), reads=[t_px], writes=[t_rs])
                S.op("dve", lambda e, rs=rs: e.reciprocal(rs[:], rs[:]), reads=[t_rs], writes=[t_rs])
                S.op("dve", lambda e, stg=stg, oa=oa, rs=rs, qs=qs: e.scalar_tensor_tensor(
                    out=stg[:, qs], in0=oa[:], scalar=wsub[:, 1:2], in1=rs[:], op0=ALU.mult, op1=ALU.mult),
                    reads=[t_oa, t_rs, t_wsub], writes=[t_stg])
            S.dma("sp", lambda e, stg=stg, h=h: e.dma_start(out=c.oaT_d[h], in_=stg[:]), reads=[t_stg], writes=[c.t_oa])
    S.barrier()


def phase3(c):
    nc, S = c.nc, c.S
    TC = c.t_const
    NB = NT
    with ExitStack() as st:
        sb = lambda name, shape, dt: st.enter_context(nc.sbuf_tensor(name, shape, dt))
        inc = [sb("p3inc%d" % d, [128, 128], F32) for d in range(2)]
        strm = [sb("p3str%d" % d, [128, 128], F32) for d in range(2)]
        mbias = [sb("p3mb%d" % d, [128, 128], F32) for d in range(2)]
        esel = [sb("p3es%d" % d, [128, 128], F32) for d in range(2)]
        t_m = Tok()
        for d in range(2):
            sgn = 1 if d == 0 else -1
            S.op("pool", lambda e, d=d: e.memset(inc[d][:], 1.0), writes=[t_m])
            S.op("pool", lambda e, d=d, sgn=sgn: e.affine_select(out=inc[d][:], in_=inc[d][:], compare_op=ALU.is_ge, fill=0.0,
                                                                base=0, pattern=[[sgn, 128]], channel_multiplier=-sgn),
                 reads=[t_m], writes=[t_m])
            S.op("pool", lambda e, d=d: e.memset(strm[d][:], 1.0), writes=[t_m])
            S.op("pool", lambda e, d=d, sgn=sgn: e.affine_select(out=strm[d][:], in_=strm[d][:], compare_op=ALU.is_ge, fill=0.0,
                                                                base=-1, pattern=[[sgn, 128]], channel_multiplier=-sgn),
                 reads=[t_m], writes=[t_m])
            S.op("pool", lambda e, d=d: e.tensor_scalar(out=mbias[d][:], in0=inc[d][:], scalar1=30000.0, scalar2=-30000.0,
                                                        op0=ALU.mult, op1=ALU.add), reads=[t_m], writes=[t_m])
            lastp = 127 if d == 0 else 0
            S.op("pool", lambda e, d=d: e.memset(esel[d][:], 0.0), writes=[t_m])
            S.op("pool", lambda e, d=d, lastp=lastp: e.affine_select(out=esel[d][:], in_=esel[d][:], compare_op=ALU.not_equal, fill=1.0,
                                                                    base=-lastp, pattern=[[0, 128]], channel_multiplier=1),
                 reads=[t_m], writes=[t_m])
        sm = sb("p3sm", [128, NT, 32], F32)
        t_sm = Tok()
        S.dma("sp", lambda e: e.dma_start(out=sm[:], in_=c.small_d), reads=[c.t_small], writes=[t_sm])
        gw = sb("p3gw", [128, 128], F32)
        t_gw = Tok()
        S.dma("sp", lambda e: e.dma_start(out=gw[:], in_=c.gdn_norm_w.broadcast_to([128, 128])), writes=[t_gw])

        def bank(name, dt=F32, n=512):
            return st.enter_context(nc.psum_tensor(name, [128, n], dt))
        b0, b2, b3, b4, b5, b6, b7 = [bank("p3b" + x) for x in "0234567"]
        b1 = bank("p3b1", BF16, 1024)
        btok = {id(b): Tok(excl=True) for b in (b0, b1, b2, b3, b4, b5, b6, b7)}
        slot = lambda b, i, w=128: (b[:, i * w:(i + 1) * w], btok[id(b)])
        s_kk, s_kq = slot(b0, 0), slot(b0, 1)
        s_gdd = [slot(b0, 2), slot(b0, 3)]
        s_misc = [slot(b6, 3)] * 4
        s_tr = [slot(b1, i) for i in range(8)]
        s_sqd = [(slot(b2, 0), slot(b2, 1)), (slot(b4, 0), slot(b4, 1))]
        s_apd = [slot(b3, 0, 256), slot(b5, 0, 256)]
        s_scan = [[slot(b6, i) for i in range(4)], [slot(b7, i) for i in range(4)]]

        kT = sb("p3kT", [128, S_LEN], BF16)
        qT = sb("p3qT", [128, S_LEN], BF16)
        ktok = sb("p3ktok", [128, NB, 128], BF16)
        vtok = sb("p3vtok", [128, NB, 128], BF16)
        zs = sb("p3zs", [128, NB, 128], BF16)
        t_in = Tok()
        ub = [sb("p3ub%d" % d, [128, NB, 128], F32) for d in range(2)]
        nwT = [sb("p3nwT%d" % d, [128, NB, 128], BF16) for d in range(2)]
        kdec = [sb("p3kdec%d" % d, [128, NB, 128], BF16) for d in range(2)]
        qkT = [sb("p3qkT%d" % d, [128, NB, 128], BF16) for d in range(2)]
        t_blk = [[Tok() for _ in range(NB)] for _ in range(2)]
        sc = [sb("p3sc%d" % d, [128, 8, NB], F32) for d in range(2)]
        t_sc = [Tok(), Tok()]
        oacc = sb("p3oacc", [128, NB, 128], F32)
        t_oacc = [Tok() for _ in range(NB)]
        S32 = [sb("p3S32_%d" % d, [128, 128], F32) for d in range(2)]
        Sbf = [sb("p3Sbf_%d" % d, [128, 128], BF16) for d in range(2)]
        t_S = [Tok(), Tok()]
        dtr = _mk_ring(st, nc, "p3dt", 5, [128, 128], F32)
        tmpr = _mk_ring(st, nc, "p3tmp", 5, [128, 128], F32)
        ntr = _mk_ring(st, nc, "p3nt", 10, [128, 128], F32)
        nnr = _mk_ring(st, nc, "p3nn", 10, [128, 128], F32)
        xr = _mk_ring(st, nc, "p3x", 5, [128, 256], F32)
        vnr = _mk_ring(st, nc, "p3vn", 4, [128, 128], BF16)
        o2r = _mk_ring(st, nc, "p3o2", 4, [128, 128], F32)
        big = sb("p3big", [128, NB, 128], F32)
        t_big = Tok()
        red = sb("p3red", [128, NB], F32)
        ogtok = sb("p3ogtok", [128, NB, 128], BF16)
        stage = sb("p3stage", [128, S_LEN], BF16)
        t_stage = Tok()

        stop = c.opts.get("p3_stop", 9)
        for h in range(c.opts.get("p3_heads", H)):
            S.dma("sp", lambda e, h=h: e.dma_start(out=kT[:], in_=c.gk_d[h]), reads=[c.t_gqk], writes=[t_in])
            S.dma("sp", lambda e, h=h: e.dma_start(out=qT[:], in_=c.gq_d[h]), reads=[c.t_gqk], writes=[t_in])
            for (buf, src, tk) in ((ktok, c.gktok_d, c.t_gtok), (vtok, c.gvtok_d, c.t_gtok), (zs, c.zs_d, c.t_vz)):
                S.dma("sp", lambda e, buf=buf, src=src, h=h: e.dma_start(
                    out=buf[:], in_=src[:, h * 128:(h + 1) * 128].rearrange("(t p) e -> p t e", p=128)), reads=[tk], writes=[t_in])
            for d in range(2):
                s_ = sc[d]
                g = sm[:, :, d * 8 + h]
                beta = sm[:, :, 16 + d * 8 + h]
                (pm, t_pm) = s_misc[d * 2]
                S.op("pe", lambda e, pm=pm, d=d, g=g: e.matmul(pm[:, 0:NB], inc[d][:], g, start=True, stop=True),
                     reads=[t_m, t_sm], writes=[t_pm])
                S.op("dve", lambda e, pm=pm, s_=s_: e.tensor_copy(s_[:, 0, :], pm[:, 0:NB]), reads=[t_pm], writes=[t_sc[d]])
                S.op("dve", lambda e, s_=s_: e.tensor_scalar(out=s_[:, 1, :], in0=s_[:, 0, :], scalar1=-1.0, scalar2=None, op0=ALU.mult),
                     reads=[t_sc[d]], writes=[t_sc[d]])
                (pm2, t_pm2) = s_misc[d * 2 + 1]
                S.op("pe", lambda e, pm2=pm2, d=d, s_=s_: e.matmul(pm2[:, 0:NB], esel[d][:], s_[:, 0, :], start=True, stop=True),
                     reads=[t_m, t_sc[d]], writes=[t_pm2])
                S.op("dve", lambda e, pm2=pm2, s_=s_: e.tensor_copy(s_[:, 2, :], pm2[:, 0:NB]), reads=[t_pm2], writes=[t_sc[d]])
                S.op("act", lambda e, s_=s_: e.activation(s_[:, 3, :], s_[:, 0, :], AF.Exp), reads=[t_sc[d]], writes=[t_sc[d]])
                S.op("dve", lambda e, s_=s_: e.tensor_tensor(out=s_[:, 4, :], in0=s_[:, 2, :], in1=s_[:, 0, :], op=ALU.subtract),
                     reads=[t_sc[d]], writes=[t_sc[d]])
                S.op("act", lambda e, s_=s_: e.activation(s_[:, 4, :], s_[:, 4, :], AF.Exp), reads=[t_sc[d]], writes=[t_sc[d]])
                S.op("act", lambda e, s_=s_: e.activation(s_[:, 5, :], s_[:, 2, :], AF.Exp), reads=[t_sc[d]], writes=[t_sc[d]])
                S.op("dve", lambda e, s_=s_, beta=beta: e.tensor_scalar(out=s_[:, 6, :], in0=beta, scalar1=-1.0, scalar2=None, op0=ALU.mult),
                     reads=[t_sm, t_sc[d]], writes=[t_sc[d]])
                S.op("dve", lambda e, s_=s_, beta=beta: e.tensor_copy(s_[:, 7, :], beta), reads=[t_sm, t_sc[d]], writes=[t_sc[d]])

            def par_chain(b, d):
                bs = slice(b * 128, (b + 1) * 128)
                s_ = sc[d]
                (pkk, t_pkk), (pkq, t_pkq), (pgd, t_pgd) = s_kk, s_kq, s_gdd[d]
                if d == 0:
                    S.op("pe", lambda e: e.matmul(pkk, kT[:, bs], kT[:, bs], start=True, stop=True), reads=[t_in], writes=[t_pkk])
                    S.op("pe", lambda e: e.matmul(pkq, kT[:, bs], qT[:, bs], start=True, stop=True), reads=[t_in], writes=[t_pkq])
                S.op("pe", lambda e: e.matmul(pgd, s_[:, 0, b:b + 1].broadcast_to([128, 128]), c.ident_f[:], start=True, stop=False),
                     reads=[t_sc[d], TC], writes=[t_pgd])
                S.op("pe", lambda e: e.matmul(pgd, c.ident_f[:], s_[:, 1, b:b + 1].broadcast_to([128, 128]), start=False, stop=False),
                     reads=[t_sc[d], TC], writes=[t_pgd])
                S.op("pe", lambda e: e.matmul(pgd, c.ident_f[:], mbias[d][:], start=False, stop=True), reads=[t_m, TC], writes=[t_pgd])
                yield
                dt_, t_dt = dtr.next()
                S.op("act", lambda e: e.activation(dt_[:], pgd, AF.Exp), reads=[t_pgd], writes=[t_dt])
                tmp, t_tmp = tmpr.next()
                S.op("dve", lambda e: e.scalar_tensor_tensor(out=tmp[:], in0=pkk, scalar=s_[:, 6, b:b + 1], in1=dt_[:], op0=ALU.mult, op1=ALU.mult),
                     reads=[t_pkk, t_sc[d], t_dt], writes=[t_tmp])
                S.op("dve", lambda e: e.tensor_tensor(out=qkT[d][:, b, :], in0=pkq, in1=dt_[:], op=ALU.mult),
                     reads=[t_pkq, t_dt], writes=[t_blk[d][b]])
                nt, t_nt = ntr.next()
                S.op("pool", lambda e, nt=nt: e.tensor_tensor(out=nt[:], in0=tmp[:], in1=strm[d][:], op=ALU.mult), reads=[t_tmp, t_m], writes=[t_nt])
                x, t_x = xr.next()
                S.op("pool", lambda e: e.tensor_copy(x[:, 0:128], vtok[:, b, :]), reads=[t_in], writes=[t_x])
                S.op("pool", lambda e: e.tensor_scalar(out=x[:, 128:256], in0=ktok[:, b, :], scalar1=s_[:, 3, b:b + 1], scalar2=None, op0=ALU.mult),
                     reads=[t_in, t_sc[d]], writes=[t_x])
                S.op("pool", lambda e: e.tensor_scalar(out=kdec[d][:, b, :], in0=ktok[:, b, :], scalar1=s_[:, 4, b:b + 1], scalar2=None, op0=ALU.mult),
                     reads=[t_in, t_sc[d]], writes=[t_blk[d][b]])
                yield
                (ptr, t_ptr) = s_sqd[d][0]
                S.op("pe", lambda e, nt=nt: e.transpose(ptr, nt[:], c.ident_f[:]), reads=[t_nt, TC], writes=[t_ptr])
                yield
                nn, t_nn = nnr.next()
                S.op("act", lambda e, nn=nn: e.copy(nn[:], ptr), reads=[t_ptr], writes=[t_nn])
                yield
                for l in range(7):
                    (pap, t_pap) = s_apd[d]
                    S.op("pe", lambda e, nt=nt: e.matmul(pap, nt[:], x[:], start=True, stop=True), reads=[t_nt, t_x], writes=[t_pap])
                    if l < 6:
                        (pn2, t_pn2), (pnt2, t_pnt2) = s_sqd[d]
                        S.op("pe", lambda e, nt=nt, nn=nn: e.matmul(pn2, nt[:], nn[:], start=True, stop=True), reads=[t_nt, t_nn], writes=[t_pn2])
                        S.op("pe", lambda e, nt=nt, nn=nn: e.matmul(pnt2, nn[:], nt[:], start=True, stop=True), reads=[t_nt, t_nn], writes=[t_pnt2])
                    yield
                    S.op("dve", lambda e: e.tensor_tensor(out=x[:], in0=pap, in1=x[:], op=ALU.add), reads=[t_pap, t_x], writes=[t_x])
                    if l < 6:
                        nn2, t_nn2 = nnr.next()
                        nt2, t_nt2 = ntr.next()
                        S.op("act", lambda e, nn2=nn2: e.copy(nn2[:], pn2), reads=[t_pn2], writes=[t_nn2])
                        S.op("act", lambda e, nt2=nt2: e.copy(nt2[:], pnt2), reads=[t_pnt2], writes=[t_nt2])
                        nn, t_nn, nt, t_nt = nn2, t_nn2, nt2, t_nt2
                    yield
                S.op("dve", lambda e: e.tensor_scalar(out=ub[d][:, b, :], in0=x[:, 0:128], scalar1=s_[:, 7, b:b + 1], scalar2=None, op0=ALU.mult),
                     reads=[t_x, t_sc[d]], writes=[t_blk[d][b]])
                (ptw, t_ptw) = s_sqd[d][1]
                S.op("pe", lambda e: e.transpose(ptw, x[:, 128:256], c.ident_f[:]), reads=[t_x, TC], writes=[t_ptw])
                yield
                S.op("act", lambda e: e.activation(nwT[d][:, b, :], ptw, AF.Copy, scale=-1.0), reads=[t_ptw], writes=[t_blk[d][b]])
                yield

            if stop >= 2:
                _interleave((par_chain(b, d) for b in range(NB) for d in range(2)), 2)

            def scan_chain(d):
                s_ = sc[d]
                S.op("pool", lambda e: e.memset(S32[d][:], 0.0), writes=[t_S[d]])
                S.op("pool", lambda e: e.memset(Sbf[d][:], 0.0), writes=[t_S[d]])
                (pv, t_pv), (po1, t_po1), (po2, t_po2), (pS, t_pS) = s_scan[d]
                for step in range(NB):
                    b = step if d == 0 else NB - 1 - step
                    bs = slice(b * 128, (b + 1) * 128)
                    S.op("pe", lambda e, b=b: e.matmul(pv, nwT[d][:, b, :], Sbf[d][:], start=True, stop=True),
                         reads=[t_blk[d][b], t_S[d]], writes=[t_pv])
                    S.op("pe", lambda e, bs=bs: e.matmul(po1, qT[:, bs], Sbf[d][:], start=True, stop=True), reads=[t_in, t_S[d]], writes=[t_po1])
                    yield
                    vn, t_vn = vnr.next()
                    S.op("dve", lambda e, vn=vn, b=b: e.scalar_tensor_tensor(
                        out=vn[:], in0=pv, scalar=s_[:, 7, b:b + 1], in1=ub[d][:, b, :], op0=ALU.mult, op1=ALU.add),
                        reads=[t_pv, t_sc[d], t_blk[d][b]], writes=[t_vn])
                    yield
                    S.op("pe", lambda e, vn=vn, b=b: e.matmul(pS, kdec[d][:, b, :], vn[:], start=True, stop=True),
                         reads=[t_blk[d][b], t_vn], writes=[t_pS])
                    S.op("pe", lambda e, vn=vn, b=b: e.matmul(po2, qkT[d][:, b, :], vn[:], start=True, stop=True),
                         reads=[t_blk[d][b], t_vn], writes=[t_po2])
                    yield
                    S.op("dve", lambda e, b=b: e.scalar_tensor_tensor(
                        out=S32[d][:], in0=S32[d][:], scalar=s_[:, 5, b:b + 1], in1=pS, op0=ALU.mult, op1=ALU.add),
                        reads=[t_pS, t_sc[d], t_S[d]], writes=[t_S[d]])
                    S.op("pool", lambda e: e.tensor_copy(Sbf[d][:], S32[d][:]), reads=[t_S[d]], writes=[t_S[d]])
                    o2, t_o2 = o2r.next()
                    S.op("act", lambda e, o2=o2: e.copy(o2[:], po2), reads=[t_po2], writes=[t_o2])
                    first = (b < NB // 2) if d == 0 else (b >= NB // 2)
                    if not first:
                        S.op("pool", lambda e, o2=o2, b=b: e.tensor_tensor(out=o2[:], in0=o2[:], in1=oacc[:, b, :], op=ALU.add),
                             reads=[t_o2, t_oacc[b]], writes=[t_o2])
                    S.op("dve", lambda e, o2=o2, b=b: e.scalar_tensor_tensor(
                        out=oacc[:, b, :], in0=po1, scalar=s_[:, 3, b:b + 1], in1=o2[:], op0=ALU.mult, op1=ALU.add),
                        reads=[t_po1, t_sc[d], t_o2], writes=[t_oacc[b]])
                    yield

            if stop >= 3:
                _interleave((scan_chain(d) for d in range(2)), 2)

            S.op("dve", lambda e: e.tensor_tensor(out=big[:], in0=oacc[:], in1=oacc[:], op=ALU.mult), reads=t_oacc, writes=[t_big])
            S.op("dve", lambda e: e.tensor_reduce(out=red[:], in_=big[:], axis=mybir.AxisListType.X, op=ALU.add), reads=[t_big], writes=[t_big])
            S.op("act", lambda e: e.activation(red[:], red[:], AF.Sqrt, bias=EPS, scale=1.0 / 128), reads=[t_big], writes=[t_big])
            S.op("dve", lambda e: e.reciprocal(red[:], red[:]), reads=[t_big], writes=[t_big])
            S.op("dve", lambda e: e.tensor_tensor(out=big[:], in0=oacc[:], in1=red[:].unsqueeze(2).broadcast_to([128, NB, 128]), op=ALU.mult),
                 reads=t_oacc + [t_big], writes=[t_big])
            S.op("pool", lambda e: e.tensor_tensor(out=big[:], in0=big[:], in1=gw[:].unsqueeze(1).broadcast_to([128, NB, 128]), op=ALU.mult),
                 reads=[t_big, t_gw], writes=[t_big])
            S.op("dve", lambda e: e.tensor_tensor(out=ogtok[:], in0=big[:], in1=zs[:], op=ALU.mult), reads=[t_big, t_in], writes=[t_big])
            for i8 in range(4):
                for ii in range(8):
                    b = i8 * 8 + ii
                    (ptr, t_ptr) = s_tr[ii]
                    S.op("pe", lambda e, ptr=ptr, b=b: e.transpose(ptr, ogtok[:, b, :], c.ident_b[:]), reads=[t_big, TC], writes=[t_ptr])
                    S.op("act", lambda e, ptr=ptr, b=b: e.copy(stage[:, b * 128:(b + 1) * 128], ptr), reads=[t_ptr], writes=[t_stage])
            S.dma("sp", lambda e, h=h: e.dma_start(out=c.ogT_d[h], in_=stage[:]), reads=[t_stage], writes=[c.t_og])
    S.barrier()


def phase4(c):
    nc, S = c.nc, c.S
    TC = c.t_const
    with ExitStack() as st:
        sb = lambda name, shape, dt: st.enter_context(nc.sbuf_tensor(name, shape, dt))
        wpa = sb("p4wpa", [128, 8, D], BF16)
        wpg = sb("p4wpg", [128, 8, D], BF16)
        wout = sb("p4wout", [128, 8, D], BF16)
        wr = sb("p4wr", [128, 8, NE], BF16)
        t_w = Tok()
        for (dst, src) in ((wpa, c.w_proj_attn), (wpg, c.w_proj_gdn), (wout, c.w_out)):
            for kc in range(8):
                S.dma("pool", lambda e, dst=dst, src=src, kc=kc: e.dma_start(out=dst[:, kc, :], in_=src[kc * 128:(kc + 1) * 128, :]),
                      writes=[Tok()])
        S.dma("pool", lambda e: e.dma_start(out=wr[:], in_=c.w_router.rearrange("(kc p) n -> p kc n", p=128)), writes=[t_w])
        S.barrier()
        n2w = sb("p4n2w", [128, D], F32)
        t_n2w = Tok()
        S.dma("sp", lambda e: e.dma_start(out=n2w[:], in_=c.norm2_w.broadcast_to([128, D])), writes=[t_n2w])
        aff = sb("p4aff", [128, NT, NE], F32)
        t_aff = Tok()
        oar = _mk_ring(st, nc, "p4oa", 2, [128, 8, 512], BF16)
        ogr = _mk_ring(st, nc, "p4og", 2, [128, 8, 512], BF16)
        sgar = _mk_ring(st, nc, "p4sga", 2, [128, 8, 512], BF16)
        sggr = _mk_ring(st, nc, "p4sgg", 2, [128, 8, 512], BF16)
        mgr = _mk_ring(st, nc, "p4mg", 2, [128, 8, 512], BF16)
        f1 = _mk_ring(st, nc, "p4f1", 2, [128, 512], F32)
        f2 = _mk_ring(st, nc, "p4f2", 2, [128, 512], F32)
        xr = _mk_ring(st, nc, "p4x", 2, [128, D], F32)
        sqr = _mk_ring(st, nc, "p4sq", 2, [128, D], BF16)
        hr = _mk_ring(st, nc, "p4h", 2, [128, D], BF16)
        hTr = _mk_ring(st, nc, "p4hT", 2, [128, 8, 128], BF16)
        ssr = _mk_ring(st, nc, "p4ss", 4, [128, 4], F32)
        lgr = _mk_ring(st, nc, "p4lg", 2, [128, NE], F32)
        psm = _mk_ring(st, nc, "p4psm", 6, [128, 512], F32, psum=True)
        pst = _mk_ring(st, nc, "p4pst", 2, [128, 8, 128], BF16, psum=True)

        for g in range(NG):
            gs = slice(g * 512, (g + 1) * 512)
            oa, t_oa = oar.next()
            og, t_og = ogr.next()
            sga, t_sga = sgar.next()
            sgg, t_sgg = sggr.next()
            for (buf, tk, src, dep) in ((oa, t_oa, c.oaT_d, c.t_oa), (og, t_og, c.ogT_d, c.t_og),
                                        (sga, t_sga, c.sga_d, c.t_gates), (sgg, t_sgg, c.sgg_d, c.t_gates)):
                S.dma("sp", lambda e, buf=buf, src=src, gs=gs: e.dma_start(out=buf[:], in_=src[:, :, gs].rearrange("h p t -> p h t")),
                      reads=[dep], writes=[tk])
            mg, t_mg = mgr.next()
            for dc in range(8):
                ds = slice(dc * 128, (dc + 1) * 128)
                pa, t_pa = psm.next()
                pg, t_pg = psm.next()
                for ec in range(8):
                    S.op("pe", lambda e, pa=pa, ec=ec, ds=ds, oa=oa: e.matmul(pa[:], wpa[:, ec, ds], oa[:, ec, :], start=(ec == 0), stop=(ec == 7)),
                         reads=[t_oa], writes=[t_pa])
                for ec in range(8):
                    S.op("pe", lambda e, pg=pg, ec=ec, ds=ds, og=og: e.matmul(pg[:], wpg[:, ec, ds], og[:, ec, :], start=(ec == 0), stop=(ec == 7)),
                         reads=[t_og], writes=[t_pg])
                a, t_a = f1.next()
                b, t_b = f2.next()
                S.op("dve", lambda e, a=a, pa=pa, sga=sga, dc=dc: e.tensor_tensor(out=a[:], in0=pa[:], in1=sga[:, dc, :], op=ALU.mult),
                     reads=[t_pa, t_sga], writes=[t_a])
                S.op("dve", lambda e, b=b, pg=pg, sgg=sgg, dc=dc: e.tensor_tensor(out=b[:], in0=pg[:], in1=sgg[:, dc, :], op=ALU.mult),
                     reads=[t_pg, t_sgg], writes=[t_b])
                S.op("pool", lambda e, mg=mg, a=a, b=b, dc=dc: e.tensor_tensor(out=mg[:, dc, :], in0=a[:], in1=b[:], op=ALU.add),
                     reads=[t_a, t_b], writes=[t_mg])
            for tl in range(4):
                i = g * 4 + tl
                xt, t_x = xr.next()
                S.dma("sp", lambda e, xt=xt, i=i: e.dma_start(out=xt[:], in_=c.x[i * 128:(i + 1) * 128, :]), writes=[t_x])
                for dh in range(2):
                    po, t_po = psm.next()
                    for dc in range(8):
                        S.op("pe", lambda e, po=po, mg=mg, dc=dc, tl=tl, dh=dh: e.matmul(
                            po[:], mg[:, dc, tl * 128:(tl + 1) * 128], wout[:, dc, dh * 512:(dh + 1) * 512], start=(dc == 0), stop=(dc == 7)),
                            reads=[t_mg], writes=[t_po])
                    S.op("dve", lambda e, xt=xt, po=po, dh=dh: e.tensor_tensor(out=xt[:, dh * 512:(dh + 1) * 512], in0=po[:],
                                                                              in1=xt[:, dh * 512:(dh + 1) * 512], op=ALU.add),
                         reads=[t_po, t_x], writes=[t_x])
                S.dma("sp", lambda e, xt=xt, i=i: e.dma_start(out=c.x1_d[i * 128:(i + 1) * 128, :], in_=xt[:]), reads=[t_x], writes=[c.t_x1])
                sq, t_sq = sqr.next()
                ss, t_ss = ssr.next()
                S.op("act", lambda e, xt=xt, sq=sq, ss=ss: e.activation(sq[:], xt[:], AF.Square, accum_out=ss[:, 0:1]),
                     reads=[t_x], writes=[t_sq, t_ss])
                S.op("act", lambda e, ss=ss: e.activation(ss[:, 1:2], ss[:, 0:1], AF.Sqrt, bias=EPS, scale=1.0 / D), reads=[t_ss], writes=[t_ss])
                S.op("dve", lambda e, ss=ss: e.reciprocal(ss[:, 1:2], ss[:, 1:2]), reads=[t_ss], writes=[t_ss])
                ht, t_h = hr.next()
                S.op("dve", lambda e, ht=ht, xt=xt, ss=ss: e.scalar_tensor_tensor(
                    out=ht[:], in0=xt[:], scalar=ss[:, 1:2], in1=n2w[:], op0=ALU.mult, op1=ALU.mult),
                    reads=[t_x, t_ss, t_n2w], writes=[t_h])
                S.dma("sp", lambda e, ht=ht, i=i: e.dma_start(out=c.h2tok_d[i * 128:(i + 1) * 128, :], in_=ht[:]), reads=[t_h], writes=[c.t_h2])
                pt, t_pt = pst.next()
                for dc in range(8):
                    S.op("pe", lambda e, pt=pt, ht=ht, dc=dc: e.transpose(pt[:, dc, :], ht[:, dc * 128:(dc + 1) * 128], c.ident_b[:]),
                         reads=[t_h, TC], writes=[t_pt])
                hT, t_hT = hTr.next()
                S.op("act", lambda e, hT=hT, pt=pt: e.copy(hT[:], pt[:]), reads=[t_pt], writes=[t_hT])
                pl, t_pl = psm.next()
                for dc in range(8):
                    S.op("pe", lambda e, pl=pl, hT=hT, dc=dc: e.matmul(pl[:, 0:NE], hT[:, dc, :], wr[:, dc, :], start=(dc == 0), stop=(dc == 7)),
                         reads=[t_hT, t_w], writes=[t_pl])
                lg, t_lg = lgr.next()
                S.op("dve", lambda e, pl=pl, ss=ss: e.tensor_reduce(out=ss[:, 2:3], in_=pl[:, 0:NE], axis=mybir.AxisListType.X, op=ALU.max),
                     reads=[t_pl, t_ss], writes=[t_ss])
                S.op("dve", lambda e, ss=ss: e.tensor_scalar(out=ss[:, 2:3], in0=ss[:, 2:3], scalar1=-1.0, scalar2=None, op0=ALU.mult),
                     reads=[t_ss], writes=[t_ss])
                S.op("act", lambda e, lg=lg, pl=pl, ss=ss: e.activation(lg[:], pl[:, 0:NE], AF.Exp, bias=ss[:, 2:3], accum_out=ss[:, 3:4]),
                     reads=[t_pl, t_ss], writes=[t_lg, t_ss])
                S.op("dve", lambda e, ss=ss: e.reciprocal(ss[:, 3:4], ss[:, 3:4]), reads=[t_ss], writes=[t_ss])
                S.op("dve", lambda e, lg=lg, ss=ss, i=i: e.tensor_scalar(out=aff[:, i, :], in0=lg[:], scalar1=ss[:, 3:4], scalar2=None, op0=ALU.mult),
                     reads=[t_lg, t_ss], writes=[t_aff])
        S.dma("sp", lambda e: e.dma_start(out=c.aff_d, in_=aff[:]), reads=[t_aff], writes=[c.t_aff])
    S.barrier()


def phase5(c):
    nc, S = c.nc, c.S
    TC = c.t_const
    with ExitStack() as st:
        sb = lambda name, shape, dt: st.enter_context(nc.sbuf_tensor(name, shape, dt))
        aff = sb("p5aff", [128, NT, NE], F32)
        t_aff = Tok()
        S.dma("sp", lambda e: e.dma_start(out=aff[:], in_=c.aff_d), reads=[c.t_aff], writes=[t_aff])
        affT = sb("p5affT", [NE, S_LEN], F32)
        work = sb("p5work", [NE, S_LEN], F32)
        ones = sb("p5ones", [NE, S_LEN], F32)
        mx = sb("p5mx", [NE, 8], F32)
        t_affT, t_work, t_mx, t_ones = Tok(), Tok(), Tok(), Tok()
        psm = _mk_ring(st, nc, "p5psm", 4, [128, 512], F32, psum=True)
        for i4 in range(NT // 4):
            ps, t_ps = psm.next()
            for ii in range(4):
                i = i4 * 4 + ii
                S.op("pe", lambda e, ps=ps, ii=ii, i=i: e.transpose(ps[0:NE, ii * 128:(ii + 1) * 128], aff[:, i, :], c.ident_f[:]),
                     reads=[t_aff, TC], writes=[t_ps])
            S.op("act", lambda e, ps=ps, i4=i4: e.copy(affT[:, i4 * 512:(i4 + 1) * 512], ps[0:NE, :]), reads=[t_ps], writes=[t_affT])
        S.op("pool", lambda e: e.memset(ones[:], 1.0), writes=[t_ones])
        src = affT
        for it in range(CAP // 8):
            S.op("dve", lambda e, src=src: e.max(out=mx[:], in_=src[:]), reads=[t_affT, t_work], writes=[t_mx])
            if it < CAP // 8 - 1:
                S.op("dve", lambda e, src=src: e.match_replace(out=work[:], in_to_replace=mx[:], in_values=src[:], imm_value=-1.0),
                     reads=[t_mx, t_affT, t_work], writes=[t_work])
            src = work
        S.op("dve", lambda e: e.tensor_scalar(out=work[:], in0=affT[:], scalar1=mx[:, 7:8], scalar2=None, op0=ALU.is_ge),
             reads=[t_affT, t_mx, t_work], writes=[t_work])
        S.op("dve", lambda e: e.tensor_tensor_scan(out=affT[:], data0=ones[:], data1=work[:], initial=0.0, op0=ALU.mult, op1=ALU.add),
             reads=[t_work, t_ones, t_affT], writes=[t_affT])
        S.op("dve", lambda e: e.tensor_tensor(out=affT[:], in0=affT[:], in1=work[:], op=ALU.mult), reads=[t_work, t_affT], writes=[t_affT])
        S.op("dve", lambda e: e.tensor_scalar(out=affT[:], in0=affT[:], scalar1=-1.0, scalar2=None, op0=ALU.add), reads=[t_affT], writes=[t_affT])
        S.dma("sp", lambda e: e.dma_start(out=c.pos_d, in_=affT[:]), reads=[t_affT], writes=[c.t_pos])
        ptok = sb("p5ptok", [128, NT, NE], F32)
        t_ptok = Tok()
        for i4 in range(NT // 4):
            ps, t_ps = psm.next()
            for ii in range(4):
                i = i4 * 4 + ii
                S.op("pe", lambda e, ps=ps, ii=ii, i=i: e.transpose(ps[:, ii * NE:(ii + 1) * NE], affT[:, i * 128:(i + 1) * 128], c.ident_f[0:NE, 0:NE]),
                     reads=[t_affT, TC], writes=[t_ps])
            S.op("act", lambda e, ps=ps, i4=i4: e.copy(ptok[:, i4 * 4:(i4 + 1) * 4, :], ps[:, 0:4 * NE].rearrange("p (a b) -> p a b", b=NE)),
                 reads=[t_ps], writes=[t_ptok])
        S.dma("sp", lambda e: e.dma_start(out=c.ptok_d, in_=ptok[:]), reads=[t_ptok], writes=[c.t_pos])
    S.barrier()


def phase6a(c):
    nc, S = c.nc, c.S
    TC = c.t_const
    with ExitStack() as st:
        sb = lambda name, shape, dt: st.enter_context(nc.sbuf_tensor(name, shape, dt))
        ptok = sb("p6ptok", [128, NT, NE], F32)
        t_ptok = Tok()
        S.dma("sp", lambda e: e.dma_start(out=ptok[:], in_=c.ptok_d), reads=[c.t_pos], writes=[t_ptok])
        iota = sb("p6iota", [128, CAP], F32)
        t_iota = Tok()
        S.op("pool", lambda e: e.iota(iota[:], pattern=[[1, CAP]], base=0, channel_multiplier=0, allow_small_or_imprecise_dtypes=True),
             writes=[t_iota])
        sel = sb("p6sel", [128, NT, CAP], BF16)
        t_sel = Tok()
        xsT = sb("p6xsT", [128, 8, CAP], BF16)
        t_xsT = Tok()
        actT = sb("p6actT", [128, 16, CAP], BF16)
        t_actT = Tok()
        yes = sb("p6ye", [128, 4, D], BF16)
        t_yes = Tok()
        h2r = _mk_ring(st, nc, "p6h2", 4, [128, D], BF16)
        wgr = _mk_ring(st, nc, "p6wg", 2, [128, 8, 1024], BF16)
        wur = _mk_ring(st, nc, "p6wu", 2, [128, 8, 1024], BF16)
        wdr = _mk_ring(st, nc, "p6wd", 2, [128, 8, D], BF16)
        gfr = _mk_ring(st, nc, "p6gf", 2, [128, CAP], F32)
        psm = _mk_ring(st, nc, "p6psm", 8, [128, 512], F32, psum=True)

        for ex in range(NE):
            for i in range(NT):
                eng = "dve" if i % 2 == 0 else "pool"
                S.op(eng, lambda e, i=i, ex=ex: e.tensor_scalar(out=sel[:, i, :], in0=iota[:], scalar1=ptok[:, i, ex:ex + 1], scalar2=None,
                                                                 op0=ALU.is_equal), reads=[t_iota, t_ptok], writes=[t_sel])
            for half in range(2):
                accs = [psm.next() for _ in range(4)]
                for i in range(NT):
                    ht, t_h = h2r.next()
                    S.dma("sp", lambda e, ht=ht, i=i: e.dma_start(out=ht[:], in_=c.h2tok_d[i * 128:(i + 1) * 128, :]), reads=[c.t_h2], writes=[t_h])
                    for dl in range(4):
                        dc = half * 4 + dl
                        ps, t_ps = accs[dl]
                        S.op("pe", lambda e, ps=ps, ht=ht, dc=dc, i=i: e.matmul(ps[:], ht[:, dc * 128:(dc + 1) * 128], sel[:, i, :],
                                                                               start=(i == 0), stop=(i == NT - 1)),
                             reads=[t_h, t_sel], writes=[t_ps])
                for dl in range(4):
                    dc = half * 4 + dl
                    ps, t_ps = accs[dl]
                    S.op("act", lambda e, ps=ps, dc=dc: e.copy(xsT[:, dc, :], ps[:]), reads=[t_ps], writes=[t_xsT])
            for fh in range(2):
                wg, t_wg = wgr.next()
                wu, t_wu = wur.next()
                for kc in range(8):
                    S.dma("pool", lambda e, wg=wg, kc=kc, ex=ex, fh=fh: e.dma_start(
                        out=wg[:, kc, :], in_=c.w_gate[ex, kc * 128:(kc + 1) * 128, fh * 1024:(fh + 1) * 1024]), writes=[t_wg])
                    S.dma("pool", lambda e, wu=wu, kc=kc, ex=ex, fh=fh: e.dma_start(
                        out=wu[:, kc, :], in_=c.w_up[ex, kc * 128:(kc + 1) * 128, fh * 1024:(fh + 1) * 1024]), writes=[t_wu])
                for fl in range(8):
                    fc = fh * 8 + fl
                    fs = slice(fl * 128, (fl + 1) * 128)
                    pg, t_pg = psm.next()
                    pu, t_pu = psm.next()
                    for kc in range(8):
                        S.op("pe", lambda e, pg=pg, wg=wg, kc=kc, fs=fs: e.matmul(pg[:], wg[:, kc, fs], xsT[:, kc, :], start=(kc == 0), stop=(kc == 7)),
                             reads=[t_wg, t_xsT], writes=[t_pg])
                    for kc in range(8):
                        S.op("pe", lambda e, pu=pu, wu=wu, kc=kc, fs=fs: e.matmul(pu[:], wu[:, kc, fs], xsT[:, kc, :], start=(kc == 0), stop=(kc == 7)),
                             reads=[t_wu, t_xsT], writes=[t_pu])
                    gf, t_gf = gfr.next()
                    S.op("act", lambda e, gf=gf, pg=pg: e.activation(gf[:], pg[:], AF.Silu), reads=[t_pg], writes=[t_gf])
                    S.op("dve", lambda e, gf=gf, pu=pu, fc=fc: e.tensor_tensor(out=actT[:, fc, :], in0=pu[:], in1=gf[:], op=ALU.mult),
                         reads=[t_pu, t_gf], writes=[t_actT])
            accs = [psm.next() for _ in range(8)]
            for fh in range(2):
                wd, t_wd = wdr.next()
                for fl in range(8):
                    fc = fh * 8 + fl
                    S.dma("pool", lambda e, wd=wd, fl=fl, fc=fc, ex=ex: e.dma_start(
                        out=wd[:, fl, :], in_=c.w_down[ex, fc * 128:(fc + 1) * 128, :]), writes=[t_wd])
                for fl in range(8):
                    fc = fh * 8 + fl
                    for sc in range(4):
                        for dh in range(2):
                            ps, t_ps = accs[sc * 2 + dh]
                            S.op("pe", lambda e, ps=ps, fc=fc, sc=sc, wd=wd, fl=fl, dh=dh: e.matmul(
                                ps[:], actT[:, fc, sc * 128:(sc + 1) * 128], wd[:, fl, dh * 512:(dh + 1) * 512], start=(fc == 0), stop=(fc == 15)),
                                reads=[t_actT, t_wd], writes=[t_ps])
            for sc in range(4):
                for dh in range(2):
                    ps, t_ps = accs[sc * 2 + dh]
                    eng = "act" if dh == 0 else "dve"
                    if eng == "act":
                        S.op("act", lambda e, ps=ps, sc=sc, dh=dh: e.copy(yes[:, sc, dh * 512:(dh + 1) * 512], ps[:]), reads=[t_ps], writes=[t_yes])
                    else:
                        S.op("dve", lambda e, ps=ps, sc=sc, dh=dh: e.tensor_copy(yes[:, sc, dh * 512:(dh + 1) * 512], ps[:]), reads=[t_ps], writes=[t_yes])
            S.dma("sp", lambda e, ex=ex: e.dma_start(out=c.ye_d[ex].rearrange("(sc p) d -> p sc d", p=128), in_=yes[:]),
                  reads=[t_yes], writes=[c.t_ye])
    S.barrier()


def phase6b(c):
    nc, S = c.nc, c.S
    TC = c.t_const
    with ExitStack() as st:
        sb = lambda name, shape, dt: st.enter_context(nc.sbuf_tensor(name, shape, dt))
        yall = sb("p7ye", [128, NE, 4, D], BF16)
        t_yall = Tok()
        for ex in range(NE):
            S.dma("sp", lambda e, ex=ex: e.dma_start(out=yall[:, ex, :, :], in_=c.ye_d[ex].rearrange("(sc p) d -> p sc d", p=128)),
                  reads=[c.t_ye], writes=[Tok()])
        S.barrier()
        aff = sb("p7aff", [128, NT, NE], F32)
        t_aff = Tok()
        S.dma("sp", lambda e: e.dma_start(out=aff[:], in_=c.aff_d), reads=[c.t_aff], writes=[t_aff])
        nfw = sb("p7nfw", [128, D], F32)
        t_nfw = Tok()
        S.dma("sp", lambda e: e.dma_start(out=nfw[:], in_=c.norm_f_w.broadcast_to([128, D])), writes=[t_nfw])
        pidx = sb("p7pidx", [128, 4], F32)
        t_pidx = Tok()
        S.op("pool", lambda e: e.iota(pidx[:], pattern=[[128, 4]], base=0, channel_multiplier=1, allow_small_or_imprecise_dtypes=True),
             writes=[t_pidx])
        posbc = sb("p7posbc", [128, NE, 512], F32)
        t_posbc = Tok()
        selT = _mk_ring(st, nc, "p7selT", 8, [128, 512], BF16)
        accr = _mk_ring(st, nc, "p7acc", 4, [128, D], F32)
        sqr = _mk_ring(st, nc, "p7sq", 2, [128, D], BF16)
        ssr = _mk_ring(st, nc, "p7ss", 4, [128, 2], F32)
        psm = _mk_ring(st, nc, "p7psm", 6, [128, 512], F32, psum=True)
        for g in range(NG):
            gs = slice(g * 512, (g + 1) * 512)
            S.dma("sp", lambda e, gs=gs: e.dma_start(out=posbc[:], in_=c.pos_d[:, gs].unsqueeze(0).broadcast_to([128, NE, 512])),
                  reads=[c.t_pos], writes=[t_posbc])
            accs = []
            for tl in range(4):
                i = g * 4 + tl
                acc, t_acc = accr.next()
                S.dma("sp", lambda e, acc=acc, i=i: e.dma_start(out=acc[:], in_=c.x1_d[i * 128:(i + 1) * 128, :]), reads=[c.t_x1], writes=[t_acc])
                accs.append((acc, t_acc))
            for ex in range(NE):
                sts = []
                for sc in range(4):
                    sT, t_sT = selT.next()
                    eng = "dve" if sc % 2 == 0 else "pool"
                    S.op(eng, lambda e, sT=sT, ex=ex, sc=sc: e.tensor_scalar(out=sT[:], in0=posbc[:, ex, :], scalar1=pidx[:, sc:sc + 1], scalar2=None,
                                                                            op0=ALU.is_equal), reads=[t_posbc, t_pidx], writes=[t_sT])
                    sts.append((sT, t_sT))
                for tl in range(4):
                    i = g * 4 + tl
                    acc, t_acc = accs[tl]
                    for dh in range(2):
                        ps, t_ps = psm.next()
                        for sc in range(4):
                            sT, t_sT = sts[sc]
                            S.op("pe", lambda e, ps=ps, sT=sT, tl=tl, ex=ex, sc=sc, dh=dh: e.matmul(
                                ps[:], sT[:, tl * 128:(tl + 1) * 128], yall[:, ex, sc, dh * 512:(dh + 1) * 512], start=(sc == 0), stop=(sc == 3)),
                                reads=[t_sT], writes=[t_ps])
                        S.op("dve", lambda e, acc=acc, ps=ps, i=i, ex=ex, dh=dh: e.scalar_tensor_tensor(
                            out=acc[:, dh * 512:(dh + 1) * 512], in0=ps[:], scalar=aff[:, i, ex:ex + 1], in1=acc[:, dh * 512:(dh + 1) * 512],
                            op0=ALU.mult, op1=ALU.add), reads=[t_ps, t_aff, t_acc], writes=[t_acc])
            for tl in range(4):
                i = g * 4 + tl
                acc, t_acc = accs[tl]
                sq, t_sq = sqr.next()
                ss, t_ss = ssr.next()
                S.op("act", lambda e, acc=acc, sq=sq, ss=ss: e.activation(sq[:], acc[:], AF.Square, accum_out=ss[:, 0:1]),
                     reads=[t_acc], writes=[t_sq, t_ss])
                S.op("act", lambda e, ss=ss: e.activation(ss[:, 1:2], ss[:, 0:1], AF.Sqrt, bias=EPS, scale=1.0 / D), reads=[t_ss], writes=[t_ss])
                S.op("dve", lambda e, ss=ss: e.reciprocal(ss[:, 1:2], ss[:, 1:2]), reads=[t_ss], writes=[t_ss])
                S.op("dve", lambda e, acc=acc, ss=ss: e.scalar_tensor_tensor(
                    out=acc[:], in0=acc[:], scalar=ss[:, 1:2], in1=nfw[:], op0=ALU.mult, op1=ALU.mult),
                    reads=[t_acc, t_ss, t_nfw], writes=[t_acc])
                S.dma("sp", lambda e, acc=acc, i=i: e.dma_start(out=c.out[i * 128:(i + 1) * 128, :], in_=acc[:]), reads=[t_acc], writes=[c.t_out])
    S.barrier()


ALL_PHASES = ("p1", "p2", "p3", "p4", "p5", "p6a", "p6b")


def kernel(**inputs):
    n_cores = 8
    shared = host_prep(inputs)
    nc, c = build_program(debug=False, phases=ALL_PHASES)
    x = np.asarray(inputs["x"], dtype=np.float32)
    in_maps = []
    for b in range(n_cores):
        m = dict(shared)
        m["x"] = np.ascontiguousarray(x[b])
        in_maps.append({k: m[k] for k in c.input_names})
    res = run_bass_kernel_spmd(nc, in_maps, core_ids=list(range(n_cores)))
    out = np.stack([np.asarray(r["out"], dtype=np.float32) for r in res.results], axis=0)
    return out
```

```python
import math
from contextlib import ExitStack
import numpy as np
import concourse.bass as bass
import concourse.mybir as mybir
from concourse.bass_utils import run_bass_kernel_spmd

F32 = mybir.dt.float32
BF16 = mybir.dt.bfloat16
AF = mybir.ActivationFunctionType
ALU = mybir.AluOpType

S_LEN = 4096
D = 1024
NT = S_LEN // 128
NG = S_LEN // 512
H = 8
IN_W = 9248
C_QA, C_KA, C_VA, C_GQ, C_GK, C_GV, C_Z, C_SM, C_GA, C_GG = 0, 1024, 2048, 3072, 4096, 5120, 6144, 7168, 7200, 8224
NE = 16
FF = 2048
CAP = 512
EPS = 1e-6
LAMBDA_INIT = 0.8 - 0.6 * math.exp(-0.3 * 0)

ENGS = ("pe", "act", "dve", "pool", "sp")


class Tok:
    __slots__ = ("w", "r", "excl")

    def __init__(self, excl=False):
        self.w = None
        self.r = {}
        self.excl = excl


class Sched:
    NDMA = 32

    def __init__(self, nc):
        self.nc = nc
        self.sems = {}
        for e in ENGS:
            self.sems[e] = nc.alloc_semaphore(name="sem_" + e)
        for i in range(self.NDMA):
            self.sems[("d", i)] = nc.alloc_semaphore(name="sem_dma%d" % i)
        self.cnt = {k: 0 for k in self.sems}
        self.seen = {e: {} for e in ENGS}
        self.prog = {e: [] for e in ENGS}
        self.ndma = 0
        self.ninstr = 0

    def _deps(self, reads, writes):
        deps = {}
        for t in reads:
            if t.w is not None:
                k, v = t.w
                if deps.get(k, 0) < v:
                    deps[k] = v
        for t in writes:
            if t.w is not None:
                k, v = t.w
                if deps.get(k, 0) < v:
                    deps[k] = v
            for k, v in t.r.items():
                if deps.get(k, 0) < v:
                    deps[k] = v
        return deps

    def _emit_waits(self, e, deps):
        seen = self.seen[e]
        for k, v in deps.items():
            if seen.get(k, 0) >= v:
                continue
            seen[k] = v
            sem = self.sems[k]
            self.prog[e].append(lambda eng, sem=sem, v=v: eng.wait_ge(sem, v))

    def op(self, e, fn, reads=(), writes=()):
        if any(t.excl for t in reads):
            writes = list(writes) + [t for t in reads if t.excl]
            reads = [t for t in reads if not t.excl]
        deps = self._deps(reads, writes)
        if e == "pe":
            deps.pop("pe", None)
        self._emit_waits(e, deps)
        sem = self.sems[e]
        self.cnt[e] += 1
        n = self.cnt[e]
        self.prog[e].append(lambda eng, fn=fn, sem=sem: fn(eng).then_inc(sem, 1))
        for t in reads:
            if t.r.get(e, 0) < n:
                t.r[e] = n
        for t in writes:
            t.w = (e, n)
            t.r = {}
        self.ninstr += 1

    def dma(self, e, fn, reads=(), writes=()):
        i = self.ndma % self.NDMA
        self.ndma += 1
        k = ("d", i)
        deps = self._deps(reads, writes)
        if self.cnt[k] > 0:
            deps[k] = max(deps.get(k, 0), self.cnt[k])
        self._emit_waits(e, deps)
        self.cnt[k] += 16
        v = self.cnt[k]
        sem = self.sems[k]
        self.prog[e].append(lambda eng, fn=fn, sem=sem: fn(eng).then_inc(sem, 16))
        for t in reads:
            if t.r.get(k, 0) < v:
                t.r[k] = v
        for t in writes:
            t.w = (k, v)
            t.r = {}
        self.ninstr += 1

    def barrier(self):
        deps = {k: v for k, v in self.cnt.items() if v > 0}
        for e in ENGS:
            self._emit_waits(e, dict(deps))

    def emit(self):
        nc = self.nc
        prog = self.prog
        with nc.Block() as block:
            @block.tensor
            def _(eng):
                for f in prog["pe"]:
                    f(eng)

            @block.scalar
            def _(eng):
                for f in prog["act"]:
                    f(eng)

            @block.vector
            def _(eng):
                for f in prog["dve"]:
                    f(eng)

            @block.gpsimd
            def _(eng):
                for f in prog["pool"]:
                    f(eng)

            @block.sync
            def _(eng):
                for f in prog["sp"]:
                    f(eng)


class Ring:
    def __init__(self, items):
        self.items = items
        self.i = 0

    def next(self):
        it = self.items[self.i % len(self.items)]
        self.i += 1
        return it


class Ctx:
    pass


def _interleave(gens, width):
    it = iter(gens)
    active = []
    exhausted = False
    while True:
        while len(active) < width and not exhausted:
            try:
                active.append(next(it))
            except StopIteration:
                exhausted = True
        if not active:
            break
        for g in list(active):
            try:
                next(g)
            except StopIteration:
                active.remove(g)


def _mk_ring(stack, nc, name, n, shape, dt, psum=False):
    items = []
    for i in range(n):
        if psum:
            t = stack.enter_context(nc.psum_tensor("%s%d" % (name, i), shape, dt))
        else:
            t = stack.enter_context(nc.sbuf_tensor("%s%d" % (name, i), shape, dt))
        items.append((t, Tok()))
    return Ring(items)


def _consts(c, stack):
    nc, S = c.nc, c.S
    c.ident_f = nc.alloc_sbuf_tensor("ident_f", [128, 128], F32)
    c.ident_b = nc.alloc_sbuf_tensor("ident_b", [128, 128], BF16)
    c.ones_b = nc.alloc_sbuf_tensor("ones_b", [128, 128], BF16)
    c.ones_f = nc.alloc_sbuf_tensor("ones_f", [128, 128], F32)
    c.t_const = Tok()
    tc = c.t_const
    S.op("pool", lambda e: e.memset(c.ident_f[:], 0.0), writes=[tc])
    S.op("pool", lambda e: e.affine_select(out=c.ident_f[:], in_=c.ident_f[:], compare_op=ALU.not_equal, fill=1.0,
                                           base=0, pattern=[[-1, 128]], channel_multiplier=1),
         reads=[tc], writes=[tc])
    S.op("pool", lambda e: e.tensor_copy(c.ident_b[:], c.ident_f[:]), reads=[tc], writes=[tc])
    S.op("pool", lambda e: e.memset(c.ones_b[:], 1.0), writes=[tc])
    S.op("pool", lambda e: e.memset(c.ones_f[:], 1.0), writes=[tc])


def phase1(c):
    nc, S = c.nc, c.S
    TC = c.t_const
    with ExitStack() as st:
        sb = lambda name, shape, dt: st.enter_context(nc.sbuf_tensor(name, shape, dt))
        hT = sb("hT", [128, 8, S_LEN], BF16)
        t_hT = [Tok() for _ in range(NT)]
        stA = ExitStack()
        sbA = lambda name, shape, dt: stA.enter_context(nc.sbuf_tensor(name, shape, dt))
        n1w = sbA("n1w", [128, D], F32)
        t_n1w = Tok()
        S.dma("sp", lambda e: e.dma_start(out=n1w[:], in_=c.norm1_w.broadcast_to([128, D])), writes=[t_n1w])
        xring = _mk_ring(stA, nc, "p1x", 3, [128, D], F32)
        sqring = _mk_ring(stA, nc, "p1sq", 2, [128, D], BF16)
        hring = _mk_ring(stA, nc, "p1h", 2, [128, D], BF16)
        ssring = _mk_ring(stA, nc, "p1ss", 4, [128, 2], F32)
        pst = _mk_ring(st, nc, "p1pst", 2, [128, 8, 128], BF16, psum=True)
        psm = _mk_ring(st, nc, "p1psm", 5, [128, 512], F32, psum=True)

        for i in range(NT):
            xt, t_x = xring.next()
            S.dma("sp", lambda e, xt=xt, i=i: e.dma_start(out=xt[:], in_=c.x[i * 128:(i + 1) * 128, :]), writes=[t_x])
            sq, t_sq = sqring.next()
            ss, t_ss = ssring.next()
            S.op("act", lambda e, xt=xt, sq=sq, ss=ss: e.activation(sq[:], xt[:], AF.Square, accum_out=ss[:, 0:1]),
                 reads=[t_x], writes=[t_sq, t_ss])
            S.op("act", lambda e, ss=ss: e.activation(ss[:, 1:2], ss[:, 0:1], AF.Sqrt, bias=EPS, scale=1.0 / D),
                 reads=[t_ss], writes=[t_ss])
            S.op("dve", lambda e, ss=ss: e.reciprocal(ss[:, 1:2], ss[:, 1:2]), reads=[t_ss], writes=[t_ss])
            ht, t_h = hring.next()
            S.op("dve", lambda e, ht=ht, xt=xt, ss=ss: e.scalar_tensor_tensor(
                out=ht[:], in0=xt[:], scalar=ss[:, 1:2], in1=n1w[:], op0=ALU.mult, op1=ALU.mult),
                reads=[t_x, t_ss, t_n1w], writes=[t_h])
            pt, t_pt = pst.next()
            for dc in range(8):
                S.op("pe", lambda e, pt=pt, ht=ht, dc=dc: e.transpose(pt[:, dc, :], ht[:, dc * 128:(dc + 1) * 128], c.ident_b[:]),
                     reads=[t_h, TC], writes=[t_pt])
            S.op("act", lambda e, pt=pt, i=i: e.copy(hT[:, :, i * 128:(i + 1) * 128], pt[:]),
                 reads=[t_pt], writes=[t_hT[i]])

        S.barrier()
        stA.close()
        wfm = _mk_ring(st, nc, "p1wfm", 4, [128, 8, 128], BF16)
        wtm = _mk_ring(st, nc, "p1wtm", 2, [128, 8, 512], BF16)

        def load_w(ring, src, col0, ncols):
            wt, t_w = ring.next()
            S.dma("pool", lambda e: e.dma_start(
                out=wt[:, :, 0:ncols], in_=src[:, col0:col0 + ncols].rearrange("(kc p) n -> p kc n", p=128)),
                writes=[t_w])
            return wt, t_w

        def proj_fm(wt, t_w, g):
            ps, t_ps = psm.next()
            for kc in range(8):
                S.op("pe", lambda e, ps=ps, kc=kc: e.matmul(ps[:], wt[:, kc, :], hT[:, kc, g * 512:(g + 1) * 512],
                                                          start=(kc == 0), stop=(kc == 7)),
                     reads=[t_w] + t_hT[g * 4:(g + 1) * 4], writes=[t_ps])
            return ps, t_ps

        def proj_tm(wt, t_w, i, ncols):
            ps, t_ps = psm.next()
            for kc in range(8):
                S.op("pe", lambda e, ps=ps, kc=kc: e.matmul(ps[:, 0:ncols], hT[:, kc, i * 128:(i + 1) * 128], wt[:, kc, 0:ncols],
                                                          start=(kc == 0), stop=(kc == 7)),
                     reads=[t_w, t_hT[i]], writes=[t_ps])
            return ps, t_ps

        stage = _mk_ring(st, nc, "p1stage", 2, [128, S_LEN], BF16)
        f32a = _mk_ring(st, nc, "p1f32a", 3, [128, 512], F32)
        f32b = _mk_ring(st, nc, "p1f32b", 3, [128, 512], F32)

        stB = ExitStack()
        cosT = stB.enter_context(nc.sbuf_tensor("cosT_sb", [128, S_LEN], F32))
        sinT = stB.enter_context(nc.sbuf_tensor("sinT_sb", [128, S_LEN], F32))
        t_rope = Tok()
        S.dma("sp", lambda e: e.dma_start(out=cosT[:], in_=c.cosT), writes=[t_rope])
        S.dma("sp", lambda e: e.dma_start(out=sinT[:], in_=c.sinT), writes=[t_rope])

        for (col0, dst) in ((C_QA, c.qT_d), (C_KA, c.kT_d)):
            for h in range(H):
                w1, t_w1 = load_w(wfm, c.w_in, col0 + h * 128, 128)
                w2, t_w2 = load_w(wfm, c.w_qkp, (col0 // 1024) * 1024 + h * 128, 128)
                stg, t_stg = stage.next()
                for g in range(NG):
                    ps1, t_ps1 = proj_fm(w1, t_w1, g)
                    ps2, t_ps2 = proj_fm(w2, t_w2, g)
                    a, t_a = f32a.next()
                    b, t_b = f32b.next()
                    sl = slice(g * 512, (g + 1) * 512)
                    S.op("dve", lambda e, a=a, ps1=ps1, sl=sl: e.tensor_tensor(out=a[:], in0=ps1[:], in1=cosT[:, sl], op=ALU.mult),
                         reads=[t_ps1, t_rope], writes=[t_a])
                    S.op("dve", lambda e, b=b, ps2=ps2, sl=sl: e.tensor_tensor(out=b[:], in0=ps2[:], in1=sinT[:, sl], op=ALU.mult),
                         reads=[t_ps2, t_rope], writes=[t_b])
                    S.op("pool", lambda e, a=a, b=b, stg=stg, sl=sl: e.tensor_tensor(out=stg[:, sl], in0=a[:], in1=b[:], op=ALU.add),
                         reads=[t_a, t_b], writes=[t_stg])
                S.dma("sp", lambda e, stg=stg, dst=dst, h=h: e.dma_start(out=dst[h], in_=stg[:]), reads=[t_stg], writes=[c.t_qk])

        S.barrier()
        stB.close()
        for (col0, dst) in ((C_GA, c.sga_d), (C_GG, c.sgg_d)):
            for h in range(8):
                w1, t_w1 = load_w(wfm, c.w_in, col0 + h * 128, 128)
                stg, t_stg = stage.next()
                for g in range(NG):
                    ps1, t_ps1 = proj_fm(w1, t_w1, g)
                    sl = slice(g * 512, (g + 1) * 512)
                    S.op("act", lambda e, ps1=ps1, stg=stg, sl=sl: e.activation(stg[:, sl], ps1[:], AF.Sigmoid),
                         reads=[t_ps1], writes=[t_stg])
                S.dma("sp", lambda e, stg=stg, dst=dst, h=h: e.dma_start(out=dst[h], in_=stg[:]), reads=[t_stg], writes=[c.t_gates])

        tmst = _mk_ring(st, nc, "p1tmst", 3, [128, 512], BF16)
        for (col0, dst, fn) in ((C_VA, c.v_d, None), (C_Z, c.zs_d, AF.Silu)):
            for half in range(2):
                wt, t_w = load_w(wtm, c.w_in, col0 + half * 512, 512)
                for i in range(NT):
                    ps, t_ps = proj_tm(wt, t_w, i, 512)
                    o, t_o = tmst.next()
                    if fn is None:
                        S.op("act", lambda e, o=o, ps=ps: e.copy(o[:], ps[:]), reads=[t_ps], writes=[t_o])
                    else:
                        S.op("act", lambda e, o=o, ps=ps, fn=fn: e.activation(o[:], ps[:], fn), reads=[t_ps], writes=[t_o])
                    S.dma("sp", lambda e, o=o, dst=dst, i=i, half=half: e.dma_start(
                        out=dst[i * 128:(i + 1) * 128, half * 512:(half + 1) * 512], in_=o[:]), reads=[t_o], writes=[c.t_vz])

        sm = sb("p1sm", [128, NT, 32], F32)
        t_sm = Tok()
        wt, t_w = load_w(wtm, c.w_in, C_SM, 32)
        for i in range(NT):
            ps, t_ps = proj_tm(wt, t_w, i, 32)
            S.op("dve", lambda e, ps=ps, i=i: e.tensor_copy(sm[:, i, :], ps[:, 0:32]), reads=[t_ps], writes=[t_sm])
        prm = sb("p1prm", [128, 32], F32)
        t_prm = Tok()
        for j, src in enumerate((c.dt_bias_fwd, c.dt_bias_bwd, c.a_log_fwd, c.a_log_bwd)):
            S.dma("sp", lambda e, j=j, src=src: e.dma_start(out=prm[:, j * 8:(j + 1) * 8], in_=src.broadcast_to([128, 8])),
                  writes=[t_prm])
        tmpa = sb("p1tmpa", [128, NT, 16], F32)
        tmpb = sb("p1tmpb", [128, NT, 16], F32)
        t_ta, t_tb = Tok(), Tok()
        dtb = prm[:, 0:16].unsqueeze(1).broadcast_to([128, NT, 16])
        S.op("act", lambda e: e.activation(prm[:, 16:32], prm[:, 16:32], AF.Exp), reads=[t_prm], writes=[t_prm])
        nA = prm[:, 16:32].unsqueeze(1).broadcast_to([128, NT, 16])
        S.op("dve", lambda e: e.tensor_tensor(out=sm[:, :, 0:16], in0=sm[:, :, 0:16], in1=dtb, op=ALU.add),
             reads=[t_sm, t_prm], writes=[t_sm])
        S.op("act", lambda e: e.activation(tmpa[:], sm[:, :, 0:16], AF.Abs), reads=[t_sm], writes=[t_ta])
        S.op("act", lambda e: e.activation(tmpa[:], tmpa[:], AF.Exp, scale=-1.0), reads=[t_ta], writes=[t_ta])
        S.op("act", lambda e: e.activation(tmpa[:], tmpa[:], AF.Ln, bias=1.0), reads=[t_ta], writes=[t_ta])
        S.op("dve", lambda e: e.scalar_tensor_tensor(out=tmpb[:], in0=sm[:, :, 0:16], scalar=0.0, in1=tmpa[:],
                                                     op0=ALU.max, op1=ALU.add), reads=[t_sm, t_ta], writes=[t_tb])
        S.op("dve", lambda e: e.scalar_tensor_tensor(out=sm[:, :, 0:16], in0=tmpb[:], scalar=-1.0, in1=nA,
                                                     op0=ALU.mult, op1=ALU.mult), reads=[t_tb, t_prm], writes=[t_sm])
        S.op("act", lambda e: e.activation(sm[:, :, 16:32], sm[:, :, 16:32], AF.Sigmoid), reads=[t_sm], writes=[t_sm])
        S.dma("sp", lambda e: e.dma_start(out=c.small_d, in_=sm[:]), reads=[t_sm], writes=[c.t_small])

        cw5 = sb("p1cw5", [5, 3072], F32)
        t_cw5 = Tok()
        S.dma("sp", lambda e: e.dma_start(out=cw5[:], in_=c.conv_w), writes=[t_cw5])
        cwT = sb("p1cwT", [128, 24, 8], F32)
        t_cwT = Tok()
        for ct in range(24):
            ps, t_ps = psm.next()
            S.op("pe", lambda e, ps=ps, ct=ct: e.transpose(ps[:, 0:5], cw5[0:5, ct * 128:(ct + 1) * 128], c.ident_f[0:5, 0:5]),
                 reads=[t_cw5, TC], writes=[t_ps])
            S.op("dve", lambda e, ps=ps, ct=ct: e.tensor_copy(cwT[:, ct, 0:5], ps[:, 0:5]), reads=[t_ps], writes=[t_cwT])
        dgs = _mk_ring(st, nc, "p1dg", 2, [128, 5, 128], BF16)
        xpre = _mk_ring(st, nc, "p1xpre", 2, [128, S_LEN + 4], BF16)
        tmstage = _mk_ring(st, nc, "p1tmstage", 2, [128, NT, 128], BF16)
        for ct in range(24):
            kind = ct // 8
            h = ct % 8
            w1, t_w1 = load_w(wfm, c.w_in, C_GQ + ct * 128, 128)
            dg, t_dg = dgs.next()
            for j in range(5):
                S.op("pool", lambda e, dg=dg, j=j, ct=ct: e.tensor_scalar(
                    out=dg[:, j, :], in0=c.ident_f[:], scalar1=cwT[:, ct, j:j + 1], scalar2=None, op0=ALU.mult),
                    reads=[TC, t_cwT], writes=[t_dg])
            xp, t_xp = xpre.next()
            S.op("pool", lambda e, xp=xp: e.memset(xp[:, 0:2], 0.0), writes=[t_xp])
            S.op("pool", lambda e, xp=xp: e.memset(xp[:, S_LEN + 2:S_LEN + 4], 0.0), writes=[t_xp])
            for g in range(NG):
                ps1, t_ps1 = proj_fm(w1, t_w1, g)
                S.op("act", lambda e, xp=xp, ps1=ps1, g=g: e.copy(xp[:, 2 + g * 512:2 + (g + 1) * 512], ps1[:]),
                     reads=[t_ps1], writes=[t_xp])
            stg, t_stg = stage.next()
            for g in range(NG):
                ps, t_ps = psm.next()
                for j in range(5):
                    S.op("pe", lambda e, ps=ps, dg=dg, xp=xp, g=g, j=j: e.matmul(
                        ps[:], dg[:, j, :], xp[:, g * 512 + j:g * 512 + j + 512], start=(j == 0), stop=(j == 4)),
                        reads=[t_dg, t_xp], writes=[t_ps])
                sl = slice(g * 512, (g + 1) * 512)
                if kind == 2:
                    S.op("act", lambda e, ps=ps, stg=stg, sl=sl: e.activation(stg[:, sl], ps[:], AF.Silu),
                         reads=[t_ps], writes=[t_stg])
                else:
                    a, t_a = f32a.next()
                    S.op("act", lambda e, ps=ps, a=a: e.activation(a[:], ps[:], AF.Silu), reads=[t_ps], writes=[t_a])
                    sqb, t_sqb = tmst.next()
                    S.op("pool", lambda e, sqb=sqb, a=a: e.tensor_tensor(out=sqb[:], in0=a[:], in1=a[:], op=ALU.mult),
                         reads=[t_a], writes=[t_sqb])
                    ps2, t_ps2 = psm.next()
                    S.op("pe", lambda e, ps2=ps2, sqb=sqb: e.matmul(ps2[:], c.ones_b[:], sqb[:], start=True, stop=True),
                         reads=[t_sqb, TC], writes=[t_ps2])
                    b, t_b = f32b.next()
                    S.op("act", lambda e, b=b, ps2=ps2: e.activation(b[:], ps2[:], AF.Sqrt, bias=EPS), reads=[t_ps2], writes=[t_b])
                    S.op("dve", lambda e, b=b: e.reciprocal(b[:], b[:]), reads=[t_b], writes=[t_b])
                    scl = (128.0 ** -0.5) if kind == 0 else 1.0
                    S.op("dve", lambda e, a=a, b=b, stg=stg, sl=sl, scl=scl: e.scalar_tensor_tensor(
                        out=stg[:, sl], in0=a[:], scalar=scl, in1=b[:], op0=ALU.mult, op1=ALU.mult),
                        reads=[t_a, t_b], writes=[t_stg])
            if kind < 2:
                dst = c.gq_d if kind == 0 else c.gk_d
                S.dma("sp", lambda e, stg=stg, dst=dst, h=h: e.dma_start(out=dst[h], in_=stg[:]), reads=[t_stg], writes=[c.t_gqk])
            if kind >= 1:
                dst = c.gktok_d if kind == 1 else c.gvtok_d
                tms, t_tms = tmstage.next()
                for i8 in range(4):
                    pt, t_pt = pst.next()
                    for ii in range(8):
                        i = i8 * 8 + ii
                        S.op("pe", lambda e, pt=pt, stg=stg, ii=ii, i=i: e.transpose(pt[:, ii, :], stg[:, i * 128:(i + 1) * 128], c.ident_b[:]),
                             reads=[t_stg, TC], writes=[t_pt])
                    S.op("dve", lambda e, pt=pt, tms=tms, i8=i8: e.tensor_copy(tms[:, i8 * 8:(i8 + 1) * 8, :], pt[:]),
                         reads=[t_pt], writes=[t_tms])
                S.dma("sp", lambda e, tms=tms, dst=dst, h=h: e.dma_start(
                    out=dst[:, h * 128:(h + 1) * 128].rearrange("(t p) c -> p t c", p=128), in_=tms[:]),
                    reads=[t_tms], writes=[c.t_gtok])
    S.barrier()


def build_program(debug=False, phases=("p1",), inject=(), opts=None):
    nc = bass.Bass("TRN2", target_bir_lowering=False)
    c = Ctx()
    c.inject = set(inject)
    c.opts = opts or {}
    c.nc = nc
    c.S = Sched(nc)

    c.input_names = []

    def din(name, shape):
        c.input_names.append(name)
        return nc.dram_tensor(name, list(shape), F32, kind="ExternalInput").ap()

    c.x = din("x", [S_LEN, D])
    c.norm1_w = din("norm1_w", [1, D])
    c.w_in = din("w_in", [D, IN_W])
    c.w_qkp = din("w_qkp", [D, 2048])
    c.cosT = din("cosT", [128, S_LEN])
    c.sinT = din("sinT", [128, S_LEN])
    c.conv_w = din("conv_w", [5, 3072])
    for n in ("a_log_fwd", "dt_bias_fwd", "a_log_bwd", "dt_bias_bwd"):
        setattr(c, n, din(n, [1, 8]))

    c.debug_names = []

    def scratch(name, shape, dt):
        kind = "ExternalOutput" if debug else "Internal"
        if name in c.inject:
            kind = "ExternalInput"
            c.input_names.append(name)
        elif debug:
            c.debug_names.append(name)
        return nc.dram_tensor(name, list(shape), dt, kind=kind).ap()

    c.qT_d = scratch("qT_d", [H, 128, S_LEN], BF16)
    c.kT_d = scratch("kT_d", [H, 128, S_LEN], BF16)
    c.v_d = scratch("v_d", [S_LEN, D], BF16)
    c.zs_d = scratch("zs_d", [S_LEN, D], BF16)
    c.sga_d = scratch("sga_d", [H, 128, S_LEN], BF16)
    c.sgg_d = scratch("sgg_d", [H, 128, S_LEN], BF16)
    c.gq_d = scratch("gq_d", [H, 128, S_LEN], BF16)
    c.gk_d = scratch("gk_d", [H, 128, S_LEN], BF16)
    c.gktok_d = scratch("gktok_d", [S_LEN, D], BF16)
    c.gvtok_d = scratch("gvtok_d", [S_LEN, D], BF16)
    c.small_d = scratch("small_d", [128, NT, 32], F32)
    for n in ("lambda_q1", "lambda_k1", "lambda_q2", "lambda_k2"):
        setattr(c, n, din(n, [1, 64]))
    c.subln_w = din("subln_w", [1, 128])
    c.gdn_norm_w = din("gdn_norm_w", [1, 128])
    c.w_proj_attn = din("w_proj_attn", [D, D])
    c.w_proj_gdn = din("w_proj_gdn", [D, D])
    c.w_out = din("w_out", [D, D])
    c.w_router = din("w_router", [D, NE])
    c.norm2_w = din("norm2_w", [1, D])
    c.norm_f_w = din("norm_f_w", [1, D])
    c.w_gate = din("w_gate", [NE, D, FF])
    c.w_up = din("w_up", [NE, D, FF])
    c.w_down = din("w_down", [NE, FF, D])
    c.out = nc.dram_tensor("out", [S_LEN, D], F32, kind="ExternalOutput").ap()
    c.x1_d = scratch("x1_d", [S_LEN, D], F32)
    c.h2tok_d = scratch("h2tok_d", [S_LEN, D], BF16)
    c.aff_d = scratch("aff_d", [128, NT, NE], F32)
    c.pos_d = scratch("pos_d", [NE, S_LEN], F32)
    c.ptok_d = scratch("ptok_d", [128, NT, NE], F32)
    c.ye_d = scratch("ye_d", [NE, CAP, D], BF16)
    for n in ("t_x1", "t_h2", "t_aff", "t_pos", "t_ye", "t_out"):
        setattr(c, n, Tok())
    c.ogT_d = scratch("ogT_d", [H, 128, S_LEN], BF16)
    c.t_og = Tok()
    c.oaT_d = scratch("oaT_d", [H, 128, S_LEN], BF16)
    for n in ("t_qk", "t_gates", "t_vz", "t_small", "t_gqk", "t_gtok", "t_oa"):
        setattr(c, n, Tok())

    with ExitStack() as st:
        _consts(c, st)
        if "p1" in phases:
            phase1(c)
        if "p2" in phases:
            phase2(c)
        if "p3" in phases:
            phase3(c)
        if "p4" in phases:
            phase4(c)
        if "p5" in phases:
            phase5(c)
        if "p6a" in phases:
            phase6a(c)
        if "p6b" in phases:
            phase6b(c)
        c.S.barrier()
        c.S.emit()
    return nc, c


def host_prep(inputs):
    f = lambda a: np.ascontiguousarray(np.asarray(a, dtype=np.float32))
    w_in = f(inputs["w_in"][0])
    perm = np.arange(2048).reshape(16, 2, 2, 32)[:, :, ::-1, :].reshape(-1)
    w_qkp = np.ascontiguousarray(w_in[:, :2048][:, perm])
    inv = 10000.0 ** (-np.arange(0, 64, 2, dtype=np.float32) / 64)
    ang = np.arange(S_LEN, dtype=np.float32)[:, None] * inv[None, :]
    ang = np.concatenate([ang, ang], axis=-1)
    cos = np.cos(ang).T.astype(np.float32)
    sin = np.sin(ang).T.astype(np.float32)
    sin[:32] *= -1.0
    shared = {
        "norm1_w": f(inputs["norm1_w"]).reshape(1, D),
        "w_in": w_in,
        "w_qkp": w_qkp,
        "cosT": np.ascontiguousarray(np.concatenate([cos, cos], 0)),
        "sinT": np.ascontiguousarray(np.concatenate([sin, sin], 0)),
        "conv_w": f(inputs["conv_w"][0]),
    }
    for n in ("a_log_fwd", "dt_bias_fwd", "a_log_bwd", "dt_bias_bwd"):
        shared[n] = f(inputs[n]).reshape(1, 8)
    for n in ("lambda_q1", "lambda_k1", "lambda_q2", "lambda_k2"):
        shared[n] = f(inputs[n]).reshape(1, 64)
    shared["subln_w"] = f(inputs["subln_w"]).reshape(1, 128)
    shared["gdn_norm_w"] = f(inputs["gdn_norm_w"]).reshape(1, 128)
    shared["w_proj_attn"] = f(inputs["w_proj_attn"][0])
    shared["w_proj_gdn"] = f(inputs["w_proj_gdn"][0])
    shared["w_out"] = f(inputs["w_out"][0])
    shared["w_router"] = f(inputs["w_router"][0])
    shared["norm2_w"] = f(inputs["norm2_w"]).reshape(1, D)
    shared["norm_f_w"] = f(inputs["norm_f_w"]).reshape(1, D)
    shared["w_gate"] = f(inputs["w_gate"][0])
    shared["w_up"] = f(inputs["w_up"][0])
    shared["w_down"] = f(inputs["w_down"][0])
    return shared


def phase2(c):
    nc, S = c.nc, c.S
    TC = c.t_const
    with ExitStack() as st:
        sb = lambda name, shape, dt: st.enter_context(nc.sbuf_tensor(name, shape, dt))
        lam = sb("p2lam", [128, 4, 64], F32)
        lsc = sb("p2lsc", [128, 8], F32)
        t_lam = Tok()
        for j, src in enumerate((c.lambda_q1, c.lambda_k1, c.lambda_q2, c.lambda_k2)):
            S.dma("sp", lambda e, j=j, src=src: e.dma_start(out=lam[:, j, :], in_=src.broadcast_to([128, 64])), writes=[t_lam])
        S.op("dve", lambda e: e.tensor_tensor(out=lam[:, 0, :], in0=lam[:, 0, :], in1=lam[:, 1, :], op=ALU.mult), reads=[t_lam], writes=[t_lam])
        S.op("dve", lambda e: e.tensor_tensor(out=lam[:, 2, :], in0=lam[:, 2, :], in1=lam[:, 3, :], op=ALU.mult), reads=[t_lam], writes=[t_lam])
        S.op("act", lambda e: e.activation(lam[:, 1, :], lam[:, 0, :], AF.Identity, accum_out=lsc[:, 0:1]), reads=[t_lam], writes=[t_lam])
        S.op("act", lambda e: e.activation(lam[:, 3, :], lam[:, 2, :], AF.Identity, accum_out=lsc[:, 1:2]), reads=[t_lam], writes=[t_lam])
        S.op("act", lambda e: e.activation(lsc[:, 2:4], lsc[:, 0:2], AF.Exp), reads=[t_lam], writes=[t_lam])
        S.op("dve", lambda e: e.scalar_tensor_tensor(out=lsc[:, 4:5], in0=lsc[:, 3:4], scalar=-LAMBDA_INIT, in1=lsc[:, 2:3],
                                                     op0=ALU.add, op1=ALU.subtract), reads=[t_lam], writes=[t_lam])
        wsub = sb("p2wsub", [128, 2], F32)
        t_wsub = Tok()
        S.dma("sp", lambda e: e.dma_start(out=wsub[:, 0:1], in_=c.subln_w.rearrange("o e -> e o")), writes=[t_wsub])
        S.op("dve", lambda e: e.tensor_scalar(out=wsub[:, 1:2], in0=wsub[:, 0:1], scalar1=(1.0 - LAMBDA_INIT), scalar2=None, op0=ALU.mult),
             reads=[t_wsub], writes=[t_wsub])

        qr = _mk_ring(st, nc, "p2q", 2, [128, S_LEN], BF16)
        kr = _mk_ring(st, nc, "p2k", 2, [128, S_LEN], BF16)
        vr = _mk_ring(st, nc, "p2v", 2, [128, NT, 128], BF16)
        pr = _mk_ring(st, nc, "p2p", 4, [128, 512], BF16)
        fr = _mk_ring(st, nc, "p2f", 6, [128, 512], F32)
        sqr = _mk_ring(st, nc, "p2sq", 2, [128, 512], BF16)
        accr = _mk_ring(st, nc, "p2acc", 4, [128, 512], F32)
        stage = _mk_ring(st, nc, "p2stage", 2, [128, S_LEN], BF16)
        ps_s = _mk_ring(st, nc, "p2pss", 3, [128, 512], F32, psum=True)
        ps_o = [_mk_ring(st, nc, "p2pso%d" % t, 1, [128, 512], F32, psum=True) for t in range(2)]
        ps_l = [_mk_ring(st, nc, "p2psl%d" % t, 1, [128, 512], F32, psum=True) for t in range(2)]
        ps_x = _mk_ring(st, nc, "p2psx", 1, [128, 512], F32, psum=True)

        for h in range(H):
            q, t_q = qr.next()
            k, t_k = kr.next()
            v, t_v = vr.next()
            S.dma("sp", lambda e, q=q, h=h: e.dma_start(out=q[:], in_=c.qT_d[h]), reads=[c.t_qk], writes=[t_q])
            S.dma("sp", lambda e, k=k, h=h: e.dma_start(out=k[:], in_=c.kT_d[h]), reads=[c.t_qk], writes=[t_k])
            S.dma("sp", lambda e, v=v, h=h: e.dma_start(
                out=v[:], in_=c.v_d[:, h * 128:(h + 1) * 128].rearrange("(t p) e -> p t e", p=128)), reads=[c.t_vz], writes=[t_v])
            stg, t_stg = stage.next()
            for g in range(NG):
                qs = slice(g * 512, (g + 1) * 512)
                accs = []
                for t in range(2):
                    po, t_po = ps_o[t].next()
                    pl, t_pl = ps_l[t].next()
                    ts = slice(t * 64, (t + 1) * 64)
                    accA, t_accA = accr.next()
                    accB, t_accB = accr.next()

                    def emit_s(j, ts=ts, qs=qs, k=k, q=q, t_k=t_k, t_q=t_q):
                        pss, t_pss = ps_s.next()
                        S.op("pe", lambda e, pss=pss, j=j: e.matmul(
                            pss[:], k[ts, j * 128:(j + 1) * 128], q[ts, qs], start=True, stop=True),
                            reads=[t_k, t_q], writes=[t_pss])
                        return pss, t_pss
                    cur = emit_s(0)
                    for j in range(NT):
                        nxt = emit_s(j + 1) if j + 1 < NT else None
                        pss, t_pss = cur
                        p, t_p = pr.next()
                        S.op("act", lambda e, p=p, pss=pss: e.activation(p[:], pss[:], AF.Exp, scale=0.125),
                             reads=[t_pss], writes=[t_p])
                        S.op("pe", lambda e, po=po, v=v, p=p, j=j: e.matmul(po[:], v[:, j, :], p[:], start=(j == 0), stop=(j == NT - 1)),
                             reads=[t_v, t_p], writes=[t_po])
                        acc, t_acc = (accA, t_accA) if j % 2 == 0 else (accB, t_accB)
                        eng = "dve" if j % 2 == 0 else "pool"
                        if j < 2:
                            S.op(eng, lambda e, acc=acc, p=p: e.tensor_copy(acc[:], p[:]), reads=[t_p], writes=[t_acc])
                        else:
                            S.op(eng, lambda e, acc=acc, p=p: e.tensor_tensor(out=acc[:], in0=acc[:], in1=p[:], op=ALU.add),
                                 reads=[t_p, t_acc], writes=[t_acc])
                        cur = nxt
                    S.op("pe", lambda e, pl=pl, accA=accA: e.matmul(pl[:], c.ones_f[:], accA[:], start=True, stop=False),
                         reads=[TC, t_accA], writes=[t_pl])
                    S.op("pe", lambda e, pl=pl, accB=accB: e.matmul(pl[:], c.ones_f[:], accB[:], start=False, stop=True),
                         reads=[TC, t_accB], writes=[t_pl])
                    accs.append((po, t_po, pl, t_pl))
                outs = []
                for t in range(2):
                    po, t_po, pl, t_pl = accs[t]
                    r, t_r = fr.next()
                    S.op("dve", lambda e, r=r, pl=pl: e.reciprocal(r[:], pl[:]), reads=[t_pl], writes=[t_r])
                    a, t_a = fr.next()
                    S.op("dve", lambda e, a=a, po=po, r=r: e.tensor_tensor(out=a[:], in0=po[:], in1=r[:], op=ALU.mult),
                         reads=[t_po, t_r], writes=[t_a])
                    outs.append((a, t_a))
                (a, t_a), (b, t_b) = outs
                oa, t_oa = fr.next()
                S.op("dve", lambda e, oa=oa, a=a, b=b: e.scalar_tensor_tensor(
                    out=oa[:], in0=b[:], scalar=lsc[:, 4:5], in1=a[:], op0=ALU.mult, op1=ALU.add),
                    reads=[t_a, t_b, t_lam], writes=[t_oa])
                sq, t_sq = sqr.next()
                S.op("pool", lambda e, sq=sq, oa=oa: e.tensor_tensor(out=sq[:], in0=oa[:], in1=oa[:], op=ALU.mult),
                     reads=[t_oa], writes=[t_sq])
                px, t_px = ps_x.next()
                S.op("pe", lambda e, px=px, sq=sq: e.matmul(px[:], c.ones_b[:], sq[:], start=True, stop=True),
                     reads=[TC, t_sq], writes=[t_px])
                rs, t_rs = fr.next()
                S.op("act", lambda e, rs=rs, px=px: e.activation(rs[:], px[:], AF.Sqrt, bias=EPS, scale=1.0 / 128), reads=[t_px], writes=[t_rs])
                S.op("dve", lambda e, rs=rs: e.reciprocal(rs[:], rs[:]), reads=[t_rs], writes=[t_rs])
                S.op("dve", lambda e, stg=stg, oa=oa, rs=rs, qs=qs: e.scalar_tensor_tensor(
                    out=stg[:, qs], in0=oa[:], scalar=wsub[:, 1:2], in1=rs[:], op0=ALU.mult, op1=ALU.mult),
                    reads=[t_oa, t_rs, t_wsub], writes=[t_stg])
            S.dma("sp", lambda e, stg=stg, h=h: e.dma_start(out=c.oaT_d[h], in_=stg[:]), reads=[t_stg], writes=[c.t_oa])
    S.barrier()


def phase3(c):
    nc, S = c.nc, c.S
    TC = c.t_const
    NB = NT
    with ExitStack() as st:
        sb = lambda name, shape, dt: st.enter_context(nc.sbuf_tensor(name, shape, dt))
        inc = [sb("p3inc%d" % d, [128, 128], F32) for d in range(2)]
        strm = [sb("p3str%d" % d, [128, 128], F32) for d in range(2)]
        mbias = [sb("p3mb%d" % d, [128, 128], F32) for d in range(2)]
        esel = [sb("p3es%d" % d, [128, 128], F32) for d in range(2)]
        t_m = Tok()
        for d in range(2):
            sgn = 1 if d == 0 else -1
            S.op("pool", lambda e, d=d: e.memset(inc[d][:], 1.0), writes=[t_m])
            S.op("pool", lambda e, d=d, sgn=sgn: e.affine_select(out=inc[d][:], in_=inc[d][:], compare_op=ALU.is_ge, fill=0.0,
                                                                base=0, pattern=[[sgn, 128]], channel_multiplier=-sgn),
                 reads=[t_m], writes=[t_m])
            S.op("pool", lambda e, d=d: e.memset(strm[d][:], 1.0), writes=[t_m])
            S.op("pool", lambda e, d=d, sgn=sgn: e.affine_select(out=strm[d][:], in_=strm[d][:], compare_op=ALU.is_ge, fill=0.0,
                                                                base=-1, pattern=[[sgn, 128]], channel_multiplier=-sgn),
                 reads=[t_m], writes=[t_m])
            S.op("pool", lambda e, d=d: e.tensor_scalar(out=mbias[d][:], in0=inc[d][:], scalar1=30000.0, scalar2=-30000.0,
                                                        op0=ALU.mult, op1=ALU.add), reads=[t_m], writes=[t_m])
            lastp = 127 if d == 0 else 0
            S.op("pool", lambda e, d=d: e.memset(esel[d][:], 0.0), writes=[t_m])
            S.op("pool", lambda e, d=d, lastp=lastp: e.affine_select(out=esel[d][:], in_=esel[d][:], compare_op=ALU.not_equal, fill=1.0,
                                                                    base=-lastp, pattern=[[0, 128]], channel_multiplier=1),
                 reads=[t_m], writes=[t_m])
        sm = sb("p3sm", [128, NT, 32], F32)
        t_sm = Tok()
        S.dma("sp", lambda e: e.dma_start(out=sm[:], in_=c.small_d), reads=[c.t_small], writes=[t_sm])
        gw = sb("p3gw", [128, 128], F32)
        t_gw = Tok()
        S.dma("sp", lambda e: e.dma_start(out=gw[:], in_=c.gdn_norm_w.broadcast_to([128, 128])), writes=[t_gw])

        def bank(name, dt=F32, n=512):
            return st.enter_context(nc.psum_tensor(name, [128, n], dt))
        b0, b2, b3, b4, b5, b6, b7 = [bank("p3b" + x) for x in "0234567"]
        b1 = bank("p3b1", BF16, 1024)
        btok = {id(b): Tok(excl=True) for b in (b0, b1, b2, b3, b4, b5, b6, b7)}
        slot = lambda b, i, w=128: (b[:, i * w:(i + 1) * w], btok[id(b)])
        s_kk, s_kq = slot(b0, 0), slot(b0, 1)
        s_gdd = [slot(b0, 2), slot(b0, 3)]
        s_misc = [slot(b6, 3)] * 4
        s_tr = [slot(b1, i) for i in range(8)]
        s_sqd = [(slot(b2, 0), slot(b2, 1)), (slot(b4, 0), slot(b4, 1))]
        s_apd = [slot(b3, 0, 256), slot(b5, 0, 256)]
        s_scan = [[slot(b6, i) for i in range(4)], [slot(b7, i) for i in range(4)]]

        kT = sb("p3kT", [128, S_LEN], BF16)
        qT = sb("p3qT", [128, S_LEN], BF16)
        ktok = sb("p3ktok", [128, NB, 128], BF16)
        vtok = sb("p3vtok", [128, NB, 128], BF16)
        zs = sb("p3zs", [128, NB, 128], BF16)
        t_in = Tok()
        ub = [sb("p3ub%d" % d, [128, NB, 128], F32) for d in range(2)]
        nwT = [sb("p3nwT%d" % d, [128, NB, 128], BF16) for d in range(2)]
        kdec = [sb("p3kdec%d" % d, [128, NB, 128], BF16) for d in range(2)]
        qkT = [sb("p3qkT%d" % d, [128, NB, 128], BF16) for d in range(2)]
        t_blk = [[Tok() for _ in range(NB)] for _ in range(2)]
        sc = [sb("p3sc%d" % d, [128, 8, NB], F32) for d in range(2)]
        t_sc = [Tok(), Tok()]
        oacc = sb("p3oacc", [128, NB, 128], F32)
        t_oacc = [Tok() for _ in range(NB)]
        S32 = [sb("p3S32_%d" % d, [128, 128], F32) for d in range(2)]
        Sbf = [sb("p3Sbf_%d" % d, [128, 128], BF16) for d in range(2)]
        t_S = [Tok(), Tok()]
        dtr = _mk_ring(st, nc, "p3dt", 5, [128, 128], F32)
        tmpr = _mk_ring(st, nc, "p3tmp", 5, [128, 128], F32)
        ntr = _mk_ring(st, nc, "p3nt", 10, [128, 128], F32)
        nnr = _mk_ring(st, nc, "p3nn", 10, [128, 128], F32)
        xr = _mk_ring(st, nc, "p3x", 5, [128, 256], F32)
        vnr = _mk_ring(st, nc, "p3vn", 4, [128, 128], BF16)
        o2r = _mk_ring(st, nc, "p3o2", 4, [128, 128], F32)
        big = sb("p3big", [128, NB, 128], F32)
        t_big = Tok()
        red = sb("p3red", [128, NB], F32)
        ogtok = sb("p3ogtok", [128, NB, 128], BF16)
        stage = sb("p3stage", [128, S_LEN], BF16)
        t_stage = Tok()

        stop = c.opts.get("p3_stop", 9)
        for h in range(c.opts.get("p3_heads", H)):
            S.dma("sp", lambda e, h=h: e.dma_start(out=kT[:], in_=c.gk_d[h]), reads=[c.t_gqk], writes=[t_in])
            S.dma("sp", lambda e, h=h: e.dma_start(out=qT[:], in_=c.gq_d[h]), reads=[c.t_gqk], writes=[t_in])
            for (buf, src, tk) in ((ktok, c.gktok_d, c.t_gtok), (vtok, c.gvtok_d, c.t_gtok), (zs, c.zs_d, c.t_vz)):
                S.dma("sp", lambda e, buf=buf, src=src, h=h: e.dma_start(
                    out=buf[:], in_=src[:, h * 128:(h + 1) * 128].rearrange("(t p) e -> p t e", p=128)), reads=[tk], writes=[t_in])
            for d in range(2):
                s_ = sc[d]
                g = sm[:, :, d * 8 + h]
                beta = sm[:, :, 16 + d * 8 + h]
                (pm, t_pm) = s_misc[d * 2]
                S.op("pe", lambda e, pm=pm, d=d, g=g: e.matmul(pm[:, 0:NB], inc[d][:], g, start=True, stop=True),
                     reads=[t_m, t_sm], writes=[t_pm])
                S.op("dve", lambda e, pm=pm, s_=s_: e.tensor_copy(s_[:, 0, :], pm[:, 0:NB]), reads=[t_pm], writes=[t_sc[d]])
                S.op("dve", lambda e, s_=s_: e.tensor_scalar(out=s_[:, 1, :], in0=s_[:, 0, :], scalar1=-1.0, scalar2=None, op0=ALU.mult),
                     reads=[t_sc[d]], writes=[t_sc[d]])
                (pm2, t_pm2) = s_misc[d * 2 + 1]
                S.op("pe", lambda e, pm2=pm2, d=d, s_=s_: e.matmul(pm2[:, 0:NB], esel[d][:], s_[:, 0, :], start=True, stop=True),
                     reads=[t_m, t_sc[d]], writes=[t_pm2])
                S.op("dve", lambda e, pm2=pm2, s_=s_: e.tensor_copy(s_[:, 2, :], pm2[:, 0:NB]), reads=[t_pm2], writes=[t_sc[d]])
                S.op("act", lambda e, s_=s_: e.activation(s_[:, 3, :], s_[:, 0, :], AF.Exp), reads=[t_sc[d]], writes=[t_sc[d]])
                S.op("dve", lambda e, s_=s_: e.tensor_tensor(out=s_[:, 4, :], in0=s_[:, 2, :], in1=s_[:, 0, :], op=ALU.subtract),
                     reads=[t_sc[d]], writes=[t_sc[d]])
                S.op("act", lambda e, s_=s_: e.activation(s_[:, 4, :], s_[:, 4, :], AF.Exp), reads=[t_sc[d]], writes=[t_sc[d]])
                S.op("act", lambda e, s_=s_: e.activation(s_[:, 5, :], s_[:, 2, :], AF.Exp), reads=[t_sc[d]], writes=[t_sc[d]])
                S.op("dve", lambda e, s_=s_, beta=beta: e.tensor_scalar(out=s_[:, 6, :], in0=beta, scalar1=-1.0, scalar2=None, op0=ALU.mult),
                     reads=[t_sm, t_sc[d]], writes=[t_sc[d]])
                S.op("dve", lambda e, s_=s_, beta=beta: e.tensor_copy(s_[:, 7, :], beta), reads=[t_sm, t_sc[d]], writes=[t_sc[d]])

            def par_chain(b, d):
                bs = slice(b * 128, (b + 1) * 128)
                s_ = sc[d]
                (pkk, t_pkk), (pkq, t_pkq), (pgd, t_pgd) = s_kk, s_kq, s_gdd[d]
                if d == 0:
                    S.op("pe", lambda e: e.matmul(pkk, kT[:, bs], kT[:, bs], start=True, stop=True), reads=[t_in], writes=[t_pkk])
                    S.op("pe", lambda e: e.matmul(pkq, kT[:, bs], qT[:, bs], start=True, stop=True), reads=[t_in], writes=[t_pkq])
                S.op("pe", lambda e: e.matmul(pgd, s_[:, 0, b:b + 1].broadcast_to([128, 128]), c.ident_f[:], start=True, stop=False),
                     reads=[t_sc[d], TC], writes=[t_pgd])
                S.op("pe", lambda e: e.matmul(pgd, c.ident_f[:], s_[:, 1, b:b + 1].broadcast_to([128, 128]), start=False, stop=False),
                     reads=[t_sc[d], TC], writes=[t_pgd])
                S.op("pe", lambda e: e.matmul(pgd, c.ident_f[:], mbias[d][:], start=False, stop=True), reads=[t_m, TC], writes=[t_pgd])
                yield
                dt_, t_dt = dtr.next()
                S.op("act", lambda e: e.activation(dt_[:], pgd, AF.Exp), reads=[t_pgd], writes=[t_dt])
                tmp, t_tmp = tmpr.next()
                S.op("dve", lambda e: e.scalar_tensor_tensor(out=tmp[:], in0=pkk, scalar=s_[:, 6, b:b + 1], in1=dt_[:], op0=ALU.mult, op1=ALU.mult),
                     reads=[t_pkk, t_sc[d], t_dt], writes=[t_tmp])
                S.op("dve", lambda e: e.tensor_tensor(out=qkT[d][:, b, :], in0=pkq, in1=dt_[:], op=ALU.mult),
                     reads=[t_pkq, t_dt], writes=[t_blk[d][b]])
                nt, t_nt = ntr.next()
                S.op("pool", lambda e, nt=nt: e.tensor_tensor(out=nt[:], in0=tmp[:], in1=strm[d][:], op=ALU.mult), reads=[t_tmp, t_m], writes=[t_nt])
                x, t_x = xr.next()
                S.op("pool", lambda e: e.tensor_copy(x[:, 0:128], vtok[:, b, :]), reads=[t_in], writes=[t_x])
                S.op("pool", lambda e: e.tensor_scalar(out=x[:, 128:256], in0=ktok[:, b, :], scalar1=s_[:, 3, b:b + 1], scalar2=None, op0=ALU.mult),
                     reads=[t_in, t_sc[d]], writes=[t_x])
                S.op("pool", lambda e: e.tensor_scalar(out=kdec[d][:, b, :], in0=ktok[:, b, :], scalar1=s_[:, 4, b:b + 1], scalar2=None, op0=ALU.mult),
                     reads=[t_in, t_sc[d]], writes=[t_blk[d][b]])
                yield
                (ptr, t_ptr) = s_sqd[d][0]
                S.op("pe", lambda e, nt=nt: e.transpose(ptr, nt[:], c.ident_f[:]), reads=[t_nt, TC], writes=[t_ptr])
                yield
                nn, t_nn = nnr.next()
                S.op("act", lambda e, nn=nn: e.copy(nn[:], ptr), reads=[t_ptr], writes=[t_nn])
                yield
                for l in range(7):
                    (pap, t_pap) = s_apd[d]
                    S.op("pe", lambda e, nt=nt: e.matmul(pap, nt[:], x[:], start=True, stop=True), reads=[t_nt, t_x], writes=[t_pap])
                    if l < 6:
                        (pn2, t_pn2), (pnt2, t_pnt2) = s_sqd[d]
                        S.op("pe", lambda e, nt=nt, nn=nn: e.matmul(pn2, nt[:], nn[:], start=True, stop=True), reads=[t_nt, t_nn], writes=[t_pn2])
                        S.op("pe", lambda e, nt=nt, nn=nn: e.matmul(pnt2, nn[:], nt[:], start=True, stop=True), reads=[t_nt, t_nn], writes=[t_pnt2])
                    yield
                    S.op("dve", lambda e: e.tensor_tensor(out=x[:], in0=pap, in1=x[:], op=ALU.add), reads=[t_pap, t_x], writes=[t_x])
                    if l < 6:
                        nn2, t_nn2 = nnr.next()
                        nt2, t_nt2 = ntr.next()
                        S.op("act", lambda e, nn2=nn2: e.copy(nn2[:], pn2), reads=[t_pn2], writes=[t_nn2])
                        S.op("act", lambda e, nt2=nt2: e.copy(nt2[:], pnt2), reads=[t_pnt2], writes=[t_nt2])
                        nn, t_nn, nt, t_nt = nn2, t_nn2, nt2, t_nt2
                    yield
                S.op("dve", lambda e: e.tensor_scalar(out=ub[d][:, b, :], in0=x[:, 0:128], scalar1=s_[:, 7, b:b + 1], scalar2=None, op0=ALU.mult),
                     reads=[t_x, t_sc[d]], writes=[t_blk[d][b]])
                (ptw, t_ptw) = s_sqd[d][1]
                S.op("pe", lambda e: e.transpose(ptw, x[:, 128:256], c.ident_f[:]), reads=[t_x, TC], writes=[t_ptw])
                yield
                S.op("act", lambda e: e.activation(nwT[d][:, b, :], ptw, AF.Copy, scale=-1.0), reads=[t_ptw], writes=[t_blk[d][b]])
                yield

            if stop >= 2:
                _interleave((par_chain(b, d) for b in range(NB) for d in range(2)), 2)

            def scan_chain(d):
                s_ = sc[d]
                S.op("pool", lambda e: e.memset(S32[d][:], 0.0), writes=[t_S[d]])
                S.op("pool", lambda e: e.memset(Sbf[d][:], 0.0), writes=[t_S[d]])
                (pv, t_pv), (po1, t_po1), (po2, t_po2), (pS, t_pS) = s_scan[d]
                for step in range(NB):
                    b = step if d == 0 else NB - 1 - step
                    bs = slice(b * 128, (b + 1) * 128)
                    S.op("pe", lambda e, b=b: e.matmul(pv, nwT[d][:, b, :], Sbf[d][:], start=True, stop=True),
                         reads=[t_blk[d][b], t_S[d]], writes=[t_pv])
                    S.op("pe", lambda e, bs=bs: e.matmul(po1, qT[:, bs], Sbf[d][:], start=True, stop=True), reads=[t_in, t_S[d]], writes=[t_po1])
                    yield
                    vn, t_vn = vnr.next()
                    S.op("dve", lambda e, vn=vn, b=b: e.scalar_tensor_tensor(
                        out=vn[:], in0=pv, scalar=s_[:, 7, b:b + 1], in1=ub[d][:, b, :], op0=ALU.mult, op1=ALU.add),
                        reads=[t_pv, t_sc[d], t_blk[d][b]], writes=[t_vn])
                    yield
                    S.op("pe", lambda e, vn=vn, b=b: e.matmul(pS, kdec[d][:, b, :], vn[:], start=True, stop=True),
                         reads=[t_blk[d][b], t_vn], writes=[t_pS])
                    S.op("pe", lambda e, vn=vn, b=b: e.matmul(po2, qkT[d][:, b, :], vn[:], start=True, stop=True),
                         reads=[t_blk[d][b], t_vn], writes=[t_po2])
                    yield
                    S.op("dve", lambda e, b=b: e.scalar_tensor_tensor(
                        out=S32[d][:], in0=S32[d][:], scalar=s_[:, 5, b:b + 1], in1=pS, op0=ALU.mult, op1=ALU.add),
                        reads=[t_pS, t_sc[d], t_S[d]], writes=[t_S[d]])
                    S.op("pool", lambda e: e.tensor_copy(Sbf[d][:], S32[d][:]), reads=[t_S[d]], writes=[t_S[d]])
                    o2, t_o2 = o2r.next()
                    S.op("act", lambda e, o2=o2: e.copy(o2[:], po2), reads=[t_po2], writes=[t_o2])
                    first = (b < NB // 2) if d == 0 else (b >= NB // 2)
                    if not first:
                        S.op("pool", lambda e, o2=o2, b=b: e.tensor_tensor(out=o2[:], in0=o2[:], in1=oacc[:, b, :], op=ALU.add),
                             reads=[t_o2, t_oacc[b]], writes=[t_o2])
                    S.op("dve", lambda e, o2=o2, b=b: e.scalar_tensor_tensor(
                        out=oacc[:, b, :], in0=po1, scalar=s_[:, 3, b:b + 1], in1=o2[:], op0=ALU.mult, op1=ALU.add),
                        reads=[t_po1, t_sc[d], t_o2], writes=[t_oacc[b]])
                    yield

            if stop >= 3:
                _interleave((scan_chain(d) for d in range(2)), 2)

            S.op("dve", lambda e: e.tensor_tensor(out=big[:], in0=oacc[:], in1=oacc[:], op=ALU.mult), reads=t_oacc, writes=[t_big])
            S.op("dve", lambda e: e.tensor_reduce(out=red[:], in_=big[:], axis=mybir.AxisListType.X, op=ALU.add), reads=[t_big], writes=[t_big])
            S.op("act", lambda e: e.activation(red[:], red[:], AF.Sqrt, bias=EPS, scale=1.0 / 128), reads=[t_big], writes=[t_big])
            S.op("dve", lambda e: e.reciprocal(red[:], red[:]), reads=[t_big], writes=[t_big])
            S.op("dve", lambda e: e.tensor_tensor(out=big[:], in0=oacc[:], in1=red[:].unsqueeze(2).broadcast_to([128, NB, 128]), op=ALU.mult),
                 reads=t_oacc + [t_big], writes=[t_big])
            S.op("pool", lambda e: e.tensor_tensor(out=big[:], in0=big[:], in1=gw[:].unsqueeze(1).broadcast_to([128, NB, 128]), op=ALU.mult),
                 reads=[t_big, t_gw], writes=[t_big])
            S.op("dve", lambda e: e.tensor_tensor(out=ogtok[:], in0=big[:], in1=zs[:], op=ALU.mult), reads=[t_big, t_in], writes=[t_big])
            for i8 in range(4):
                for ii in range(8):
                    b = i8 * 8 + ii
                    (ptr, t_ptr) = s_tr[ii]
                    S.op("pe", lambda e, ptr=ptr, b=b: e.transpose(ptr, ogtok[:, b, :], c.ident_b[:]), reads=[t_big, TC], writes=[t_ptr])
                    S.op("act", lambda e, ptr=ptr, b=b: e.copy(stage[:, b * 128:(b + 1) * 128], ptr), reads=[t_ptr], writes=[t_stage])
            S.dma("sp", lambda e, h=h: e.dma_start(out=c.ogT_d[h], in_=stage[:]), reads=[t_stage], writes=[c.t_og])
    S.barrier()


def phase4(c):
    nc, S = c.nc, c.S
    TC = c.t_const
    with ExitStack() as st:
        sb = lambda name, shape, dt: st.enter_context(nc.sbuf_tensor(name, shape, dt))
        wpa = sb("p4wpa", [128, 8, D], BF16)
        wpg = sb("p4wpg", [128, 8, D], BF16)
        wout = sb("p4wout", [128, 8, D], BF16)
        wr = sb("p4wr", [128, 8, NE], BF16)
        t_w = Tok()
        for (dst, src) in ((wpa, c.w_proj_attn), (wpg, c.w_proj_gdn), (wout, c.w_out)):
            for kc in range(8):
                S.dma("pool", lambda e, dst=dst, src=src, kc=kc: e.dma_start(out=dst[:, kc, :], in_=src[kc * 128:(kc + 1) * 128, :]),
                      writes=[Tok()])
        S.dma("pool", lambda e: e.dma_start(out=wr[:], in_=c.w_router.rearrange("(kc p) n -> p kc n", p=128)), writes=[t_w])
        S.barrier()
        n2w = sb("p4n2w", [128, D], F32)
        t_n2w = Tok()
        S.dma("sp", lambda e: e.dma_start(out=n2w[:], in_=c.norm2_w.broadcast_to([128, D])), writes=[t_n2w])
        aff = sb("p4aff", [128, NT, NE], F32)
        t_aff = Tok()
        oar = _mk_ring(st, nc, "p4oa", 2, [128, 8, 512], BF16)
        ogr = _mk_ring(st, nc, "p4og", 2, [128, 8, 512], BF16)
        sgar = _mk_ring(st, nc, "p4sga", 2, [128, 8, 512], BF16)
        sggr = _mk_ring(st, nc, "p4sgg", 2, [128, 8, 512], BF16)
        mgr = _mk_ring(st, nc, "p4mg", 2, [128, 8, 512], BF16)
        f1 = _mk_ring(st, nc, "p4f1", 2, [128, 512], F32)
        f2 = _mk_ring(st, nc, "p4f2", 2, [128, 512], F32)
        xr = _mk_ring(st, nc, "p4x", 2, [128, D], F32)
        sqr = _mk_ring(st, nc, "p4sq", 2, [128, D], BF16)
        hr = _mk_ring(st, nc, "p4h", 2, [128, D], BF16)
        hTr = _mk_ring(st, nc, "p4hT", 2, [128, 8, 128], BF16)
        ssr = _mk_ring(st, nc, "p4ss", 4, [128, 4], F32)
        lgr = _mk_ring(st, nc, "p4lg", 2, [128, NE], F32)
        psm = _mk_ring(st, nc, "p4psm", 6, [128, 512], F32, psum=True)
        pst = _mk_ring(st, nc, "p4pst", 2, [128, 8, 128], BF16, psum=True)

        for g in range(NG):
            gs = slice(g * 512, (g + 1) * 512)
            oa, t_oa = oar.next()
            og, t_og = ogr.next()
            sga, t_sga = sgar.next()
            sgg, t_sgg = sggr.next()
            for (buf, tk, src, dep) in ((oa, t_oa, c.oaT_d, c.t_oa), (og, t_og, c.ogT_d, c.t_og),
                                        (sga, t_sga, c.sga_d, c.t_gates), (sgg, t_sgg, c.sgg_d, c.t_gates)):
                S.dma("sp", lambda e, buf=buf, src=src, gs=gs: e.dma_start(out=buf[:], in_=src[:, :, gs].rearrange("h p t -> p h t")),
                      reads=[dep], writes=[tk])
            mg, t_mg = mgr.next()
            for dc in range(8):
                ds = slice(dc * 128, (dc + 1) * 128)
                pa, t_pa = psm.next()
                pg, t_pg = psm.next()
                for ec in range(8):
                    S.op("pe", lambda e, pa=pa, ec=ec, ds=ds, oa=oa: e.matmul(pa[:], wpa[:, ec, ds], oa[:, ec, :], start=(ec == 0), stop=(ec == 7)),
                         reads=[t_oa], writes=[t_pa])
                for ec in range(8):
                    S.op("pe", lambda e, pg=pg, ec=ec, ds=ds, og=og: e.matmul(pg[:], wpg[:, ec, ds], og[:, ec, :], start=(ec == 0), stop=(ec == 7)),
                         reads=[t_og], writes=[t_pg])
                a, t_a = f1.next()
                b, t_b = f2.next()
                S.op("dve", lambda e, a=a, pa=pa, sga=sga, dc=dc: e.tensor_tensor(out=a[:], in0=pa[:], in1=sga[:, dc, :], op=ALU.mult),
                     reads=[t_pa, t_sga], writes=[t_a])
                S.op("dve", lambda e, b=b, pg=pg, sgg=sgg, dc=dc: e.tensor_tensor(out=b[:], in0=pg[:], in1=sgg[:, dc, :], op=ALU.mult),
                     reads=[t_pg, t_sgg], writes=[t_b])
                S.op("pool", lambda e, mg=mg, a=a, b=b, dc=dc: e.tensor_tensor(out=mg[:, dc, :], in0=a[:], in1=b[:], op=ALU.add),
                     reads=[t_a, t_b], writes=[t_mg])
            for tl in range(4):
                i = g * 4 + tl
                xt, t_x = xr.next()
                S.dma("sp", lambda e, xt=xt, i=i: e.dma_start(out=xt[:], in_=c.x[i * 128:(i + 1) * 128, :]), writes=[t_x])
                for dh in range(2):
                    po, t_po = psm.next()
                    for dc in range(8):
                        S.op("pe", lambda e, po=po, mg=mg, dc=dc, tl=tl, dh=dh: e.matmul(
                            po[:], mg[:, dc, tl * 128:(tl + 1) * 128], wout[:, dc, dh * 512:(dh + 1) * 512], start=(dc == 0), stop=(dc == 7)),
                            reads=[t_mg], writes=[t_po])
                    S.op("dve", lambda e, xt=xt, po=po, dh=dh: e.tensor_tensor(out=xt[:, dh * 512:(dh + 1) * 512], in0=po[:],
                                                                              in1=xt[:, dh * 512:(dh + 1) * 512], op=ALU.add),
                         reads=[t_po, t_x], writes=[t_x])
                S.dma("sp", lambda e, xt=xt, i=i: e.dma_start(out=c.x1_d[i * 128:(i + 1) * 128, :], in_=xt[:]), reads=[t_x], writes=[c.t_x1])
                sq, t_sq = sqr.next()
                ss, t_ss = ssr.next()
                S.op("act", lambda e, xt=xt, sq=sq, ss=ss: e.activation(sq[:], xt[:], AF.Square, accum_out=ss[:, 0:1]),
                     reads=[t_x], writes=[t_sq, t_ss])
                S.op("act", lambda e, ss=ss: e.activation(ss[:, 1:2], ss[:, 0:1], AF.Sqrt, bias=EPS, scale=1.0 / D), reads=[t_ss], writes=[t_ss])
                S.op("dve", lambda e, ss=ss: e.reciprocal(ss[:, 1:2], ss[:, 1:2]), reads=[t_ss], writes=[t_ss])
                ht, t_h = hr.next()
                S.op("dve", lambda e, ht=ht, xt=xt, ss=ss: e.scalar_tensor_tensor(
                    out=ht[:], in0=xt[:], scalar=ss[:, 1:2], in1=n2w[:], op0=ALU.mult, op1=ALU.mult),
                    reads=[t_x, t_ss, t_n2w], writes=[t_h])
                S.dma("sp", lambda e, ht=ht, i=i: e.dma_start(out=c.h2tok_d[i * 128:(i + 1) * 128, :], in_=ht[:]), reads=[t_h], writes=[c.t_h2])
                pt, t_pt = pst.next()
                for dc in range(8):
                    S.op("pe", lambda e, pt=pt, ht=ht, dc=dc: e.transpose(pt[:, dc, :], ht[:, dc * 128:(dc + 1) * 128], c.ident_b[:]),
                         reads=[t_h, TC], writes=[t_pt])
                hT, t_hT = hTr.next()
                S.op("act", lambda e, hT=hT, pt=pt: e.copy(hT[:], pt[:]), reads=[t_pt], writes=[t_hT])
                pl, t_pl = psm.next()
                for dc in range(8):
                    S.op("pe", lambda e, pl=pl, hT=hT, dc=dc: e.matmul(pl[:, 0:NE], hT[:, dc, :], wr[:, dc, :], start=(dc == 0), stop=(dc == 7)),
                         reads=[t_hT, t_w], writes=[t_pl])
                lg, t_lg = lgr.next()
                S.op("dve", lambda e, pl=pl, ss=ss: e.tensor_reduce(out=ss[:, 2:3], in_=pl[:, 0:NE], axis=mybir.AxisListType.X, op=ALU.max),
                     reads=[t_pl, t_ss], writes=[t_ss])
                S.op("dve", lambda e, ss=ss: e.tensor_scalar(out=ss[:, 2:3], in0=ss[:, 2:3], scalar1=-1.0, scalar2=None, op0=ALU.mult),
                     reads=[t_ss], writes=[t_ss])
                S.op("act", lambda e, lg=lg, pl=pl, ss=ss: e.activation(lg[:], pl[:, 0:NE], AF.Exp, bias=ss[:, 2:3], accum_out=ss[:, 3:4]),
                     reads=[t_pl, t_ss], writes=[t_lg, t_ss])
                S.op("dve", lambda e, ss=ss: e.reciprocal(ss[:, 3:4], ss[:, 3:4]), reads=[t_ss], writes=[t_ss])
                S.op("dve", lambda e, lg=lg, ss=ss, i=i: e.tensor_scalar(out=aff[:, i, :], in0=lg[:], scalar1=ss[:, 3:4], scalar2=None, op0=ALU.mult),
                     reads=[t_lg, t_ss], writes=[t_aff])
        S.dma("sp", lambda e: e.dma_start(out=c.aff_d, in_=aff[:]), reads=[t_aff], writes=[c.t_aff])
    S.barrier()


def phase5(c):
    nc, S = c.nc, c.S
    TC = c.t_const
    with ExitStack() as st:
        sb = lambda name, shape, dt: st.enter_context(nc.sbuf_tensor(name, shape, dt))
        aff = sb("p5aff", [128, NT, NE], F32)
        t_aff = Tok()
        S.dma("sp", lambda e: e.dma_start(out=aff[:], in_=c.aff_d), reads=[c.t_aff], writes=[t_aff])
        affT = sb("p5affT", [NE, S_LEN], F32)
        work = sb("p5work", [NE, S_LEN], F32)
        ones = sb("p5ones", [NE, S_LEN], F32)
        mx = sb("p5mx", [NE, 8], F32)
        t_affT, t_work, t_mx, t_ones = Tok(), Tok(), Tok(), Tok()
        psm = _mk_ring(st, nc, "p5psm", 4, [128, 512], F32, psum=True)
        for i4 in range(NT // 4):
            ps, t_ps = psm.next()
            for ii in range(4):
                i = i4 * 4 + ii
                S.op("pe", lambda e, ps=ps, ii=ii, i=i: e.transpose(ps[0:NE, ii * 128:(ii + 1) * 128], aff[:, i, :], c.ident_f[:]),
                     reads=[t_aff, TC], writes=[t_ps])
            S.op("act", lambda e, ps=ps, i4=i4: e.copy(affT[:, i4 * 512:(i4 + 1) * 512], ps[0:NE, :]), reads=[t_ps], writes=[t_affT])
        S.op("pool", lambda e: e.memset(ones[:], 1.0), writes=[t_ones])
        src = affT
        for it in range(CAP // 8):
            S.op("dve", lambda e, src=src: e.max(out=mx[:], in_=src[:]), reads=[t_affT, t_work], writes=[t_mx])
            if it < CAP // 8 - 1:
                S.op("dve", lambda e, src=src: e.match_replace(out=work[:], in_to_replace=mx[:], in_values=src[:], imm_value=-1.0),
                     reads=[t_mx, t_affT, t_work], writes=[t_work])
            src = work
        S.op("dve", lambda e: e.tensor_scalar(out=work[:], in0=affT[:], scalar1=mx[:, 7:8], scalar2=None, op0=ALU.is_ge),
             reads=[t_affT, t_mx, t_work], writes=[t_work])
        S.op("dve", lambda e: e.tensor_tensor_scan(out=affT[:], data0=ones[:], data1=work[:], initial=0.0, op0=ALU.mult, op1=ALU.add),
             reads=[t_work, t_ones, t_affT], writes=[t_affT])
        S.op("dve", lambda e: e.tensor_tensor(out=affT[:], in0=affT[:], in1=work[:], op=ALU.mult), reads=[t_work, t_affT], writes=[t_affT])
        S.op("dve", lambda e: e.tensor_scalar(out=affT[:], in0=affT[:], scalar1=-1.0, scalar2=None, op0=ALU.add), reads=[t_affT], writes=[t_affT])
        S.dma("sp", lambda e: e.dma_start(out=c.pos_d, in_=affT[:]), reads=[t_affT], writes=[c.t_pos])
        ptok = sb("p5ptok", [128, NT, NE], F32)
        t_ptok = Tok()
        for i4 in range(NT // 4):
            ps, t_ps = psm.next()
            for ii in range(4):
                i = i4 * 4 + ii
                S.op("pe", lambda e, ps=ps, ii=ii, i=i: e.transpose(ps[:, ii * NE:(ii + 1) * NE], affT[:, i * 128:(i + 1) * 128], c.ident_f[0:NE, 0:NE]),
                     reads=[t_affT, TC], writes=[t_ps])
            S.op("act", lambda e, ps=ps, i4=i4: e.copy(ptok[:, i4 * 4:(i4 + 1) * 4, :], ps[:, 0:4 * NE].rearrange("p (a b) -> p a b", b=NE)),
                 reads=[t_ps], writes=[t_ptok])
        S.dma("sp", lambda e: e.dma_start(out=c.ptok_d, in_=ptok[:]), reads=[t_ptok], writes=[c.t_pos])
    S.barrier()


def phase6a(c):
    nc, S = c.nc, c.S
    TC = c.t_const
    with ExitStack() as st:
        sb = lambda name, shape, dt: st.enter_context(nc.sbuf_tensor(name, shape, dt))
        ptok = sb("p6ptok", [128, NT, NE], F32)
        t_ptok = Tok()
        S.dma("sp", lambda e: e.dma_start(out=ptok[:], in_=c.ptok_d), reads=[c.t_pos], writes=[t_ptok])
        iota = sb("p6iota", [128, CAP], F32)
        t_iota = Tok()
        S.op("pool", lambda e: e.iota(iota[:], pattern=[[1, CAP]], base=0, channel_multiplier=0, allow_small_or_imprecise_dtypes=True),
             writes=[t_iota])
        sel = sb("p6sel", [128, NT, CAP], BF16)
        t_sel = Tok()
        xsT = sb("p6xsT", [128, 8, CAP], BF16)
        t_xsT = Tok()
        actT = sb("p6actT", [128, 16, CAP], BF16)
        t_actT = Tok()
        yes = sb("p6ye", [128, 4, D], BF16)
        t_yes = Tok()
        h2r = _mk_ring(st, nc, "p6h2", 4, [128, D], BF16)
        wgr = _mk_ring(st, nc, "p6wg", 2, [128, 8, 1024], BF16)
        wur = _mk_ring(st, nc, "p6wu", 2, [128, 8, 1024], BF16)
        wdr = _mk_ring(st, nc, "p6wd", 2, [128, 8, D], BF16)
        gfr = _mk_ring(st, nc, "p6gf", 2, [128, CAP], F32)
        wst = _mk_ring(st, nc, "p6wst", 6, [128, 1024], F32)
        psm = _mk_ring(st, nc, "p6psm", 8, [128, 512], F32, psum=True)

        for ex in range(NE):
            for i in range(NT):
                eng = "dve" if i % 2 == 0 else "pool"
                S.op(eng, lambda e, i=i, ex=ex: e.tensor_scalar(out=sel[:, i, :], in0=iota[:], scalar1=ptok[:, i, ex:ex + 1], scalar2=None,
                                                                 op0=ALU.is_equal), reads=[t_iota, t_ptok], writes=[t_sel])
            for half in range(2):
                accs = [psm.next() for _ in range(4)]
                for i in range(NT):
                    ht, t_h = h2r.next()
                    S.dma("pool", lambda e, ht=ht, i=i: e.dma_start(out=ht[:], in_=c.h2tok_d[i * 128:(i + 1) * 128, :]), reads=[c.t_h2], writes=[t_h])
                    for dl in range(4):
                        dc = half * 4 + dl
                        ps, t_ps = accs[dl]
                        S.op("pe", lambda e, ps=ps, ht=ht, dc=dc, i=i: e.matmul(ps[:], ht[:, dc * 128:(dc + 1) * 128], sel[:, i, :],
                                                                               start=(i == 0), stop=(i == NT - 1)),
                             reads=[t_h, t_sel], writes=[t_ps])
                for dl in range(4):
                    dc = half * 4 + dl
                    ps, t_ps = accs[dl]
                    S.op("act", lambda e, ps=ps, dc=dc: e.copy(xsT[:, dc, :], ps[:]), reads=[t_ps], writes=[t_xsT])
            for fh in range(2):
                wg, t_wg = wgr.next()
                wu, t_wu = wur.next()
                for kc in range(8):
                    for (wdst, t_wdst, wsrc) in ((wg, t_wg, c.w_gate), (wu, t_wu, c.w_up)):
                        stg, t_stg = wst.next()
                        S.dma("sp", lambda e, stg=stg, wsrc=wsrc, kc=kc, ex=ex, fh=fh: e.dma_start(
                            out=stg[:], in_=wsrc[ex, kc * 128:(kc + 1) * 128, fh * 1024:(fh + 1) * 1024]), writes=[t_stg])
                        S.op("act", lambda e, stg=stg, wdst=wdst, kc=kc: e.copy(wdst[:, kc, :], stg[:]), reads=[t_stg], writes=[t_wdst])
                for fl in range(8):
                    fc = fh * 8 + fl
                    fs = slice(fl * 128, (fl + 1) * 128)
                    pg, t_pg = psm.next()
                    pu, t_pu = psm.next()
                    for kc in range(8):
                        S.op("pe", lambda e, pg=pg, wg=wg, kc=kc, fs=fs: e.matmul(pg[:], wg[:, kc, fs], xsT[:, kc, :], start=(kc == 0), stop=(kc == 7)),
                             reads=[t_wg, t_xsT], writes=[t_pg])
                    for kc in range(8):
                        S.op("pe", lambda e, pu=pu, wu=wu, kc=kc, fs=fs: e.matmul(pu[:], wu[:, kc, fs], xsT[:, kc, :], start=(kc == 0), stop=(kc == 7)),
                             reads=[t_wu, t_xsT], writes=[t_pu])
                    gf, t_gf = gfr.next()
                    S.op("act", lambda e, gf=gf, pg=pg: e.activation(gf[:], pg[:], AF.Silu), reads=[t_pg], writes=[t_gf])
                    S.op("dve", lambda e, gf=gf, pu=pu, fc=fc: e.tensor_tensor(out=actT[:, fc, :], in0=pu[:], in1=gf[:], op=ALU.mult),
                         reads=[t_pu, t_gf], writes=[t_actT])
            accs = [psm.next() for _ in range(8)]
            for fh in range(2):
                wd, t_wd = wdr.next()
                for fl in range(8):
                    fc = fh * 8 + fl
                    stg, t_stg = wst.next()
                    S.dma("sp", lambda e, stg=stg, fc=fc, ex=ex: e.dma_start(out=stg[:], in_=c.w_down[ex, fc * 128:(fc + 1) * 128, :]),
                          writes=[t_stg])
                    S.op("act", lambda e, stg=stg, wd=wd, fl=fl: e.copy(wd[:, fl, :], stg[:]), reads=[t_stg], writes=[t_wd])
                for fl in range(8):
                    fc = fh * 8 + fl
                    for sc in range(4):
                        for dh in range(2):
                            ps, t_ps = accs[sc * 2 + dh]
                            S.op("pe", lambda e, ps=ps, fc=fc, sc=sc, wd=wd, fl=fl, dh=dh: e.matmul(
                                ps[:], actT[:, fc, sc * 128:(sc + 1) * 128], wd[:, fl, dh * 512:(dh + 1) * 512], start=(fc == 0), stop=(fc == 15)),
                                reads=[t_actT, t_wd], writes=[t_ps])
            for sc in range(4):
                for dh in range(2):
                    ps, t_ps = accs[sc * 2 + dh]
                    eng = "act" if dh == 0 else "dve"
                    if eng == "act":
                        S.op("act", lambda e, ps=ps, sc=sc, dh=dh: e.copy(yes[:, sc, dh * 512:(dh + 1) * 512], ps[:]), reads=[t_ps], writes=[t_yes])
                    else:
                        S.op("dve", lambda e, ps=ps, sc=sc, dh=dh: e.tensor_copy(yes[:, sc, dh * 512:(dh + 1) * 512], ps[:]), reads=[t_ps], writes=[t_yes])
            S.dma("sp", lambda e, ex=ex: e.dma_start(out=c.ye_d[ex].rearrange("(sc p) d -> p sc d", p=128), in_=yes[:]),
                  reads=[t_yes], writes=[c.t_ye])
    S.barrier()


def phase6b(c):
    nc, S = c.nc, c.S
    TC = c.t_const
    with ExitStack() as st:
        sb = lambda name, shape, dt: st.enter_context(nc.sbuf_tensor(name, shape, dt))
        yall = sb("p7ye", [128, NE, 4, D], BF16)
        t_yall = Tok()
        for ex in range(NE):
            S.dma("sp", lambda e, ex=ex: e.dma_start(out=yall[:, ex, :, :], in_=c.ye_d[ex].rearrange("(sc p) d -> p sc d", p=128)),
                  reads=[c.t_ye], writes=[Tok()])
        S.barrier()
        aff = sb("p7aff", [128, NT, NE], F32)
        t_aff = Tok()
        S.dma("sp", lambda e: e.dma_start(out=aff[:], in_=c.aff_d), reads=[c.t_aff], writes=[t_aff])
        nfw = sb("p7nfw", [128, D], F32)
        t_nfw = Tok()
        S.dma("sp", lambda e: e.dma_start(out=nfw[:], in_=c.norm_f_w.broadcast_to([128, D])), writes=[t_nfw])
        pidx = sb("p7pidx", [128, 4], F32)
        t_pidx = Tok()
        S.op("pool", lambda e: e.iota(pidx[:], pattern=[[128, 4]], base=0, channel_multiplier=1, allow_small_or_imprecise_dtypes=True),
             writes=[t_pidx])
        posbc = sb("p7posbc", [128, NE, 512], F32)
        t_posbc = Tok()
        selT = _mk_ring(st, nc, "p7selT", 8, [128, 512], BF16)
        accr = _mk_ring(st, nc, "p7acc", 4, [128, D], F32)
        sqr = _mk_ring(st, nc, "p7sq", 2, [128, D], BF16)
        ssr = _mk_ring(st, nc, "p7ss", 4, [128, 2], F32)
        psm = _mk_ring(st, nc, "p7psm", 6, [128, 512], F32, psum=True)
        for g in range(NG):
            gs = slice(g * 512, (g + 1) * 512)
            S.dma("sp", lambda e, gs=gs: e.dma_start(out=posbc[:], in_=c.pos_d[:, gs].unsqueeze(0).broadcast_to([128, NE, 512])),
                  reads=[c.t_pos], writes=[t_posbc])
            accs = []
            for tl in range(4):
                i = g * 4 + tl
                acc, t_acc = accr.next()
                S.dma("sp", lambda e, acc=acc, i=i: e.dma_start(out=acc[:], in_=c.x1_d[i * 128:(i + 1) * 128, :]), reads=[c.t_x1], writes=[t_acc])
                accs.append((acc, t_acc))
            def build_sts(ex):
                sts = []
                for sc in range(4):
                    sT, t_sT = selT.next()
                    eng = "dve" if sc % 2 == 0 else "pool"
                    S.op(eng, lambda e, sT=sT, ex=ex, sc=sc: e.tensor_scalar(out=sT[:], in0=posbc[:, ex, :], scalar1=pidx[:, sc:sc + 1], scalar2=None,
                                                                            op0=ALU.is_equal), reads=[t_posbc, t_pidx], writes=[t_sT])
                    sts.append((sT, t_sT))
                return sts
            nxt_sts = build_sts(0)
            for ex in range(NE):
                sts = nxt_sts
                if ex + 1 < NE:
                    nxt_sts = build_sts(ex + 1)
                for tl in range(4):
                    i = g * 4 + tl
                    acc, t_acc = accs[tl]
                    for dh in range(2):
                        ps, t_ps = psm.next()
                        for sc in range(4):
                            sT, t_sT = sts[sc]
                            S.op("pe", lambda e, ps=ps, sT=sT, tl=tl, ex=ex, sc=sc, dh=dh: e.matmul(
                                ps[:], sT[:, tl * 128:(tl + 1) * 128], yall[:, ex, sc, dh * 512:(dh + 1) * 512], start=(sc == 0), stop=(sc == 3)),
                                reads=[t_sT], writes=[t_ps])
                        S.op("dve", lambda e, acc=acc, ps=ps, i=i, ex=ex, dh=dh: e.scalar_tensor_tensor(
                            out=acc[:, dh * 512:(dh + 1) * 512], in0=ps[:], scalar=aff[:, i, ex:ex + 1], in1=acc[:, dh * 512:(dh + 1) * 512],
                            op0=ALU.mult, op1=ALU.add), reads=[t_ps, t_aff, t_acc], writes=[t_acc])
            for tl in range(4):
                i = g * 4 + tl
                acc, t_acc = accs[tl]
                sq, t_sq = sqr.next()
                ss, t_ss = ssr.next()
                S.op("act", lambda e, acc=acc, sq=sq, ss=ss: e.activation(sq[:], acc[:], AF.Square, accum_out=ss[:, 0:1]),
                     reads=[t_acc], writes=[t_sq, t_ss])
                S.op("act", lambda e, ss=ss: e.activation(ss[:, 1:2], ss[:, 0:1], AF.Sqrt, bias=EPS, scale=1.0 / D), reads=[t_ss], writes=[t_ss])
                S.op("dve", lambda e, ss=ss: e.reciprocal(ss[:, 1:2], ss[:, 1:2]), reads=[t_ss], writes=[t_ss])
                S.op("dve", lambda e, acc=acc, ss=ss: e.scalar_tensor_tensor(
                    out=acc[:], in0=acc[:], scalar=ss[:, 1:2], in1=nfw[:], op0=ALU.mult, op1=ALU.mult),
                    reads=[t_acc, t_ss, t_nfw], writes=[t_acc])
                S.dma("sp", lambda e, acc=acc, i=i: e.dma_start(out=c.out[i * 128:(i + 1) * 128, :], in_=acc[:]), reads=[t_acc], writes=[c.t_out])
    S.barrier()


ALL_PHASES = ("p1", "p2", "p3", "p4", "p5", "p6a", "p6b")


def kernel(**inputs):
    n_cores = 8
    shared = host_prep(inputs)
    nc, c = build_program(debug=False, phases=ALL_PHASES)
    x = np.asarray(inputs["x"], dtype=np.float32)
    in_maps = []
    for b in range(n_cores):
        m = dict(shared)
        m["x"] = np.ascontiguousarray(x[b])
        in_maps.append({k: m[k] for k in c.input_names})
    res = run_bass_kernel_spmd(nc, in_maps, core_ids=list(range(n_cores)))
    out = np.stack([np.asarray(r["out"], dtype=np.float32) for r in res.results], axis=0)
    return out
```

```python
import math
from contextlib import ExitStack
import numpy as np
import concourse.bass as bass
import concourse.mybir as mybir
from concourse.bass_utils import run_bass_kernel_spmd

F32 = mybir.dt.float32
BF16 = mybir.dt.bfloat16
AF = mybir.ActivationFunctionType
ALU = mybir.AluOpType

S_LEN = 4096
D = 1024
NT = S_LEN // 128
NG = S_LEN // 512
H = 8
IN_W = 9248
C_QA, C_KA, C_VA, C_GQ, C_GK, C_GV, C_Z, C_SM, C_GA, C_GG = 0, 1024, 2048, 3072, 4096, 5120, 6144, 7168, 7200, 8224
NE = 16
FF = 2048
CAP = 512
EPS = 1e-6
LAMBDA_INIT = 0.8 - 0.6 * math.exp(-0.3 * 0)

ENGS = ("pe", "act", "dve", "pool", "sp")


class Tok:
    __slots__ = ("w", "r", "excl")

    def __init__(self, excl=False):
        self.w = None
        self.r = {}
        self.excl = excl


class Sched:
    NDMA = 32

    def __init__(self, nc):
        self.nc = nc
        self.sems = {}
        for e in ENGS:
            self.sems[e] = nc.alloc_semaphore(name="sem_" + e)
        for i in range(self.NDMA):
            self.sems[("d", i)] = nc.alloc_semaphore(name="sem_dma%d" % i)
        self.cnt = {k: 0 for k in self.sems}
        self.seen = {e: {} for e in ENGS}
        self.prog = {e: [] for e in ENGS}
        self.ndma = 0
        self.ninstr = 0

    def _deps(self, reads, writes):
        deps = {}
        for t in reads:
            if t.w is not None:
                k, v = t.w
                if deps.get(k, 0) < v:
                    deps[k] = v
        for t in writes:
            if t.w is not None:
                k, v = t.w
                if deps.get(k, 0) < v:
                    deps[k] = v
            for k, v in t.r.items():
                if deps.get(k, 0) < v:
                    deps[k] = v
        return deps

    def _emit_waits(self, e, deps):
        seen = self.seen[e]
        for k, v in deps.items():
            if seen.get(k, 0) >= v:
                continue
            seen[k] = v
            sem = self.sems[k]
            self.prog[e].append(lambda eng, sem=sem, v=v: eng.wait_ge(sem, v))

    def op(self, e, fn, reads=(), writes=()):
        if any(t.excl for t in reads):
            writes = list(writes) + [t for t in reads if t.excl]
            reads = [t for t in reads if not t.excl]
        deps = self._deps(reads, writes)
        if e == "pe":
            deps.pop("pe", None)
        self._emit_waits(e, deps)
        sem = self.sems[e]
        self.cnt[e] += 1
        n = self.cnt[e]
        self.prog[e].append(lambda eng, fn=fn, sem=sem: fn(eng).then_inc(sem, 1))
        for t in reads:
            if t.r.get(e, 0) < n:
                t.r[e] = n
        for t in writes:
            t.w = (e, n)
            t.r = {}
        self.ninstr += 1

    def dma(self, e, fn, reads=(), writes=()):
        i = self.ndma % self.NDMA
        self.ndma += 1
        k = ("d", i)
        deps = self._deps(reads, writes)
        if self.cnt[k] > 0:
            deps[k] = max(deps.get(k, 0), self.cnt[k])
        self._emit_waits(e, deps)
        self.cnt[k] += 16
        v = self.cnt[k]
        sem = self.sems[k]
        self.prog[e].append(lambda eng, fn=fn, sem=sem: fn(eng).then_inc(sem, 16))
        for t in reads:
            if t.r.get(k, 0) < v:
                t.r[k] = v
        for t in writes:
            t.w = (k, v)
            t.r = {}
        self.ninstr += 1

    def barrier(self):
        deps = {k: v for k, v in self.cnt.items() if v > 0}
        for e in ENGS:
            self._emit_waits(e, dict(deps))

    def emit(self):
        nc = self.nc
        prog = self.prog
        with nc.Block() as block:
            @block.tensor
            def _(eng):
                for f in prog["pe"]:
                    f(eng)

            @block.scalar
            def _(eng):
                for f in prog["act"]:
                    f(eng)

            @block.vector
            def _(eng):
                for f in prog["dve"]:
                    f(eng)

            @block.gpsimd
            def _(eng):
                for f in prog["pool"]:
                    f(eng)

            @block.sync
            def _(eng):
                for f in prog["sp"]:
                    f(eng)


class Ring:
    def __init__(self, items):
        self.items = items
        self.i = 0

    def next(self):
        it = self.items[self.i % len(self.items)]
        self.i += 1
        return it


class Ctx:
    pass


def _interleave(gens, width):
    it = iter(gens)
    active = []
    exhausted = False
    while True:
        while len(active) < width and not exhausted:
            try:
                active.append(next(it))
            except StopIteration:
                exhausted = True
        if not active:
            break
        for g in list(active):
            try:
                next(g)
            except StopIteration:
                active.remove(g)


def _mk_ring(stack, nc, name, n, shape, dt, psum=False):
    items = []
    for i in range(n):
        if psum:
            t = stack.enter_context(nc.psum_tensor("%s%d" % (name, i), shape, dt))
        else:
            t = stack.enter_context(nc.sbuf_tensor("%s%d" % (name, i), shape, dt))
        items.append((t, Tok()))
    return Ring(items)


def _consts(c, stack):
    nc, S = c.nc, c.S
    c.ident_f = nc.alloc_sbuf_tensor("ident_f", [128, 128], F32)
    c.ident_b = nc.alloc_sbuf_tensor("ident_b", [128, 128], BF16)
    c.ones_b = nc.alloc_sbuf_tensor("ones_b", [128, 128], BF16)
    c.ones_f = nc.alloc_sbuf_tensor("ones_f", [128, 128], F32)
    c.t_const = Tok()
    tc = c.t_const
    S.op("pool", lambda e: e.memset(c.ident_f[:], 0.0), writes=[tc])
    S.op("pool", lambda e: e.affine_select(out=c.ident_f[:], in_=c.ident_f[:], compare_op=ALU.not_equal, fill=1.0,
                                           base=0, pattern=[[-1, 128]], channel_multiplier=1),
         reads=[tc], writes=[tc])
    S.op("pool", lambda e: e.tensor_copy(c.ident_b[:], c.ident_f[:]), reads=[tc], writes=[tc])
    S.op("pool", lambda e: e.memset(c.ones_b[:], 1.0), writes=[tc])
    S.op("pool", lambda e: e.memset(c.ones_f[:], 1.0), writes=[tc])


def phase1(c):
    nc, S = c.nc, c.S
    TC = c.t_const
    with ExitStack() as st:
        sb = lambda name, shape, dt: st.enter_context(nc.sbuf_tensor(name, shape, dt))
        hT = sb("hT", [128, 8, S_LEN], BF16)
        t_hT = [Tok() for _ in range(NT)]
        stA = ExitStack()
        sbA = lambda name, shape, dt: stA.enter_context(nc.sbuf_tensor(name, shape, dt))
        n1w = sbA("n1w", [128, D], F32)
        t_n1w = Tok()
        S.dma("sp", lambda e: e.dma_start(out=n1w[:], in_=c.norm1_w.broadcast_to([128, D])), writes=[t_n1w])
        xring = _mk_ring(stA, nc, "p1x", 3, [128, D], F32)
        sqring = _mk_ring(stA, nc, "p1sq", 2, [128, D], BF16)
        hring = _mk_ring(stA, nc, "p1h", 2, [128, D], BF16)
        ssring = _mk_ring(stA, nc, "p1ss", 4, [128, 2], F32)
        pst = _mk_ring(st, nc, "p1pst", 2, [128, 8, 128], BF16, psum=True)
        psm = _mk_ring(st, nc, "p1psm", 5, [128, 512], F32, psum=True)

        for i in range(NT):
            xt, t_x = xring.next()
            S.dma("sp", lambda e, xt=xt, i=i: e.dma_start(out=xt[:], in_=c.x[i * 128:(i + 1) * 128, :]), writes=[t_x])
            sq, t_sq = sqring.next()
            ss, t_ss = ssring.next()
            S.op("act", lambda e, xt=xt, sq=sq, ss=ss: e.activation(sq[:], xt[:], AF.Square, accum_out=ss[:, 0:1]),
                 reads=[t_x], writes=[t_sq, t_ss])
            S.op("act", lambda e, ss=ss: e.activation(ss[:, 1:2], ss[:, 0:1], AF.Sqrt, bias=EPS, scale=1.0 / D),
                 reads=[t_ss], writes=[t_ss])
            S.op("dve", lambda e, ss=ss: e.reciprocal(ss[:, 1:2], ss[:, 1:2]), reads=[t_ss], writes=[t_ss])
            ht, t_h = hring.next()
            S.op("dve", lambda e, ht=ht, xt=xt, ss=ss: e.scalar_tensor_tensor(
                out=ht[:], in0=xt[:], scalar=ss[:, 1:2], in1=n1w[:], op0=ALU.mult, op1=ALU.mult),
                reads=[t_x, t_ss, t_n1w], writes=[t_h])
            pt, t_pt = pst.next()
            for dc in range(8):
                S.op("pe", lambda e, pt=pt, ht=ht, dc=dc: e.transpose(pt[:, dc, :], ht[:, dc * 128:(dc + 1) * 128], c.ident_b[:]),
                     reads=[t_h, TC], writes=[t_pt])
            S.op("act", lambda e, pt=pt, i=i: e.copy(hT[:, :, i * 128:(i + 1) * 128], pt[:]),
                 reads=[t_pt], writes=[t_hT[i]])

        S.barrier()
        stA.close()
        wfm = _mk_ring(st, nc, "p1wfm", 4, [128, 8, 128], BF16)
        wtm = _mk_ring(st, nc, "p1wtm", 2, [128, 8, 512], BF16)

        def load_w(ring, src, col0, ncols):
            wt, t_w = ring.next()
            S.dma("pool", lambda e: e.dma_start(
                out=wt[:, :, 0:ncols], in_=src[:, col0:col0 + ncols].rearrange("(kc p) n -> p kc n", p=128)),
                writes=[t_w])
            return wt, t_w

        def proj_fm(wt, t_w, g):
            ps, t_ps = psm.next()
            for kc in range(8):
                S.op("pe", lambda e, ps=ps, kc=kc: e.matmul(ps[:], wt[:, kc, :], hT[:, kc, g * 512:(g + 1) * 512],
                                                          start=(kc == 0), stop=(kc == 7)),
                     reads=[t_w] + t_hT[g * 4:(g + 1) * 4], writes=[t_ps])
            return ps, t_ps

        def proj_tm(wt, t_w, i, ncols):
            ps, t_ps = psm.next()
            for kc in range(8):
                S.op("pe", lambda e, ps=ps, kc=kc: e.matmul(ps[:, 0:ncols], hT[:, kc, i * 128:(i + 1) * 128], wt[:, kc, 0:ncols],
                                                          start=(kc == 0), stop=(kc == 7)),
                     reads=[t_w, t_hT[i]], writes=[t_ps])
            return ps, t_ps

        stage = _mk_ring(st, nc, "p1stage", 2, [128, S_LEN], BF16)
        f32a = _mk_ring(st, nc, "p1f32a", 3, [128, 512], F32)
        f32b = _mk_ring(st, nc, "p1f32b", 3, [128, 512], F32)

        stB = ExitStack()
        cosT = stB.enter_context(nc.sbuf_tensor("cosT_sb", [128, S_LEN], F32))
        sinT = stB.enter_context(nc.sbuf_tensor("sinT_sb", [128, S_LEN], F32))
        t_rope = Tok()
        S.dma("sp", lambda e: e.dma_start(out=cosT[:], in_=c.cosT), writes=[t_rope])
        S.dma("sp", lambda e: e.dma_start(out=sinT[:], in_=c.sinT), writes=[t_rope])

        for (col0, dst) in ((C_QA, c.qT_d), (C_KA, c.kT_d)):
            for h in range(H):
                w1, t_w1 = load_w(wfm, c.w_in, col0 + h * 128, 128)
                w2, t_w2 = load_w(wfm, c.w_qkp, (col0 // 1024) * 1024 + h * 128, 128)
                stg, t_stg = stage.next()
                for g in range(NG):
                    ps1, t_ps1 = proj_fm(w1, t_w1, g)
                    ps2, t_ps2 = proj_fm(w2, t_w2, g)
                    a, t_a = f32a.next()
                    b, t_b = f32b.next()
                    sl = slice(g * 512, (g + 1) * 512)
                    S.op("dve", lambda e, a=a, ps1=ps1, sl=sl: e.tensor_tensor(out=a[:], in0=ps1[:], in1=cosT[:, sl], op=ALU.mult),
                         reads=[t_ps1, t_rope], writes=[t_a])
                    S.op("dve", lambda e, b=b, ps2=ps2, sl=sl: e.tensor_tensor(out=b[:], in0=ps2[:], in1=sinT[:, sl], op=ALU.mult),
                         reads=[t_ps2, t_rope], writes=[t_b])
                    S.op("pool", lambda e, a=a, b=b, stg=stg, sl=sl: e.tensor_tensor(out=stg[:, sl], in0=a[:], in1=b[:], op=ALU.add),
                         reads=[t_a, t_b], writes=[t_stg])
                S.dma("sp", lambda e, stg=stg, dst=dst, h=h: e.dma_start(out=dst[h], in_=stg[:]), reads=[t_stg], writes=[c.t_qk])

        S.barrier()
        stB.close()
        for (col0, dst) in ((C_GA, c.sga_d), (C_GG, c.sgg_d)):
            for h in range(8):
                w1, t_w1 = load_w(wfm, c.w_in, col0 + h * 128, 128)
                stg, t_stg = stage.next()
                for g in range(NG):
                    ps1, t_ps1 = proj_fm(w1, t_w1, g)
                    sl = slice(g * 512, (g + 1) * 512)
                    S.op("act", lambda e, ps1=ps1, stg=stg, sl=sl: e.activation(stg[:, sl], ps1[:], AF.Sigmoid),
                         reads=[t_ps1], writes=[t_stg])
                S.dma("sp", lambda e, stg=stg, dst=dst, h=h: e.dma_start(out=dst[h], in_=stg[:]), reads=[t_stg], writes=[c.t_gates])

        tmst = _mk_ring(st, nc, "p1tmst", 3, [128, 512], BF16)
        for (col0, dst, fn) in ((C_VA, c.v_d, None), (C_Z, c.zs_d, AF.Silu)):
            for half in range(2):
                wt, t_w = load_w(wtm, c.w_in, col0 + half * 512, 512)
                for i in range(NT):
                    ps, t_ps = proj_tm(wt, t_w, i, 512)
                    o, t_o = tmst.next()
                    if fn is None:
                        S.op("act", lambda e, o=o, ps=ps: e.copy(o[:], ps[:]), reads=[t_ps], writes=[t_o])
                    else:
                        S.op("act", lambda e, o=o, ps=ps, fn=fn: e.activation(o[:], ps[:], fn), reads=[t_ps], writes=[t_o])
                    S.dma("sp", lambda e, o=o, dst=dst, i=i, half=half: e.dma_start(
                        out=dst[i * 128:(i + 1) * 128, half * 512:(half + 1) * 512], in_=o[:]), reads=[t_o], writes=[c.t_vz])

        sm = sb("p1sm", [128, NT, 32], F32)
        t_sm = Tok()
        wt, t_w = load_w(wtm, c.w_in, C_SM, 32)
        for i in range(NT):
            ps, t_ps = proj_tm(wt, t_w, i, 32)
            S.op("dve", lambda e, ps=ps, i=i: e.tensor_copy(sm[:, i, :], ps[:, 0:32]), reads=[t_ps], writes=[t_sm])
        prm = sb("p1prm", [128, 32], F32)
        t_prm = Tok()
        for j, src in enumerate((c.dt_bias_fwd, c.dt_bias_bwd, c.a_log_fwd, c.a_log_bwd)):
            S.dma("sp", lambda e, j=j, src=src: e.dma_start(out=prm[:, j * 8:(j + 1) * 8], in_=src.broadcast_to([128, 8])),
                  writes=[t_prm])
        tmpa = sb("p1tmpa", [128, NT, 16], F32)
        tmpb = sb("p1tmpb", [128, NT, 16], F32)
        t_ta, t_tb = Tok(), Tok()
        dtb = prm[:, 0:16].unsqueeze(1).broadcast_to([128, NT, 16])
        S.op("act", lambda e: e.activation(prm[:, 16:32], prm[:, 16:32], AF.Exp), reads=[t_prm], writes=[t_prm])
        nA = prm[:, 16:32].unsqueeze(1).broadcast_to([128, NT, 16])
        S.op("dve", lambda e: e.tensor_tensor(out=sm[:, :, 0:16], in0=sm[:, :, 0:16], in1=dtb, op=ALU.add),
             reads=[t_sm, t_prm], writes=[t_sm])
        S.op("act", lambda e: e.activation(tmpa[:], sm[:, :, 0:16], AF.Abs), reads=[t_sm], writes=[t_ta])
        S.op("act", lambda e: e.activation(tmpa[:], tmpa[:], AF.Exp, scale=-1.0), reads=[t_ta], writes=[t_ta])
        S.op("act", lambda e: e.activation(tmpa[:], tmpa[:], AF.Ln, bias=1.0), reads=[t_ta], writes=[t_ta])
        S.op("dve", lambda e: e.scalar_tensor_tensor(out=tmpb[:], in0=sm[:, :, 0:16], scalar=0.0, in1=tmpa[:],
                                                     op0=ALU.max, op1=ALU.add), reads=[t_sm, t_ta], writes=[t_tb])
        S.op("dve", lambda e: e.scalar_tensor_tensor(out=sm[:, :, 0:16], in0=tmpb[:], scalar=-1.0, in1=nA,
                                                     op0=ALU.mult, op1=ALU.mult), reads=[t_tb, t_prm], writes=[t_sm])
        S.op("act", lambda e: e.activation(sm[:, :, 16:32], sm[:, :, 16:32], AF.Sigmoid), reads=[t_sm], writes=[t_sm])
        S.dma("sp", lambda e: e.dma_start(out=c.small_d, in_=sm[:]), reads=[t_sm], writes=[c.t_small])

        cw5 = sb("p1cw5", [5, 3072], F32)
        t_cw5 = Tok()
        S.dma("sp", lambda e: e.dma_start(out=cw5[:], in_=c.conv_w), writes=[t_cw5])
        cwT = sb("p1cwT", [128, 24, 8], F32)
        t_cwT = Tok()
        for ct in range(24):
            ps, t_ps = psm.next()
            S.op("pe", lambda e, ps=ps, ct=ct: e.transpose(ps[:, 0:5], cw5[0:5, ct * 128:(ct + 1) * 128], c.ident_f[0:5, 0:5]),
                 reads=[t_cw5, TC], writes=[t_ps])
            S.op("dve", lambda e, ps=ps, ct=ct: e.tensor_copy(cwT[:, ct, 0:5], ps[:, 0:5]), reads=[t_ps], writes=[t_cwT])
        dgs = _mk_ring(st, nc, "p1dg", 2, [128, 5, 128], BF16)
        xpre = _mk_ring(st, nc, "p1xpre", 2, [128, S_LEN + 4], BF16)
        tmstage = _mk_ring(st, nc, "p1tmstage", 2, [128, NT, 128], BF16)
        for ct in range(24):
            kind = ct // 8
            h = ct % 8
            w1, t_w1 = load_w(wfm, c.w_in, C_GQ + ct * 128, 128)
            dg, t_dg = dgs.next()
            for j in range(5):
                S.op("pool", lambda e, dg=dg, j=j, ct=ct: e.tensor_scalar(
                    out=dg[:, j, :], in0=c.ident_f[:], scalar1=cwT[:, ct, j:j + 1], scalar2=None, op0=ALU.mult),
                    reads=[TC, t_cwT], writes=[t_dg])
            xp, t_xp = xpre.next()
            S.op("pool", lambda e, xp=xp: e.memset(xp[:, 0:2], 0.0), writes=[t_xp])
            S.op("pool", lambda e, xp=xp: e.memset(xp[:, S_LEN + 2:S_LEN + 4], 0.0), writes=[t_xp])
            for g in range(NG):
                ps1, t_ps1 = proj_fm(w1, t_w1, g)
                S.op("act", lambda e, xp=xp, ps1=ps1, g=g: e.copy(xp[:, 2 + g * 512:2 + (g + 1) * 512], ps1[:]),
                     reads=[t_ps1], writes=[t_xp])
            stg, t_stg = stage.next()
            for g in range(NG):
                ps, t_ps = psm.next()
                for j in range(5):
                    S.op("pe", lambda e, ps=ps, dg=dg, xp=xp, g=g, j=j: e.matmul(
                        ps[:], dg[:, j, :], xp[:, g * 512 + j:g * 512 + j + 512], start=(j == 0), stop=(j == 4)),
                        reads=[t_dg, t_xp], writes=[t_ps])
                sl = slice(g * 512, (g + 1) * 512)
                if kind == 2:
                    S.op("act", lambda e, ps=ps, stg=stg, sl=sl: e.activation(stg[:, sl], ps[:], AF.Silu),
                         reads=[t_ps], writes=[t_stg])
                else:
                    a, t_a = f32a.next()
                    S.op("act", lambda e, ps=ps, a=a: e.activation(a[:], ps[:], AF.Silu), reads=[t_ps], writes=[t_a])
                    sqb, t_sqb = tmst.next()
                    S.op("pool", lambda e, sqb=sqb, a=a: e.tensor_tensor(out=sqb[:], in0=a[:], in1=a[:], op=ALU.mult),
                         reads=[t_a], writes=[t_sqb])
                    ps2, t_ps2 = psm.next()
                    S.op("pe", lambda e, ps2=ps2, sqb=sqb: e.matmul(ps2[:], c.ones_b[:], sqb[:], start=True, stop=True),
                         reads=[t_sqb, TC], writes=[t_ps2])
                    b, t_b = f32b.next()
                    S.op("act", lambda e, b=b, ps2=ps2: e.activation(b[:], ps2[:], AF.Sqrt, bias=EPS), reads=[t_ps2], writes=[t_b])
                    S.op("dve", lambda e, b=b: e.reciprocal(b[:], b[:]), reads=[t_b], writes=[t_b])
                    scl = (128.0 ** -0.5) if kind == 0 else 1.0
                    S.op("dve", lambda e, a=a, b=b, stg=stg, sl=sl, scl=scl: e.scalar_tensor_tensor(
                        out=stg[:, sl], in0=a[:], scalar=scl, in1=b[:], op0=ALU.mult, op1=ALU.mult),
                        reads=[t_a, t_b], writes=[t_stg])
            if kind < 2:
                dst = c.gq_d if kind == 0 else c.gk_d
                S.dma("sp", lambda e, stg=stg, dst=dst, h=h: e.dma_start(out=dst[h], in_=stg[:]), reads=[t_stg], writes=[c.t_gqk])
            if kind >= 1:
                dst = c.gktok_d if kind == 1 else c.gvtok_d
                tms, t_tms = tmstage.next()
                for i8 in range(4):
                    pt, t_pt = pst.next()
                    for ii in range(8):
                        i = i8 * 8 + ii
                        S.op("pe", lambda e, pt=pt, stg=stg, ii=ii, i=i: e.transpose(pt[:, ii, :], stg[:, i * 128:(i + 1) * 128], c.ident_b[:]),
                             reads=[t_stg, TC], writes=[t_pt])
                    S.op("dve", lambda e, pt=pt, tms=tms, i8=i8: e.tensor_copy(tms[:, i8 * 8:(i8 + 1) * 8, :], pt[:]),
                         reads=[t_pt], writes=[t_tms])
                S.dma("sp", lambda e, tms=tms, dst=dst, h=h: e.dma_start(
                    out=dst[:, h * 128:(h + 1) * 128].rearrange("(t p) c -> p t c", p=128), in_=tms[:]),
                    reads=[t_tms], writes=[c.t_gtok])
    S.barrier()


def build_program(debug=False, phases=("p1",), inject=(), opts=None):
    nc = bass.Bass("TRN2", target_bir_lowering=False)
    c = Ctx()
    c.inject = set(inject)
    c.opts = opts or {}
    c.nc = nc
    c.S = Sched(nc)

    c.input_names = []

    def din(name, shape):
        c.input_names.append(name)
        return nc.dram_tensor(name, list(shape), F32, kind="ExternalInput").ap()

    c.x = din("x", [S_LEN, D])
    c.norm1_w = din("norm1_w", [1, D])
    c.w_in = din("w_in", [D, IN_W])
    c.w_qkp = din("w_qkp", [D, 2048])
    c.cosT = din("cosT", [128, S_LEN])
    c.sinT = din("sinT", [128, S_LEN])
    c.conv_w = din("conv_w", [5, 3072])
    for n in ("a_log_fwd", "dt_bias_fwd", "a_log_bwd", "dt_bias_bwd"):
        setattr(c, n, din(n, [1, 8]))

    c.debug_names = []

    def scratch(name, shape, dt):
        kind = "ExternalOutput" if debug else "Internal"
        if name in c.inject:
            kind = "ExternalInput"
            c.input_names.append(name)
        elif debug:
            c.debug_names.append(name)
        return nc.dram_tensor(name, list(shape), dt, kind=kind).ap()

    c.qT_d = scratch("qT_d", [H, 128, S_LEN], BF16)
    c.kT_d = scratch("kT_d", [H, 128, S_LEN], BF16)
    c.v_d = scratch("v_d", [S_LEN, D], BF16)
    c.zs_d = scratch("zs_d", [S_LEN, D], BF16)
    c.sga_d = scratch("sga_d", [H, 128, S_LEN], BF16)
    c.sgg_d = scratch("sgg_d", [H, 128, S_LEN], BF16)
    c.gq_d = scratch("gq_d", [H, 128, S_LEN], BF16)
    c.gk_d = scratch("gk_d", [H, 128, S_LEN], BF16)
    c.gktok_d = scratch("gktok_d", [S_LEN, D], BF16)
    c.gvtok_d = scratch("gvtok_d", [S_LEN, D], BF16)
    c.small_d = scratch("small_d", [128, NT, 32], F32)
    for n in ("lambda_q1", "lambda_k1", "lambda_q2", "lambda_k2"):
        setattr(c, n, din(n, [1, 64]))
    c.subln_w = din("subln_w", [1, 128])
    c.gdn_norm_w = din("gdn_norm_w", [1, 128])
    c.w_proj_attn = din("w_proj_attn", [D, D])
    c.w_proj_gdn = din("w_proj_gdn", [D, D])
    c.w_out = din("w_out", [D, D])
    c.w_router = din("w_router", [D, NE])
    c.norm2_w = din("norm2_w", [1, D])
    c.norm_f_w = din("norm_f_w", [1, D])
    c.w_gate = din("w_gate", [NE, D, FF])
    c.w_up = din("w_up", [NE, D, FF])
    c.w_down = din("w_down", [NE, FF, D])
    c.out = nc.dram_tensor("out", [S_LEN, D], F32, kind="ExternalOutput").ap()
    c.x1_d = scratch("x1_d", [S_LEN, D], F32)
    c.h2tok_d = scratch("h2tok_d", [S_LEN, D], BF16)
    c.aff_d = scratch("aff_d", [128, NT, NE], F32)
    c.pos_d = scratch("pos_d", [NE, S_LEN], F32)
    c.ptok_d = scratch("ptok_d", [128, NT, NE], F32)
    c.ye_d = scratch("ye_d", [NE, CAP, D], BF16)
    for n in ("t_x1", "t_h2", "t_aff", "t_pos", "t_ye", "t_out"):
        setattr(c, n, Tok())
    c.ogT_d = scratch("ogT_d", [H, 128, S_LEN], BF16)
    c.t_og = Tok()
    c.oaT_d = scratch("oaT_d", [H, 128, S_LEN], BF16)
    for n in ("t_qk", "t_gates", "t_vz", "t_small", "t_gqk", "t_gtok", "t_oa"):
        setattr(c, n, Tok())

    with ExitStack() as st:
        _consts(c, st)
        if "p1" in phases:
            phase1(c)
        if "p2" in phases:
            phase2(c)
        if "p3" in phases:
            phase3(c)
        if "p4" in phases:
            phase4(c)
        if "p5" in phases:
            phase5(c)
        if "p6a" in phases:
            phase6a(c)
        if "p6b" in phases:
            phase6b(c)
        c.S.barrier()
        c.S.emit()
    return nc, c


def host_prep(inputs):
    f = lambda a: np.ascontiguousarray(np.asarray(a, dtype=np.float32))
    w_in = f(inputs["w_in"][0])
    perm = np.arange(2048).reshape(16, 2, 2, 32)[:, :, ::-1, :].reshape(-1)
    w_qkp = np.ascontiguousarray(w_in[:, :2048][:, perm])
    inv = 10000.0 ** (-np.arange(0, 64, 2, dtype=np.float32) / 64)
    ang = np.arange(S_LEN, dtype=np.float32)[:, None] * inv[None, :]
    ang = np.concatenate([ang, ang], axis=-1)
    cos = np.cos(ang).T.astype(np.float32)
    sin = np.sin(ang).T.astype(np.float32)
    sin[:32] *= -1.0
    shared = {
        "norm1_w": f(inputs["norm1_w"]).reshape(1, D),
        "w_in": w_in,
        "w_qkp": w_qkp,
        "cosT": np.ascontiguousarray(np.concatenate([cos, cos], 0)),
        "sinT": np.ascontiguousarray(np.concatenate([sin, sin], 0)),
        "conv_w": f(inputs["conv_w"][0]),
    }
    for n in ("a_log_fwd", "dt_bias_fwd", "a_log_bwd", "dt_bias_bwd"):
        shared[n] = f(inputs[n]).reshape(1, 8)
    for n in ("lambda_q1", "lambda_k1", "lambda_q2", "lambda_k2"):
        shared[n] = f(inputs[n]).reshape(1, 64)
    shared["subln_w"] = f(inputs["subln_w"]).reshape(1, 128)
    shared["gdn_norm_w"] = f(inputs["gdn_norm_w"]).reshape(1, 128)
    shared["w_proj_attn"] = f(inputs["w_proj_attn"][0])
    shared["w_proj_gdn"] = f(inputs["w_proj_gdn"][0])
    shared["w_out"] = f(inputs["w_out"][0])
    shared["w_router"] = f(inputs["w_router"][0])
    shared["norm2_w"] = f(inputs["norm2_w"]).reshape(1, D)
    shared["norm_f_w"] = f(inputs["norm_f_w"]).reshape(1, D)
    shared["w_gate"] = f(inputs["w_gate"][0])
    shared["w_up"] = f(inputs["w_up"][0])
    shared["w_down"] = f(inputs["w_down"][0])
    return shared


def phase2(c):
    nc, S = c.nc, c.S
    TC = c.t_const
    with ExitStack() as st:
        sb = lambda name, shape, dt: st.enter_context(nc.sbuf_tensor(name, shape, dt))
        lam = sb("p2lam", [128, 4, 64], F32)
        lsc = sb("p2lsc", [128, 8], F32)
        t_lam = Tok()
        for j, src in enumerate((c.lambda_q1, c.lambda_k1, c.lambda_q2, c.lambda_k2)):
            S.dma("sp", lambda e, j=j, src=src: e.dma_start(out=lam[:, j, :], in_=src.broadcast_to([128, 64])), writes=[t_lam])
        S.op("dve", lambda e: e.tensor_tensor(out=lam[:, 0, :], in0=lam[:, 0, :], in1=lam[:, 1, :], op=ALU.mult), reads=[t_lam], writes=[t_lam])
        S.op("dve", lambda e: e.tensor_tensor(out=lam[:, 2, :], in0=lam[:, 2, :], in1=lam[:, 3, :], op=ALU.mult), reads=[t_lam], writes=[t_lam])
        S.op("act", lambda e: e.activation(lam[:, 1, :], lam[:, 0, :], AF.Identity, accum_out=lsc[:, 0:1]), reads=[t_lam], writes=[t_lam])
        S.op("act", lambda e: e.activation(lam[:, 3, :], lam[:, 2, :], AF.Identity, accum_out=lsc[:, 1:2]), reads=[t_lam], writes=[t_lam])
        S.op("act", lambda e: e.activation(lsc[:, 2:4], lsc[:, 0:2], AF.Exp), reads=[t_lam], writes=[t_lam])
        S.op("dve", lambda e: e.scalar_tensor_tensor(out=lsc[:, 4:5], in0=lsc[:, 3:4], scalar=-LAMBDA_INIT, in1=lsc[:, 2:3],
                                                     op0=ALU.add, op1=ALU.subtract), reads=[t_lam], writes=[t_lam])
        wsub = sb("p2wsub", [128, 2], F32)
        t_wsub = Tok()
        S.dma("sp", lambda e: e.dma_start(out=wsub[:, 0:1], in_=c.subln_w.rearrange("o e -> e o")), writes=[t_wsub])
        S.op("dve", lambda e: e.tensor_scalar(out=wsub[:, 1:2], in0=wsub[:, 0:1], scalar1=(1.0 - LAMBDA_INIT), scalar2=None, op0=ALU.mult),
             reads=[t_wsub], writes=[t_wsub])

        qr = _mk_ring(st, nc, "p2q", 2, [128, S_LEN], BF16)
        kr = _mk_ring(st, nc, "p2k", 2, [128, S_LEN], BF16)
        vr = _mk_ring(st, nc, "p2v", 2, [128, NT, 128], BF16)
        pr = _mk_ring(st, nc, "p2p", 4, [128, 512], BF16)
        fr = _mk_ring(st, nc, "p2f", 6, [128, 512], F32)
        sqr = _mk_ring(st, nc, "p2sq", 2, [128, 512], BF16)
        accr = _mk_ring(st, nc, "p2acc", 4, [128, 512], F32)
        stage = _mk_ring(st, nc, "p2stage", 2, [128, S_LEN], BF16)
        ps_s = _mk_ring(st, nc, "p2pss", 3, [128, 512], F32, psum=True)
        ps_o = [_mk_ring(st, nc, "p2pso%d" % t, 1, [128, 512], F32, psum=True) for t in range(2)]
        ps_l = [_mk_ring(st, nc, "p2psl%d" % t, 1, [128, 512], F32, psum=True) for t in range(2)]
        ps_x = _mk_ring(st, nc, "p2psx", 1, [128, 512], F32, psum=True)

        for h in range(H):
            q, t_q = qr.next()
            k, t_k = kr.next()
            v, t_v = vr.next()
            S.dma("sp", lambda e, q=q, h=h: e.dma_start(out=q[:], in_=c.qT_d[h]), reads=[c.t_qk], writes=[t_q])
            S.dma("sp", lambda e, k=k, h=h: e.dma_start(out=k[:], in_=c.kT_d[h]), reads=[c.t_qk], writes=[t_k])
            S.dma("sp", lambda e, v=v, h=h: e.dma_start(
                out=v[:], in_=c.v_d[:, h * 128:(h + 1) * 128].rearrange("(t p) e -> p t e", p=128)), reads=[c.t_vz], writes=[t_v])
            stg, t_stg = stage.next()
            for g in range(NG):
                qs = slice(g * 512, (g + 1) * 512)
                accs = []
                for t in range(2):
                    po, t_po = ps_o[t].next()
                    pl, t_pl = ps_l[t].next()
                    ts = slice(t * 64, (t + 1) * 64)
                    accA, t_accA = accr.next()
                    accB, t_accB = accr.next()

                    def emit_s(j, ts=ts, qs=qs, k=k, q=q, t_k=t_k, t_q=t_q):
                        pss, t_pss = ps_s.next()
                        S.op("pe", lambda e, pss=pss, j=j: e.matmul(
                            pss[:], k[ts, j * 128:(j + 1) * 128], q[ts, qs], start=True, stop=True),
                            reads=[t_k, t_q], writes=[t_pss])
                        return pss, t_pss
                    cur = emit_s(0)
                    for j in range(NT):
                        nxt = emit_s(j + 1) if j + 1 < NT else None
                        pss, t_pss = cur
                        p, t_p = pr.next()
                        S.op("act", lambda e, p=p, pss=pss: e.activation(p[:], pss[:], AF.Exp, scale=0.125),
                             reads=[t_pss], writes=[t_p])
                        S.op("pe", lambda e, po=po, v=v, p=p, j=j: e.matmul(po[:], v[:, j, :], p[:], start=(j == 0), stop=(j == NT - 1)),
                             reads=[t_v, t_p], writes=[t_po])
                        acc, t_acc = (accA, t_accA) if j % 2 == 0 else (accB, t_accB)
                        eng = "dve" if j % 2 == 0 else "pool"
                        if j < 2:
                            S.op(eng, lambda e, acc=acc, p=p: e.tensor_copy(acc[:], p[:]), reads=[t_p], writes=[t_acc])
                        else:
                            S.op(eng, lambda e, acc=acc, p=p: e.tensor_tensor(out=acc[:], in0=acc[:], in1=p[:], op=ALU.add),
                                 reads=[t_p, t_acc], writes=[t_acc])
                        cur = nxt
                    S.op("pe", lambda e, pl=pl, accA=accA: e.matmul(pl[:], c.ones_f[:], accA[:], start=True, stop=False),
                         reads=[TC, t_accA], writes=[t_pl])
                    S.op("pe", lambda e, pl=pl, accB=accB: e.matmul(pl[:], c.ones_f[:], accB[:], start=False, stop=True),
                         reads=[TC, t_accB], writes=[t_pl])
                    accs.append((po, t_po, pl, t_pl))
                outs = []
                for t in range(2):
                    po, t_po, pl, t_pl = accs[t]
                    r, t_r = fr.next()
                    S.op("dve", lambda e, r=r, pl=pl: e.reciprocal(r[:], pl[:]), reads=[t_pl], writes=[t_r])
                    a, t_a = fr.next()
                    S.op("dve", lambda e, a=a, po=po, r=r: e.tensor_tensor(out=a[:], in0=po[:], in1=r[:], op=ALU.mult),
                         reads=[t_po, t_r], writes=[t_a])
                    outs.append((a, t_a))
                (a, t_a), (b, t_b) = outs
                oa, t_oa = fr.next()
                S.op("dve", lambda e, oa=oa, a=a, b=b: e.scalar_tensor_tensor(
                    out=oa[:], in0=b[:], scalar=lsc[:, 4:5], in1=a[:], op0=ALU.mult, op1=ALU.add),
                    reads=[t_a, t_b, t_lam], writes=[t_oa])
                sq, t_sq = sqr.next()
                S.op("pool", lambda e, sq=sq, oa=oa: e.tensor_tensor(out=sq[:], in0=oa[:], in1=oa[:], op=ALU.mult),
                     reads=[t_oa], writes=[t_sq])
                px, t_px = ps_x.next()
                S.op("pe", lambda e, px=px, sq=sq: e.matmul(px[:], c.ones_b[:], sq[:], start=True, stop=True),
                     reads=[TC, t_sq], writes=[t_px])
                rs, t_rs = fr.next()
                S.op("act", lambda e, rs=rs, px=px: e.activation(rs[:], px[:], AF.Sqrt, bias=EPS, scale=1.0 / 128), reads=[t_px], writes=[t_rs])
                S.op("dve", lambda e, rs=rs: e.reciprocal(rs[:], rs[:]), reads=[t_rs], writes=[t_rs])
                S.op("dve", lambda e, stg=stg, oa=oa, rs=rs, qs=qs: e.scalar_tensor_tensor(
                    out=stg[:, qs], in0=oa[:], scalar=wsub[:, 1:2], in1=rs[:], op0=ALU.mult, op1=ALU.mult),
                    reads=[t_oa, t_rs, t_wsub], writes=[t_stg])
            S.dma("sp", lambda e, stg=stg, h=h: e.dma_start(out=c.oaT_d[h], in_=stg[:]), reads=[t_stg], writes=[c.t_oa])
    S.barrier()


def phase3(c):
    nc, S = c.nc, c.S
    TC = c.t_const
    NB = NT
    with ExitStack() as st:
        sb = lambda name, shape, dt: st.enter_context(nc.sbuf_tensor(name, shape, dt))
        inc = [sb("p3inc%d" % d, [128, 128], F32) for d in range(2)]
        strm = [sb("p3str%d" % d, [128, 128], F32) for d in range(2)]
        mbias = [sb("p3mb%d" % d, [128, 128], F32) for d in range(2)]
        esel = [sb("p3es%d" % d, [128, 128], F32) for d in range(2)]
        t_m = Tok()
        for d in range(2):
            sgn = 1 if d == 0 else -1
            S.op("pool", lambda e, d=d: e.memset(inc[d][:], 1.0), writes=[t_m])
            S.op("pool", lambda e, d=d, sgn=sgn: e.affine_select(out=inc[d][:], in_=inc[d][:], compare_op=ALU.is_ge, fill=0.0,
                                                                base=0, pattern=[[sgn, 128]], channel_multiplier=-sgn),
                 reads=[t_m], writes=[t_m])
            S.op("pool", lambda e, d=d: e.memset(strm[d][:], 1.0), writes=[t_m])
            S.op("pool", lambda e, d=d, sgn=sgn: e.affine_select(out=strm[d][:], in_=strm[d][:], compare_op=ALU.is_ge, fill=0.0,
                                                                base=-1, pattern=[[sgn, 128]], channel_multiplier=-sgn),
                 reads=[t_m], writes=[t_m])
            S.op("pool", lambda e, d=d: e.tensor_scalar(out=mbias[d][:], in0=inc[d][:], scalar1=30000.0, scalar2=-30000.0,
                                                        op0=ALU.mult, op1=ALU.add), reads=[t_m], writes=[t_m])
            lastp = 127 if d == 0 else 0
            S.op("pool", lambda e, d=d: e.memset(esel[d][:], 0.0), writes=[t_m])
            S.op("pool", lambda e, d=d, lastp=lastp: e.affine_select(out=esel[d][:], in_=esel[d][:], compare_op=ALU.not_equal, fill=1.0,
                                                                    base=-lastp, pattern=[[0, 128]], channel_multiplier=1),
                 reads=[t_m], writes=[t_m])
        sm = sb("p3sm", [128, NT, 32], F32)
        t_sm = Tok()
        S.dma("sp", lambda e: e.dma_start(out=sm[:], in_=c.small_d), reads=[c.t_small], writes=[t_sm])
        gw = sb("p3gw", [128, 128], F32)
        t_gw = Tok()
        S.dma("sp", lambda e: e.dma_start(out=gw[:], in_=c.gdn_norm_w.broadcast_to([128, 128])), writes=[t_gw])

        def bank(name, dt=F32, n=512):
            return st.enter_context(nc.psum_tensor(name, [128, n], dt))
        b0, b2, b3, b4, b5, b6, b7 = [bank("p3b" + x) for x in "0234567"]
        b1 = bank("p3b1", BF16, 1024)
        btok = {id(b): Tok(excl=True) for b in (b0, b1, b2, b3, b4, b5, b6, b7)}
        slot = lambda b, i, w=128: (b[:, i * w:(i + 1) * w], btok[id(b)])
        s_kk, s_kq = slot(b0, 0), slot(b0, 1)
        s_gdd = [slot(b0, 2), slot(b0, 3)]
        s_misc = [slot(b6, 3)] * 4
        s_tr = [slot(b1, i) for i in range(8)]
        s_sqd = [(slot(b2, 0), slot(b2, 1)), (slot(b4, 0), slot(b4, 1))]
        s_apd = [slot(b3, 0, 256), slot(b5, 0, 256)]
        s_scan = [[slot(b6, i) for i in range(4)], [slot(b7, i) for i in range(4)]]

        kT = sb("p3kT", [128, S_LEN], BF16)
        qT = sb("p3qT", [128, S_LEN], BF16)
        ktok = sb("p3ktok", [128, NB, 128], BF16)
        vtok = sb("p3vtok", [128, NB, 128], BF16)
        zs = sb("p3zs", [128, NB, 128], BF16)
        t_in = Tok()
        ub = [sb("p3ub%d" % d, [128, NB, 128], F32) for d in range(2)]
        nwT = [sb("p3nwT%d" % d, [128, NB, 128], BF16) for d in range(2)]
        kdec = [sb("p3kdec%d" % d, [128, NB, 128], BF16) for d in range(2)]
        qkT = [sb("p3qkT%d" % d, [128, NB, 128], BF16) for d in range(2)]
        t_blk = [[Tok() for _ in range(NB)] for _ in range(2)]
        sc = [sb("p3sc%d" % d, [128, 8, NB], F32) for d in range(2)]
        t_sc = [Tok(), Tok()]
        oacc = sb("p3oacc", [128, NB, 128], F32)
        t_oacc = [Tok() for _ in range(NB)]
        S32 = [sb("p3S32_%d" % d, [128, 128], F32) for d in range(2)]
        Sbf = [sb("p3Sbf_%d" % d, [128, 128], BF16) for d in range(2)]
        t_S = [Tok(), Tok()]
        dtr = _mk_ring(st, nc, "p3dt", 5, [128, 128], F32)
        tmpr = _mk_ring(st, nc, "p3tmp", 5, [128, 128], F32)
        ntr = _mk_ring(st, nc, "p3nt", 10, [128, 128], F32)
        nnr = _mk_ring(st, nc, "p3nn", 10, [128, 128], F32)
        xr = _mk_ring(st, nc, "p3x", 5, [128, 256], F32)
        vnr = _mk_ring(st, nc, "p3vn", 4, [128, 128], BF16)
        o2r = _mk_ring(st, nc, "p3o2", 4, [128, 128], F32)
        big = sb("p3big", [128, NB, 128], F32)
        t_big = Tok()
        red = sb("p3red", [128, NB], F32)
        ogtok = sb("p3ogtok", [128, NB, 128], BF16)
        stage = sb("p3stage", [128, S_LEN], BF16)
        t_stage = Tok()

        stop = c.opts.get("p3_stop", 9)
        for h in range(c.opts.get("p3_heads", H)):
            S.dma("sp", lambda e, h=h: e.dma_start(out=kT[:], in_=c.gk_d[h]), reads=[c.t_gqk], writes=[t_in])
            S.dma("sp", lambda e, h=h: e.dma_start(out=qT[:], in_=c.gq_d[h]), reads=[c.t_gqk], writes=[t_in])
            for (buf, src, tk) in ((ktok, c.gktok_d, c.t_gtok), (vtok, c.gvtok_d, c.t_gtok), (zs, c.zs_d, c.t_vz)):
                S.dma("sp", lambda e, buf=buf, src=src, h=h: e.dma_start(
                    out=buf[:], in_=src[:, h * 128:(h + 1) * 128].rearrange("(t p) e -> p t e", p=128)), reads=[tk], writes=[t_in])
            for d in range(2):
                s_ = sc[d]
                g = sm[:, :, d * 8 + h]
                beta = sm[:, :, 16 + d * 8 + h]
                (pm, t_pm) = s_misc[d * 2]
                S.op("pe", lambda e, pm=pm, d=d, g=g: e.matmul(pm[:, 0:NB], inc[d][:], g, start=True, stop=True),
                     reads=[t_m, t_sm], writes=[t_pm])
                S.op("dve", lambda e, pm=pm, s_=s_: e.tensor_copy(s_[:, 0, :], pm[:, 0:NB]), reads=[t_pm], writes=[t_sc[d]])
                S.op("dve", lambda e, s_=s_: e.tensor_scalar(out=s_[:, 1, :], in0=s_[:, 0, :], scalar1=-1.0, scalar2=None, op0=ALU.mult),
                     reads=[t_sc[d]], writes=[t_sc[d]])
                (pm2, t_pm2) = s_misc[d * 2 + 1]
                S.op("pe", lambda e, pm2=pm2, d=d, s_=s_: e.matmul(pm2[:, 0:NB], esel[d][:], s_[:, 0, :], start=True, stop=True),
                     reads=[t_m, t_sc[d]], writes=[t_pm2])
                S.op("dve", lambda e, pm2=pm2, s_=s_: e.tensor_copy(s_[:, 2, :], pm2[:, 0:NB]), reads=[t_pm2], writes=[t_sc[d]])
                S.op("act", lambda e, s_=s_: e.activation(s_[:, 3, :], s_[:, 0, :], AF.Exp), reads=[t_sc[d]], writes=[t_sc[d]])
                S.op("dve", lambda e, s_=s_: e.tensor_tensor(out=s_[:, 4, :], in0=s_[:, 2, :], in1=s_[:, 0, :], op=ALU.subtract),
                     reads=[t_sc[d]], writes=[t_sc[d]])
                S.op("act", lambda e, s_=s_: e.activation(s_[:, 4, :], s_[:, 4, :], AF.Exp), reads=[t_sc[d]], writes=[t_sc[d]])
                S.op("act", lambda e, s_=s_: e.activation(s_[:, 5, :], s_[:, 2, :], AF.Exp), reads=[t_sc[d]], writes=[t_sc[d]])
                S.op("dve", lambda e, s_=s_, beta=beta: e.tensor_scalar(out=s_[:, 6, :], in0=beta, scalar1=-1.0, scalar2=None, op0=ALU.mult),
                     reads=[t_sm, t_sc[d]], writes=[t_sc[d]])
                S.op("dve", lambda e, s_=s_, beta=beta: e.tensor_copy(s_[:, 7, :], beta), reads=[t_sm, t_sc[d]], writes=[t_sc[d]])

            def par_chain(b, d):
                bs = slice(b * 128, (b + 1) * 128)
                s_ = sc[d]
                (pkk, t_pkk), (pkq, t_pkq), (pgd, t_pgd) = s_kk, s_kq, s_gdd[d]
                if d == 0:
                    S.op("pe", lambda e: e.matmul(pkk, kT[:, bs], kT[:, bs], start=True, stop=True), reads=[t_in], writes=[t_pkk])
                    S.op("pe", lambda e: e.matmul(pkq, kT[:, bs], qT[:, bs], start=True, stop=True), reads=[t_in], writes=[t_pkq])
                S.op("pe", lambda e: e.matmul(pgd, s_[:, 0, b:b + 1].broadcast_to([128, 128]), c.ident_f[:], start=True, stop=False),
                     reads=[t_sc[d], TC], writes=[t_pgd])
                S.op("pe", lambda e: e.matmul(pgd, c.ident_f[:], s_[:, 1, b:b + 1].broadcast_to([128, 128]), start=False, stop=False),
                     reads=[t_sc[d], TC], writes=[t_pgd])
                S.op("pe", lambda e: e.matmul(pgd, c.ident_f[:], mbias[d][:], start=False, stop=True), reads=[t_m, TC], writes=[t_pgd])
                yield
                dt_, t_dt = dtr.next()
                S.op("act", lambda e: e.activation(dt_[:], pgd, AF.Exp), reads=[t_pgd], writes=[t_dt])
                tmp, t_tmp = tmpr.next()
                S.op("dve", lambda e: e.scalar_tensor_tensor(out=tmp[:], in0=pkk, scalar=s_[:, 6, b:b + 1], in1=dt_[:], op0=ALU.mult, op1=ALU.mult),
                     reads=[t_pkk, t_sc[d], t_dt], writes=[t_tmp])
                S.op("dve", lambda e: e.tensor_tensor(out=qkT[d][:, b, :], in0=pkq, in1=dt_[:], op=ALU.mult),
                     reads=[t_pkq, t_dt], writes=[t_blk[d][b]])
                nt, t_nt = ntr.next()
                S.op("pool", lambda e, nt=nt: e.tensor_tensor(out=nt[:], in0=tmp[:], in1=strm[d][:], op=ALU.mult), reads=[t_tmp, t_m], writes=[t_nt])
                x, t_x = xr.next()
                S.op("pool", lambda e: e.tensor_copy(x[:, 0:128], vtok[:, b, :]), reads=[t_in], writes=[t_x])
                S.op("pool", lambda e: e.tensor_scalar(out=x[:, 128:256], in0=ktok[:, b, :], scalar1=s_[:, 3, b:b + 1], scalar2=None, op0=ALU.mult),
                     reads=[t_in, t_sc[d]], writes=[t_x])
                S.op("pool", lambda e: e.tensor_scalar(out=kdec[d][:, b, :], in0=ktok[:, b, :], scalar1=s_[:, 4, b:b + 1], scalar2=None, op0=ALU.mult),
                     reads=[t_in, t_sc[d]], writes=[t_blk[d][b]])
                yield
                (ptr, t_ptr) = s_sqd[d][0]
                S.op("pe", lambda e, nt=nt: e.transpose(ptr, nt[:], c.ident_f[:]), reads=[t_nt, TC], writes=[t_ptr])
                yield
                nn, t_nn = nnr.next()
                S.op("act", lambda e, nn=nn: e.copy(nn[:], ptr), reads=[t_ptr], writes=[t_nn])
                yield
                for l in range(7):
                    (pap, t_pap) = s_apd[d]
                    S.op("pe", lambda e, nt=nt: e.matmul(pap, nt[:], x[:], start=True, stop=True), reads=[t_nt, t_x], writes=[t_pap])
                    if l < 6:
                        (pn2, t_pn2), (pnt2, t_pnt2) = s_sqd[d]
                        S.op("pe", lambda e, nt=nt, nn=nn: e.matmul(pn2, nt[:], nn[:], start=True, stop=True), reads=[t_nt, t_nn], writes=[t_pn2])
                        S.op("pe", lambda e, nt=nt, nn=nn: e.matmul(pnt2, nn[:], nt[:], start=True, stop=True), reads=[t_nt, t_nn], writes=[t_pnt2])
                    yield
                    S.op("dve", lambda e: e.tensor_tensor(out=x[:], in0=pap, in1=x[:], op=ALU.add), reads=[t_pap, t_x], writes=[t_x])
                    if l < 6:
                        nn2, t_nn2 = nnr.next()
                        nt2, t_nt2 = ntr.next()
                        S.op("act", lambda e, nn2=nn2: e.copy(nn2[:], pn2), reads=[t_pn2], writes=[t_nn2])
                        S.op("act", lambda e, nt2=nt2: e.copy(nt2[:], pnt2), reads=[t_pnt2], writes=[t_nt2])
                        nn, t_nn, nt, t_nt = nn2, t_nn2, nt2, t_nt2
                    yield
                S.op("dve", lambda e: e.tensor_scalar(out=ub[d][:, b, :], in0=x[:, 0:128], scalar1=s_[:, 7, b:b + 1], scalar2=None, op0=ALU.mult),
                     reads=[t_x, t_sc[d]], writes=[t_blk[d][b]])
                (ptw, t_ptw) = s_sqd[d][1]
                S.op("pe", lambda e: e.transpose(ptw, x[:, 128:256], c.ident_f[:]), reads=[t_x, TC], writes=[t_ptw])
                yield
                S.op("act", lambda e: e.activation(nwT[d][:, b, :], ptw, AF.Copy, scale=-1.0), reads=[t_ptw], writes=[t_blk[d][b]])
                yield

            if stop >= 2:
                _interleave((par_chain(b, d) for b in range(NB) for d in range(2)), 2)

            def scan_chain(d):
                s_ = sc[d]
                S.op("pool", lambda e: e.memset(S32[d][:], 0.0), writes=[t_S[d]])
                S.op("pool", lambda e: e.memset(Sbf[d][:], 0.0), writes=[t_S[d]])
                (pv, t_pv), (po1, t_po1), (po2, t_po2), (pS, t_pS) = s_scan[d]
                for step in range(NB):
                    b = step if d == 0 else NB - 1 - step
                    bs = slice(b * 128, (b + 1) * 128)
                    S.op("pe", lambda e, b=b: e.matmul(pv, nwT[d][:, b, :], Sbf[d][:], start=True, stop=True),
                         reads=[t_blk[d][b], t_S[d]], writes=[t_pv])
                    S.op("pe", lambda e, bs=bs: e.matmul(po1, qT[:, bs], Sbf[d][:], start=True, stop=True), reads=[t_in, t_S[d]], writes=[t_po1])
                    yield
                    vn, t_vn = vnr.next()
                    S.op("dve", lambda e, vn=vn, b=b: e.scalar_tensor_tensor(
                        out=vn[:], in0=pv, scalar=s_[:, 7, b:b + 1], in1=ub[d][:, b, :], op0=ALU.mult, op1=ALU.add),
                        reads=[t_pv, t_sc[d], t_blk[d][b]], writes=[t_vn])
                    yield
                    S.op("pe", lambda e, vn=vn, b=b: e.matmul(pS, kdec[d][:, b, :], vn[:], start=True, stop=True),
                         reads=[t_blk[d][b], t_vn], writes=[t_pS])
                    S.op("pe", lambda e, vn=vn, b=b: e.matmul(po2, qkT[d][:, b, :], vn[:], start=True, stop=True),
                         reads=[t_blk[d][b], t_vn], writes=[t_po2])
                    yield
                    S.op("dve", lambda e, b=b: e.scalar_tensor_tensor(
                        out=S32[d][:], in0=S32[d][:], scalar=s_[:, 5, b:b + 1], in1=pS, op0=ALU.mult, op1=ALU.add),
                        reads=[t_pS, t_sc[d], t_S[d]], writes=[t_S[d]])
                    S.op("pool", lambda e: e.tensor_copy(Sbf[d][:], S32[d][:]), reads=[t_S[d]], writes=[t_S[d]])
                    o2, t_o2 = o2r.next()
                    S.op("act", lambda e, o2=o2: e.copy(o2[:], po2), reads=[t_po2], writes=[t_o2])
                    first = (b < NB // 2) if d == 0 else (b >= NB // 2)
                    if not first:
                        S.op("pool", lambda e, o2=o2, b=b: e.tensor_tensor(out=o2[:], in0=o2[:], in1=oacc[:, b, :], op=ALU.add),
                             reads=[t_o2, t_oacc[b]], writes=[t_o2])
                    S.op("dve", lambda e, o2=o2, b=b: e.scalar_tensor_tensor(
                        out=oacc[:, b, :], in0=po1, scalar=s_[:, 3, b:b + 1], in1=o2[:], op0=ALU.mult, op1=ALU.add),
                        reads=[t_po1, t_sc[d], t_o2], writes=[t_oacc[b]])
                    yield

            if stop >= 3:
                _interleave((scan_chain(d) for d in range(2)), 2)

            S.op("dve", lambda e: e.tensor_tensor(out=big[:], in0=oacc[:], in1=oacc[:], op=ALU.mult), reads=t_oacc, writes=[t_big])
            S.op("dve", lambda e: e.tensor_reduce(out=red[:], in_=big[:], axis=mybir.AxisListType.X, op=ALU.add), reads=[t_big], writes=[t_big])
            S.op("act", lambda e: e.activation(red[:], red[:], AF.Sqrt, bias=EPS, scale=1.0 / 128), reads=[t_big], writes=[t_big])
            S.op("dve", lambda e: e.reciprocal(red[:], red[:]), reads=[t_big], writes=[t_big])
            S.op("dve", lambda e: e.tensor_tensor(out=big[:], in0=oacc[:], in1=red[:].unsqueeze(2).broadcast_to([128, NB, 128]), op=ALU.mult),
                 reads=t_oacc + [t_big], writes=[t_big])
            S.op("pool", lambda e: e.tensor_tensor(out=big[:], in0=big[:], in1=gw[:].unsqueeze(1).broadcast_to([128, NB, 128]), op=ALU.mult),
                 reads=[t_big, t_gw], writes=[t_big])
            S.op("dve", lambda e: e.tensor_tensor(out=ogtok[:], in0=big[:], in1=zs[:], op=ALU.mult), reads=[t_big, t_in], writes=[t_big])
            for i8 in range(4):
                for ii in range(8):
                    b = i8 * 8 + ii
                    (ptr, t_ptr) = s_tr[ii]
                    S.op("pe", lambda e, ptr=ptr, b=b: e.transpose(ptr, ogtok[:, b, :], c.ident_b[:]), reads=[t_big, TC], writes=[t_ptr])
                    S.op("act", lambda e, ptr=ptr, b=b: e.copy(stage[:, b * 128:(b + 1) * 128], ptr), reads=[t_ptr], writes=[t_stage])
            S.dma("sp", lambda e, h=h: e.dma_start(out=c.ogT_d[h], in_=stage[:]), reads=[t_stage], writes=[c.t_og])
    S.barrier()


def phase4(c):
    nc, S = c.nc, c.S
    TC = c.t_const
    with ExitStack() as st:
        sb = lambda name, shape, dt: st.enter_context(nc.sbuf_tensor(name, shape, dt))
        wpa = sb("p4wpa", [128, 8, D], BF16)
        wpg = sb("p4wpg", [128, 8, D], BF16)
        wout = sb("p4wout", [128, 8, D], BF16)
        wr = sb("p4wr", [128, 8, NE], BF16)
        t_w = Tok()
        for (dst, src) in ((wpa, c.w_proj_attn), (wpg, c.w_proj_gdn), (wout, c.w_out)):
            for kc in range(8):
                S.dma("pool", lambda e, dst=dst, src=src, kc=kc: e.dma_start(out=dst[:, kc, :], in_=src[kc * 128:(kc + 1) * 128, :]),
                      writes=[Tok()])
        S.dma("pool", lambda e: e.dma_start(out=wr[:], in_=c.w_router.rearrange("(kc p) n -> p kc n", p=128)), writes=[t_w])
        S.barrier()
        n2w = sb("p4n2w", [128, D], F32)
        t_n2w = Tok()
        S.dma("sp", lambda e: e.dma_start(out=n2w[:], in_=c.norm2_w.broadcast_to([128, D])), writes=[t_n2w])
        aff = sb("p4aff", [128, NT, NE], F32)
        t_aff = Tok()
        oar = _mk_ring(st, nc, "p4oa", 2, [128, 8, 512], BF16)
        ogr = _mk_ring(st, nc, "p4og", 2, [128, 8, 512], BF16)
        sgar = _mk_ring(st, nc, "p4sga", 2, [128, 8, 512], BF16)
        sggr = _mk_ring(st, nc, "p4sgg", 2, [128, 8, 512], BF16)
        mgr = _mk_ring(st, nc, "p4mg", 2, [128, 8, 512], BF16)
        f1 = _mk_ring(st, nc, "p4f1", 2, [128, 512], F32)
        f2 = _mk_ring(st, nc, "p4f2", 2, [128, 512], F32)
        xr = _mk_ring(st, nc, "p4x", 2, [128, D], F32)
        sqr = _mk_ring(st, nc, "p4sq", 2, [128, D], BF16)
        hr = _mk_ring(st, nc, "p4h", 2, [128, D], BF16)
        hTr = _mk_ring(st, nc, "p4hT", 2, [128, 8, 128], BF16)
        ssr = _mk_ring(st, nc, "p4ss", 4, [128, 4], F32)
        lgr = _mk_ring(st, nc, "p4lg", 2, [128, NE], F32)
        psm = _mk_ring(st, nc, "p4psm", 6, [128, 512], F32, psum=True)
        pst = _mk_ring(st, nc, "p4pst", 2, [128, 8, 128], BF16, psum=True)

        for g in range(NG):
            gs = slice(g * 512, (g + 1) * 512)
            oa, t_oa = oar.next()
            og, t_og = ogr.next()
            sga, t_sga = sgar.next()
            sgg, t_sgg = sggr.next()
            for (buf, tk, src, dep) in ((oa, t_oa, c.oaT_d, c.t_oa), (og, t_og, c.ogT_d, c.t_og),
                                        (sga, t_sga, c.sga_d, c.t_gates), (sgg, t_sgg, c.sgg_d, c.t_gates)):
                S.dma("sp", lambda e, buf=buf, src=src, gs=gs: e.dma_start(out=buf[:], in_=src[:, :, gs].rearrange("h p t -> p h t")),
                      reads=[dep], writes=[tk])
            mg, t_mg = mgr.next()
            for dc in range(8):
                ds = slice(dc * 128, (dc + 1) * 128)
                pa, t_pa = psm.next()
                pg, t_pg = psm.next()
                for ec in range(8):
                    S.op("pe", lambda e, pa=pa, ec=ec, ds=ds, oa=oa: e.matmul(pa[:], wpa[:, ec, ds], oa[:, ec, :], start=(ec == 0), stop=(ec == 7)),
                         reads=[t_oa], writes=[t_pa])
                for ec in range(8):
                    S.op("pe", lambda e, pg=pg, ec=ec, ds=ds, og=og: e.matmul(pg[:], wpg[:, ec, ds], og[:, ec, :], start=(ec == 0), stop=(ec == 7)),
                         reads=[t_og], writes=[t_pg])
                a, t_a = f1.next()
                b, t_b = f2.next()
                S.op("dve", lambda e, a=a, pa=pa, sga=sga, dc=dc: e.tensor_tensor(out=a[:], in0=pa[:], in1=sga[:, dc, :], op=ALU.mult),
                     reads=[t_pa, t_sga], writes=[t_a])
                S.op("dve", lambda e, b=b, pg=pg, sgg=sgg, dc=dc: e.tensor_tensor(out=b[:], in0=pg[:], in1=sgg[:, dc, :], op=ALU.mult),
                     reads=[t_pg, t_sgg], writes=[t_b])
                S.op("pool", lambda e, mg=mg, a=a, b=b, dc=dc: e.tensor_tensor(out=mg[:, dc, :], in0=a[:], in1=b[:], op=ALU.add),
                     reads=[t_a, t_b], writes=[t_mg])
            for tl in range(4):
                i = g * 4 + tl
                xt, t_x = xr.next()
                S.dma("sp", lambda e, xt=xt, i=i: e.dma_start(out=xt[:], in_=c.x[i * 128:(i + 1) * 128, :]), writes=[t_x])
                for dh in range(2):
                    po, t_po = psm.next()
                    for dc in range(8):
                        S.op("pe", lambda e, po=po, mg=mg, dc=dc, tl=tl, dh=dh: e.matmul(
                            po[:], mg[:, dc, tl * 128:(tl + 1) * 128], wout[:, dc, dh * 512:(dh + 1) * 512], start=(dc == 0), stop=(dc == 7)),
                            reads=[t_mg], writes=[t_po])
                    S.op("dve", lambda e, xt=xt, po=po, dh=dh: e.tensor_tensor(out=xt[:, dh * 512:(dh + 1) * 512], in0=po[:],
                                                                              in1=xt[:, dh * 512:(dh + 1) * 512], op=ALU.add),
                         reads=[t_po, t_x], writes=[t_x])
                S.dma("sp", lambda e, xt=xt, i=i: e.dma_start(out=c.x1_d[i * 128:(i + 1) * 128, :], in_=xt[:]), reads=[t_x], writes=[c.t_x1])
                sq, t_sq = sqr.next()
                ss, t_ss = ssr.next()
                S.op("act", lambda e, xt=xt, sq=sq, ss=ss: e.activation(sq[:], xt[:], AF.Square, accum_out=ss[:, 0:1]),
                     reads=[t_x], writes=[t_sq, t_ss])
                S.op("act", lambda e, ss=ss: e.activation(ss[:, 1:2], ss[:, 0:1], AF.Sqrt, bias=EPS, scale=1.0 / D), reads=[t_ss], writes=[t_ss])
                S.op("dve", lambda e, ss=ss: e.reciprocal(ss[:, 1:2], ss[:, 1:2]), reads=[t_ss], writes=[t_ss])
                ht, t_h = hr.next()
                S.op("dve", lambda e, ht=ht, xt=xt, ss=ss: e.scalar_tensor_tensor(
                    out=ht[:], in0=xt[:], scalar=ss[:, 1:2], in1=n2w[:], op0=ALU.mult, op1=ALU.mult),
                    reads=[t_x, t_ss, t_n2w], writes=[t_h])
                S.dma("sp", lambda e, ht=ht, i=i: e.dma_start(out=c.h2tok_d[i * 128:(i + 1) * 128, :], in_=ht[:]), reads=[t_h], writes=[c.t_h2])
                pt, t_pt = pst.next()
                for dc in range(8):
                    S.op("pe", lambda e, pt=pt, ht=ht, dc=dc: e.transpose(pt[:, dc, :], ht[:, dc * 128:(dc + 1) * 128], c.ident_b[:]),
                         reads=[t_h, TC], writes=[t_pt])
                hT, t_hT = hTr.next()
                S.op("act", lambda e, hT=hT, pt=pt: e.copy(hT[:], pt[:]), reads=[t_pt], writes=[t_hT])
                pl, t_pl = psm.next()
                for dc in range(8):
                    S.op("pe", lambda e, pl=pl, hT=hT, dc=dc: e.matmul(pl[:, 0:NE], hT[:, dc, :], wr[:, dc, :], start=(dc == 0), stop=(dc == 7)),
                         reads=[t_hT, t_w], writes=[t_pl])
                lg, t_lg = lgr.next()
                S.op("dve", lambda e, pl=pl, ss=ss: e.tensor_reduce(out=ss[:, 2:3], in_=pl[:, 0:NE], axis=mybir.AxisListType.X, op=ALU.max),
                     reads=[t_pl, t_ss], writes=[t_ss])
                S.op("dve", lambda e, ss=ss: e.tensor_scalar(out=ss[:, 2:3], in0=ss[:, 2:3], scalar1=-1.0, scalar2=None, op0=ALU.mult),
                     reads=[t_ss], writes=[t_ss])
                S.op("act", lambda e, lg=lg, pl=pl, ss=ss: e.activation(lg[:], pl[:, 0:NE], AF.Exp, bias=ss[:, 2:3], accum_out=ss[:, 3:4]),
                     reads=[t_pl, t_ss], writes=[t_lg, t_ss])
                S.op("dve", lambda e, ss=ss: e.reciprocal(ss[:, 3:4], ss[:, 3:4]), reads=[t_ss], writes=[t_ss])
                S.op("dve", lambda e, lg=lg, ss=ss, i=i: e.tensor_scalar(out=aff[:, i, :], in0=lg[:], scalar1=ss[:, 3:4], scalar2=None, op0=ALU.mult),
                     reads=[t_lg, t_ss], writes=[t_aff])
        S.dma("sp", lambda e: e.dma_start(out=c.aff_d, in_=aff[:]), reads=[t_aff], writes=[c.t_aff])
    S.barrier()


def phase5(c):
    nc, S = c.nc, c.S
    TC = c.t_const
    with ExitStack() as st:
        sb = lambda name, shape, dt: st.enter_context(nc.sbuf_tensor(name, shape, dt))
        aff = sb("p5aff", [128, NT, NE], F32)
        t_aff = Tok()
        S.dma("sp", lambda e: e.dma_start(out=aff[:], in_=c.aff_d), reads=[c.t_aff], writes=[t_aff])
        affT = sb("p5affT", [NE, S_LEN], F32)
        work = sb("p5work", [NE, S_LEN], F32)
        ones = sb("p5ones", [NE, S_LEN], F32)
        mx = sb("p5mx", [NE, 8], F32)
        t_affT, t_work, t_mx, t_ones = Tok(), Tok(), Tok(), Tok()
        psm = _mk_ring(st, nc, "p5psm", 4, [128, 512], F32, psum=True)
        for i4 in range(NT // 4):
            ps, t_ps = psm.next()
            for ii in range(4):
                i = i4 * 4 + ii
                S.op("pe", lambda e, ps=ps, ii=ii, i=i: e.transpose(ps[0:NE, ii * 128:(ii + 1) * 128], aff[:, i, :], c.ident_f[:]),
                     reads=[t_aff, TC], writes=[t_ps])
            S.op("act", lambda e, ps=ps, i4=i4: e.copy(affT[:, i4 * 512:(i4 + 1) * 512], ps[0:NE, :]), reads=[t_ps], writes=[t_affT])
        S.op("pool", lambda e: e.memset(ones[:], 1.0), writes=[t_ones])
        src = affT
        for it in range(CAP // 8):
            S.op("dve", lambda e, src=src: e.max(out=mx[:], in_=src[:]), reads=[t_affT, t_work], writes=[t_mx])
            if it < CAP // 8 - 1:
                S.op("dve", lambda e, src=src: e.match_replace(out=work[:], in_to_replace=mx[:], in_values=src[:], imm_value=-1.0),
                     reads=[t_mx, t_affT, t_work], writes=[t_work])
            src = work
        S.op("dve", lambda e: e.tensor_scalar(out=work[:], in0=affT[:], scalar1=mx[:, 7:8], scalar2=None, op0=ALU.is_ge),
             reads=[t_affT, t_mx, t_work], writes=[t_work])
        S.op("dve", lambda e: e.tensor_tensor_scan(out=affT[:], data0=ones[:], data1=work[:], initial=0.0, op0=ALU.mult, op1=ALU.add),
             reads=[t_work, t_ones, t_affT], writes=[t_affT])
        S.op("dve", lambda e: e.tensor_tensor(out=affT[:], in0=affT[:], in1=work[:], op=ALU.mult), reads=[t_work, t_affT], writes=[t_affT])
        S.op("dve", lambda e: e.tensor_scalar(out=affT[:], in0=affT[:], scalar1=-1.0, scalar2=None, op0=ALU.add), reads=[t_affT], writes=[t_affT])
        S.dma("sp", lambda e: e.dma_start(out=c.pos_d, in_=affT[:]), reads=[t_affT], writes=[c.t_pos])
        ptok = sb("p5ptok", [128, NT, NE], F32)
        t_ptok = Tok()
        for i4 in range(NT // 4):
            ps, t_ps = psm.next()
            for ii in range(4):
                i = i4 * 4 + ii
                S.op("pe", lambda e, ps=ps, ii=ii, i=i: e.transpose(ps[:, ii * NE:(ii + 1) * NE], affT[:, i * 128:(i + 1) * 128], c.ident_f[0:NE, 0:NE]),
                     reads=[t_affT, TC], writes=[t_ps])
            S.op("act", lambda e, ps=ps, i4=i4: e.copy(ptok[:, i4 * 4:(i4 + 1) * 4, :], ps[:, 0:4 * NE].rearrange("p (a b) -> p a b", b=NE)),
                 reads=[t_ps], writes=[t_ptok])
        S.dma("sp", lambda e: e.dma_start(out=c.ptok_d, in_=ptok[:]), reads=[t_ptok], writes=[c.t_pos])
    S.barrier()


def phase6a(c):
    nc, S = c.nc, c.S
    TC = c.t_const
    with ExitStack() as st:
        sb = lambda name, shape, dt: st.enter_context(nc.sbuf_tensor(name, shape, dt))
        ptok = sb("p6ptok", [128, NT, NE], F32)
        t_ptok = Tok()
        S.dma("sp", lambda e: e.dma_start(out=ptok[:], in_=c.ptok_d), reads=[c.t_pos], writes=[t_ptok])
        iota = sb("p6iota", [128, CAP], F32)
        t_iota = Tok()
        S.op("pool", lambda e: e.iota(iota[:], pattern=[[1, CAP]], base=0, channel_multiplier=0, allow_small_or_imprecise_dtypes=True),
             writes=[t_iota])
        sel = sb("p6sel", [128, NT, CAP], BF16)
        t_sel = Tok()
        xsT = sb("p6xsT", [128, 8, CAP], BF16)
        t_xsT = Tok()
        actT = sb("p6actT", [128, 16, CAP], BF16)
        t_actT = Tok()
        yes = sb("p6ye", [128, 4, D], BF16)
        t_yes = Tok()
        h2r = _mk_ring(st, nc, "p6h2", 4, [128, D], BF16)
        wgr = _mk_ring(st, nc, "p6wg", 2, [128, 8, 1024], BF16)
        wur = _mk_ring(st, nc, "p6wu", 2, [128, 8, 1024], BF16)
        wdr = _mk_ring(st, nc, "p6wd", 2, [128, 8, D], BF16)
        gfr = _mk_ring(st, nc, "p6gf", 2, [128, CAP], F32)
        wst = _mk_ring(st, nc, "p6wst", 6, [128, 1024], F32)
        psm = _mk_ring(st, nc, "p6psm", 8, [128, 512], F32, psum=True)

        def load_gu(ex):
            bufs = []
            for fh in range(2):
                wg, t_wg = wgr.next()
                wu, t_wu = wur.next()
                for kc in range(8):
                    for (wdst, t_wdst, wsrc) in ((wg, t_wg, c.w_gate), (wu, t_wu, c.w_up)):
                        stg, t_stg = wst.next()
                        S.dma("sp", lambda e, stg=stg, wsrc=wsrc, kc=kc, ex=ex, fh=fh: e.dma_start(
                            out=stg[:], in_=wsrc[ex, kc * 128:(kc + 1) * 128, fh * 1024:(fh + 1) * 1024]), writes=[t_stg])
                        S.op("act", lambda e, stg=stg, wdst=wdst, kc=kc: e.copy(wdst[:, kc, :], stg[:]), reads=[t_stg], writes=[t_wdst])
                bufs.append(((wg, t_wg), (wu, t_wu)))
            return bufs

        def load_wd(ex):
            bufs = []
            for fh in range(2):
                wd, t_wd = wdr.next()
                for fl in range(8):
                    fc = fh * 8 + fl
                    stg, t_stg = wst.next()
                    S.dma("sp", lambda e, stg=stg, fc=fc, ex=ex: e.dma_start(out=stg[:], in_=c.w_down[ex, fc * 128:(fc + 1) * 128, :]),
                          writes=[t_stg])
                    S.op("act", lambda e, stg=stg, wd=wd, fl=fl: e.copy(wd[:, fl, :], stg[:]), reads=[t_stg], writes=[t_wd])
                bufs.append((wd, t_wd))
            return bufs

        gu_bufs = None
        for ex in range(NE):
            if ex == 0:
                gu_bufs = load_gu(0)
            wd_bufs = load_wd(ex)
            for i in range(NT):
                eng = "dve" if i % 2 == 0 else "pool"
                S.op(eng, lambda e, i=i, ex=ex: e.tensor_scalar(out=sel[:, i, :], in0=iota[:], scalar1=ptok[:, i, ex:ex + 1], scalar2=None,
                                                                 op0=ALU.is_equal), reads=[t_iota, t_ptok], writes=[t_sel])
            for half in range(2):
                accs = [psm.next() for _ in range(4)]
                for i in range(NT):
                    ht, t_h = h2r.next()
                    S.dma("pool", lambda e, ht=ht, i=i: e.dma_start(out=ht[:], in_=c.h2tok_d[i * 128:(i + 1) * 128, :]), reads=[c.t_h2], writes=[t_h])
                    for dl in range(4):
                        dc = half * 4 + dl
                        ps, t_ps = accs[dl]
                        S.op("pe", lambda e, ps=ps, ht=ht, dc=dc, i=i: e.matmul(ps[:], ht[:, dc * 128:(dc + 1) * 128], sel[:, i, :],
                                                                               start=(i == 0), stop=(i == NT - 1)),
                             reads=[t_h, t_sel], writes=[t_ps])
                for dl in range(4):
                    dc = half * 4 + dl
                    ps, t_ps = accs[dl]
                    S.op("act", lambda e, ps=ps, dc=dc: e.copy(xsT[:, dc, :], ps[:]), reads=[t_ps], writes=[t_xsT])
            for fh in range(2):
                (wg, t_wg), (wu, t_wu) = gu_bufs[fh]
                for fl in range(8):
                    fc = fh * 8 + fl
                    fs = slice(fl * 128, (fl + 1) * 128)
                    pg, t_pg = psm.next()
                    pu, t_pu = psm.next()
                    for kc in range(8):
                        S.op("pe", lambda e, pg=pg, wg=wg, kc=kc, fs=fs: e.matmul(pg[:], wg[:, kc, fs], xsT[:, kc, :], start=(kc == 0), stop=(kc == 7)),
                             reads=[t_wg, t_xsT], writes=[t_pg])
                    for kc in range(8):
                        S.op("pe", lambda e, pu=pu, wu=wu, kc=kc, fs=fs: e.matmul(pu[:], wu[:, kc, fs], xsT[:, kc, :], start=(kc == 0), stop=(kc == 7)),
                             reads=[t_wu, t_xsT], writes=[t_pu])
                    gf, t_gf = gfr.next()
                    S.op("act", lambda e, gf=gf, pg=pg: e.activation(gf[:], pg[:], AF.Silu), reads=[t_pg], writes=[t_gf])
                    S.op("dve", lambda e, gf=gf, pu=pu, fc=fc: e.tensor_tensor(out=actT[:, fc, :], in0=pu[:], in1=gf[:], op=ALU.mult),
                         reads=[t_pu, t_gf], writes=[t_actT])
            if ex + 1 < NE:
                gu_bufs = load_gu(ex + 1)
            accs = [psm.next() for _ in range(8)]
            for fh in range(2):
                wd, t_wd = wd_bufs[fh]
                for fl in range(8):
                    fc = fh * 8 + fl
                    for sc in range(4):
                        for dh in range(2):
                            ps, t_ps = accs[sc * 2 + dh]
                            S.op("pe", lambda e, ps=ps, fc=fc, sc=sc, wd=wd, fl=fl, dh=dh: e.matmul(
                                ps[:], actT[:, fc, sc * 128:(sc + 1) * 128], wd[:, fl, dh * 512:(dh + 1) * 512], start=(fc == 0), stop=(fc == 15)),
                                reads=[t_actT, t_wd], writes=[t_ps])
            for sc in range(4):
                for dh in range(2):
                    ps, t_ps = accs[sc * 2 + dh]
                    eng = "act" if dh == 0 else "dve"
                    if eng == "act":
                        S.op("act", lambda e, ps=ps, sc=sc, dh=dh: e.copy(yes[:, sc, dh * 512:(dh + 1) * 512], ps[:]), reads=[t_ps], writes=[t_yes])
                    else:
                        S.op("dve", lambda e, ps=ps, sc=sc, dh=dh: e.tensor_copy(yes[:, sc, dh * 512:(dh + 1) * 512], ps[:]), reads=[t_ps], writes=[t_yes])
            S.dma("sp", lambda e, ex=ex: e.dma_start(out=c.ye_d[ex].rearrange("(sc p) d -> p sc d", p=128), in_=yes[:]),
                  reads=[t_yes], writes=[c.t_ye])
    S.barrier()


def phase6b(c):
    nc, S = c.nc, c.S
    TC = c.t_const
    with ExitStack() as st:
        sb = lambda name, shape, dt: st.enter_context(nc.sbuf_tensor(name, shape, dt))
        yall = sb("p7ye", [128, NE, 4, D], BF16)
        t_yall = Tok()
        for ex in range(NE):
            S.dma("sp", lambda e, ex=ex: e.dma_start(out=yall[:, ex, :, :], in_=c.ye_d[ex].rearrange("(sc p) d -> p sc d", p=128)),
                  reads=[c.t_ye], writes=[Tok()])
        S.barrier()
        aff = sb("p7aff", [128, NT, NE], F32)
        t_aff = Tok()
        S.dma("sp", lambda e: e.dma_start(out=aff[:], in_=c.aff_d), reads=[c.t_aff], writes=[t_aff])
        nfw = sb("p7nfw", [128, D], F32)
        t_nfw = Tok()
        S.dma("sp", lambda e: e.dma_start(out=nfw[:], in_=c.norm_f_w.broadcast_to([128, D])), writes=[t_nfw])
        pidx = sb("p7pidx", [128, 4], F32)
        t_pidx = Tok()
        S.op("pool", lambda e: e.iota(pidx[:], pattern=[[128, 4]], base=0, channel_multiplier=1, allow_small_or_imprecise_dtypes=True),
             writes=[t_pidx])
        posbc = sb("p7posbc", [128, NE, 512], F32)
        t_posbc = Tok()
        selT = _mk_ring(st, nc, "p7selT", 8, [128, 512], BF16)
        accr = _mk_ring(st, nc, "p7acc", 4, [128, D], F32)
        sqr = _mk_ring(st, nc, "p7sq", 2, [128, D], BF16)
        ssr = _mk_ring(st, nc, "p7ss", 4, [128, 2], F32)
        psm = _mk_ring(st, nc, "p7psm", 6, [128, 512], F32, psum=True)
        for g in range(NG):
            gs = slice(g * 512, (g + 1) * 512)
            S.dma("sp", lambda e, gs=gs: e.dma_start(out=posbc[:], in_=c.pos_d[:, gs].unsqueeze(0).broadcast_to([128, NE, 512])),
                  reads=[c.t_pos], writes=[t_posbc])
            accs = []
            for tl in range(4):
                i = g * 4 + tl
                acc, t_acc = accr.next()
                S.dma("sp", lambda e, acc=acc, i=i: e.dma_start(out=acc[:], in_=c.x1_d[i * 128:(i + 1) * 128, :]), reads=[c.t_x1], writes=[t_acc])
                accs.append((acc, t_acc))
            def build_sts(ex):
                sts = []
                for sc in range(4):
                    sT, t_sT = selT.next()
                    eng = "dve" if sc % 2 == 0 else "pool"
                    S.op(eng, lambda e, sT=sT, ex=ex, sc=sc: e.tensor_scalar(out=sT[:], in0=posbc[:, ex, :], scalar1=pidx[:, sc:sc + 1], scalar2=None,
                                                                            op0=ALU.is_equal), reads=[t_posbc, t_pidx], writes=[t_sT])
                    sts.append((sT, t_sT))
                return sts
            nxt_sts = build_sts(0)
            for ex in range(NE):
                sts = nxt_sts
                if ex + 1 < NE:
                    nxt_sts = build_sts(ex + 1)
                for tl in range(4):
                    i = g * 4 + tl
                    acc, t_acc = accs[tl]
                    for dh in range(2):
                        ps, t_ps = psm.next()
                        for sc in range(4):
                            sT, t_sT = sts[sc]
                            S.op("pe", lambda e, ps=ps, sT=sT, tl=tl, ex=ex, sc=sc, dh=dh: e.matmul(
                                ps[:], sT[:, tl * 128:(tl + 1) * 128], yall[:, ex, sc, dh * 512:(dh + 1) * 512], start=(sc == 0), stop=(sc == 3)),
                                reads=[t_sT], writes=[t_ps])
                        S.op("dve", lambda e, acc=acc, ps=ps, i=i, ex=ex, dh=dh: e.scalar_tensor_tensor(
                            out=acc[:, dh * 512:(dh + 1) * 512], in0=ps[:], scalar=aff[:, i, ex:ex + 1], in1=acc[:, dh * 512:(dh + 1) * 512],
                            op0=ALU.mult, op1=ALU.add), reads=[t_ps, t_aff, t_acc], writes=[t_acc])
            for tl in range(4):
                i = g * 4 + tl
                acc, t_acc = accs[tl]
                sq, t_sq = sqr.next()
                ss, t_ss = ssr.next()
                S.op("act", lambda e, acc=acc, sq=sq, ss=ss: e.activation(sq[:], acc[:], AF.Square, accum_out=ss[:, 0:1]),
                     reads=[t_acc], writes=[t_sq, t_ss])
                S.op("act", lambda e, ss=ss: e.activation(ss[:, 1:2], ss[:, 0:1], AF.Sqrt, bias=EPS, scale=1.0 / D), reads=[t_ss], writes=[t_ss])
                S.op("dve", lambda e, ss=ss: e.reciprocal(ss[:, 1:2], ss[:, 1:2]), reads=[t_ss], writes=[t_ss])
                S.op("dve", lambda e, acc=acc, ss=ss: e.scalar_tensor_tensor(
                    out=acc[:], in0=acc[:], scalar=ss[:, 1:2], in1=nfw[:], op0=ALU.mult, op1=ALU.mult),
                    reads=[t_acc, t_ss, t_nfw], writes=[t_acc])
                S.dma("sp", lambda e, acc=acc, i=i: e.dma_start(out=c.out[i * 128:(i + 1) * 128, :], in_=acc[:]), reads=[t_acc], writes=[c.t_out])
    S.barrier()


ALL_PHASES = ("p1", "p2", "p3", "p4", "p5", "p6a", "p6b")


def kernel(**inputs):
    n_cores = 8
    shared = host_prep(inputs)
    nc, c = build_program(debug=False, phases=ALL_PHASES)
    x = np.asarray(inputs["x"], dtype=np.float32)
    in_maps = []
    for b in range(n_cores):
        m = dict(shared)
        m["x"] = np.ascontiguousarray(x[b])
        in_maps.append({k: m[k] for k in c.input_names})
    res = run_bass_kernel_spmd(nc, in_maps, core_ids=list(range(n_cores)))
    out = np.stack([np.asarray(r["out"], dtype=np.float32) for r in res.results], axis=0)
    return out
```

```python
import math
from contextlib import ExitStack
import numpy as np
import concourse.bass as bass
import concourse.mybir as mybir
from concourse.bass_utils import run_bass_kernel_spmd

F32 = mybir.dt.float32
BF16 = mybir.dt.bfloat16
AF = mybir.ActivationFunctionType
ALU = mybir.AluOpType

S_LEN = 4096
D = 1024
NT = S_LEN // 128
NG = S_LEN // 512
H = 8
IN_W = 9248
C_QA, C_KA, C_VA, C_GQ, C_GK, C_GV, C_Z, C_SM, C_GA, C_GG = 0, 1024, 2048, 3072, 4096, 5120, 6144, 7168, 7200, 8224
NE = 16
FF = 2048
CAP = 512
EPS = 1e-6
LAMBDA_INIT = 0.8 - 0.6 * math.exp(-0.3 * 0)

ENGS = ("pe", "act", "dve", "pool", "sp")


class Tok:
    __slots__ = ("w", "r", "excl")

    def __init__(self, excl=False):
        self.w = None
        self.r = {}
        self.excl = excl


class Sched:
    NDMA = 32

    def __init__(self, nc):
        self.nc = nc
        self.sems = {}
        for e in ENGS:
            self.sems[e] = nc.alloc_semaphore(name="sem_" + e)
        for i in range(self.NDMA):
            self.sems[("d", i)] = nc.alloc_semaphore(name="sem_dma%d" % i)
        self.cnt = {k: 0 for k in self.sems}
        self.seen = {e: {} for e in ENGS}
        self.prog = {e: [] for e in ENGS}
        self.ndma = 0
        self.ninstr = 0

    def _deps(self, reads, writes):
        deps = {}
        for t in reads:
            if t.w is not None:
                k, v = t.w
                if deps.get(k, 0) < v:
                    deps[k] = v
        for t in writes:
            if t.w is not None:
                k, v = t.w
                if deps.get(k, 0) < v:
                    deps[k] = v
            for k, v in t.r.items():
                if deps.get(k, 0) < v:
                    deps[k] = v
        return deps

    def _emit_waits(self, e, deps):
        seen = self.seen[e]
        for k, v in deps.items():
            if seen.get(k, 0) >= v:
                continue
            seen[k] = v
            sem = self.sems[k]
            self.prog[e].append(lambda eng, sem=sem, v=v: eng.wait_ge(sem, v))

    def op(self, e, fn, reads=(), writes=()):
        if any(t.excl for t in reads):
            writes = list(writes) + [t for t in reads if t.excl]
            reads = [t for t in reads if not t.excl]
        deps = self._deps(reads, writes)
        if e == "pe":
            deps.pop("pe", None)
        self._emit_waits(e, deps)
        sem = self.sems[e]
        self.cnt[e] += 1
        n = self.cnt[e]
        self.prog[e].append(lambda eng, fn=fn, sem=sem: fn(eng).then_inc(sem, 1))
        for t in reads:
            if t.r.get(e, 0) < n:
                t.r[e] = n
        for t in writes:
            t.w = (e, n)
            t.r = {}
        self.ninstr += 1

    def dma(self, e, fn, reads=(), writes=()):
        i = self.ndma % self.NDMA
        self.ndma += 1
        k = ("d", i)
        deps = self._deps(reads, writes)
        if self.cnt[k] > 0:
            deps[k] = max(deps.get(k, 0), self.cnt[k])
        self._emit_waits(e, deps)
        self.cnt[k] += 16
        v = self.cnt[k]
        sem = self.sems[k]
        self.prog[e].append(lambda eng, fn=fn, sem=sem: fn(eng).then_inc(sem, 16))
        for t in reads:
            if t.r.get(k, 0) < v:
                t.r[k] = v
        for t in writes:
            t.w = (k, v)
            t.r = {}
        self.ninstr += 1

    def barrier(self):
        deps = {k: v for k, v in self.cnt.items() if v > 0}
        for e in ENGS:
            self._emit_waits(e, dict(deps))

    def emit(self):
        nc = self.nc
        prog = self.prog
        with nc.Block() as block:
            @block.tensor
            def _(eng):
                for f in prog["pe"]:
                    f(eng)

            @block.scalar
            def _(eng):
                for f in prog["act"]:
                    f(eng)

            @block.vector
            def _(eng):
                for f in prog["dve"]:
                    f(eng)

            @block.gpsimd
            def _(eng):
                for f in prog["pool"]:
                    f(eng)

            @block.sync
            def _(eng):
                for f in prog["sp"]:
                    f(eng)


class Ring:
    def __init__(self, items):
        self.items = items
        self.i = 0

    def next(self):
        it = self.items[self.i % len(self.items)]
        self.i += 1
        return it


class Ctx:
    pass


def _interleave(gens, width):
    it = iter(gens)
    active = []
    exhausted = False
    while True:
        while len(active) < width and not exhausted:
            try:
                active.append(next(it))
            except StopIteration:
                exhausted = True
        if not active:
            break
        for g in list(active):
            try:
                next(g)
            except StopIteration:
                active.remove(g)


def _mk_ring(stack, nc, name, n, shape, dt, psum=False):
    items = []
    for i in range(n):
        if psum:
            t = stack.enter_context(nc.psum_tensor("%s%d" % (name, i), shape, dt))
        else:
            t = stack.enter_context(nc.sbuf_tensor("%s%d" % (name, i), shape, dt))
        items.append((t, Tok()))
    return Ring(items)


def _consts(c, stack):
    nc, S = c.nc, c.S
    c.ident_f = nc.alloc_sbuf_tensor("ident_f", [128, 128], F32)
    c.ident_b = nc.alloc_sbuf_tensor("ident_b", [128, 128], BF16)
    c.ones_b = nc.alloc_sbuf_tensor("ones_b", [128, 128], BF16)
    c.ones_f = nc.alloc_sbuf_tensor("ones_f", [128, 128], F32)
    c.t_const = Tok()
    tc = c.t_const
    S.op("pool", lambda e: e.memset(c.ident_f[:], 0.0), writes=[tc])
    S.op("pool", lambda e: e.affine_select(out=c.ident_f[:], in_=c.ident_f[:], compare_op=ALU.not_equal, fill=1.0,
                                           base=0, pattern=[[-1, 128]], channel_multiplier=1),
         reads=[tc], writes=[tc])
    S.op("pool", lambda e: e.tensor_copy(c.ident_b[:], c.ident_f[:]), reads=[tc], writes=[tc])
    S.op("pool", lambda e: e.memset(c.ones_b[:], 1.0), writes=[tc])
    S.op("pool", lambda e: e.memset(c.ones_f[:], 1.0), writes=[tc])


def phase1(c):
    nc, S = c.nc, c.S
    TC = c.t_const
    with ExitStack() as st:
        sb = lambda name, shape, dt: st.enter_context(nc.sbuf_tensor(name, shape, dt))
        hT = sb("hT", [128, 8, S_LEN], BF16)
        t_hT = [Tok() for _ in range(NT)]
        stA = ExitStack()
        sbA = lambda name, shape, dt: stA.enter_context(nc.sbuf_tensor(name, shape, dt))
        n1w = sbA("n1w", [128, D], F32)
        t_n1w = Tok()
        S.dma("sp", lambda e: e.dma_start(out=n1w[:], in_=c.norm1_w.broadcast_to([128, D])), writes=[t_n1w])
        xring = _mk_ring(stA, nc, "p1x", 3, [128, D], F32)
        sqring = _mk_ring(stA, nc, "p1sq", 2, [128, D], BF16)
        hring = _mk_ring(stA, nc, "p1h", 2, [128, D], BF16)
        ssring = _mk_ring(stA, nc, "p1ss", 4, [128, 2], F32)
        pst = _mk_ring(st, nc, "p1pst", 2, [128, 8, 128], BF16, psum=True)
        psm = _mk_ring(st, nc, "p1psm", 5, [128, 512], F32, psum=True)

        for i in range(NT):
            xt, t_x = xring.next()
            S.dma("sp", lambda e, xt=xt, i=i: e.dma_start(out=xt[:], in_=c.x[i * 128:(i + 1) * 128, :]), writes=[t_x])
            sq, t_sq = sqring.next()
            ss, t_ss = ssring.next()
            S.op("act", lambda e, xt=xt, sq=sq, ss=ss: e.activation(sq[:], xt[:], AF.Square, accum_out=ss[:, 0:1]),
                 reads=[t_x], writes=[t_sq, t_ss])
            S.op("act", lambda e, ss=ss: e.activation(ss[:, 1:2], ss[:, 0:1], AF.Sqrt, bias=EPS, scale=1.0 / D),
                 reads=[t_ss], writes=[t_ss])
            S.op("dve", lambda e, ss=ss: e.reciprocal(ss[:, 1:2], ss[:, 1:2]), reads=[t_ss], writes=[t_ss])
            ht, t_h = hring.next()
            S.op("dve", lambda e, ht=ht, xt=xt, ss=ss: e.scalar_tensor_tensor(
                out=ht[:], in0=xt[:], scalar=ss[:, 1:2], in1=n1w[:], op0=ALU.mult, op1=ALU.mult),
                reads=[t_x, t_ss, t_n1w], writes=[t_h])
            pt, t_pt = pst.next()
            for dc in range(8):
                S.op("pe", lambda e, pt=pt, ht=ht, dc=dc: e.transpose(pt[:, dc, :], ht[:, dc * 128:(dc + 1) * 128], c.ident_b[:]),
                     reads=[t_h, TC], writes=[t_pt])
            S.op("act", lambda e, pt=pt, i=i: e.copy(hT[:, :, i * 128:(i + 1) * 128], pt[:]),
                 reads=[t_pt], writes=[t_hT[i]])

        S.barrier()
        stA.close()
        wfm = _mk_ring(st, nc, "p1wfm", 4, [128, 8, 128], BF16)
        wtm = _mk_ring(st, nc, "p1wtm", 2, [128, 8, 512], BF16)

        def load_w(ring, src, col0, ncols):
            wt, t_w = ring.next()
            S.dma("pool", lambda e: e.dma_start(
                out=wt[:, :, 0:ncols], in_=src[:, col0:col0 + ncols].rearrange("(kc p) n -> p kc n", p=128)),
                writes=[t_w])
            return wt, t_w

        def proj_fm(wt, t_w, g):
            ps, t_ps = psm.next()
            for kc in range(8):
                S.op("pe", lambda e, ps=ps, kc=kc: e.matmul(ps[:], wt[:, kc, :], hT[:, kc, g * 512:(g + 1) * 512],
                                                          start=(kc == 0), stop=(kc == 7)),
                     reads=[t_w] + t_hT[g * 4:(g + 1) * 4], writes=[t_ps])
            return ps, t_ps

        def proj_tm(wt, t_w, i, ncols):
            ps, t_ps = psm.next()
            for kc in range(8):
                S.op("pe", lambda e, ps=ps, kc=kc: e.matmul(ps[:, 0:ncols], hT[:, kc, i * 128:(i + 1) * 128], wt[:, kc, 0:ncols],
                                                          start=(kc == 0), stop=(kc == 7)),
                     reads=[t_w, t_hT[i]], writes=[t_ps])
            return ps, t_ps

        stage = _mk_ring(st, nc, "p1stage", 2, [128, S_LEN], BF16)
        f32a = _mk_ring(st, nc, "p1f32a", 3, [128, 512], F32)
        f32b = _mk_ring(st, nc, "p1f32b", 3, [128, 512], F32)

        stB = ExitStack()
        cosT = stB.enter_context(nc.sbuf_tensor("cosT_sb", [128, S_LEN], F32))
        sinT = stB.enter_context(nc.sbuf_tensor("sinT_sb", [128, S_LEN], F32))
        t_rope = Tok()
        S.dma("sp", lambda e: e.dma_start(out=cosT[:], in_=c.cosT), writes=[t_rope])
        S.dma("sp", lambda e: e.dma_start(out=sinT[:], in_=c.sinT), writes=[t_rope])

        for (col0, dst) in ((C_QA, c.qT_d), (C_KA, c.kT_d)):
            for h in range(H):
                w1, t_w1 = load_w(wfm, c.w_in, col0 + h * 128, 128)
                w2, t_w2 = load_w(wfm, c.w_qkp, (col0 // 1024) * 1024 + h * 128, 128)
                stg, t_stg = stage.next()
                for g in range(NG):
                    ps1, t_ps1 = proj_fm(w1, t_w1, g)
                    ps2, t_ps2 = proj_fm(w2, t_w2, g)
                    a, t_a = f32a.next()
                    b, t_b = f32b.next()
                    sl = slice(g * 512, (g + 1) * 512)
                    S.op("dve", lambda e, a=a, ps1=ps1, sl=sl: e.tensor_tensor(out=a[:], in0=ps1[:], in1=cosT[:, sl], op=ALU.mult),
                         reads=[t_ps1, t_rope], writes=[t_a])
                    S.op("dve", lambda e, b=b, ps2=ps2, sl=sl: e.tensor_tensor(out=b[:], in0=ps2[:], in1=sinT[:, sl], op=ALU.mult),
                         reads=[t_ps2, t_rope], writes=[t_b])
                    S.op("pool", lambda e, a=a, b=b, stg=stg, sl=sl: e.tensor_tensor(out=stg[:, sl], in0=a[:], in1=b[:], op=ALU.add),
                         reads=[t_a, t_b], writes=[t_stg])
                S.dma("sp", lambda e, stg=stg, dst=dst, h=h: e.dma_start(out=dst[h], in_=stg[:]), reads=[t_stg], writes=[c.t_qk])

        S.barrier()
        stB.close()
        for (col0, dst) in ((C_GA, c.sga_d), (C_GG, c.sgg_d)):
            for h in range(8):
                w1, t_w1 = load_w(wfm, c.w_in, col0 + h * 128, 128)
                stg, t_stg = stage.next()
                for g in range(NG):
                    ps1, t_ps1 = proj_fm(w1, t_w1, g)
                    sl = slice(g * 512, (g + 1) * 512)
                    S.op("act", lambda e, ps1=ps1, stg=stg, sl=sl: e.activation(stg[:, sl], ps1[:], AF.Sigmoid),
                         reads=[t_ps1], writes=[t_stg])
                S.dma("sp", lambda e, stg=stg, dst=dst, h=h: e.dma_start(out=dst[h], in_=stg[:]), reads=[t_stg], writes=[c.t_gates])

        tmst = _mk_ring(st, nc, "p1tmst", 3, [128, 512], BF16)
        for (col0, dst, fn) in ((C_VA, c.v_d, None), (C_Z, c.zs_d, AF.Silu)):
            for half in range(2):
                wt, t_w = load_w(wtm, c.w_in, col0 + half * 512, 512)
                for i in range(NT):
                    ps, t_ps = proj_tm(wt, t_w, i, 512)
                    o, t_o = tmst.next()
                    if fn is None:
                        S.op("act", lambda e, o=o, ps=ps: e.copy(o[:], ps[:]), reads=[t_ps], writes=[t_o])
                    else:
                        S.op("act", lambda e, o=o, ps=ps, fn=fn: e.activation(o[:], ps[:], fn), reads=[t_ps], writes=[t_o])
                    S.dma("sp", lambda e, o=o, dst=dst, i=i, half=half: e.dma_start(
                        out=dst[i * 128:(i + 1) * 128, half * 512:(half + 1) * 512], in_=o[:]), reads=[t_o], writes=[c.t_vz])

        sm = sb("p1sm", [128, NT, 32], F32)
        t_sm = Tok()
        wt, t_w = load_w(wtm, c.w_in, C_SM, 32)
        for i in range(NT):
            ps, t_ps = proj_tm(wt, t_w, i, 32)
            S.op("dve", lambda e, ps=ps, i=i: e.tensor_copy(sm[:, i, :], ps[:, 0:32]), reads=[t_ps], writes=[t_sm])
        prm = sb("p1prm", [128, 32], F32)
        t_prm = Tok()
        for j, src in enumerate((c.dt_bias_fwd, c.dt_bias_bwd, c.a_log_fwd, c.a_log_bwd)):
            S.dma("sp", lambda e, j=j, src=src: e.dma_start(out=prm[:, j * 8:(j + 1) * 8], in_=src.broadcast_to([128, 8])),
                  writes=[t_prm])
        tmpa = sb("p1tmpa", [128, NT, 16], F32)
        tmpb = sb("p1tmpb", [128, NT, 16], F32)
        t_ta, t_tb = Tok(), Tok()
        dtb = prm[:, 0:16].unsqueeze(1).broadcast_to([128, NT, 16])
        S.op("act", lambda e: e.activation(prm[:, 16:32], prm[:, 16:32], AF.Exp), reads=[t_prm], writes=[t_prm])
        nA = prm[:, 16:32].unsqueeze(1).broadcast_to([128, NT, 16])
        S.op("dve", lambda e: e.tensor_tensor(out=sm[:, :, 0:16], in0=sm[:, :, 0:16], in1=dtb, op=ALU.add),
             reads=[t_sm, t_prm], writes=[t_sm])
        S.op("act", lambda e: e.activation(tmpa[:], sm[:, :, 0:16], AF.Abs), reads=[t_sm], writes=[t_ta])
        S.op("act", lambda e: e.activation(tmpa[:], tmpa[:], AF.Exp, scale=-1.0), reads=[t_ta], writes=[t_ta])
        S.op("act", lambda e: e.activation(tmpa[:], tmpa[:], AF.Ln, bias=1.0), reads=[t_ta], writes=[t_ta])
        S.op("dve", lambda e: e.scalar_tensor_tensor(out=tmpb[:], in0=sm[:, :, 0:16], scalar=0.0, in1=tmpa[:],
                                                     op0=ALU.max, op1=ALU.add), reads=[t_sm, t_ta], writes=[t_tb])
        S.op("dve", lambda e: e.scalar_tensor_tensor(out=sm[:, :, 0:16], in0=tmpb[:], scalar=-1.0, in1=nA,
                                                     op0=ALU.mult, op1=ALU.mult), reads=[t_tb, t_prm], writes=[t_sm])
        S.op("act", lambda e: e.activation(sm[:, :, 16:32], sm[:, :, 16:32], AF.Sigmoid), reads=[t_sm], writes=[t_sm])
        S.dma("sp", lambda e: e.dma_start(out=c.small_d, in_=sm[:]), reads=[t_sm], writes=[c.t_small])

        cw5 = sb("p1cw5", [5, 3072], F32)
        t_cw5 = Tok()
        S.dma("sp", lambda e: e.dma_start(out=cw5[:], in_=c.conv_w), writes=[t_cw5])
        cwT = sb("p1cwT", [128, 24, 8], F32)
        t_cwT = Tok()
        for ct in range(24):
            ps, t_ps = psm.next()
            S.op("pe", lambda e, ps=ps, ct=ct: e.transpose(ps[:, 0:5], cw5[0:5, ct * 128:(ct + 1) * 128], c.ident_f[0:5, 0:5]),
                 reads=[t_cw5, TC], writes=[t_ps])
            S.op("dve", lambda e, ps=ps, ct=ct: e.tensor_copy(cwT[:, ct, 0:5], ps[:, 0:5]), reads=[t_ps], writes=[t_cwT])
        dgs = _mk_ring(st, nc, "p1dg", 2, [128, 5, 128], BF16)
        xpre = _mk_ring(st, nc, "p1xpre", 2, [128, S_LEN + 4], BF16)
        tmstage = _mk_ring(st, nc, "p1tmstage", 2, [128, NT, 128], BF16)
        for ct in range(24):
            kind = ct // 8
            h = ct % 8
            w1, t_w1 = load_w(wfm, c.w_in, C_GQ + ct * 128, 128)
            dg, t_dg = dgs.next()
            for j in range(5):
                S.op("pool", lambda e, dg=dg, j=j, ct=ct: e.tensor_scalar(
                    out=dg[:, j, :], in0=c.ident_f[:], scalar1=cwT[:, ct, j:j + 1], scalar2=None, op0=ALU.mult),
                    reads=[TC, t_cwT], writes=[t_dg])
            xp, t_xp = xpre.next()
            S.op("pool", lambda e, xp=xp: e.memset(xp[:, 0:2], 0.0), writes=[t_xp])
            S.op("pool", lambda e, xp=xp: e.memset(xp[:, S_LEN + 2:S_LEN + 4], 0.0), writes=[t_xp])
            for g in range(NG):
                ps1, t_ps1 = proj_fm(w1, t_w1, g)
                S.op("act", lambda e, xp=xp, ps1=ps1, g=g: e.copy(xp[:, 2 + g * 512:2 + (g + 1) * 512], ps1[:]),
                     reads=[t_ps1], writes=[t_xp])
            stg, t_stg = stage.next()
            for g in range(NG):
                ps, t_ps = psm.next()
                for j in range(5):
                    S.op("pe", lambda e, ps=ps, dg=dg, xp=xp, g=g, j=j: e.matmul(
                        ps[:], dg[:, j, :], xp[:, g * 512 + j:g * 512 + j + 512], start=(j == 0), stop=(j == 4)),
                        reads=[t_dg, t_xp], writes=[t_ps])
                sl = slice(g * 512, (g + 1) * 512)
                if kind == 2:
                    S.op("act", lambda e, ps=ps, stg=stg, sl=sl: e.activation(stg[:, sl], ps[:], AF.Silu),
                         reads=[t_ps], writes=[t_stg])
                else:
                    a, t_a = f32a.next()
                    S.op("act", lambda e, ps=ps, a=a: e.activation(a[:], ps[:], AF.Silu), reads=[t_ps], writes=[t_a])
                    sqb, t_sqb = tmst.next()
                    S.op("pool", lambda e, sqb=sqb, a=a: e.tensor_tensor(out=sqb[:], in0=a[:], in1=a[:], op=ALU.mult),
                         reads=[t_a], writes=[t_sqb])
                    ps2, t_ps2 = psm.next()
                    S.op("pe", lambda e, ps2=ps2, sqb=sqb: e.matmul(ps2[:], c.ones_b[:], sqb[:], start=True, stop=True),
                         reads=[t_sqb, TC], writes=[t_ps2])
                    b, t_b = f32b.next()
                    S.op("act", lambda e, b=b, ps2=ps2: e.activation(b[:], ps2[:], AF.Sqrt, bias=EPS), reads=[t_ps2], writes=[t_b])
                    S.op("dve", lambda e, b=b: e.reciprocal(b[:], b[:]), reads=[t_b], writes=[t_b])
                    scl = (128.0 ** -0.5) if kind == 0 else 1.0
                    S.op("dve", lambda e, a=a, b=b, stg=stg, sl=sl, scl=scl: e.scalar_tensor_tensor(
                        out=stg[:, sl], in0=a[:], scalar=scl, in1=b[:], op0=ALU.mult, op1=ALU.mult),
                        reads=[t_a, t_b], writes=[t_stg])
            if kind < 2:
                dst = c.gq_d if kind == 0 else c.gk_d
                S.dma("sp", lambda e, stg=stg, dst=dst, h=h: e.dma_start(out=dst[h], in_=stg[:]), reads=[t_stg], writes=[c.t_gqk])
            if kind >= 1:
                dst = c.gktok_d if kind == 1 else c.gvtok_d
                tms, t_tms = tmstage.next()
                for i8 in range(4):
                    pt, t_pt = pst.next()
                    for ii in range(8):
                        i = i8 * 8 + ii
                        S.op("pe", lambda e, pt=pt, stg=stg, ii=ii, i=i: e.transpose(pt[:, ii, :], stg[:, i * 128:(i + 1) * 128], c.ident_b[:]),
                             reads=[t_stg, TC], writes=[t_pt])
                    S.op("dve", lambda e, pt=pt, tms=tms, i8=i8: e.tensor_copy(tms[:, i8 * 8:(i8 + 1) * 8, :], pt[:]),
                         reads=[t_pt], writes=[t_tms])
                S.dma("sp", lambda e, tms=tms, dst=dst, h=h: e.dma_start(
                    out=dst[:, h * 128:(h + 1) * 128].rearrange("(t p) c -> p t c", p=128), in_=tms[:]),
                    reads=[t_tms], writes=[c.t_gtok])
    S.barrier()


def build_program(debug=False, phases=("p1",), inject=(), opts=None):
    nc = bass.Bass("TRN2", target_bir_lowering=False)
    c = Ctx()
    c.inject = set(inject)
    c.opts = opts or {}
    c.nc = nc
    c.S = Sched(nc)

    c.input_names = []

    def din(name, shape):
        c.input_names.append(name)
        return nc.dram_tensor(name, list(shape), F32, kind="ExternalInput").ap()

    c.x = din("x", [S_LEN, D])
    c.norm1_w = din("norm1_w", [1, D])
    c.w_in = din("w_in", [D, IN_W])
    c.w_qkp = din("w_qkp", [D, 2048])
    c.cosT = din("cosT", [128, S_LEN])
    c.sinT = din("sinT", [128, S_LEN])
    c.conv_w = din("conv_w", [5, 3072])
    for n in ("a_log_fwd", "dt_bias_fwd", "a_log_bwd", "dt_bias_bwd"):
        setattr(c, n, din(n, [1, 8]))

    c.debug_names = []

    def scratch(name, shape, dt):
        kind = "ExternalOutput" if debug else "Internal"
        if name in c.inject:
            kind = "ExternalInput"
            c.input_names.append(name)
        elif debug:
            c.debug_names.append(name)
        return nc.dram_tensor(name, list(shape), dt, kind=kind).ap()

    c.qT_d = scratch("qT_d", [H, 128, S_LEN], BF16)
    c.kT_d = scratch("kT_d", [H, 128, S_LEN], BF16)
    c.v_d = scratch("v_d", [S_LEN, D], BF16)
    c.zs_d = scratch("zs_d", [S_LEN, D], BF16)
    c.sga_d = scratch("sga_d", [H, 128, S_LEN], BF16)
    c.sgg_d = scratch("sgg_d", [H, 128, S_LEN], BF16)
    c.gq_d = scratch("gq_d", [H, 128, S_LEN], BF16)
    c.gk_d = scratch("gk_d", [H, 128, S_LEN], BF16)
    c.gktok_d = scratch("gktok_d", [S_LEN, D], BF16)
    c.gvtok_d = scratch("gvtok_d", [S_LEN, D], BF16)
    c.small_d = scratch("small_d", [128, NT, 32], F32)
    for n in ("lambda_q1", "lambda_k1", "lambda_q2", "lambda_k2"):
        setattr(c, n, din(n, [1, 64]))
    c.subln_w = din("subln_w", [1, 128])
    c.gdn_norm_w = din("gdn_norm_w", [1, 128])
    c.w_proj_attn = din("w_proj_attn", [D, D])
    c.w_proj_gdn = din("w_proj_gdn", [D, D])
    c.w_out = din("w_out", [D, D])
    c.w_router = din("w_router", [D, NE])
    c.norm2_w = din("norm2_w", [1, D])
    c.norm_f_w = din("norm_f_w", [1, D])
    c.w_gate = din("w_gate", [NE, D, FF])
    c.w_up = din("w_up", [NE, D, FF])
    c.w_down = din("w_down", [NE, FF, D])
    c.out = nc.dram_tensor("out", [S_LEN, D], F32, kind="ExternalOutput").ap()
    c.x1_d = scratch("x1_d", [S_LEN, D], F32)
    c.h2tok_d = scratch("h2tok_d", [S_LEN, D], BF16)
    c.aff_d = scratch("aff_d", [128, NT, NE], F32)
    c.pos_d = scratch("pos_d", [NE, S_LEN], F32)
    c.ptok_d = scratch("ptok_d", [128, NT, NE], F32)
    c.ye_d = scratch("ye_d", [NE, CAP, D], BF16)
    for n in ("t_x1", "t_h2", "t_aff", "t_pos", "t_ye", "t_out"):
        setattr(c, n, Tok())
    c.ogT_d = scratch("ogT_d", [H, 128, S_LEN], BF16)
    c.t_og = Tok()
    c.oaT_d = scratch("oaT_d", [H, 128, S_LEN], BF16)
    for n in ("t_qk", "t_gates", "t_vz", "t_small", "t_gqk", "t_gtok", "t_oa"):
        setattr(c, n, Tok())

    with ExitStack() as st:
        _consts(c, st)
        if "p1" in phases:
            phase1(c)
        if "p2" in phases:
            phase2(c)
        if "p3" in phases:
            phase3(c)
        if "p4" in phases:
            phase4(c)
        if "p5" in phases:
            phase5(c)
        if "p6a" in phases:
            phase6a(c)
        if "p6b" in phases:
            phase6b(c)
        c.S.barrier()
        c.S.emit()
    return nc, c


def host_prep(inputs):
    f = lambda a: np.ascontiguousarray(np.asarray(a, dtype=np.float32))
    w_in = f(inputs["w_in"][0])
    perm = np.arange(2048).reshape(16, 2, 2, 32)[:, :, ::-1, :].reshape(-1)
    w_qkp = np.ascontiguousarray(w_in[:, :2048][:, perm])
    inv = 10000.0 ** (-np.arange(0, 64, 2, dtype=np.float32) / 64)
    ang = np.arange(S_LEN, dtype=np.float32)[:, None] * inv[None, :]
    ang = np.concatenate([ang, ang], axis=-1)
    cos = np.cos(ang).T.astype(np.float32)
    sin = np.sin(ang).T.astype(np.float32)
    sin[:32] *= -1.0
    shared = {
        "norm1_w": f(inputs["norm1_w"]).reshape(1, D),
        "w_in": w_in,
        "w_qkp": w_qkp,
        "cosT": np.ascontiguousarray(np.concatenate([cos, cos], 0)),
        "sinT": np.ascontiguousarray(np.concatenate([sin, sin], 0)),
        "conv_w": f(inputs["conv_w"][0]),
    }
    for n in ("a_log_fwd", "dt_bias_fwd", "a_log_bwd", "dt_bias_bwd"):
        shared[n] = f(inputs[n]).reshape(1, 8)
    for n in ("lambda_q1", "lambda_k1", "lambda_q2", "lambda_k2"):
        shared[n] = f(inputs[n]).reshape(1, 64)
    shared["subln_w"] = f(inputs["subln_w"]).reshape(1, 128)
    shared["gdn_norm_w"] = f(inputs["gdn_norm_w"]).reshape(1, 128)
    shared["w_proj_attn"] = f(inputs["w_proj_attn"][0])
    shared["w_proj_gdn"] = f(inputs["w_proj_gdn"][0])
    shared["w_out"] = f(inputs["w_out"][0])
    shared["w_router"] = f(inputs["w_router"][0])
    shared["norm2_w"] = f(inputs["norm2_w"]).reshape(1, D)
    shared["norm_f_w"] = f(inputs["norm_f_w"]).reshape(1, D)
    shared["w_gate"] = f(inputs["w_gate"][0])
    shared["w_up"] = f(inputs["w_up"][0])
    shared["w_down"] = f(inputs["w_down"][0])
    return shared


def phase2(c):
    nc, S = c.nc, c.S
    TC = c.t_const
    with ExitStack() as st:
        sb = lambda name, shape, dt: st.enter_context(nc.sbuf_tensor(name, shape, dt))
        lam = sb("p2lam", [128, 4, 64], F32)
        lsc = sb("p2lsc", [128, 8], F32)
        t_lam = Tok()
        for j, src in enumerate((c.lambda_q1, c.lambda_k1, c.lambda_q2, c.lambda_k2)):
            S.dma("sp", lambda e, j=j, src=src: e.dma_start(out=lam[:, j, :], in_=src.broadcast_to([128, 64])), writes=[t_lam])
        S.op("dve", lambda e: e.tensor_tensor(out=lam[:, 0, :], in0=lam[:, 0, :], in1=lam[:, 1, :], op=ALU.mult), reads=[t_lam], writes=[t_lam])
        S.op("dve", lambda e: e.tensor_tensor(out=lam[:, 2, :], in0=lam[:, 2, :], in1=lam[:, 3, :], op=ALU.mult), reads=[t_lam], writes=[t_lam])
        S.op("act", lambda e: e.activation(lam[:, 1, :], lam[:, 0, :], AF.Identity, accum_out=lsc[:, 0:1]), reads=[t_lam], writes=[t_lam])
        S.op("act", lambda e: e.activation(lam[:, 3, :], lam[:, 2, :], AF.Identity, accum_out=lsc[:, 1:2]), reads=[t_lam], writes=[t_lam])
        S.op("act", lambda e: e.activation(lsc[:, 2:4], lsc[:, 0:2], AF.Exp), reads=[t_lam], writes=[t_lam])
        S.op("dve", lambda e: e.scalar_tensor_tensor(out=lsc[:, 4:5], in0=lsc[:, 3:4], scalar=-LAMBDA_INIT, in1=lsc[:, 2:3],
                                                     op0=ALU.add, op1=ALU.subtract), reads=[t_lam], writes=[t_lam])
        wsub = sb("p2wsub", [128, 2], F32)
        t_wsub = Tok()
        S.dma("sp", lambda e: e.dma_start(out=wsub[:, 0:1], in_=c.subln_w.rearrange("o e -> e o")), writes=[t_wsub])
        S.op("dve", lambda e: e.tensor_scalar(out=wsub[:, 1:2], in0=wsub[:, 0:1], scalar1=(1.0 - LAMBDA_INIT), scalar2=None, op0=ALU.mult),
             reads=[t_wsub], writes=[t_wsub])

        qr = _mk_ring(st, nc, "p2q", 2, [128, S_LEN], BF16)
        kr = _mk_ring(st, nc, "p2k", 4, [128, S_LEN], BF16)
        for i_, (kb_, t_kb_) in enumerate(kr.items):
            zs_ = slice(64, 128) if i_ % 2 == 0 else slice(0, 64)
            S.op("pool", lambda e, kb_=kb_, zs_=zs_: e.memset(kb_[zs_, :], 0.0), writes=[t_kb_])
        vr = _mk_ring(st, nc, "p2v", 2, [128, NT, 128], BF16)
        pr = _mk_ring(st, nc, "p2p", 4, [128, 512], BF16)
        fr = _mk_ring(st, nc, "p2f", 6, [128, 512], F32)
        sqr = _mk_ring(st, nc, "p2sq", 2, [128, 512], BF16)
        accr = _mk_ring(st, nc, "p2acc", 4, [128, 512], F32)
        stage = _mk_ring(st, nc, "p2stage", 2, [128, S_LEN], BF16)
        ps_s = _mk_ring(st, nc, "p2pss", 3, [128, 512], F32, psum=True)
        ps_o = [_mk_ring(st, nc, "p2pso%d" % t, 1, [128, 512], F32, psum=True) for t in range(2)]
        ps_l = [_mk_ring(st, nc, "p2psl%d" % t, 1, [128, 512], F32, psum=True) for t in range(2)]
        ps_x = _mk_ring(st, nc, "p2psx", 1, [128, 512], F32, psum=True)

        for h in range(H):
            q, t_q = qr.next()
            k0, t_k0 = kr.next()
            k1, t_k1 = kr.next()
            kz = ((k0, t_k0), (k1, t_k1))
            v, t_v = vr.next()
            S.dma("sp", lambda e, q=q, h=h: e.dma_start(out=q[:], in_=c.qT_d[h]), reads=[c.t_qk], writes=[t_q])
            S.dma("sp", lambda e, k0=k0, h=h: e.dma_start(out=k0[0:64, :], in_=c.kT_d[h, 0:64, :]), reads=[c.t_qk], writes=[t_k0])
            S.dma("sp", lambda e, k1=k1, h=h: e.dma_start(out=k1[64:128, :], in_=c.kT_d[h, 64:128, :]), reads=[c.t_qk], writes=[t_k1])
            S.dma("sp", lambda e, v=v, h=h: e.dma_start(
                out=v[:], in_=c.v_d[:, h * 128:(h + 1) * 128].rearrange("(t p) e -> p t e", p=128)), reads=[c.t_vz], writes=[t_v])
            stg, t_stg = stage.next()
            for g in range(NG):
                qs = slice(g * 512, (g + 1) * 512)
                accs = []
                for t in range(2):
                    po, t_po = ps_o[t].next()
                    pl, t_pl = ps_l[t].next()
                    ts = slice(t * 64, (t + 1) * 64)
                    accA, t_accA = accr.next()
                    accB, t_accB = accr.next()

                    def emit_s(j, qs=qs, k=kz[t][0], q=q, t_k=kz[t][1], t_q=t_q):
                        pss, t_pss = ps_s.next()
                        S.op("pe", lambda e, pss=pss, j=j: e.matmul(
                            pss[:], k[:, j * 128:(j + 1) * 128], q[:, qs], start=True, stop=True),
                            reads=[t_k, t_q], writes=[t_pss])
                        return pss, t_pss
                    cur = emit_s(0)
                    for j in range(NT):
                        nxt = emit_s(j + 1) if j + 1 < NT else None
                        pss, t_pss = cur
                        p, t_p = pr.next()
                        S.op("act", lambda e, p=p, pss=pss: e.activation(p[:], pss[:], AF.Exp, scale=0.125),
                             reads=[t_pss], writes=[t_p])
                        S.op("pe", lambda e, po=po, v=v, p=p, j=j: e.matmul(po[:], v[:, j, :], p[:], start=(j == 0), stop=(j == NT - 1)),
                             reads=[t_v, t_p], writes=[t_po])
                        acc, t_acc = (accA, t_accA) if j % 2 == 0 else (accB, t_accB)
                        eng = "dve" if j % 2 == 0 else "pool"
                        if j < 2:
                            S.op(eng, lambda e, acc=acc, p=p: e.tensor_copy(acc[:], p[:]), reads=[t_p], writes=[t_acc])
                        else:
                            S.op(eng, lambda e, acc=acc, p=p: e.tensor_tensor(out=acc[:], in0=acc[:], in1=p[:], op=ALU.add),
                                 reads=[t_p, t_acc], writes=[t_acc])
                        cur = nxt
                    S.op("pe", lambda e, pl=pl, accA=accA: e.matmul(pl[:], c.ones_f[:], accA[:], start=True, stop=False),
                         reads=[TC, t_accA], writes=[t_pl])
                    S.op("pe", lambda e, pl=pl, accB=accB: e.matmul(pl[:], c.ones_f[:], accB[:], start=False, stop=True),
                         reads=[TC, t_accB], writes=[t_pl])
                    accs.append((po, t_po, pl, t_pl))
                outs = []
                for t in range(2):
                    po, t_po, pl, t_pl = accs[t]
                    r, t_r = fr.next()
                    S.op("dve", lambda e, r=r, pl=pl: e.reciprocal(r[:], pl[:]), reads=[t_pl], writes=[t_r])
                    a, t_a = fr.next()
                    S.op("dve", lambda e, a=a, po=po, r=r: e.tensor_tensor(out=a[:], in0=po[:], in1=r[:], op=ALU.mult),
                         reads=[t_po, t_r], writes=[t_a])
                    outs.append((a, t_a))
                (a, t_a), (b, t_b) = outs
                oa, t_oa = fr.next()
                S.op("dve", lambda e, oa=oa, a=a, b=b: e.scalar_tensor_tensor(
                    out=oa[:], in0=b[:], scalar=lsc[:, 4:5], in1=a[:], op0=ALU.mult, op1=ALU.add),
                    reads=[t_a, t_b, t_lam], writes=[t_oa])
                sq, t_sq = sqr.next()
                S.op("pool", lambda e, sq=sq, oa=oa: e.tensor_tensor(out=sq[:], in0=oa[:], in1=oa[:], op=ALU.mult),
                     reads=[t_oa], writes=[t_sq])
                px, t_px = ps_x.next()
                S.op("pe", lambda e, px=px, sq=sq: e.matmul(px[:], c.ones_b[:], sq[:], start=True, stop=True),
                     reads=[TC, t_sq], writes=[t_px])
                rs, t_rs = fr.next()
                S.op("act", lambda e, rs=rs, px=px: e.activation(rs[:], px[:], AF.Sqrt, bias=EPS, scale=1.0 / 128), reads=[t_px], writes=[t_rs])
                S.op("dve", lambda e, rs=rs: e.reciprocal(rs[:], rs[:]), reads=[t_rs], writes=[t_rs])
                S.op("dve", lambda e, stg=stg, oa=oa, rs=rs, qs=qs: e.scalar_tensor_tensor(
                    out=stg[:, qs], in0=oa[:], scalar=wsub[:, 1:2], in1=rs[:], op0=ALU.mult, op1=ALU.mult),
                    reads=[t_oa, t_rs, t_wsub], writes=[t_stg])
            S.dma("sp", lambda e, stg=stg, h=h: e.dma_start(out=c.oaT_d[h], in_=stg[:]), reads=[t_stg], writes=[c.t_oa])
    S.barrier()


def phase3(c):
    nc, S = c.nc, c.S
    TC = c.t_const
    NB = NT
    with ExitStack() as st:
        sb = lambda name, shape, dt: st.enter_context(nc.sbuf_tensor(name, shape, dt))
        inc = [sb("p3inc%d" % d, [128, 128], F32) for d in range(2)]
        strm = [sb("p3str%d" % d, [128, 128], F32) for d in range(2)]
        mbias = [sb("p3mb%d" % d, [128, 128], F32) for d in range(2)]
        esel = [sb("p3es%d" % d, [128, 128], F32) for d in range(2)]
        t_m = Tok()
        for d in range(2):
            sgn = 1 if d == 0 else -1
            S.op("pool", lambda e, d=d: e.memset(inc[d][:], 1.0), writes=[t_m])
            S.op("pool", lambda e, d=d, sgn=sgn: e.affine_select(out=inc[d][:], in_=inc[d][:], compare_op=ALU.is_ge, fill=0.0,
                                                                base=0, pattern=[[sgn, 128]], channel_multiplier=-sgn),
                 reads=[t_m], writes=[t_m])
            S.op("pool", lambda e, d=d: e.memset(strm[d][:], 1.0), writes=[t_m])
            S.op("pool", lambda e, d=d, sgn=sgn: e.affine_select(out=strm[d][:], in_=strm[d][:], compare_op=ALU.is_ge, fill=0.0,
                                                                base=-1, pattern=[[sgn, 128]], channel_multiplier=-sgn),
                 reads=[t_m], writes=[t_m])
            S.op("pool", lambda e, d=d: e.tensor_scalar(out=mbias[d][:], in0=inc[d][:], scalar1=30000.0, scalar2=-30000.0,
                                                        op0=ALU.mult, op1=ALU.add), reads=[t_m], writes=[t_m])
            lastp = 127 if d == 0 else 0
            S.op("pool", lambda e, d=d: e.memset(esel[d][:], 0.0), writes=[t_m])
            S.op("pool", lambda e, d=d, lastp=lastp: e.affine_select(out=esel[d][:], in_=esel[d][:], compare_op=ALU.not_equal, fill=1.0,
                                                                    base=-lastp, pattern=[[0, 128]], channel_multiplier=1),
                 reads=[t_m], writes=[t_m])
        sm = sb("p3sm", [128, NT, 32], F32)
        t_sm = Tok()
        S.dma("sp", lambda e: e.dma_start(out=sm[:], in_=c.small_d), reads=[c.t_small], writes=[t_sm])
        gw = sb("p3gw", [128, 128], F32)
        t_gw = Tok()
        S.dma("sp", lambda e: e.dma_start(out=gw[:], in_=c.gdn_norm_w.broadcast_to([128, 128])), writes=[t_gw])

        def bank(name, dt=F32, n=512):
            return st.enter_context(nc.psum_tensor(name, [128, n], dt))
        b0, b2, b3, b4, b5, b6, b7 = [bank("p3b" + x) for x in "0234567"]
        b1 = bank("p3b1", BF16, 1024)
        btok = {id(b): Tok(excl=True) for b in (b0, b1, b2, b3, b4, b5, b6, b7)}
        slot = lambda b, i, w=128: (b[:, i * w:(i + 1) * w], btok[id(b)])
        s_kk, s_kq = slot(b0, 0), slot(b0, 1)
        s_gdd = [slot(b0, 2), slot(b0, 3)]
        s_misc = [slot(b6, 3)] * 4
        s_tr = [slot(b1, i) for i in range(8)]
        s_sqd = [(slot(b2, 0), slot(b2, 1)), (slot(b4, 0), slot(b4, 1))]
        s_apd = [slot(b3, 0, 256), slot(b5, 0, 256)]
        s_scan = [[slot(b6, i) for i in range(4)], [slot(b7, i) for i in range(4)]]

        kT = sb("p3kT", [128, S_LEN], BF16)
        qT = sb("p3qT", [128, S_LEN], BF16)
        ktok = sb("p3ktok", [128, NB, 128], BF16)
        vtok = sb("p3vtok", [128, NB, 128], BF16)
        zs = sb("p3zs", [128, NB, 128], BF16)
        t_in = Tok()
        ub = [sb("p3ub%d" % d, [128, NB, 128], F32) for d in range(2)]
        nwT = [sb("p3nwT%d" % d, [128, NB, 128], BF16) for d in range(2)]
        kdec = [sb("p3kdec%d" % d, [128, NB, 128], BF16) for d in range(2)]
        qkT = [sb("p3qkT%d" % d, [128, NB, 128], BF16) for d in range(2)]
        t_blk = [[Tok() for _ in range(NB)] for _ in range(2)]
        sc = [sb("p3sc%d" % d, [128, 8, NB], F32) for d in range(2)]
        t_sc = [Tok(), Tok()]
        oacc = sb("p3oacc", [128, NB, 128], F32)
        t_oacc = [Tok() for _ in range(NB)]
        S32 = [sb("p3S32_%d" % d, [128, 128], F32) for d in range(2)]
        Sbf = [sb("p3Sbf_%d" % d, [128, 128], BF16) for d in range(2)]
        t_S = [Tok(), Tok()]
        dtr = _mk_ring(st, nc, "p3dt", 5, [128, 128], F32)
        tmpr = _mk_ring(st, nc, "p3tmp", 5, [128, 128], F32)
        ntr = _mk_ring(st, nc, "p3nt", 10, [128, 128], F32)
        nnr = _mk_ring(st, nc, "p3nn", 10, [128, 128], F32)
        xr = _mk_ring(st, nc, "p3x", 5, [128, 256], F32)
        vnr = _mk_ring(st, nc, "p3vn", 4, [128, 128], BF16)
        o2r = _mk_ring(st, nc, "p3o2", 4, [128, 128], F32)
        big = sb("p3big", [128, NB, 128], F32)
        t_big = Tok()
        red = sb("p3red", [128, NB], F32)
        ogtok = sb("p3ogtok", [128, NB, 128], BF16)
        stage = sb("p3stage", [128, S_LEN], BF16)
        t_stage = Tok()

        stop = c.opts.get("p3_stop", 9)
        for h in range(c.opts.get("p3_heads", H)):
            S.dma("sp", lambda e, h=h: e.dma_start(out=kT[:], in_=c.gk_d[h]), reads=[c.t_gqk], writes=[t_in])
            S.dma("sp", lambda e, h=h: e.dma_start(out=qT[:], in_=c.gq_d[h]), reads=[c.t_gqk], writes=[t_in])
            for (buf, src, tk) in ((ktok, c.gktok_d, c.t_gtok), (vtok, c.gvtok_d, c.t_gtok), (zs, c.zs_d, c.t_vz)):
                S.dma("sp", lambda e, buf=buf, src=src, h=h: e.dma_start(
                    out=buf[:], in_=src[:, h * 128:(h + 1) * 128].rearrange("(t p) e -> p t e", p=128)), reads=[tk], writes=[t_in])
            for d in range(2):
                s_ = sc[d]
                g = sm[:, :, d * 8 + h]
                beta = sm[:, :, 16 + d * 8 + h]
                (pm, t_pm) = s_misc[d * 2]
                S.op("pe", lambda e, pm=pm, d=d, g=g: e.matmul(pm[:, 0:NB], inc[d][:], g, start=True, stop=True),
                     reads=[t_m, t_sm], writes=[t_pm])
                S.op("dve", lambda e, pm=pm, s_=s_: e.tensor_copy(s_[:, 0, :], pm[:, 0:NB]), reads=[t_pm], writes=[t_sc[d]])
                S.op("dve", lambda e, s_=s_: e.tensor_scalar(out=s_[:, 1, :], in0=s_[:, 0, :], scalar1=-1.0, scalar2=None, op0=ALU.mult),
                     reads=[t_sc[d]], writes=[t_sc[d]])
                (pm2, t_pm2) = s_misc[d * 2 + 1]
                S.op("pe", lambda e, pm2=pm2, d=d, s_=s_: e.matmul(pm2[:, 0:NB], esel[d][:], s_[:, 0, :], start=True, stop=True),
                     reads=[t_m, t_sc[d]], writes=[t_pm2])
                S.op("dve", lambda e, pm2=pm2, s_=s_: e.tensor_copy(s_[:, 2, :], pm2[:, 0:NB]), reads=[t_pm2], writes=[t_sc[d]])
                S.op("act", lambda e, s_=s_: e.activation(s_[:, 3, :], s_[:, 0, :], AF.Exp), reads=[t_sc[d]], writes=[t_sc[d]])
                S.op("dve", lambda e, s_=s_: e.tensor_tensor(out=s_[:, 4, :], in0=s_[:, 2, :], in1=s_[:, 0, :], op=ALU.subtract),
                     reads=[t_sc[d]], writes=[t_sc[d]])
                S.op("act", lambda e, s_=s_: e.activation(s_[:, 4, :], s_[:, 4, :], AF.Exp), reads=[t_sc[d]], writes=[t_sc[d]])
                S.op("act", lambda e, s_=s_: e.activation(s_[:, 5, :], s_[:, 2, :], AF.Exp), reads=[t_sc[d]], writes=[t_sc[d]])
                S.op("dve", lambda e, s_=s_, beta=beta: e.tensor_scalar(out=s_[:, 6, :], in0=beta, scalar1=-1.0, scalar2=None, op0=ALU.mult),
                     reads=[t_sm, t_sc[d]], writes=[t_sc[d]])
                S.op("dve", lambda e, s_=s_, beta=beta: e.tensor_copy(s_[:, 7, :], beta), reads=[t_sm, t_sc[d]], writes=[t_sc[d]])

            def par_chain(b, d):
                bs = slice(b * 128, (b + 1) * 128)
                s_ = sc[d]
                (pkk, t_pkk), (pkq, t_pkq), (pgd, t_pgd) = s_kk, s_kq, s_gdd[d]
                if d == 0:
                    S.op("pe", lambda e: e.matmul(pkk, kT[:, bs], kT[:, bs], start=True, stop=True), reads=[t_in], writes=[t_pkk])
                    S.op("pe", lambda e: e.matmul(pkq, kT[:, bs], qT[:, bs], start=True, stop=True), reads=[t_in], writes=[t_pkq])
                S.op("pe", lambda e: e.matmul(pgd, s_[:, 0, b:b + 1].broadcast_to([128, 128]), c.ident_f[:], start=True, stop=False),
                     reads=[t_sc[d], TC], writes=[t_pgd])
                S.op("pe", lambda e: e.matmul(pgd, c.ident_f[:], s_[:, 1, b:b + 1].broadcast_to([128, 128]), start=False, stop=False),
                     reads=[t_sc[d], TC], writes=[t_pgd])
                S.op("pe", lambda e: e.matmul(pgd, c.ident_f[:], mbias[d][:], start=False, stop=True), reads=[t_m, TC], writes=[t_pgd])
                yield
                dt_, t_dt = dtr.next()
                S.op("act", lambda e: e.activation(dt_[:], pgd, AF.Exp), reads=[t_pgd], writes=[t_dt])
                tmp, t_tmp = tmpr.next()
                S.op("dve", lambda e: e.scalar_tensor_tensor(out=tmp[:], in0=pkk, scalar=s_[:, 6, b:b + 1], in1=dt_[:], op0=ALU.mult, op1=ALU.mult),
                     reads=[t_pkk, t_sc[d], t_dt], writes=[t_tmp])
                S.op("dve", lambda e: e.tensor_tensor(out=qkT[d][:, b, :], in0=pkq, in1=dt_[:], op=ALU.mult),
                     reads=[t_pkq, t_dt], writes=[t_blk[d][b]])
                nt, t_nt = ntr.next()
                S.op("pool", lambda e, nt=nt: e.tensor_tensor(out=nt[:], in0=tmp[:], in1=strm[d][:], op=ALU.mult), reads=[t_tmp, t_m], writes=[t_nt])
                x, t_x = xr.next()
                S.op("pool", lambda e: e.tensor_copy(x[:, 0:128], vtok[:, b, :]), reads=[t_in], writes=[t_x])
                S.op("pool", lambda e: e.tensor_scalar(out=x[:, 128:256], in0=ktok[:, b, :], scalar1=s_[:, 3, b:b + 1], scalar2=None, op0=ALU.mult),
                     reads=[t_in, t_sc[d]], writes=[t_x])
                S.op("pool", lambda e: e.tensor_scalar(out=kdec[d][:, b, :], in0=ktok[:, b, :], scalar1=s_[:, 4, b:b + 1], scalar2=None, op0=ALU.mult),
                     reads=[t_in, t_sc[d]], writes=[t_blk[d][b]])
                yield
                (ptr, t_ptr) = s_sqd[d][0]
                S.op("pe", lambda e, nt=nt: e.transpose(ptr, nt[:], c.ident_f[:]), reads=[t_nt, TC], writes=[t_ptr])
                yield
                nn, t_nn = nnr.next()
                S.op("act", lambda e, nn=nn: e.copy(nn[:], ptr), reads=[t_ptr], writes=[t_nn])
                yield
                for l in range(7):
                    (pap, t_pap) = s_apd[d]
                    S.op("pe", lambda e, nt=nt: e.matmul(pap, nt[:], x[:], start=True, stop=True), reads=[t_nt, t_x], writes=[t_pap])
                    if l < 6:
                        (pn2, t_pn2), (pnt2, t_pnt2) = s_sqd[d]
                        S.op("pe", lambda e, nt=nt, nn=nn: e.matmul(pn2, nt[:], nn[:], start=True, stop=True), reads=[t_nt, t_nn], writes=[t_pn2])
                        S.op("pe", lambda e, nt=nt, nn=nn: e.matmul(pnt2, nn[:], nt[:], start=True, stop=True), reads=[t_nt, t_nn], writes=[t_pnt2])
                    yield
                    S.op("dve", lambda e: e.tensor_tensor(out=x[:], in0=pap, in1=x[:], op=ALU.add), reads=[t_pap, t_x], writes=[t_x])
                    if l < 6:
                        nn2, t_nn2 = nnr.next()
                        nt2, t_nt2 = ntr.next()
                        S.op("act", lambda e, nn2=nn2: e.copy(nn2[:], pn2), reads=[t_pn2], writes=[t_nn2])
                        S.op("act", lambda e, nt2=nt2: e.copy(nt2[:], pnt2), reads=[t_pnt2], writes=[t_nt2])
                        nn, t_nn, nt, t_nt = nn2, t_nn2, nt2, t_nt2
                    yield
                S.op("dve", lambda e: e.tensor_scalar(out=ub[d][:, b, :], in0=x[:, 0:128], scalar1=s_[:, 7, b:b + 1], scalar2=None, op0=ALU.mult),
                     reads=[t_x, t_sc[d]], writes=[t_blk[d][b]])
                (ptw, t_ptw) = s_sqd[d][1]
                S.op("pe", lambda e: e.transpose(ptw, x[:, 128:256], c.ident_f[:]), reads=[t_x, TC], writes=[t_ptw])
                yield
                S.op("act", lambda e: e.activation(nwT[d][:, b, :], ptw, AF.Copy, scale=-1.0), reads=[t_ptw], writes=[t_blk[d][b]])
                yield

            if stop >= 2:
                _interleave((par_chain(b, d) for b in range(NB) for d in range(2)), 2)

            def scan_chain(d):
                s_ = sc[d]
                S.op("pool", lambda e: e.memset(S32[d][:], 0.0), writes=[t_S[d]])
                S.op("pool", lambda e: e.memset(Sbf[d][:], 0.0), writes=[t_S[d]])
                (pv, t_pv), (po1, t_po1), (po2, t_po2), (pS, t_pS) = s_scan[d]
                for step in range(NB):
                    b = step if d == 0 else NB - 1 - step
                    bs = slice(b * 128, (b + 1) * 128)
                    S.op("pe", lambda e, b=b: e.matmul(pv, nwT[d][:, b, :], Sbf[d][:], start=True, stop=True),
                         reads=[t_blk[d][b], t_S[d]], writes=[t_pv])
                    S.op("pe", lambda e, bs=bs: e.matmul(po1, qT[:, bs], Sbf[d][:], start=True, stop=True), reads=[t_in, t_S[d]], writes=[t_po1])
                    yield
                    vn, t_vn = vnr.next()
                    S.op("dve", lambda e, vn=vn, b=b: e.scalar_tensor_tensor(
                        out=vn[:], in0=pv, scalar=s_[:, 7, b:b + 1], in1=ub[d][:, b, :], op0=ALU.mult, op1=ALU.add),
                        reads=[t_pv, t_sc[d], t_blk[d][b]], writes=[t_vn])
                    yield
                    S.op("pe", lambda e, vn=vn, b=b: e.matmul(pS, kdec[d][:, b, :], vn[:], start=True, stop=True),
                         reads=[t_blk[d][b], t_vn], writes=[t_pS])
                    S.op("pe", lambda e, vn=vn, b=b: e.matmul(po2, qkT[d][:, b, :], vn[:], start=True, stop=True),
                         reads=[t_blk[d][b], t_vn], writes=[t_po2])
                    yield
                    S.op("dve", lambda e, b=b: e.scalar_tensor_tensor(
                        out=S32[d][:], in0=S32[d][:], scalar=s_[:, 5, b:b + 1], in1=pS, op0=ALU.mult, op1=ALU.add),
                        reads=[t_pS, t_sc[d], t_S[d]], writes=[t_S[d]])
                    S.op("pool", lambda e: e.tensor_copy(Sbf[d][:], S32[d][:]), reads=[t_S[d]], writes=[t_S[d]])
                    o2, t_o2 = o2r.next()
                    S.op("act", lambda e, o2=o2: e.copy(o2[:], po2), reads=[t_po2], writes=[t_o2])
                    first = (b < NB // 2) if d == 0 else (b >= NB // 2)
                    if not first:
                        S.op("pool", lambda e, o2=o2, b=b: e.tensor_tensor(out=o2[:], in0=o2[:], in1=oacc[:, b, :], op=ALU.add),
                             reads=[t_o2, t_oacc[b]], writes=[t_o2])
                    S.op("dve", lambda e, o2=o2, b=b: e.scalar_tensor_tensor(
                        out=oacc[:, b, :], in0=po1, scalar=s_[:, 3, b:b + 1], in1=o2[:], op0=ALU.mult, op1=ALU.add),
                        reads=[t_po1, t_sc[d], t_o2], writes=[t_oacc[b]])
                    yield

            if stop >= 3:
                _interleave((scan_chain(d) for d in range(2)), 2)

            S.op("dve", lambda e: e.tensor_tensor(out=big[:], in0=oacc[:], in1=oacc[:], op=ALU.mult), reads=t_oacc, writes=[t_big])
            S.op("dve", lambda e: e.tensor_reduce(out=red[:], in_=big[:], axis=mybir.AxisListType.X, op=ALU.add), reads=[t_big], writes=[t_big])
            S.op("act", lambda e: e.activation(red[:], red[:], AF.Sqrt, bias=EPS, scale=1.0 / 128), reads=[t_big], writes=[t_big])
            S.op("dve", lambda e: e.reciprocal(red[:], red[:]), reads=[t_big], writes=[t_big])
            S.op("dve", lambda e: e.tensor_tensor(out=big[:], in0=oacc[:], in1=red[:].unsqueeze(2).broadcast_to([128, NB, 128]), op=ALU.mult),
                 reads=t_oacc + [t_big], writes=[t_big])
            S.op("pool", lambda e: e.tensor_tensor(out=big[:], in0=big[:], in1=gw[:].unsqueeze(1).broadcast_to([128, NB, 128]), op=ALU.mult),
                 reads=[t_big, t_gw], writes=[t_big])
            S.op("dve", lambda e: e.tensor_tensor(out=ogtok[:], in0=big[:], in1=zs[:], op=ALU.mult), reads=[t_big, t_in], writes=[t_big])
            for i8 in range(4):
                for ii in range(8):
                    b = i8 * 8 + ii
                    (ptr, t_ptr) = s_tr[ii]
                    S.op("pe", lambda e, ptr=ptr, b=b: e.transpose(ptr, ogtok[:, b, :], c.ident_b[:]), reads=[t_big, TC], writes=[t_ptr])
                    S.op("act", lambda e, ptr=ptr, b=b: e.copy(stage[:, b * 128:(b + 1) * 128], ptr), reads=[t_ptr], writes=[t_stage])
            S.dma("sp", lambda e, h=h: e.dma_start(out=c.ogT_d[h], in_=stage[:]), reads=[t_stage], writes=[c.t_og])
    S.barrier()


def phase4(c):
    nc, S = c.nc, c.S
    TC = c.t_const
    with ExitStack() as st:
        sb = lambda name, shape, dt: st.enter_context(nc.sbuf_tensor(name, shape, dt))
        wpa = sb("p4wpa", [128, 8, D], BF16)
        wpg = sb("p4wpg", [128, 8, D], BF16)
        wout = sb("p4wout", [128, 8, D], BF16)
        wr = sb("p4wr", [128, 8, NE], BF16)
        t_w = Tok()
        for (dst, src) in ((wpa, c.w_proj_attn), (wpg, c.w_proj_gdn), (wout, c.w_out)):
            for kc in range(8):
                S.dma("pool", lambda e, dst=dst, src=src, kc=kc: e.dma_start(out=dst[:, kc, :], in_=src[kc * 128:(kc + 1) * 128, :]),
                      writes=[Tok()])
        S.dma("pool", lambda e: e.dma_start(out=wr[:], in_=c.w_router.rearrange("(kc p) n -> p kc n", p=128)), writes=[t_w])
        S.barrier()
        n2w = sb("p4n2w", [128, D], F32)
        t_n2w = Tok()
        S.dma("sp", lambda e: e.dma_start(out=n2w[:], in_=c.norm2_w.broadcast_to([128, D])), writes=[t_n2w])
        aff = sb("p4aff", [128, NT, NE], F32)
        t_aff = Tok()
        oar = _mk_ring(st, nc, "p4oa", 2, [128, 8, 512], BF16)
        ogr = _mk_ring(st, nc, "p4og", 2, [128, 8, 512], BF16)
        sgar = _mk_ring(st, nc, "p4sga", 2, [128, 8, 512], BF16)
        sggr = _mk_ring(st, nc, "p4sgg", 2, [128, 8, 512], BF16)
        mgr = _mk_ring(st, nc, "p4mg", 2, [128, 8, 512], BF16)
        f1 = _mk_ring(st, nc, "p4f1", 2, [128, 512], F32)
        f2 = _mk_ring(st, nc, "p4f2", 2, [128, 512], F32)
        xr = _mk_ring(st, nc, "p4x", 2, [128, D], F32)
        sqr = _mk_ring(st, nc, "p4sq", 2, [128, D], BF16)
        hr = _mk_ring(st, nc, "p4h", 2, [128, D], BF16)
        hTr = _mk_ring(st, nc, "p4hT", 2, [128, 8, 128], BF16)
        ssr = _mk_ring(st, nc, "p4ss", 4, [128, 4], F32)
        lgr = _mk_ring(st, nc, "p4lg", 2, [128, NE], F32)
        psm = _mk_ring(st, nc, "p4psm", 6, [128, 512], F32, psum=True)
        pst = _mk_ring(st, nc, "p4pst", 2, [128, 8, 128], BF16, psum=True)

        for g in range(NG):
            gs = slice(g * 512, (g + 1) * 512)
            oa, t_oa = oar.next()
            og, t_og = ogr.next()
            sga, t_sga = sgar.next()
            sgg, t_sgg = sggr.next()
            for (buf, tk, src, dep) in ((oa, t_oa, c.oaT_d, c.t_oa), (og, t_og, c.ogT_d, c.t_og),
                                        (sga, t_sga, c.sga_d, c.t_gates), (sgg, t_sgg, c.sgg_d, c.t_gates)):
                S.dma("sp", lambda e, buf=buf, src=src, gs=gs: e.dma_start(out=buf[:], in_=src[:, :, gs].rearrange("h p t -> p h t")),
                      reads=[dep], writes=[tk])
            mg, t_mg = mgr.next()
            for dc in range(8):
                ds = slice(dc * 128, (dc + 1) * 128)
                pa, t_pa = psm.next()
                pg, t_pg = psm.next()
                for ec in range(8):
                    S.op("pe", lambda e, pa=pa, ec=ec, ds=ds, oa=oa: e.matmul(pa[:], wpa[:, ec, ds], oa[:, ec, :], start=(ec == 0), stop=(ec == 7)),
                         reads=[t_oa], writes=[t_pa])
                for ec in range(8):
                    S.op("pe", lambda e, pg=pg, ec=ec, ds=ds, og=og: e.matmul(pg[:], wpg[:, ec, ds], og[:, ec, :], start=(ec == 0), stop=(ec == 7)),
                         reads=[t_og], writes=[t_pg])
                a, t_a = f1.next()
                b, t_b = f2.next()
                S.op("dve", lambda e, a=a, pa=pa, sga=sga, dc=dc: e.tensor_tensor(out=a[:], in0=pa[:], in1=sga[:, dc, :], op=ALU.mult),
                     reads=[t_pa, t_sga], writes=[t_a])
                S.op("dve", lambda e, b=b, pg=pg, sgg=sgg, dc=dc: e.tensor_tensor(out=b[:], in0=pg[:], in1=sgg[:, dc, :], op=ALU.mult),
                     reads=[t_pg, t_sgg], writes=[t_b])
                S.op("pool", lambda e, mg=mg, a=a, b=b, dc=dc: e.tensor_tensor(out=mg[:, dc, :], in0=a[:], in1=b[:], op=ALU.add),
                     reads=[t_a, t_b], writes=[t_mg])
            for tl in range(4):
                i = g * 4 + tl
                xt, t_x = xr.next()
                S.dma("sp", lambda e, xt=xt, i=i: e.dma_start(out=xt[:], in_=c.x[i * 128:(i + 1) * 128, :]), writes=[t_x])
                for dh in range(2):
                    po, t_po = psm.next()
                    for dc in range(8):
                        S.op("pe", lambda e, po=po, mg=mg, dc=dc, tl=tl, dh=dh: e.matmul(
                            po[:], mg[:, dc, tl * 128:(tl + 1) * 128], wout[:, dc, dh * 512:(dh + 1) * 512], start=(dc == 0), stop=(dc == 7)),
                            reads=[t_mg], writes=[t_po])
                    S.op("dve", lambda e, xt=xt, po=po, dh=dh: e.tensor_tensor(out=xt[:, dh * 512:(dh + 1) * 512], in0=po[:],
                                                                              in1=xt[:, dh * 512:(dh + 1) * 512], op=ALU.add),
                         reads=[t_po, t_x], writes=[t_x])
                S.dma("sp", lambda e, xt=xt, i=i: e.dma_start(out=c.x1_d[i * 128:(i + 1) * 128, :], in_=xt[:]), reads=[t_x], writes=[c.t_x1])
                sq, t_sq = sqr.next()
                ss, t_ss = ssr.next()
                S.op("act", lambda e, xt=xt, sq=sq, ss=ss: e.activation(sq[:], xt[:], AF.Square, accum_out=ss[:, 0:1]),
                     reads=[t_x], writes=[t_sq, t_ss])
                S.op("act", lambda e, ss=ss: e.activation(ss[:, 1:2], ss[:, 0:1], AF.Sqrt, bias=EPS, scale=1.0 / D), reads=[t_ss], writes=[t_ss])
                S.op("dve", lambda e, ss=ss: e.reciprocal(ss[:, 1:2], ss[:, 1:2]), reads=[t_ss], writes=[t_ss])
                ht, t_h = hr.next()
                S.op("dve", lambda e, ht=ht, xt=xt, ss=ss: e.scalar_tensor_tensor(
                    out=ht[:], in0=xt[:], scalar=ss[:, 1:2], in1=n2w[:], op0=ALU.mult, op1=ALU.mult),
                    reads=[t_x, t_ss, t_n2w], writes=[t_h])
                S.dma("sp", lambda e, ht=ht, i=i: e.dma_start(out=c.h2tok_d[i * 128:(i + 1) * 128, :], in_=ht[:]), reads=[t_h], writes=[c.t_h2])
                pt, t_pt = pst.next()
                for dc in range(8):
                    S.op("pe", lambda e, pt=pt, ht=ht, dc=dc: e.transpose(pt[:, dc, :], ht[:, dc * 128:(dc + 1) * 128], c.ident_b[:]),
                         reads=[t_h, TC], writes=[t_pt])
                hT, t_hT = hTr.next()
                S.op("act", lambda e, hT=hT, pt=pt: e.copy(hT[:], pt[:]), reads=[t_pt], writes=[t_hT])
                pl, t_pl = psm.next()
                for dc in range(8):
                    S.op("pe", lambda e, pl=pl, hT=hT, dc=dc: e.matmul(pl[:, 0:NE], hT[:, dc, :], wr[:, dc, :], start=(dc == 0), stop=(dc == 7)),
                         reads=[t_hT, t_w], writes=[t_pl])
                lg, t_lg = lgr.next()
                S.op("dve", lambda e, pl=pl, ss=ss: e.tensor_reduce(out=ss[:, 2:3], in_=pl[:, 0:NE], axis=mybir.AxisListType.X, op=ALU.max),
                     reads=[t_pl, t_ss], writes=[t_ss])
                S.op("dve", lambda e, ss=ss: e.tensor_scalar(out=ss[:, 2:3], in0=ss[:, 2:3], scalar1=-1.0, scalar2=None, op0=ALU.mult),
                     reads=[t_ss], writes=[t_ss])
                S.op("act", lambda e, lg=lg, pl=pl, ss=ss: e.activation(lg[:], pl[:, 0:NE], AF.Exp, bias=ss[:, 2:3], accum_out=ss[:, 3:4]),
                     reads=[t_pl, t_ss], writes=[t_lg, t_ss])
                S.op("dve", lambda e, ss=ss: e.reciprocal(ss[:, 3:4], ss[:, 3:4]), reads=[t_ss], writes=[t_ss])
                S.op("dve", lambda e, lg=lg, ss=ss, i=i: e.tensor_scalar(out=aff[:, i, :], in0=lg[:], scalar1=ss[:, 3:4], scalar2=None, op0=ALU.mult),
                     reads=[t_lg, t_ss], writes=[t_aff])
        S.dma("sp", lambda e: e.dma_start(out=c.aff_d, in_=aff[:]), reads=[t_aff], writes=[c.t_aff])
    S.barrier()


def phase5(c):
    nc, S = c.nc, c.S
    TC = c.t_const
    with ExitStack() as st:
        sb = lambda name, shape, dt: st.enter_context(nc.sbuf_tensor(name, shape, dt))
        aff = sb("p5aff", [128, NT, NE], F32)
        t_aff = Tok()
        S.dma("sp", lambda e: e.dma_start(out=aff[:], in_=c.aff_d), reads=[c.t_aff], writes=[t_aff])
        affT = sb("p5affT", [NE, S_LEN], F32)
        work = sb("p5work", [NE, S_LEN], F32)
        ones = sb("p5ones", [NE, S_LEN], F32)
        mx = sb("p5mx", [NE, 8], F32)
        t_affT, t_work, t_mx, t_ones = Tok(), Tok(), Tok(), Tok()
        psm = _mk_ring(st, nc, "p5psm", 4, [128, 512], F32, psum=True)
        for i4 in range(NT // 4):
            ps, t_ps = psm.next()
            for ii in range(4):
                i = i4 * 4 + ii
                S.op("pe", lambda e, ps=ps, ii=ii, i=i: e.transpose(ps[0:NE, ii * 128:(ii + 1) * 128], aff[:, i, :], c.ident_f[:]),
                     reads=[t_aff, TC], writes=[t_ps])
            S.op("act", lambda e, ps=ps, i4=i4: e.copy(affT[:, i4 * 512:(i4 + 1) * 512], ps[0:NE, :]), reads=[t_ps], writes=[t_affT])
        S.op("pool", lambda e: e.memset(ones[:], 1.0), writes=[t_ones])
        src = affT
        for it in range(CAP // 8):
            S.op("dve", lambda e, src=src: e.max(out=mx[:], in_=src[:]), reads=[t_affT, t_work], writes=[t_mx])
            if it < CAP // 8 - 1:
                S.op("dve", lambda e, src=src: e.match_replace(out=work[:], in_to_replace=mx[:], in_values=src[:], imm_value=-1.0),
                     reads=[t_mx, t_affT, t_work], writes=[t_work])
            src = work
        S.op("dve", lambda e: e.tensor_scalar(out=work[:], in0=affT[:], scalar1=mx[:, 7:8], scalar2=None, op0=ALU.is_ge),
             reads=[t_affT, t_mx, t_work], writes=[t_work])
        S.op("dve", lambda e: e.tensor_tensor_scan(out=affT[:], data0=ones[:], data1=work[:], initial=0.0, op0=ALU.mult, op1=ALU.add),
             reads=[t_work, t_ones, t_affT], writes=[t_affT])
        S.op("dve", lambda e: e.tensor_tensor(out=affT[:], in0=affT[:], in1=work[:], op=ALU.mult), reads=[t_work, t_affT], writes=[t_affT])
        S.op("dve", lambda e: e.tensor_scalar(out=affT[:], in0=affT[:], scalar1=-1.0, scalar2=None, op0=ALU.add), reads=[t_affT], writes=[t_affT])
        S.dma("sp", lambda e: e.dma_start(out=c.pos_d, in_=affT[:]), reads=[t_affT], writes=[c.t_pos])
        ptok = sb("p5ptok", [128, NT, NE], F32)
        t_ptok = Tok()
        for i4 in range(NT // 4):
            ps, t_ps = psm.next()
            for ii in range(4):
                i = i4 * 4 + ii
                S.op("pe", lambda e, ps=ps, ii=ii, i=i: e.transpose(ps[:, ii * NE:(ii + 1) * NE], affT[:, i * 128:(i + 1) * 128], c.ident_f[0:NE, 0:NE]),
                     reads=[t_affT, TC], writes=[t_ps])
            S.op("act", lambda e, ps=ps, i4=i4: e.copy(ptok[:, i4 * 4:(i4 + 1) * 4, :], ps[:, 0:4 * NE].rearrange("p (a b) -> p a b", b=NE)),
                 reads=[t_ps], writes=[t_ptok])
        S.dma("sp", lambda e: e.dma_start(out=c.ptok_d, in_=ptok[:]), reads=[t_ptok], writes=[c.t_pos])
    S.barrier()


def phase6a(c):
    nc, S = c.nc, c.S
    TC = c.t_const
    with ExitStack() as st:
        sb = lambda name, shape, dt: st.enter_context(nc.sbuf_tensor(name, shape, dt))
        ptok = sb("p6ptok", [128, NT, NE], F32)
        t_ptok = Tok()
        S.dma("sp", lambda e: e.dma_start(out=ptok[:], in_=c.ptok_d), reads=[c.t_pos], writes=[t_ptok])
        iota = sb("p6iota", [128, CAP], F32)
        t_iota = Tok()
        S.op("pool", lambda e: e.iota(iota[:], pattern=[[1, CAP]], base=0, channel_multiplier=0, allow_small_or_imprecise_dtypes=True),
             writes=[t_iota])
        sel = sb("p6sel", [128, NT, CAP], BF16)
        t_sel = Tok()
        xsT = sb("p6xsT", [128, 8, CAP], BF16)
        t_xsT = Tok()
        actT = sb("p6actT", [128, 16, CAP], BF16)
        t_actT = Tok()
        yes = sb("p6ye", [128, 4, D], BF16)
        t_yes = Tok()
        h2r = _mk_ring(st, nc, "p6h2", 2, [128, 4, D], BF16)
        wgr = _mk_ring(st, nc, "p6wg", 2, [128, 8, 1024], BF16)
        wur = _mk_ring(st, nc, "p6wu", 2, [128, 8, 1024], BF16)
        wdr = _mk_ring(st, nc, "p6wd", 2, [128, 8, D], BF16)
        gfr = _mk_ring(st, nc, "p6gf", 2, [128, CAP], F32)
        wst = _mk_ring(st, nc, "p6wst", 4, [128, 1024], F32)
        psm = _mk_ring(st, nc, "p6psm", 8, [128, 512], F32, psum=True)

        def load_gu(ex):
            bufs = []
            for fh in range(2):
                wg, t_wg = wgr.next()
                wu, t_wu = wur.next()
                for kc in range(8):
                    for (wdst, t_wdst, wsrc) in ((wg, t_wg, c.w_gate), (wu, t_wu, c.w_up)):
                        stg, t_stg = wst.next()
                        S.dma("sp", lambda e, stg=stg, wsrc=wsrc, kc=kc, ex=ex, fh=fh: e.dma_start(
                            out=stg[:], in_=wsrc[ex, kc * 128:(kc + 1) * 128, fh * 1024:(fh + 1) * 1024]), writes=[t_stg])
                        S.op("act", lambda e, stg=stg, wdst=wdst, kc=kc: e.copy(wdst[:, kc, :], stg[:]), reads=[t_stg], writes=[t_wdst])
                bufs.append(((wg, t_wg), (wu, t_wu)))
            return bufs

        def load_wd(ex):
            bufs = []
            for fh in range(2):
                wd, t_wd = wdr.next()
                for fl in range(8):
                    fc = fh * 8 + fl
                    stg, t_stg = wst.next()
                    S.dma("sp", lambda e, stg=stg, fc=fc, ex=ex: e.dma_start(out=stg[:], in_=c.w_down[ex, fc * 128:(fc + 1) * 128, :]),
                          writes=[t_stg])
                    S.op("act", lambda e, stg=stg, wd=wd, fl=fl: e.copy(wd[:, fl, :], stg[:]), reads=[t_stg], writes=[t_wd])
                bufs.append((wd, t_wd))
            return bufs

        gu_bufs = None
        for ex in range(NE):
            if ex == 0:
                gu_bufs = load_gu(0)
            wd_bufs = load_wd(ex)
            for i in range(NT):
                eng = "dve" if i % 2 == 0 else "pool"
                S.op(eng, lambda e, i=i, ex=ex: e.tensor_scalar(out=sel[:, i, :], in0=iota[:], scalar1=ptok[:, i, ex:ex + 1], scalar2=None,
                                                                 op0=ALU.is_equal), reads=[t_iota, t_ptok], writes=[t_sel])
            for half in range(2):
                accs = [psm.next() for _ in range(4)]
                for i4 in range(NT // 4):
                    ht, t_h = h2r.next()
                    S.dma("sp", lambda e, ht=ht, i4=i4: e.dma_start(
                        out=ht[:], in_=c.h2tok_d[i4 * 512:(i4 + 1) * 512, :].rearrange("(a p) d -> p a d", p=128)), reads=[c.t_h2], writes=[t_h])
                    for ii in range(4):
                        i = i4 * 4 + ii
                        for dl in range(4):
                            dc = half * 4 + dl
                            ps, t_ps = accs[dl]
                            S.op("pe", lambda e, ps=ps, ht=ht, dc=dc, i=i, ii=ii: e.matmul(ps[:], ht[:, ii, dc * 128:(dc + 1) * 128], sel[:, i, :],
                                                                                          start=(i == 0), stop=(i == NT - 1)),
                                 reads=[t_h, t_sel], writes=[t_ps])
                for dl in range(4):
                    dc = half * 4 + dl
                    ps, t_ps = accs[dl]
                    S.op("act", lambda e, ps=ps, dc=dc: e.copy(xsT[:, dc, :], ps[:]), reads=[t_ps], writes=[t_xsT])
            for fh in range(2):
                (wg, t_wg), (wu, t_wu) = gu_bufs[fh]
                for fl in range(8):
                    fc = fh * 8 + fl
                    fs = slice(fl * 128, (fl + 1) * 128)
                    pg, t_pg = psm.next()
                    pu, t_pu = psm.next()
                    for kc in range(8):
                        S.op("pe", lambda e, pg=pg, wg=wg, kc=kc, fs=fs: e.matmul(pg[:], wg[:, kc, fs], xsT[:, kc, :], start=(kc == 0), stop=(kc == 7)),
                             reads=[t_wg, t_xsT], writes=[t_pg])
                    for kc in range(8):
                        S.op("pe", lambda e, pu=pu, wu=wu, kc=kc, fs=fs: e.matmul(pu[:], wu[:, kc, fs], xsT[:, kc, :], start=(kc == 0), stop=(kc == 7)),
                             reads=[t_wu, t_xsT], writes=[t_pu])
                    gf, t_gf = gfr.next()
                    S.op("act", lambda e, gf=gf, pg=pg: e.activation(gf[:], pg[:], AF.Silu), reads=[t_pg], writes=[t_gf])
                    S.op("dve", lambda e, gf=gf, pu=pu, fc=fc: e.tensor_tensor(out=actT[:, fc, :], in0=pu[:], in1=gf[:], op=ALU.mult),
                         reads=[t_pu, t_gf], writes=[t_actT])
            if ex + 1 < NE:
                gu_bufs = load_gu(ex + 1)
            accs = [psm.next() for _ in range(8)]
            for fh in range(2):
                wd, t_wd = wd_bufs[fh]
                for fl in range(8):
                    fc = fh * 8 + fl
                    for sc in range(4):
                        for dh in range(2):
                            ps, t_ps = accs[sc * 2 + dh]
                            S.op("pe", lambda e, ps=ps, fc=fc, sc=sc, wd=wd, fl=fl, dh=dh: e.matmul(
                                ps[:], actT[:, fc, sc * 128:(sc + 1) * 128], wd[:, fl, dh * 512:(dh + 1) * 512], start=(fc == 0), stop=(fc == 15)),
                                reads=[t_actT, t_wd], writes=[t_ps])
            for sc in range(4):
                for dh in range(2):
                    ps, t_ps = accs[sc * 2 + dh]
                    eng = "act" if dh == 0 else "dve"
                    if eng == "act":
                        S.op("act", lambda e, ps=ps, sc=sc, dh=dh: e.copy(yes[:, sc, dh * 512:(dh + 1) * 512], ps[:]), reads=[t_ps], writes=[t_yes])
                    else:
                        S.op("dve", lambda e, ps=ps, sc=sc, dh=dh: e.tensor_copy(yes[:, sc, dh * 512:(dh + 1) * 512], ps[:]), reads=[t_ps], writes=[t_yes])
            S.dma("sp", lambda e, ex=ex: e.dma_start(out=c.ye_d[ex].rearrange("(sc p) d -> p sc d", p=128), in_=yes[:]),
                  reads=[t_yes], writes=[c.t_ye])
    S.barrier()


def phase6b(c):
    nc, S = c.nc, c.S
    TC = c.t_const
    with ExitStack() as st:
        sb = lambda name, shape, dt: st.enter_context(nc.sbuf_tensor(name, shape, dt))
        yall = sb("p7ye", [128, NE, 4, D], BF16)
        t_yall = Tok()
        for ex in range(NE):
            S.dma("sp", lambda e, ex=ex: e.dma_start(out=yall[:, ex, :, :], in_=c.ye_d[ex].rearrange("(sc p) d -> p sc d", p=128)),
                  reads=[c.t_ye], writes=[Tok()])
        S.barrier()
        aff = sb("p7aff", [128, NT, NE], F32)
        t_aff = Tok()
        S.dma("sp", lambda e: e.dma_start(out=aff[:], in_=c.aff_d), reads=[c.t_aff], writes=[t_aff])
        nfw = sb("p7nfw", [128, D], F32)
        t_nfw = Tok()
        S.dma("sp", lambda e: e.dma_start(out=nfw[:], in_=c.norm_f_w.broadcast_to([128, D])), writes=[t_nfw])
        pidx = sb("p7pidx", [128, 4], F32)
        t_pidx = Tok()
        S.op("pool", lambda e: e.iota(pidx[:], pattern=[[128, 4]], base=0, channel_multiplier=1, allow_small_or_imprecise_dtypes=True),
             writes=[t_pidx])
        posbc = sb("p7posbc", [128, NE, 512], F32)
        t_posbc = Tok()
        selT = _mk_ring(st, nc, "p7selT", 8, [128, 512], BF16)
        accr = _mk_ring(st, nc, "p7acc", 4, [128, D], F32)
        sqr = _mk_ring(st, nc, "p7sq", 2, [128, D], BF16)
        ssr = _mk_ring(st, nc, "p7ss", 4, [128, 2], F32)
        psm = _mk_ring(st, nc, "p7psm", 6, [128, 512], F32, psum=True)
        for g in range(NG):
            gs = slice(g * 512, (g + 1) * 512)
            S.dma("sp", lambda e, gs=gs: e.dma_start(out=posbc[:], in_=c.pos_d[:, gs].unsqueeze(0).broadcast_to([128, NE, 512])),
                  reads=[c.t_pos], writes=[t_posbc])
            accs = []
            for tl in range(4):
                i = g * 4 + tl
                acc, t_acc = accr.next()
                S.dma("sp", lambda e, acc=acc, i=i: e.dma_start(out=acc[:], in_=c.x1_d[i * 128:(i + 1) * 128, :]), reads=[c.t_x1], writes=[t_acc])
                accs.append((acc, t_acc))
            def build_sts(ex):
                sts = []
                for sc in range(4):
                    sT, t_sT = selT.next()
                    eng = "dve" if sc % 2 == 0 else "pool"
                    S.op(eng, lambda e, sT=sT, ex=ex, sc=sc: e.tensor_scalar(out=sT[:], in0=posbc[:, ex, :], scalar1=pidx[:, sc:sc + 1], scalar2=None,
                                                                            op0=ALU.is_equal), reads=[t_posbc, t_pidx], writes=[t_sT])
                    sts.append((sT, t_sT))
                return sts
            nxt_sts = build_sts(0)
            for ex in range(NE):
                sts = nxt_sts
                if ex + 1 < NE:
                    nxt_sts = build_sts(ex + 1)
                for tl in range(4):
                    i = g * 4 + tl
                    acc, t_acc = accs[tl]
                    for dh in range(2):
                        ps, t_ps = psm.next()
                        for sc in range(4):
                            sT, t_sT = sts[sc]
                            S.op("pe", lambda e, ps=ps, sT=sT, tl=tl, ex=ex, sc=sc, dh=dh: e.matmul(
                                ps[:], sT[:, tl * 128:(tl + 1) * 128], yall[:, ex, sc, dh * 512:(dh + 1) * 512], start=(sc == 0), stop=(sc == 3)),
                                reads=[t_sT], writes=[t_ps])
                        S.op("dve", lambda e, acc=acc, ps=ps, i=i, ex=ex, dh=dh: e.scalar_tensor_tensor(
                            out=acc[:, dh * 512:(dh + 1) * 512], in0=ps[:], scalar=aff[:, i, ex:ex + 1], in1=acc[:, dh * 512:(dh + 1) * 512],
                            op0=ALU.mult, op1=ALU.add), reads=[t_ps, t_aff, t_acc], writes=[t_acc])
            for tl in range(4):
                i = g * 4 + tl
                acc, t_acc = accs[tl]
                sq, t_sq = sqr.next()
                ss, t_ss = ssr.next()
                S.op("act", lambda e, acc=acc, sq=sq, ss=ss: e.activation(sq[:], acc[:], AF.Square, accum_out=ss[:, 0:1]),
                     reads=[t_acc], writes=[t_sq, t_ss])
                S.op("act", lambda e, ss=ss: e.activation(ss[:, 1:2], ss[:, 0:1], AF.Sqrt, bias=EPS, scale=1.0 / D), reads=[t_ss], writes=[t_ss])
                S.op("dve", lambda e, ss=ss: e.reciprocal(ss[:, 1:2], ss[:, 1:2]), reads=[t_ss], writes=[t_ss])
                S.op("dve", lambda e, acc=acc, ss=ss: e.scalar_tensor_tensor(
                    out=acc[:], in0=acc[:], scalar=ss[:, 1:2], in1=nfw[:], op0=ALU.mult, op1=ALU.mult),
                    reads=[t_acc, t_ss, t_nfw], writes=[t_acc])
                S.dma("sp", lambda e, acc=acc, i=i: e.dma_start(out=c.out[i * 128:(i + 1) * 128, :], in_=acc[:]), reads=[t_acc], writes=[c.t_out])
    S.barrier()


ALL_PHASES = ("p1", "p2", "p3", "p4", "p5", "p6a", "p6b")


def kernel(**inputs):
    n_cores = 8
    shared = host_prep(inputs)
    nc, c = build_program(debug=False, phases=ALL_PHASES)
    x = np.asarray(inputs["x"], dtype=np.float32)
    in_maps = []
    for b in range(n_cores):
        m = dict(shared)
        m["x"] = np.ascontiguousarray(x[b])
        in_maps.append({k: m[k] for k in c.input_names})
    res = run_bass_kernel_spmd(nc, in_maps, core_ids=list(range(n_cores)))
    out = np.stack([np.asarray(r["out"], dtype=np.float32) for r in res.results], axis=0)
    return out
```

```python
import math
from contextlib import ExitStack
import numpy as np
import concourse.bass as bass
import concourse.mybir as mybir
from concourse.bass_utils import run_bass_kernel_spmd

F32 = mybir.dt.float32
BF16 = mybir.dt.bfloat16
AF = mybir.ActivationFunctionType
ALU = mybir.AluOpType

S_LEN = 4096
D = 1024
NT = S_LEN // 128
NG = S_LEN // 512
H = 8
IN_W = 9248
C_QA, C_KA, C_VA, C_GQ, C_GK, C_GV, C_Z, C_SM, C_GA, C_GG = 0, 1024, 2048, 3072, 4096, 5120, 6144, 7168, 7200, 8224
NE = 16
FF = 2048
CAP = 512
EPS = 1e-6
LAMBDA_INIT = 0.8 - 0.6 * math.exp(-0.3 * 0)

ENGS = ("pe", "act", "dve", "pool", "sp")


class Tok:
    __slots__ = ("w", "r", "excl")

    def __init__(self, excl=False):
        self.w = None
        self.r = {}
        self.excl = excl


class Sched:
    NDMA = 32

    def __init__(self, nc):
        self.nc = nc
        self.sems = {}
        for e in ENGS:
            self.sems[e] = nc.alloc_semaphore(name="sem_" + e)
        for i in range(self.NDMA):
            self.sems[("d", i)] = nc.alloc_semaphore(name="sem_dma%d" % i)
        self.cnt = {k: 0 for k in self.sems}
        self.seen = {e: {} for e in ENGS}
        self.prog = {e: [] for e in ENGS}
        self.ndma = 0
        self.ninstr = 0

    def _deps(self, reads, writes):
        deps = {}
        for t in reads:
            if t.w is not None:
                k, v = t.w
                if deps.get(k, 0) < v:
                    deps[k] = v
        for t in writes:
            if t.w is not None:
                k, v = t.w
                if deps.get(k, 0) < v:
                    deps[k] = v
            for k, v in t.r.items():
                if deps.get(k, 0) < v:
                    deps[k] = v
        return deps

    def _emit_waits(self, e, deps):
        seen = self.seen[e]
        for k, v in deps.items():
            if seen.get(k, 0) >= v:
                continue
            seen[k] = v
            sem = self.sems[k]
            self.prog[e].append(lambda eng, sem=sem, v=v: eng.wait_ge(sem, v))

    def op(self, e, fn, reads=(), writes=()):
        if any(t.excl for t in reads):
            writes = list(writes) + [t for t in reads if t.excl]
            reads = [t for t in reads if not t.excl]
        deps = self._deps(reads, writes)
        if e == "pe":
            deps.pop("pe", None)
        self._emit_waits(e, deps)
        sem = self.sems[e]
        self.cnt[e] += 1
        n = self.cnt[e]
        self.prog[e].append(lambda eng, fn=fn, sem=sem: fn(eng).then_inc(sem, 1))
        for t in reads:
            if t.r.get(e, 0) < n:
                t.r[e] = n
        for t in writes:
            t.w = (e, n)
            t.r = {}
        self.ninstr += 1

    def dma(self, e, fn, reads=(), writes=()):
        i = self.ndma % self.NDMA
        self.ndma += 1
        k = ("d", i)
        deps = self._deps(reads, writes)
        if self.cnt[k] > 0:
            deps[k] = max(deps.get(k, 0), self.cnt[k])
        self._emit_waits(e, deps)
        self.cnt[k] += 16
        v = self.cnt[k]
        sem = self.sems[k]
        self.prog[e].append(lambda eng, fn=fn, sem=sem: fn(eng).then_inc(sem, 16))
        for t in reads:
            if t.r.get(k, 0) < v:
                t.r[k] = v
        for t in writes:
            t.w = (k, v)
            t.r = {}
        self.ninstr += 1

    def barrier(self):
        deps = {k: v for k, v in self.cnt.items() if v > 0}
        for e in ENGS:
            self._emit_waits(e, dict(deps))

    def emit(self):
        nc = self.nc
        prog = self.prog
        with nc.Block() as block:
            @block.tensor
            def _(eng):
                for f in prog["pe"]:
                    f(eng)

            @block.scalar
            def _(eng):
                for f in prog["act"]:
                    f(eng)

            @block.vector
            def _(eng):
                for f in prog["dve"]:
                    f(eng)

            @block.gpsimd
            def _(eng):
                for f in prog["pool"]:
                    f(eng)

            @block.sync
            def _(eng):
                for f in prog["sp"]:
                    f(eng)


class Ring:
    def __init__(self, items):
        self.items = items
        self.i = 0

    def next(self):
        it = self.items[self.i % len(self.items)]
        self.i += 1
        return it


class Ctx:
    pass


def _interleave(gens, width):
    it = iter(gens)
    active = []
    exhausted = False
    while True:
        while len(active) < width and not exhausted:
            try:
                active.append(next(it))
            except StopIteration:
                exhausted = True
        if not active:
            break
        for g in list(active):
            try:
                next(g)
            except StopIteration:
                active.remove(g)


def _mk_ring(stack, nc, name, n, shape, dt, psum=False):
    items = []
    for i in range(n):
        if psum:
            t = stack.enter_context(nc.psum_tensor("%s%d" % (name, i), shape, dt))
        else:
            t = stack.enter_context(nc.sbuf_tensor("%s%d" % (name, i), shape, dt))
        items.append((t, Tok()))
    return Ring(items)


def _consts(c, stack):
    nc, S = c.nc, c.S
    c.ident_f = nc.alloc_sbuf_tensor("ident_f", [128, 128], F32)
    c.ident_b = nc.alloc_sbuf_tensor("ident_b", [128, 128], BF16)
    c.ones_b = nc.alloc_sbuf_tensor("ones_b", [128, 128], BF16)
    c.ones_f = nc.alloc_sbuf_tensor("ones_f", [128, 128], F32)
    c.t_const = Tok()
    tc = c.t_const
    S.op("pool", lambda e: e.memset(c.ident_f[:], 0.0), writes=[tc])
    S.op("pool", lambda e: e.affine_select(out=c.ident_f[:], in_=c.ident_f[:], compare_op=ALU.not_equal, fill=1.0,
                                           base=0, pattern=[[-1, 128]], channel_multiplier=1),
         reads=[tc], writes=[tc])
    S.op("pool", lambda e: e.tensor_copy(c.ident_b[:], c.ident_f[:]), reads=[tc], writes=[tc])
    S.op("pool", lambda e: e.memset(c.ones_b[:], 1.0), writes=[tc])
    S.op("pool", lambda e: e.memset(c.ones_f[:], 1.0), writes=[tc])


def phase1(c):
    nc, S = c.nc, c.S
    TC = c.t_const
    with ExitStack() as st:
        sb = lambda name, shape, dt: st.enter_context(nc.sbuf_tensor(name, shape, dt))
        hT = sb("hT", [128, 8, S_LEN], BF16)
        t_hT = [Tok() for _ in range(NT)]
        stA = ExitStack()
        sbA = lambda name, shape, dt: stA.enter_context(nc.sbuf_tensor(name, shape, dt))
        n1w = sbA("n1w", [128, D], F32)
        t_n1w = Tok()
        S.dma("sp", lambda e: e.dma_start(out=n1w[:], in_=c.norm1_w.broadcast_to([128, D])), writes=[t_n1w])
        xring = _mk_ring(stA, nc, "p1x", 3, [128, D], F32)
        sqring = _mk_ring(stA, nc, "p1sq", 2, [128, D], BF16)
        hring = _mk_ring(stA, nc, "p1h", 2, [128, D], BF16)
        ssring = _mk_ring(stA, nc, "p1ss", 4, [128, 2], F32)
        pst = _mk_ring(st, nc, "p1pst", 2, [128, 8, 128], BF16, psum=True)
        psm = _mk_ring(st, nc, "p1psm", 5, [128, 512], F32, psum=True)

        for i in range(NT):
            xt, t_x = xring.next()
            S.dma("sp", lambda e, xt=xt, i=i: e.dma_start(out=xt[:], in_=c.x[i * 128:(i + 1) * 128, :]), writes=[t_x])
            sq, t_sq = sqring.next()
            ss, t_ss = ssring.next()
            S.op("act", lambda e, xt=xt, sq=sq, ss=ss: e.activation(sq[:], xt[:], AF.Square, accum_out=ss[:, 0:1]),
                 reads=[t_x], writes=[t_sq, t_ss])
            S.op("act", lambda e, ss=ss: e.activation(ss[:, 1:2], ss[:, 0:1], AF.Sqrt, bias=EPS, scale=1.0 / D),
                 reads=[t_ss], writes=[t_ss])
            S.op("dve", lambda e, ss=ss: e.reciprocal(ss[:, 1:2], ss[:, 1:2]), reads=[t_ss], writes=[t_ss])
            ht, t_h = hring.next()
            S.op("dve", lambda e, ht=ht, xt=xt, ss=ss: e.scalar_tensor_tensor(
                out=ht[:], in0=xt[:], scalar=ss[:, 1:2], in1=n1w[:], op0=ALU.mult, op1=ALU.mult),
                reads=[t_x, t_ss, t_n1w], writes=[t_h])
            pt, t_pt = pst.next()
            for dc in range(8):
                S.op("pe", lambda e, pt=pt, ht=ht, dc=dc: e.transpose(pt[:, dc, :], ht[:, dc * 128:(dc + 1) * 128], c.ident_b[:]),
                     reads=[t_h, TC], writes=[t_pt])
            S.op("act", lambda e, pt=pt, i=i: e.copy(hT[:, :, i * 128:(i + 1) * 128], pt[:]),
                 reads=[t_pt], writes=[t_hT[i]])

        S.barrier()
        stA.close()
        wfm = _mk_ring(st, nc, "p1wfm", 4, [128, 8, 128], BF16)
        wtm = _mk_ring(st, nc, "p1wtm", 2, [128, 8, 512], BF16)

        def load_w(ring, src, col0, ncols):
            wt, t_w = ring.next()
            S.dma("pool", lambda e: e.dma_start(
                out=wt[:, :, 0:ncols], in_=src[:, col0:col0 + ncols].rearrange("(kc p) n -> p kc n", p=128)),
                writes=[t_w])
            return wt, t_w

        def proj_fm(wt, t_w, g):
            ps, t_ps = psm.next()
            for kc in range(8):
                S.op("pe", lambda e, ps=ps, kc=kc: e.matmul(ps[:], wt[:, kc, :], hT[:, kc, g * 512:(g + 1) * 512],
                                                          start=(kc == 0), stop=(kc == 7)),
                     reads=[t_w] + t_hT[g * 4:(g + 1) * 4], writes=[t_ps])
            return ps, t_ps

        def proj_tm(wt, t_w, i, ncols):
            ps, t_ps = psm.next()
            for kc in range(8):
                S.op("pe", lambda e, ps=ps, kc=kc: e.matmul(ps[:, 0:ncols], hT[:, kc, i * 128:(i + 1) * 128], wt[:, kc, 0:ncols],
                                                          start=(kc == 0), stop=(kc == 7)),
                     reads=[t_w, t_hT[i]], writes=[t_ps])
            return ps, t_ps

        stage = _mk_ring(st, nc, "p1stage", 2, [128, S_LEN], BF16)
        f32a = _mk_ring(st, nc, "p1f32a", 3, [128, 512], F32)
        f32b = _mk_ring(st, nc, "p1f32b", 3, [128, 512], F32)

        stB = ExitStack()
        cosT = stB.enter_context(nc.sbuf_tensor("cosT_sb", [128, S_LEN], F32))
        sinT = stB.enter_context(nc.sbuf_tensor("sinT_sb", [128, S_LEN], F32))
        t_rope = Tok()
        S.dma("sp", lambda e: e.dma_start(out=cosT[:], in_=c.cosT), writes=[t_rope])
        S.dma("sp", lambda e: e.dma_start(out=sinT[:], in_=c.sinT), writes=[t_rope])

        for (col0, dst) in ((C_QA, c.qT_d), (C_KA, c.kT_d)):
            for h in range(H):
                w1, t_w1 = load_w(wfm, c.w_in, col0 + h * 128, 128)
                w2, t_w2 = load_w(wfm, c.w_qkp, (col0 // 1024) * 1024 + h * 128, 128)
                stg, t_stg = stage.next()
                for g in range(NG):
                    ps1, t_ps1 = proj_fm(w1, t_w1, g)
                    ps2, t_ps2 = proj_fm(w2, t_w2, g)
                    a, t_a = f32a.next()
                    b, t_b = f32b.next()
                    sl = slice(g * 512, (g + 1) * 512)
                    S.op("dve", lambda e, a=a, ps1=ps1, sl=sl: e.tensor_tensor(out=a[:], in0=ps1[:], in1=cosT[:, sl], op=ALU.mult),
                         reads=[t_ps1, t_rope], writes=[t_a])
                    S.op("dve", lambda e, b=b, ps2=ps2, sl=sl: e.tensor_tensor(out=b[:], in0=ps2[:], in1=sinT[:, sl], op=ALU.mult),
                         reads=[t_ps2, t_rope], writes=[t_b])
                    S.op("pool", lambda e, a=a, b=b, stg=stg, sl=sl: e.tensor_tensor(out=stg[:, sl], in0=a[:], in1=b[:], op=ALU.add),
                         reads=[t_a, t_b], writes=[t_stg])
                S.dma("sp", lambda e, stg=stg, dst=dst, h=h: e.dma_start(out=dst[h], in_=stg[:]), reads=[t_stg], writes=[c.t_qk])

        S.barrier()
        stB.close()
        for (col0, dst) in ((C_GA, c.sga_d), (C_GG, c.sgg_d)):
            for h in range(8):
                w1, t_w1 = load_w(wfm, c.w_in, col0 + h * 128, 128)
                stg, t_stg = stage.next()
                for g in range(NG):
                    ps1, t_ps1 = proj_fm(w1, t_w1, g)
                    sl = slice(g * 512, (g + 1) * 512)
                    S.op("act", lambda e, ps1=ps1, stg=stg, sl=sl: e.activation(stg[:, sl], ps1[:], AF.Sigmoid),
                         reads=[t_ps1], writes=[t_stg])
                S.dma("sp", lambda e, stg=stg, dst=dst, h=h: e.dma_start(out=dst[h], in_=stg[:]), reads=[t_stg], writes=[c.t_gates])

        tmst = _mk_ring(st, nc, "p1tmst", 3, [128, 512], BF16)
        for (col0, dst, fn) in ((C_VA, c.v_d, None), (C_Z, c.zs_d, AF.Silu)):
            for half in range(2):
                wt, t_w = load_w(wtm, c.w_in, col0 + half * 512, 512)
                for i in range(NT):
                    ps, t_ps = proj_tm(wt, t_w, i, 512)
                    o, t_o = tmst.next()
                    if fn is None:
                        S.op("act", lambda e, o=o, ps=ps: e.copy(o[:], ps[:]), reads=[t_ps], writes=[t_o])
                    else:
                        S.op("act", lambda e, o=o, ps=ps, fn=fn: e.activation(o[:], ps[:], fn), reads=[t_ps], writes=[t_o])
                    S.dma("sp", lambda e, o=o, dst=dst, i=i, half=half: e.dma_start(
                        out=dst[i * 128:(i + 1) * 128, half * 512:(half + 1) * 512], in_=o[:]), reads=[t_o], writes=[c.t_vz])

        sm = sb("p1sm", [128, NT, 32], F32)
        t_sm = Tok()
        wt, t_w = load_w(wtm, c.w_in, C_SM, 32)
        for i in range(NT):
            ps, t_ps = proj_tm(wt, t_w, i, 32)
            S.op("dve", lambda e, ps=ps, i=i: e.tensor_copy(sm[:, i, :], ps[:, 0:32]), reads=[t_ps], writes=[t_sm])
        prm = sb("p1prm", [128, 32], F32)
        t_prm = Tok()
        for j, src in enumerate((c.dt_bias_fwd, c.dt_bias_bwd, c.a_log_fwd, c.a_log_bwd)):
            S.dma("sp", lambda e, j=j, src=src: e.dma_start(out=prm[:, j * 8:(j + 1) * 8], in_=src.broadcast_to([128, 8])),
                  writes=[t_prm])
        tmpa = sb("p1tmpa", [128, NT, 16], F32)
        tmpb = sb("p1tmpb", [128, NT, 16], F32)
        t_ta, t_tb = Tok(), Tok()
        dtb = prm[:, 0:16].unsqueeze(1).broadcast_to([128, NT, 16])
        S.op("act", lambda e: e.activation(prm[:, 16:32], prm[:, 16:32], AF.Exp), reads=[t_prm], writes=[t_prm])
        nA = prm[:, 16:32].unsqueeze(1).broadcast_to([128, NT, 16])
        S.op("dve", lambda e: e.tensor_tensor(out=sm[:, :, 0:16], in0=sm[:, :, 0:16], in1=dtb, op=ALU.add),
             reads=[t_sm, t_prm], writes=[t_sm])
        S.op("act", lambda e: e.activation(tmpa[:], sm[:, :, 0:16], AF.Abs), reads=[t_sm], writes=[t_ta])
        S.op("act", lambda e: e.activation(tmpa[:], tmpa[:], AF.Exp, scale=-1.0), reads=[t_ta], writes=[t_ta])
        S.op("act", lambda e: e.activation(tmpa[:], tmpa[:], AF.Ln, bias=1.0), reads=[t_ta], writes=[t_ta])
        S.op("dve", lambda e: e.scalar_tensor_tensor(out=tmpb[:], in0=sm[:, :, 0:16], scalar=0.0, in1=tmpa[:],
                                                     op0=ALU.max, op1=ALU.add), reads=[t_sm, t_ta], writes=[t_tb])
        S.op("dve", lambda e: e.scalar_tensor_tensor(out=sm[:, :, 0:16], in0=tmpb[:], scalar=-1.0, in1=nA,
                                                     op0=ALU.mult, op1=ALU.mult), reads=[t_tb, t_prm], writes=[t_sm])
        S.op("act", lambda e: e.activation(sm[:, :, 16:32], sm[:, :, 16:32], AF.Sigmoid), reads=[t_sm], writes=[t_sm])
        S.dma("sp", lambda e: e.dma_start(out=c.small_d, in_=sm[:]), reads=[t_sm], writes=[c.t_small])

        cw5 = sb("p1cw5", [5, 3072], F32)
        t_cw5 = Tok()
        S.dma("sp", lambda e: e.dma_start(out=cw5[:], in_=c.conv_w), writes=[t_cw5])
        cwT = sb("p1cwT", [128, 24, 8], F32)
        t_cwT = Tok()
        for ct in range(24):
            ps, t_ps = psm.next()
            S.op("pe", lambda e, ps=ps, ct=ct: e.transpose(ps[:, 0:5], cw5[0:5, ct * 128:(ct + 1) * 128], c.ident_f[0:5, 0:5]),
                 reads=[t_cw5, TC], writes=[t_ps])
            S.op("dve", lambda e, ps=ps, ct=ct: e.tensor_copy(cwT[:, ct, 0:5], ps[:, 0:5]), reads=[t_ps], writes=[t_cwT])
        dgs = _mk_ring(st, nc, "p1dg", 2, [128, 5, 128], BF16)
        xpre = _mk_ring(st, nc, "p1xpre", 2, [128, S_LEN + 4], BF16)
        tmstage = _mk_ring(st, nc, "p1tmstage", 2, [128, NT, 128], BF16)
        for ct in range(24):
            kind = ct // 8
            h = ct % 8
            w1, t_w1 = load_w(wfm, c.w_in, C_GQ + ct * 128, 128)
            dg, t_dg = dgs.next()
            for j in range(5):
                S.op("pool", lambda e, dg=dg, j=j, ct=ct: e.tensor_scalar(
                    out=dg[:, j, :], in0=c.ident_f[:], scalar1=cwT[:, ct, j:j + 1], scalar2=None, op0=ALU.mult),
                    reads=[TC, t_cwT], writes=[t_dg])
            xp, t_xp = xpre.next()
            S.op("pool", lambda e, xp=xp: e.memset(xp[:, 0:2], 0.0), writes=[t_xp])
            S.op("pool", lambda e, xp=xp: e.memset(xp[:, S_LEN + 2:S_LEN + 4], 0.0), writes=[t_xp])
            for g in range(NG):
                ps1, t_ps1 = proj_fm(w1, t_w1, g)
                S.op("act", lambda e, xp=xp, ps1=ps1, g=g: e.copy(xp[:, 2 + g * 512:2 + (g + 1) * 512], ps1[:]),
                     reads=[t_ps1], writes=[t_xp])
            stg, t_stg = stage.next()
            for g in range(NG):
                ps, t_ps = psm.next()
                for j in range(5):
                    S.op("pe", lambda e, ps=ps, dg=dg, xp=xp, g=g, j=j: e.matmul(
                        ps[:], dg[:, j, :], xp[:, g * 512 + j:g * 512 + j + 512], start=(j == 0), stop=(j == 4)),
                        reads=[t_dg, t_xp], writes=[t_ps])
                sl = slice(g * 512, (g + 1) * 512)
                if kind == 2:
                    S.op("act", lambda e, ps=ps, stg=stg, sl=sl: e.activation(stg[:, sl], ps[:], AF.Silu),
                         reads=[t_ps], writes=[t_stg])
                else:
                    a, t_a = f32a.next()
                    S.op("act", lambda e, ps=ps, a=a: e.activation(a[:], ps[:], AF.Silu), reads=[t_ps], writes=[t_a])
                    sqb, t_sqb = tmst.next()
                    S.op("pool", lambda e, sqb=sqb, a=a: e.tensor_tensor(out=sqb[:], in0=a[:], in1=a[:], op=ALU.mult),
                         reads=[t_a], writes=[t_sqb])
                    ps2, t_ps2 = psm.next()
                    S.op("pe", lambda e, ps2=ps2, sqb=sqb: e.matmul(ps2[:], c.ones_b[:], sqb[:], start=True, stop=True),
                         reads=[t_sqb, TC], writes=[t_ps2])
                    b, t_b = f32b.next()
                    S.op("act", lambda e, b=b, ps2=ps2: e.activation(b[:], ps2[:], AF.Sqrt, bias=EPS), reads=[t_ps2], writes=[t_b])
                    S.op("dve", lambda e, b=b: e.reciprocal(b[:], b[:]), reads=[t_b], writes=[t_b])
                    scl = (128.0 ** -0.5) if kind == 0 else 1.0
                    S.op("dve", lambda e, a=a, b=b, stg=stg, sl=sl, scl=scl: e.scalar_tensor_tensor(
                        out=stg[:, sl], in0=a[:], scalar=scl, in1=b[:], op0=ALU.mult, op1=ALU.mult),
                        reads=[t_a, t_b], writes=[t_stg])
            if kind < 2:
                dst = c.gq_d if kind == 0 else c.gk_d
                S.dma("sp", lambda e, stg=stg, dst=dst, h=h: e.dma_start(out=dst[h], in_=stg[:]), reads=[t_stg], writes=[c.t_gqk])
            if kind >= 1:
                dst = c.gktok_d if kind == 1 else c.gvtok_d
                tms, t_tms = tmstage.next()
                for i8 in range(4):
                    pt, t_pt = pst.next()
                    for ii in range(8):
                        i = i8 * 8 + ii
                        S.op("pe", lambda e, pt=pt, stg=stg, ii=ii, i=i: e.transpose(pt[:, ii, :], stg[:, i * 128:(i + 1) * 128], c.ident_b[:]),
                             reads=[t_stg, TC], writes=[t_pt])
                    S.op("dve", lambda e, pt=pt, tms=tms, i8=i8: e.tensor_copy(tms[:, i8 * 8:(i8 + 1) * 8, :], pt[:]),
                         reads=[t_pt], writes=[t_tms])
                S.dma("sp", lambda e, tms=tms, dst=dst, h=h: e.dma_start(
                    out=dst[:, h * 128:(h + 1) * 128].rearrange("(t p) c -> p t c", p=128), in_=tms[:]),
                    reads=[t_tms], writes=[c.t_gtok])
    S.barrier()


def build_program(debug=False, phases=("p1",), inject=(), opts=None):
    nc = bass.Bass("TRN2", target_bir_lowering=False)
    c = Ctx()
    c.inject = set(inject)
    c.opts = opts or {}
    c.nc = nc
    c.S = Sched(nc)

    c.input_names = []

    def din(name, shape):
        c.input_names.append(name)
        return nc.dram_tensor(name, list(shape), F32, kind="ExternalInput").ap()

    c.x = din("x", [S_LEN, D])
    c.norm1_w = din("norm1_w", [1, D])
    c.w_in = din("w_in", [D, IN_W])
    c.w_qkp = din("w_qkp", [D, 2048])
    c.cosT = din("cosT", [128, S_LEN])
    c.sinT = din("sinT", [128, S_LEN])
    c.conv_w = din("conv_w", [5, 3072])
    for n in ("a_log_fwd", "dt_bias_fwd", "a_log_bwd", "dt_bias_bwd"):
        setattr(c, n, din(n, [1, 8]))

    c.debug_names = []

    def scratch(name, shape, dt):
        kind = "ExternalOutput" if debug else "Internal"
        if name in c.inject:
            kind = "ExternalInput"
            c.input_names.append(name)
        elif debug:
            c.debug_names.append(name)
        return nc.dram_tensor(name, list(shape), dt, kind=kind).ap()

    c.qT_d = scratch("qT_d", [H, 128, S_LEN], BF16)
    c.kT_d = scratch("kT_d", [H, 128, S_LEN], BF16)
    c.v_d = scratch("v_d", [S_LEN, D], BF16)
    c.zs_d = scratch("zs_d", [S_LEN, D], BF16)
    c.sga_d = scratch("sga_d", [H, 128, S_LEN], BF16)
    c.sgg_d = scratch("sgg_d", [H, 128, S_LEN], BF16)
    c.gq_d = scratch("gq_d", [H, 128, S_LEN], BF16)
    c.gk_d = scratch("gk_d", [H, 128, S_LEN], BF16)
    c.gktok_d = scratch("gktok_d", [S_LEN, D], BF16)
    c.gvtok_d = scratch("gvtok_d", [S_LEN, D], BF16)
    c.small_d = scratch("small_d", [128, NT, 32], F32)
    for n in ("lambda_q1", "lambda_k1", "lambda_q2", "lambda_k2"):
        setattr(c, n, din(n, [1, 64]))
    c.subln_w = din("subln_w", [1, 128])
    c.gdn_norm_w = din("gdn_norm_w", [1, 128])
    c.w_proj_attn = din("w_proj_attn", [D, D])
    c.w_proj_gdn = din("w_proj_gdn", [D, D])
    c.w_out = din("w_out", [D, D])
    c.w_router = din("w_router", [D, NE])
    c.norm2_w = din("norm2_w", [1, D])
    c.norm_f_w = din("norm_f_w", [1, D])
    c.w_gate = din("w_gate", [NE, D, FF])
    c.w_up = din("w_up", [NE, D, FF])
    c.w_down = din("w_down", [NE, FF, D])
    c.out = nc.dram_tensor("out", [S_LEN, D], F32, kind="ExternalOutput").ap()
    c.x1_d = scratch("x1_d", [S_LEN, D], F32)
    c.h2tok_d = scratch("h2tok_d", [S_LEN, D], BF16)
    c.aff_d = scratch("aff_d", [128, NT, NE], F32)
    c.pos_d = scratch("pos_d", [NE, S_LEN], F32)
    c.ptok_d = scratch("ptok_d", [128, NT, NE], F32)
    c.ye_d = scratch("ye_d", [NE, CAP, D], BF16)
    for n in ("t_x1", "t_h2", "t_aff", "t_pos", "t_ye", "t_out"):
        setattr(c, n, Tok())
    c.ogT_d = scratch("ogT_d", [H, 128, S_LEN], BF16)
    c.t_og = Tok()
    c.oaT_d = scratch("oaT_d", [H, 128, S_LEN], BF16)
    for n in ("t_qk", "t_gates", "t_vz", "t_small", "t_gqk", "t_gtok", "t_oa"):
        setattr(c, n, Tok())

    with ExitStack() as st:
        _consts(c, st)
        if "p1" in phases:
            phase1(c)
        if "p2" in phases:
            phase2(c)
        if "p3" in phases:
            phase3(c)
        if "p4" in phases:
            phase4(c)
        if "p5" in phases:
            phase5(c)
        if "p6a" in phases:
            phase6a(c)
        if "p6b" in phases:
            phase6b(c)
        c.S.barrier()
        c.S.emit()
    return nc, c


def host_prep(inputs):
    f = lambda a: np.ascontiguousarray(np.asarray(a, dtype=np.float32))
    w_in = f(inputs["w_in"][0])
    perm = np.arange(2048).reshape(16, 2, 2, 32)[:, :, ::-1, :].reshape(-1)
    w_qkp = np.ascontiguousarray(w_in[:, :2048][:, perm])
    inv = 10000.0 ** (-np.arange(0, 64, 2, dtype=np.float32) / 64)
    ang = np.arange(S_LEN, dtype=np.float32)[:, None] * inv[None, :]
    ang = np.concatenate([ang, ang], axis=-1)
    cos = np.cos(ang).T.astype(np.float32)
    sin = np.sin(ang).T.astype(np.float32)
    sin[:32] *= -1.0
    shared = {
        "norm1_w": f(inputs["norm1_w"]).reshape(1, D),
        "w_in": w_in,
        "w_qkp": w_qkp,
        "cosT": np.ascontiguousarray(np.concatenate([cos, cos], 0)),
        "sinT": np.ascontiguousarray(np.concatenate([sin, sin], 0)),
        "conv_w": f(inputs["conv_w"][0]),
    }
    for n in ("a_log_fwd", "dt_bias_fwd", "a_log_bwd", "dt_bias_bwd"):
        shared[n] = f(inputs[n]).reshape(1, 8)
    for n in ("lambda_q1", "lambda_k1", "lambda_q2", "lambda_k2"):
        shared[n] = f(inputs[n]).reshape(1, 64)
    shared["subln_w"] = f(inputs["subln_w"]).reshape(1, 128)
    shared["gdn_norm_w"] = f(inputs["gdn_norm_w"]).reshape(1, 128)
    shared["w_proj_attn"] = f(inputs["w_proj_attn"][0])
    shared["w_proj_gdn"] = f(inputs["w_proj_gdn"][0])
    shared["w_out"] = f(inputs["w_out"][0])
    shared["w_router"] = f(inputs["w_router"][0])
    shared["norm2_w"] = f(inputs["norm2_w"]).reshape(1, D)
    shared["norm_f_w"] = f(inputs["norm_f_w"]).reshape(1, D)
    shared["w_gate"] = f(inputs["w_gate"][0])
    shared["w_up"] = f(inputs["w_up"][0])
    shared["w_down"] = f(inputs["w_down"][0])
    return shared


def phase2(c):
    nc, S = c.nc, c.S
    TC = c.t_const
    with ExitStack() as st:
        sb = lambda name, shape, dt: st.enter_context(nc.sbuf_tensor(name, shape, dt))
        lam = sb("p2lam", [128, 4, 64], F32)
        lsc = sb("p2lsc", [128, 8], F32)
        t_lam = Tok()
        for j, src in enumerate((c.lambda_q1, c.lambda_k1, c.lambda_q2, c.lambda_k2)):
            S.dma("sp", lambda e, j=j, src=src: e.dma_start(out=lam[:, j, :], in_=src.broadcast_to([128, 64])), writes=[t_lam])
        S.op("dve", lambda e: e.tensor_tensor(out=lam[:, 0, :], in0=lam[:, 0, :], in1=lam[:, 1, :], op=ALU.mult), reads=[t_lam], writes=[t_lam])
        S.op("dve", lambda e: e.tensor_tensor(out=lam[:, 2, :], in0=lam[:, 2, :], in1=lam[:, 3, :], op=ALU.mult), reads=[t_lam], writes=[t_lam])
        S.op("act", lambda e: e.activation(lam[:, 1, :], lam[:, 0, :], AF.Identity, accum_out=lsc[:, 0:1]), reads=[t_lam], writes=[t_lam])
        S.op("act", lambda e: e.activation(lam[:, 3, :], lam[:, 2, :], AF.Identity, accum_out=lsc[:, 1:2]), reads=[t_lam], writes=[t_lam])
        S.op("act", lambda e: e.activation(lsc[:, 2:4], lsc[:, 0:2], AF.Exp), reads=[t_lam], writes=[t_lam])
        S.op("dve", lambda e: e.scalar_tensor_tensor(out=lsc[:, 4:5], in0=lsc[:, 3:4], scalar=-LAMBDA_INIT, in1=lsc[:, 2:3],
                                                     op0=ALU.add, op1=ALU.subtract), reads=[t_lam], writes=[t_lam])
        wsub = sb("p2wsub", [128, 2], F32)
        t_wsub = Tok()
        S.dma("sp", lambda e: e.dma_start(out=wsub[:, 0:1], in_=c.subln_w.rearrange("o e -> e o")), writes=[t_wsub])
        S.op("dve", lambda e: e.tensor_scalar(out=wsub[:, 1:2], in0=wsub[:, 0:1], scalar1=(1.0 - LAMBDA_INIT), scalar2=None, op0=ALU.mult),
             reads=[t_wsub], writes=[t_wsub])

        qr = _mk_ring(st, nc, "p2q", 2, [128, S_LEN], BF16)
        kr = _mk_ring(st, nc, "p2k", 4, [128, S_LEN], BF16)
        for i_, (kb_, t_kb_) in enumerate(kr.items):
            zs_ = slice(64, 128) if i_ % 2 == 0 else slice(0, 64)
            S.op("pool", lambda e, kb_=kb_, zs_=zs_: e.memset(kb_[zs_, :], 0.0), writes=[t_kb_])
        vr = _mk_ring(st, nc, "p2v", 2, [128, NT, 128], BF16)
        pr = _mk_ring(st, nc, "p2p", 4, [128, 512], BF16)
        fr = _mk_ring(st, nc, "p2f", 6, [128, 512], F32)
        sqr = _mk_ring(st, nc, "p2sq", 2, [128, 512], BF16)
        accr = _mk_ring(st, nc, "p2acc", 4, [128, 512], F32)
        stage = _mk_ring(st, nc, "p2stage", 2, [128, S_LEN], BF16)
        ps_s = _mk_ring(st, nc, "p2pss", 3, [128, 512], F32, psum=True)
        ps_o = [_mk_ring(st, nc, "p2pso%d" % t, 1, [128, 512], F32, psum=True) for t in range(2)]
        ps_l = [_mk_ring(st, nc, "p2psl%d" % t, 1, [128, 512], F32, psum=True) for t in range(2)]
        ps_x = _mk_ring(st, nc, "p2psx", 1, [128, 512], F32, psum=True)

        for h in range(H):
            q, t_q = qr.next()
            k0, t_k0 = kr.next()
            k1, t_k1 = kr.next()
            kz = ((k0, t_k0), (k1, t_k1))
            v, t_v = vr.next()
            S.dma("sp", lambda e, q=q, h=h: e.dma_start(out=q[:], in_=c.qT_d[h]), reads=[c.t_qk], writes=[t_q])
            S.dma("sp", lambda e, k0=k0, h=h: e.dma_start(out=k0[0:64, :], in_=c.kT_d[h, 0:64, :]), reads=[c.t_qk], writes=[t_k0])
            S.dma("sp", lambda e, k1=k1, h=h: e.dma_start(out=k1[64:128, :], in_=c.kT_d[h, 64:128, :]), reads=[c.t_qk], writes=[t_k1])
            S.dma("sp", lambda e, v=v, h=h: e.dma_start(
                out=v[:], in_=c.v_d[:, h * 128:(h + 1) * 128].rearrange("(t p) e -> p t e", p=128)), reads=[c.t_vz], writes=[t_v])
            stg, t_stg = stage.next()
            for g in range(NG):
                qs = slice(g * 512, (g + 1) * 512)
                accs = []
                for t in range(2):
                    po, t_po = ps_o[t].next()
                    pl, t_pl = ps_l[t].next()
                    ts = slice(t * 64, (t + 1) * 64)
                    accA, t_accA = accr.next()
                    accB, t_accB = accr.next()

                    def emit_s(j, qs=qs, k=kz[t][0], q=q, t_k=kz[t][1], t_q=t_q):
                        pss, t_pss = ps_s.next()
                        S.op("pe", lambda e, pss=pss, j=j: e.matmul(
                            pss[:], k[:, j * 128:(j + 1) * 128], q[:, qs], start=True, stop=True),
                            reads=[t_k, t_q], writes=[t_pss])
                        return pss, t_pss
                    cur = emit_s(0)
                    for j in range(NT):
                        nxt = emit_s(j + 1) if j + 1 < NT else None
                        pss, t_pss = cur
                        p, t_p = pr.next()
                        S.op("act", lambda e, p=p, pss=pss: e.activation(p[:], pss[:], AF.Exp, scale=0.125),
                             reads=[t_pss], writes=[t_p])
                        S.op("pe", lambda e, po=po, v=v, p=p, j=j: e.matmul(po[:], v[:, j, :], p[:], start=(j == 0), stop=(j == NT - 1)),
                             reads=[t_v, t_p], writes=[t_po])
                        acc, t_acc = (accA, t_accA) if j % 2 == 0 else (accB, t_accB)
                        eng = "dve" if j % 2 == 0 else "pool"
                        if j < 2:
                            S.op(eng, lambda e, acc=acc, p=p: e.tensor_copy(acc[:], p[:]), reads=[t_p], writes=[t_acc])
                        else:
                            S.op(eng, lambda e, acc=acc, p=p: e.tensor_tensor(out=acc[:], in0=acc[:], in1=p[:], op=ALU.add),
                                 reads=[t_p, t_acc], writes=[t_acc])
                        cur = nxt
                    S.op("pe", lambda e, pl=pl, accA=accA: e.matmul(pl[:], c.ones_f[:], accA[:], start=True, stop=False),
                         reads=[TC, t_accA], writes=[t_pl])
                    S.op("pe", lambda e, pl=pl, accB=accB: e.matmul(pl[:], c.ones_f[:], accB[:], start=False, stop=True),
                         reads=[TC, t_accB], writes=[t_pl])
                    accs.append((po, t_po, pl, t_pl))
                outs = []
                for t in range(2):
                    po, t_po, pl, t_pl = accs[t]
                    r, t_r = fr.next()
                    S.op("dve", lambda e, r=r, pl=pl: e.reciprocal(r[:], pl[:]), reads=[t_pl], writes=[t_r])
                    a, t_a = fr.next()
                    S.op("dve", lambda e, a=a, po=po, r=r: e.tensor_tensor(out=a[:], in0=po[:], in1=r[:], op=ALU.mult),
                         reads=[t_po, t_r], writes=[t_a])
                    outs.append((a, t_a))
                (a, t_a), (b, t_b) = outs
                oa, t_oa = fr.next()
                S.op("dve", lambda e, oa=oa, a=a, b=b: e.scalar_tensor_tensor(
                    out=oa[:], in0=b[:], scalar=lsc[:, 4:5], in1=a[:], op0=ALU.mult, op1=ALU.add),
                    reads=[t_a, t_b, t_lam], writes=[t_oa])
                sq, t_sq = sqr.next()
                S.op("pool", lambda e, sq=sq, oa=oa: e.tensor_tensor(out=sq[:], in0=oa[:], in1=oa[:], op=ALU.mult),
                     reads=[t_oa], writes=[t_sq])
                px, t_px = ps_x.next()
                S.op("pe", lambda e, px=px, sq=sq: e.matmul(px[:], c.ones_b[:], sq[:], start=True, stop=True),
                     reads=[TC, t_sq], writes=[t_px])
                rs, t_rs = fr.next()
                S.op("act", lambda e, rs=rs, px=px: e.activation(rs[:], px[:], AF.Sqrt, bias=EPS, scale=1.0 / 128), reads=[t_px], writes=[t_rs])
                S.op("dve", lambda e, rs=rs: e.reciprocal(rs[:], rs[:]), reads=[t_rs], writes=[t_rs])
                S.op("dve", lambda e, stg=stg, oa=oa, rs=rs, qs=qs: e.scalar_tensor_tensor(
                    out=stg[:, qs], in0=oa[:], scalar=wsub[:, 1:2], in1=rs[:], op0=ALU.mult, op1=ALU.mult),
                    reads=[t_oa, t_rs, t_wsub], writes=[t_stg])
            S.dma("sp", lambda e, stg=stg, h=h: e.dma_start(out=c.oaT_d[h], in_=stg[:]), reads=[t_stg], writes=[c.t_oa])
    S.barrier()


def phase3(c):
    nc, S = c.nc, c.S
    TC = c.t_const
    NB = NT
    with ExitStack() as st:
        sb = lambda name, shape, dt: st.enter_context(nc.sbuf_tensor(name, shape, dt))
        inc = [sb("p3inc%d" % d, [128, 128], F32) for d in range(2)]
        strm = [sb("p3str%d" % d, [128, 128], F32) for d in range(2)]
        mbias = [sb("p3mb%d" % d, [128, 128], F32) for d in range(2)]
        esel = [sb("p3es%d" % d, [128, 128], F32) for d in range(2)]
        t_m = Tok()
        for d in range(2):
            sgn = 1 if d == 0 else -1
            S.op("pool", lambda e, d=d: e.memset(inc[d][:], 1.0), writes=[t_m])
            S.op("pool", lambda e, d=d, sgn=sgn: e.affine_select(out=inc[d][:], in_=inc[d][:], compare_op=ALU.is_ge, fill=0.0,
                                                                base=0, pattern=[[sgn, 128]], channel_multiplier=-sgn),
                 reads=[t_m], writes=[t_m])
            S.op("pool", lambda e, d=d: e.memset(strm[d][:], 1.0), writes=[t_m])
            S.op("pool", lambda e, d=d, sgn=sgn: e.affine_select(out=strm[d][:], in_=strm[d][:], compare_op=ALU.is_ge, fill=0.0,
                                                                base=-1, pattern=[[sgn, 128]], channel_multiplier=-sgn),
                 reads=[t_m], writes=[t_m])
            S.op("pool", lambda e, d=d: e.tensor_scalar(out=mbias[d][:], in0=inc[d][:], scalar1=30000.0, scalar2=-30000.0,
                                                        op0=ALU.mult, op1=ALU.add), reads=[t_m], writes=[t_m])
            lastp = 127 if d == 0 else 0
            S.op("pool", lambda e, d=d: e.memset(esel[d][:], 0.0), writes=[t_m])
            S.op("pool", lambda e, d=d, lastp=lastp: e.affine_select(out=esel[d][:], in_=esel[d][:], compare_op=ALU.not_equal, fill=1.0,
                                                                    base=-lastp, pattern=[[0, 128]], channel_multiplier=1),
                 reads=[t_m], writes=[t_m])
        sm = sb("p3sm", [128, NT, 32], F32)
        t_sm = Tok()
        S.dma("sp", lambda e: e.dma_start(out=sm[:], in_=c.small_d), reads=[c.t_small], writes=[t_sm])
        gw = sb("p3gw", [128, 128], F32)
        t_gw = Tok()
        S.dma("sp", lambda e: e.dma_start(out=gw[:], in_=c.gdn_norm_w.broadcast_to([128, 128])), writes=[t_gw])

        def bank(name, dt=F32, n=512):
            return st.enter_context(nc.psum_tensor(name, [128, n], dt))
        b0, b2, b3, b4, b5, b6, b7 = [bank("p3b" + x) for x in "0234567"]
        b1 = bank("p3b1", BF16, 1024)
        btok = {id(b): Tok(excl=True) for b in (b0, b1, b2, b3, b4, b5, b6, b7)}
        slot = lambda b, i, w=128: (b[:, i * w:(i + 1) * w], btok[id(b)])
        s_kkp = [slot(b0, 0), slot(b6, 0)]
        s_kqp = [slot(b0, 1), slot(b6, 1)]
        s_gdp = [[slot(b0, 2), slot(b0, 3)], [slot(b6, 2), slot(b6, 3)]]
        s_misc = [slot(b7, 3)] * 4
        s_tr = [slot(b1, i) for i in range(8)]
        s_sqdp = [[(slot(b2, 0), slot(b2, 1)), (slot(b2, 2), slot(b2, 3))], [(slot(b4, 0), slot(b4, 1)), (slot(b4, 2), slot(b4, 3))]]
        s_apdp = [[slot(b3, 0, 256), slot(b3, 1, 256)], [slot(b5, 0, 256), slot(b5, 1, 256)]]
        s_scan = [[slot(b6, i) for i in range(4)], [slot(b7, i) for i in range(4)]]

        kT = sb("p3kT", [128, S_LEN], BF16)
        qT = sb("p3qT", [128, S_LEN], BF16)
        ktok = sb("p3ktok", [128, NB, 128], BF16)
        vtok = sb("p3vtok", [128, NB, 128], BF16)
        zs = sb("p3zs", [128, NB, 128], BF16)
        t_in = Tok()
        ub = [sb("p3ub%d" % d, [128, NB, 128], F32) for d in range(2)]
        nwT = [sb("p3nwT%d" % d, [128, NB, 128], BF16) for d in range(2)]
        kdec = [sb("p3kdec%d" % d, [128, NB, 128], BF16) for d in range(2)]
        qkT = [sb("p3qkT%d" % d, [128, NB, 128], BF16) for d in range(2)]
        t_blk = [[Tok() for _ in range(NB)] for _ in range(2)]
        sc = [sb("p3sc%d" % d, [128, 8, NB], F32) for d in range(2)]
        t_sc = [Tok(), Tok()]
        oacc = sb("p3oacc", [128, NB, 128], F32)
        t_oacc = [Tok() for _ in range(NB)]
        S32 = [sb("p3S32_%d" % d, [128, 128], F32) for d in range(2)]
        Sbf = [sb("p3Sbf_%d" % d, [128, 128], BF16) for d in range(2)]
        t_S = [Tok(), Tok()]
        dtr = _mk_ring(st, nc, "p3dt", 5, [128, 128], F32)
        tmpr = _mk_ring(st, nc, "p3tmp", 5, [128, 128], F32)
        ntr = _mk_ring(st, nc, "p3nt", 10, [128, 128], F32)
        nnr = _mk_ring(st, nc, "p3nn", 10, [128, 128], F32)
        xr = _mk_ring(st, nc, "p3x", 5, [128, 256], F32)
        vnr = _mk_ring(st, nc, "p3vn", 4, [128, 128], BF16)
        o2r = _mk_ring(st, nc, "p3o2", 4, [128, 128], F32)
        big = sb("p3big", [128, NB, 128], F32)
        t_big = Tok()
        red = sb("p3red", [128, NB], F32)
        ogtok = sb("p3ogtok", [128, NB, 128], BF16)
        stage = sb("p3stage", [128, S_LEN], BF16)
        t_stage = Tok()

        stop = c.opts.get("p3_stop", 9)
        for h in range(c.opts.get("p3_heads", H)):
            S.dma("sp", lambda e, h=h: e.dma_start(out=kT[:], in_=c.gk_d[h]), reads=[c.t_gqk], writes=[t_in])
            S.dma("sp", lambda e, h=h: e.dma_start(out=qT[:], in_=c.gq_d[h]), reads=[c.t_gqk], writes=[t_in])
            for (buf, src, tk) in ((ktok, c.gktok_d, c.t_gtok), (vtok, c.gvtok_d, c.t_gtok), (zs, c.zs_d, c.t_vz)):
                S.dma("sp", lambda e, buf=buf, src=src, h=h: e.dma_start(
                    out=buf[:], in_=src[:, h * 128:(h + 1) * 128].rearrange("(t p) e -> p t e", p=128)), reads=[tk], writes=[t_in])
            for d in range(2):
                s_ = sc[d]
                g = sm[:, :, d * 8 + h]
                beta = sm[:, :, 16 + d * 8 + h]
                (pm, t_pm) = s_misc[d * 2]
                S.op("pe", lambda e, pm=pm, d=d, g=g: e.matmul(pm[:, 0:NB], inc[d][:], g, start=True, stop=True),
                     reads=[t_m, t_sm], writes=[t_pm])
                S.op("dve", lambda e, pm=pm, s_=s_: e.tensor_copy(s_[:, 0, :], pm[:, 0:NB]), reads=[t_pm], writes=[t_sc[d]])
                S.op("dve", lambda e, s_=s_: e.tensor_scalar(out=s_[:, 1, :], in0=s_[:, 0, :], scalar1=-1.0, scalar2=None, op0=ALU.mult),
                     reads=[t_sc[d]], writes=[t_sc[d]])
                (pm2, t_pm2) = s_misc[d * 2 + 1]
                S.op("pe", lambda e, pm2=pm2, d=d, s_=s_: e.matmul(pm2[:, 0:NB], esel[d][:], s_[:, 0, :], start=True, stop=True),
                     reads=[t_m, t_sc[d]], writes=[t_pm2])
                S.op("dve", lambda e, pm2=pm2, s_=s_: e.tensor_copy(s_[:, 2, :], pm2[:, 0:NB]), reads=[t_pm2], writes=[t_sc[d]])
                S.op("act", lambda e, s_=s_: e.activation(s_[:, 3, :], s_[:, 0, :], AF.Exp), reads=[t_sc[d]], writes=[t_sc[d]])
                S.op("dve", lambda e, s_=s_: e.tensor_tensor(out=s_[:, 4, :], in0=s_[:, 2, :], in1=s_[:, 0, :], op=ALU.subtract),
                     reads=[t_sc[d]], writes=[t_sc[d]])
                S.op("act", lambda e, s_=s_: e.activation(s_[:, 4, :], s_[:, 4, :], AF.Exp), reads=[t_sc[d]], writes=[t_sc[d]])
                S.op("act", lambda e, s_=s_: e.activation(s_[:, 5, :], s_[:, 2, :], AF.Exp), reads=[t_sc[d]], writes=[t_sc[d]])
                S.op("dve", lambda e, s_=s_, beta=beta: e.tensor_scalar(out=s_[:, 6, :], in0=beta, scalar1=-1.0, scalar2=None, op0=ALU.mult),
                     reads=[t_sm, t_sc[d]], writes=[t_sc[d]])
                S.op("dve", lambda e, s_=s_, beta=beta: e.tensor_copy(s_[:, 7, :], beta), reads=[t_sm, t_sc[d]], writes=[t_sc[d]])

            def par_chain(b, d):
                bs = slice(b * 128, (b + 1) * 128)
                s_ = sc[d]
                par = b % 2
                s_sqd = [s_sqdp[0][par], s_sqdp[1][par]]
                s_apd = [s_apdp[0][par], s_apdp[1][par]]
                (pkk, t_pkk), (pkq, t_pkq), (pgd, t_pgd) = s_kkp[par], s_kqp[par], s_gdp[par][d]
                if d == 0:
                    S.op("pe", lambda e: e.matmul(pkk, kT[:, bs], kT[:, bs], start=True, stop=True), reads=[t_in], writes=[t_pkk])
                    S.op("pe", lambda e: e.matmul(pkq, kT[:, bs], qT[:, bs], start=True, stop=True), reads=[t_in], writes=[t_pkq])
                S.op("pe", lambda e: e.matmul(pgd, s_[:, 0, b:b + 1].broadcast_to([128, 128]), c.ident_f[:], start=True, stop=False),
                     reads=[t_sc[d], TC], writes=[t_pgd])
                S.op("pe", lambda e: e.matmul(pgd, c.ident_f[:], s_[:, 1, b:b + 1].broadcast_to([128, 128]), start=False, stop=False),
                     reads=[t_sc[d], TC], writes=[t_pgd])
                S.op("pe", lambda e: e.matmul(pgd, c.ident_f[:], mbias[d][:], start=False, stop=True), reads=[t_m, TC], writes=[t_pgd])
                yield
                dt_, t_dt = dtr.next()
                S.op("act", lambda e: e.activation(dt_[:], pgd, AF.Exp), reads=[t_pgd], writes=[t_dt])
                tmp, t_tmp = tmpr.next()
                S.op("dve", lambda e: e.scalar_tensor_tensor(out=tmp[:], in0=pkk, scalar=s_[:, 6, b:b + 1], in1=dt_[:], op0=ALU.mult, op1=ALU.mult),
                     reads=[t_pkk, t_sc[d], t_dt], writes=[t_tmp])
                S.op("dve", lambda e: e.tensor_tensor(out=qkT[d][:, b, :], in0=pkq, in1=dt_[:], op=ALU.mult),
                     reads=[t_pkq, t_dt], writes=[t_blk[d][b]])
                nt, t_nt = ntr.next()
                S.op("pool", lambda e, nt=nt: e.tensor_tensor(out=nt[:], in0=tmp[:], in1=strm[d][:], op=ALU.mult), reads=[t_tmp, t_m], writes=[t_nt])
                x, t_x = xr.next()
                S.op("pool", lambda e: e.tensor_copy(x[:, 0:128], vtok[:, b, :]), reads=[t_in], writes=[t_x])
                S.op("pool", lambda e: e.tensor_scalar(out=x[:, 128:256], in0=ktok[:, b, :], scalar1=s_[:, 3, b:b + 1], scalar2=None, op0=ALU.mult),
                     reads=[t_in, t_sc[d]], writes=[t_x])
                S.op("pool", lambda e: e.tensor_scalar(out=kdec[d][:, b, :], in0=ktok[:, b, :], scalar1=s_[:, 4, b:b + 1], scalar2=None, op0=ALU.mult),
                     reads=[t_in, t_sc[d]], writes=[t_blk[d][b]])
                yield
                (ptr, t_ptr) = s_sqd[d][0]
                S.op("pe", lambda e, nt=nt: e.transpose(ptr, nt[:], c.ident_f[:]), reads=[t_nt, TC], writes=[t_ptr])
                yield
                nn, t_nn = nnr.next()
                S.op("act", lambda e, nn=nn: e.copy(nn[:], ptr), reads=[t_ptr], writes=[t_nn])
                yield
                for l in range(7):
                    (pap, t_pap) = s_apd[d]
                    S.op("pe", lambda e, nt=nt: e.matmul(pap, nt[:], x[:], start=True, stop=True), reads=[t_nt, t_x], writes=[t_pap])
                    if l < 6:
                        (pn2, t_pn2), (pnt2, t_pnt2) = s_sqd[d]
                        S.op("pe", lambda e, nt=nt, nn=nn: e.matmul(pn2, nt[:], nn[:], start=True, stop=True), reads=[t_nt, t_nn], writes=[t_pn2])
                        S.op("pe", lambda e, nt=nt, nn=nn: e.matmul(pnt2, nn[:], nt[:], start=True, stop=True), reads=[t_nt, t_nn], writes=[t_pnt2])
                    yield
                    S.op("dve", lambda e: e.tensor_tensor(out=x[:], in0=pap, in1=x[:], op=ALU.add), reads=[t_pap, t_x], writes=[t_x])
                    if l < 6:
                        nn2, t_nn2 = nnr.next()
                        nt2, t_nt2 = ntr.next()
                        S.op("act", lambda e, nn2=nn2: e.copy(nn2[:], pn2), reads=[t_pn2], writes=[t_nn2])
                        S.op("act", lambda e, nt2=nt2: e.copy(nt2[:], pnt2), reads=[t_pnt2], writes=[t_nt2])
                        nn, t_nn, nt, t_nt = nn2, t_nn2, nt2, t_nt2
                    yield
                S.op("dve", lambda e: e.tensor_scalar(out=ub[d][:, b, :], in0=x[:, 0:128], scalar1=s_[:, 7, b:b + 1], scalar2=None, op0=ALU.mult),
                     reads=[t_x, t_sc[d]], writes=[t_blk[d][b]])
                (ptw, t_ptw) = s_sqd[d][1]
                S.op("pe", lambda e: e.transpose(ptw, x[:, 128:256], c.ident_f[:]), reads=[t_x, TC], writes=[t_ptw])
                yield
                S.op("act", lambda e: e.activation(nwT[d][:, b, :], ptw, AF.Copy, scale=-1.0), reads=[t_ptw], writes=[t_blk[d][b]])
                yield

            if stop >= 2:
                _interleave((par_chain(b, d) for b in range(NB) for d in range(2)), 4)

            def scan_chain(d):
                s_ = sc[d]
                S.op("pool", lambda e: e.memset(S32[d][:], 0.0), writes=[t_S[d]])
                S.op("pool", lambda e: e.memset(Sbf[d][:], 0.0), writes=[t_S[d]])
                (pv, t_pv), (po1, t_po1), (po2, t_po2), (pS, t_pS) = s_scan[d]
                for step in range(NB):
                    b = step if d == 0 else NB - 1 - step
                    bs = slice(b * 128, (b + 1) * 128)
                    S.op("pe", lambda e, b=b: e.matmul(pv, nwT[d][:, b, :], Sbf[d][:], start=True, stop=True),
                         reads=[t_blk[d][b], t_S[d]], writes=[t_pv])
                    S.op("pe", lambda e, bs=bs: e.matmul(po1, qT[:, bs], Sbf[d][:], start=True, stop=True), reads=[t_in, t_S[d]], writes=[t_po1])
                    yield
                    vn, t_vn = vnr.next()
                    S.op("dve", lambda e, vn=vn, b=b: e.scalar_tensor_tensor(
                        out=vn[:], in0=pv, scalar=s_[:, 7, b:b + 1], in1=ub[d][:, b, :], op0=ALU.mult, op1=ALU.add),
                        reads=[t_pv, t_sc[d], t_blk[d][b]], writes=[t_vn])
                    yield
                    S.op("pe", lambda e, vn=vn, b=b: e.matmul(pS, kdec[d][:, b, :], vn[:], start=True, stop=True),
                         reads=[t_blk[d][b], t_vn], writes=[t_pS])
                    S.op("pe", lambda e, vn=vn, b=b: e.matmul(po2, qkT[d][:, b, :], vn[:], start=True, stop=True),
                         reads=[t_blk[d][b], t_vn], writes=[t_po2])
                    yield
                    S.op("dve", lambda e, b=b: e.scalar_tensor_tensor(
                        out=S32[d][:], in0=S32[d][:], scalar=s_[:, 5, b:b + 1], in1=pS, op0=ALU.mult, op1=ALU.add),
                        reads=[t_pS, t_sc[d], t_S[d]], writes=[t_S[d]])
                    S.op("pool", lambda e: e.tensor_copy(Sbf[d][:], S32[d][:]), reads=[t_S[d]], writes=[t_S[d]])
                    o2, t_o2 = o2r.next()
                    S.op("act", lambda e, o2=o2: e.copy(o2[:], po2), reads=[t_po2], writes=[t_o2])
                    first = (b < NB // 2) if d == 0 else (b >= NB // 2)
                    if not first:
                        S.op("pool", lambda e, o2=o2, b=b: e.tensor_tensor(out=o2[:], in0=o2[:], in1=oacc[:, b, :], op=ALU.add),
                             reads=[t_o2, t_oacc[b]], writes=[t_o2])
                    S.op("dve", lambda e, o2=o2, b=b: e.scalar_tensor_tensor(
                        out=oacc[:, b, :], in0=po1, scalar=s_[:, 3, b:b + 1], in1=o2[:], op0=ALU.mult, op1=ALU.add),
                        reads=[t_po1, t_sc[d], t_o2], writes=[t_oacc[b]])
                    yield

            if stop >= 3:
                _interleave((scan_chain(d) for d in range(2)), 2)

            S.op("dve", lambda e: e.tensor_tensor(out=big[:], in0=oacc[:], in1=oacc[:], op=ALU.mult), reads=t_oacc, writes=[t_big])
            S.op("dve", lambda e: e.tensor_reduce(out=red[:], in_=big[:], axis=mybir.AxisListType.X, op=ALU.add), reads=[t_big], writes=[t_big])
            S.op("act", lambda e: e.activation(red[:], red[:], AF.Sqrt, bias=EPS, scale=1.0 / 128), reads=[t_big], writes=[t_big])
            S.op("dve", lambda e: e.reciprocal(red[:], red[:]), reads=[t_big], writes=[t_big])
            S.op("dve", lambda e: e.tensor_tensor(out=big[:], in0=oacc[:], in1=red[:].unsqueeze(2).broadcast_to([128, NB, 128]), op=ALU.mult),
                 reads=t_oacc + [t_big], writes=[t_big])
            S.op("pool", lambda e: e.tensor_tensor(out=big[:], in0=big[:], in1=gw[:].unsqueeze(1).broadcast_to([128, NB, 128]), op=ALU.mult),
                 reads=[t_big, t_gw], writes=[t_big])
            S.op("dve", lambda e: e.tensor_tensor(out=ogtok[:], in0=big[:], in1=zs[:], op=ALU.mult), reads=[t_big, t_in], writes=[t_big])
            for i8 in range(4):
                for ii in range(8):
                    b = i8 * 8 + ii
                    (ptr, t_ptr) = s_tr[ii]
                    S.op("pe", lambda e, ptr=ptr, b=b: e.transpose(ptr, ogtok[:, b, :], c.ident_b[:]), reads=[t_big, TC], writes=[t_ptr])
                    S.op("act", lambda e, ptr=ptr, b=b: e.copy(stage[:, b * 128:(b + 1) * 128], ptr), reads=[t_ptr], writes=[t_stage])
            S.dma("sp", lambda e, h=h: e.dma_start(out=c.ogT_d[h], in_=stage[:]), reads=[t_stage], writes=[c.t_og])
    S.barrier()


def phase4(c):
    nc, S = c.nc, c.S
    TC = c.t_const
    with ExitStack() as st:
        sb = lambda name, shape, dt: st.enter_context(nc.sbuf_tensor(name, shape, dt))
        wpa = sb("p4wpa", [128, 8, D], BF16)
        wpg = sb("p4wpg", [128, 8, D], BF16)
        wout = sb("p4wout", [128, 8, D], BF16)
        wr = sb("p4wr", [128, 8, NE], BF16)
        t_w = Tok()
        for (dst, src) in ((wpa, c.w_proj_attn), (wpg, c.w_proj_gdn), (wout, c.w_out)):
            for kc in range(8):
                S.dma("pool", lambda e, dst=dst, src=src, kc=kc: e.dma_start(out=dst[:, kc, :], in_=src[kc * 128:(kc + 1) * 128, :]),
                      writes=[Tok()])
        S.dma("pool", lambda e: e.dma_start(out=wr[:], in_=c.w_router.rearrange("(kc p) n -> p kc n", p=128)), writes=[t_w])
        S.barrier()
        n2w = sb("p4n2w", [128, D], F32)
        t_n2w = Tok()
        S.dma("sp", lambda e: e.dma_start(out=n2w[:], in_=c.norm2_w.broadcast_to([128, D])), writes=[t_n2w])
        aff = sb("p4aff", [128, NT, NE], F32)
        t_aff = Tok()
        oar = _mk_ring(st, nc, "p4oa", 2, [128, 8, 512], BF16)
        ogr = _mk_ring(st, nc, "p4og", 2, [128, 8, 512], BF16)
        sgar = _mk_ring(st, nc, "p4sga", 2, [128, 8, 512], BF16)
        sggr = _mk_ring(st, nc, "p4sgg", 2, [128, 8, 512], BF16)
        mgr = _mk_ring(st, nc, "p4mg", 2, [128, 8, 512], BF16)
        f1 = _mk_ring(st, nc, "p4f1", 2, [128, 512], F32)
        f2 = _mk_ring(st, nc, "p4f2", 2, [128, 512], F32)
        xr = _mk_ring(st, nc, "p4x", 2, [128, D], F32)
        sqr = _mk_ring(st, nc, "p4sq", 2, [128, D], BF16)
        hr = _mk_ring(st, nc, "p4h", 2, [128, D], BF16)
        hTr = _mk_ring(st, nc, "p4hT", 2, [128, 8, 128], BF16)
        ssr = _mk_ring(st, nc, "p4ss", 4, [128, 4], F32)
        lgr = _mk_ring(st, nc, "p4lg", 2, [128, NE], F32)
        psm = _mk_ring(st, nc, "p4psm", 6, [128, 512], F32, psum=True)
        pst = _mk_ring(st, nc, "p4pst", 2, [128, 8, 128], BF16, psum=True)

        for g in range(NG):
            gs = slice(g * 512, (g + 1) * 512)
            oa, t_oa = oar.next()
            og, t_og = ogr.next()
            sga, t_sga = sgar.next()
            sgg, t_sgg = sggr.next()
            for (buf, tk, src, dep) in ((oa, t_oa, c.oaT_d, c.t_oa), (og, t_og, c.ogT_d, c.t_og),
                                        (sga, t_sga, c.sga_d, c.t_gates), (sgg, t_sgg, c.sgg_d, c.t_gates)):
                S.dma("sp", lambda e, buf=buf, src=src, gs=gs: e.dma_start(out=buf[:], in_=src[:, :, gs].rearrange("h p t -> p h t")),
                      reads=[dep], writes=[tk])
            mg, t_mg = mgr.next()
            for dc in range(8):
                ds = slice(dc * 128, (dc + 1) * 128)
                pa, t_pa = psm.next()
                pg, t_pg = psm.next()
                for ec in range(8):
                    S.op("pe", lambda e, pa=pa, ec=ec, ds=ds, oa=oa: e.matmul(pa[:], wpa[:, ec, ds], oa[:, ec, :], start=(ec == 0), stop=(ec == 7)),
                         reads=[t_oa], writes=[t_pa])
                for ec in range(8):
                    S.op("pe", lambda e, pg=pg, ec=ec, ds=ds, og=og: e.matmul(pg[:], wpg[:, ec, ds], og[:, ec, :], start=(ec == 0), stop=(ec == 7)),
                         reads=[t_og], writes=[t_pg])
                a, t_a = f1.next()
                b, t_b = f2.next()
                S.op("dve", lambda e, a=a, pa=pa, sga=sga, dc=dc: e.tensor_tensor(out=a[:], in0=pa[:], in1=sga[:, dc, :], op=ALU.mult),
                     reads=[t_pa, t_sga], writes=[t_a])
                S.op("dve", lambda e, b=b, pg=pg, sgg=sgg, dc=dc: e.tensor_tensor(out=b[:], in0=pg[:], in1=sgg[:, dc, :], op=ALU.mult),
                     reads=[t_pg, t_sgg], writes=[t_b])
                S.op("pool", lambda e, mg=mg, a=a, b=b, dc=dc: e.tensor_tensor(out=mg[:, dc, :], in0=a[:], in1=b[:], op=ALU.add),
                     reads=[t_a, t_b], writes=[t_mg])
            for tl in range(4):
                i = g * 4 + tl
                xt, t_x = xr.next()
                S.dma("sp", lambda e, xt=xt, i=i: e.dma_start(out=xt[:], in_=c.x[i * 128:(i + 1) * 128, :]), writes=[t_x])
                for dh in range(2):
                    po, t_po = psm.next()
                    for dc in range(8):
                        S.op("pe", lambda e, po=po, mg=mg, dc=dc, tl=tl, dh=dh: e.matmul(
                            po[:], mg[:, dc, tl * 128:(tl + 1) * 128], wout[:, dc, dh * 512:(dh + 1) * 512], start=(dc == 0), stop=(dc == 7)),
                            reads=[t_mg], writes=[t_po])
                    S.op("dve", lambda e, xt=xt, po=po, dh=dh: e.tensor_tensor(out=xt[:, dh * 512:(dh + 1) * 512], in0=po[:],
                                                                              in1=xt[:, dh * 512:(dh + 1) * 512], op=ALU.add),
                         reads=[t_po, t_x], writes=[t_x])
                S.dma("sp", lambda e, xt=xt, i=i: e.dma_start(out=c.x1_d[i * 128:(i + 1) * 128, :], in_=xt[:]), reads=[t_x], writes=[c.t_x1])
                sq, t_sq = sqr.next()
                ss, t_ss = ssr.next()
                S.op("act", lambda e, xt=xt, sq=sq, ss=ss: e.activation(sq[:], xt[:], AF.Square, accum_out=ss[:, 0:1]),
                     reads=[t_x], writes=[t_sq, t_ss])
                S.op("act", lambda e, ss=ss: e.activation(ss[:, 1:2], ss[:, 0:1], AF.Sqrt, bias=EPS, scale=1.0 / D), reads=[t_ss], writes=[t_ss])
                S.op("dve", lambda e, ss=ss: e.reciprocal(ss[:, 1:2], ss[:, 1:2]), reads=[t_ss], writes=[t_ss])
                ht, t_h = hr.next()
                S.op("dve", lambda e, ht=ht, xt=xt, ss=ss: e.scalar_tensor_tensor(
                    out=ht[:], in0=xt[:], scalar=ss[:, 1:2], in1=n2w[:], op0=ALU.mult, op1=ALU.mult),
                    reads=[t_x, t_ss, t_n2w], writes=[t_h])
                S.dma("sp", lambda e, ht=ht, i=i: e.dma_start(out=c.h2tok_d[i * 128:(i + 1) * 128, :], in_=ht[:]), reads=[t_h], writes=[c.t_h2])
                pt, t_pt = pst.next()
                for dc in range(8):
                    S.op("pe", lambda e, pt=pt, ht=ht, dc=dc: e.transpose(pt[:, dc, :], ht[:, dc * 128:(dc + 1) * 128], c.ident_b[:]),
                         reads=[t_h, TC], writes=[t_pt])
                hT, t_hT = hTr.next()
                S.op("act", lambda e, hT=hT, pt=pt: e.copy(hT[:], pt[:]), reads=[t_pt], writes=[t_hT])
                pl, t_pl = psm.next()
                for dc in range(8):
                    S.op("pe", lambda e, pl=pl, hT=hT, dc=dc: e.matmul(pl[:, 0:NE], hT[:, dc, :], wr[:, dc, :], start=(dc == 0), stop=(dc == 7)),
                         reads=[t_hT, t_w], writes=[t_pl])
                lg, t_lg = lgr.next()
                S.op("dve", lambda e, pl=pl, ss=ss: e.tensor_reduce(out=ss[:, 2:3], in_=pl[:, 0:NE], axis=mybir.AxisListType.X, op=ALU.max),
                     reads=[t_pl, t_ss], writes=[t_ss])
                S.op("dve", lambda e, ss=ss: e.tensor_scalar(out=ss[:, 2:3], in0=ss[:, 2:3], scalar1=-1.0, scalar2=None, op0=ALU.mult),
                     reads=[t_ss], writes=[t_ss])
                S.op("act", lambda e, lg=lg, pl=pl, ss=ss: e.activation(lg[:], pl[:, 0:NE], AF.Exp, bias=ss[:, 2:3], accum_out=ss[:, 3:4]),
                     reads=[t_pl, t_ss], writes=[t_lg, t_ss])
                S.op("dve", lambda e, ss=ss: e.reciprocal(ss[:, 3:4], ss[:, 3:4]), reads=[t_ss], writes=[t_ss])
                S.op("dve", lambda e, lg=lg, ss=ss, i=i: e.tensor_scalar(out=aff[:, i, :], in0=lg[:], scalar1=ss[:, 3:4], scalar2=None, op0=ALU.mult),
                     reads=[t_lg, t_ss], writes=[t_aff])
        S.dma("sp", lambda e: e.dma_start(out=c.aff_d, in_=aff[:]), reads=[t_aff], writes=[c.t_aff])
    S.barrier()


def phase5(c):
    nc, S = c.nc, c.S
    TC = c.t_const
    with ExitStack() as st:
        sb = lambda name, shape, dt: st.enter_context(nc.sbuf_tensor(name, shape, dt))
        aff = sb("p5aff", [128, NT, NE], F32)
        t_aff = Tok()
        S.dma("sp", lambda e: e.dma_start(out=aff[:], in_=c.aff_d), reads=[c.t_aff], writes=[t_aff])
        affT = sb("p5affT", [NE, S_LEN], F32)
        work = sb("p5work", [NE, S_LEN], F32)
        ones = sb("p5ones", [NE, S_LEN], F32)
        mx = sb("p5mx", [NE, 8], F32)
        t_affT, t_work, t_mx, t_ones = Tok(), Tok(), Tok(), Tok()
        psm = _mk_ring(st, nc, "p5psm", 4, [128, 512], F32, psum=True)
        for i4 in range(NT // 4):
            ps, t_ps = psm.next()
            for ii in range(4):
                i = i4 * 4 + ii
                S.op("pe", lambda e, ps=ps, ii=ii, i=i: e.transpose(ps[0:NE, ii * 128:(ii + 1) * 128], aff[:, i, :], c.ident_f[:]),
                     reads=[t_aff, TC], writes=[t_ps])
            S.op("act", lambda e, ps=ps, i4=i4: e.copy(affT[:, i4 * 512:(i4 + 1) * 512], ps[0:NE, :]), reads=[t_ps], writes=[t_affT])
        S.op("pool", lambda e: e.memset(ones[:], 1.0), writes=[t_ones])
        src = affT
        for it in range(CAP // 8):
            S.op("dve", lambda e, src=src: e.max(out=mx[:], in_=src[:]), reads=[t_affT, t_work], writes=[t_mx])
            if it < CAP // 8 - 1:
                S.op("dve", lambda e, src=src: e.match_replace(out=work[:], in_to_replace=mx[:], in_values=src[:], imm_value=-1.0),
                     reads=[t_mx, t_affT, t_work], writes=[t_work])
            src = work
        S.op("dve", lambda e: e.tensor_scalar(out=work[:], in0=affT[:], scalar1=mx[:, 7:8], scalar2=None, op0=ALU.is_ge),
             reads=[t_affT, t_mx, t_work], writes=[t_work])
        S.op("dve", lambda e: e.tensor_tensor_scan(out=affT[:], data0=ones[:], data1=work[:], initial=0.0, op0=ALU.mult, op1=ALU.add),
             reads=[t_work, t_ones, t_affT], writes=[t_affT])
        S.op("dve", lambda e: e.tensor_tensor(out=affT[:], in0=affT[:], in1=work[:], op=ALU.mult), reads=[t_work, t_affT], writes=[t_affT])
        S.op("dve", lambda e: e.tensor_scalar(out=affT[:], in0=affT[:], scalar1=-1.0, scalar2=None, op0=ALU.add), reads=[t_affT], writes=[t_affT])
        S.dma("sp", lambda e: e.dma_start(out=c.pos_d, in_=affT[:]), reads=[t_affT], writes=[c.t_pos])
        ptok = sb("p5ptok", [128, NT, NE], F32)
        t_ptok = Tok()
        for i4 in range(NT // 4):
            ps, t_ps = psm.next()
            for ii in range(4):
                i = i4 * 4 + ii
                S.op("pe", lambda e, ps=ps, ii=ii, i=i: e.transpose(ps[:, ii * NE:(ii + 1) * NE], affT[:, i * 128:(i + 1) * 128], c.ident_f[0:NE, 0:NE]),
                     reads=[t_affT, TC], writes=[t_ps])
            S.op("act", lambda e, ps=ps, i4=i4: e.copy(ptok[:, i4 * 4:(i4 + 1) * 4, :], ps[:, 0:4 * NE].rearrange("p (a b) -> p a b", b=NE)),
                 reads=[t_ps], writes=[t_ptok])
        S.dma("sp", lambda e: e.dma_start(out=c.ptok_d, in_=ptok[:]), reads=[t_ptok], writes=[c.t_pos])
    S.barrier()


def phase6a(c):
    nc, S = c.nc, c.S
    TC = c.t_const
    with ExitStack() as st:
        sb = lambda name, shape, dt: st.enter_context(nc.sbuf_tensor(name, shape, dt))
        ptok = sb("p6ptok", [128, NT, NE], F32)
        t_ptok = Tok()
        S.dma("sp", lambda e: e.dma_start(out=ptok[:], in_=c.ptok_d), reads=[c.t_pos], writes=[t_ptok])
        iota = sb("p6iota", [128, CAP], F32)
        t_iota = Tok()
        S.op("pool", lambda e: e.iota(iota[:], pattern=[[1, CAP]], base=0, channel_multiplier=0, allow_small_or_imprecise_dtypes=True),
             writes=[t_iota])
        sel = sb("p6sel", [128, NT, CAP], BF16)
        t_sel = Tok()
        xsT = sb("p6xsT", [128, 8, CAP], BF16)
        t_xsT = Tok()
        actT = sb("p6actT", [128, 16, CAP], BF16)
        t_actT = Tok()
        yes = sb("p6ye", [128, 4, D], BF16)
        t_yes = Tok()
        h2r = _mk_ring(st, nc, "p6h2", 2, [128, 4, D], BF16)
        wgr = _mk_ring(st, nc, "p6wg", 2, [128, 8, 1024], BF16)
        wur = _mk_ring(st, nc, "p6wu", 2, [128, 8, 1024], BF16)
        wdr = _mk_ring(st, nc, "p6wd", 2, [128, 8, D], BF16)
        gfr = _mk_ring(st, nc, "p6gf", 2, [128, CAP], F32)
        wst = _mk_ring(st, nc, "p6wst", 4, [128, 1024], F32)
        psm = _mk_ring(st, nc, "p6psm", 8, [128, 512], F32, psum=True)

        def load_gu(ex):
            bufs = []
            for fh in range(2):
                wg, t_wg = wgr.next()
                wu, t_wu = wur.next()
                for kc in range(8):
                    for (wdst, t_wdst, wsrc) in ((wg, t_wg, c.w_gate), (wu, t_wu, c.w_up)):
                        stg, t_stg = wst.next()
                        S.dma("sp", lambda e, stg=stg, wsrc=wsrc, kc=kc, ex=ex, fh=fh: e.dma_start(
                            out=stg[:], in_=wsrc[ex, kc * 128:(kc + 1) * 128, fh * 1024:(fh + 1) * 1024]), writes=[t_stg])
                        S.op("act", lambda e, stg=stg, wdst=wdst, kc=kc: e.copy(wdst[:, kc, :], stg[:]), reads=[t_stg], writes=[t_wdst])
                bufs.append(((wg, t_wg), (wu, t_wu)))
            return bufs

        def load_wd(ex):
            bufs = []
            for fh in range(2):
                wd, t_wd = wdr.next()
                for fl in range(8):
                    fc = fh * 8 + fl
                    stg, t_stg = wst.next()
                    S.dma("sp", lambda e, stg=stg, fc=fc, ex=ex: e.dma_start(out=stg[:], in_=c.w_down[ex, fc * 128:(fc + 1) * 128, :]),
                          writes=[t_stg])
                    S.op("act", lambda e, stg=stg, wd=wd, fl=fl: e.copy(wd[:, fl, :], stg[:]), reads=[t_stg], writes=[t_wd])
                bufs.append((wd, t_wd))
            return bufs

        gu_bufs = None
        for ex in range(NE):
            if ex == 0:
                gu_bufs = load_gu(0)
            wd_bufs = load_wd(ex)
            for i in range(NT):
                eng = "dve" if i % 2 == 0 else "pool"
                S.op(eng, lambda e, i=i, ex=ex: e.tensor_scalar(out=sel[:, i, :], in0=iota[:], scalar1=ptok[:, i, ex:ex + 1], scalar2=None,
                                                                 op0=ALU.is_equal), reads=[t_iota, t_ptok], writes=[t_sel])
            for half in range(2):
                accs = [psm.next() for _ in range(4)]
                for i4 in range(NT // 4):
                    ht, t_h = h2r.next()
                    S.dma("sp", lambda e, ht=ht, i4=i4: e.dma_start(
                        out=ht[:], in_=c.h2tok_d[i4 * 512:(i4 + 1) * 512, :].rearrange("(a p) d -> p a d", p=128)), reads=[c.t_h2], writes=[t_h])
                    for ii in range(4):
                        i = i4 * 4 + ii
                        for dl in range(4):
                            dc = half * 4 + dl
                            ps, t_ps = accs[dl]
                            S.op("pe", lambda e, ps=ps, ht=ht, dc=dc, i=i, ii=ii: e.matmul(ps[:], ht[:, ii, dc * 128:(dc + 1) * 128], sel[:, i, :],
                                                                                          start=(i == 0), stop=(i == NT - 1)),
                                 reads=[t_h, t_sel], writes=[t_ps])
                for dl in range(4):
                    dc = half * 4 + dl
                    ps, t_ps = accs[dl]
                    S.op("act", lambda e, ps=ps, dc=dc: e.copy(xsT[:, dc, :], ps[:]), reads=[t_ps], writes=[t_xsT])
            for fh in range(2):
                (wg, t_wg), (wu, t_wu) = gu_bufs[fh]
                for fl in range(8):
                    fc = fh * 8 + fl
                    fs = slice(fl * 128, (fl + 1) * 128)
                    pg, t_pg = psm.next()
                    pu, t_pu = psm.next()
                    for kc in range(8):
                        S.op("pe", lambda e, pg=pg, wg=wg, kc=kc, fs=fs: e.matmul(pg[:], wg[:, kc, fs], xsT[:, kc, :], start=(kc == 0), stop=(kc == 7)),
                             reads=[t_wg, t_xsT], writes=[t_pg])
                    for kc in range(8):
                        S.op("pe", lambda e, pu=pu, wu=wu, kc=kc, fs=fs: e.matmul(pu[:], wu[:, kc, fs], xsT[:, kc, :], start=(kc == 0), stop=(kc == 7)),
                             reads=[t_wu, t_xsT], writes=[t_pu])
                    gf, t_gf = gfr.next()
                    S.op("act", lambda e, gf=gf, pg=pg: e.activation(gf[:], pg[:], AF.Silu), reads=[t_pg], writes=[t_gf])
                    S.op("dve", lambda e, gf=gf, pu=pu, fc=fc: e.tensor_tensor(out=actT[:, fc, :], in0=pu[:], in1=gf[:], op=ALU.mult),
                         reads=[t_pu, t_gf], writes=[t_actT])
            if ex + 1 < NE:
                gu_bufs = load_gu(ex + 1)
            accs = [psm.next() for _ in range(8)]
            for fh in range(2):
                wd, t_wd = wd_bufs[fh]
                for fl in range(8):
                    fc = fh * 8 + fl
                    for sc in range(4):
                        for dh in range(2):
                            ps, t_ps = accs[sc * 2 + dh]
                            S.op("pe", lambda e, ps=ps, fc=fc, sc=sc, wd=wd, fl=fl, dh=dh: e.matmul(
                                ps[:], actT[:, fc, sc * 128:(sc + 1) * 128], wd[:, fl, dh * 512:(dh + 1) * 512], start=(fc == 0), stop=(fc == 15)),
                                reads=[t_actT, t_wd], writes=[t_ps])
            for sc in range(4):
                for dh in range(2):
                    ps, t_ps = accs[sc * 2 + dh]
                    eng = "act" if dh == 0 else "dve"
                    if eng == "act":
                        S.op("act", lambda e, ps=ps, sc=sc, dh=dh: e.copy(yes[:, sc, dh * 512:(dh + 1) * 512], ps[:]), reads=[t_ps], writes=[t_yes])
                    else:
                        S.op("dve", lambda e, ps=ps, sc=sc, dh=dh: e.tensor_copy(yes[:, sc, dh * 512:(dh + 1) * 512], ps[:]), reads=[t_ps], writes=[t_yes])
            S.dma("sp", lambda e, ex=ex: e.dma_start(out=c.ye_d[ex].rearrange("(sc p) d -> p sc d", p=128), in_=yes[:]),
                  reads=[t_yes], writes=[c.t_ye])
    S.barrier()


def phase6b(c):
    nc, S = c.nc, c.S
    TC = c.t_const
    with ExitStack() as st:
        sb = lambda name, shape, dt: st.enter_context(nc.sbuf_tensor(name, shape, dt))
        yall = sb("p7ye", [128, NE, 4, D], BF16)
        t_yall = Tok()
        for ex in range(NE):
            S.dma("sp", lambda e, ex=ex: e.dma_start(out=yall[:, ex, :, :], in_=c.ye_d[ex].rearrange("(sc p) d -> p sc d", p=128)),
                  reads=[c.t_ye], writes=[Tok()])
        S.barrier()
        aff = sb("p7aff", [128, NT, NE], F32)
        t_aff = Tok()
        S.dma("sp", lambda e: e.dma_start(out=aff[:], in_=c.aff_d), reads=[c.t_aff], writes=[t_aff])
        nfw = sb("p7nfw", [128, D], F32)
        t_nfw = Tok()
        S.dma("sp", lambda e: e.dma_start(out=nfw[:], in_=c.norm_f_w.broadcast_to([128, D])), writes=[t_nfw])
        pidx = sb("p7pidx", [128, 4], F32)
        t_pidx = Tok()
        S.op("pool", lambda e: e.iota(pidx[:], pattern=[[128, 4]], base=0, channel_multiplier=1, allow_small_or_imprecise_dtypes=True),
             writes=[t_pidx])
        posbc = sb("p7posbc", [128, NE, 512], F32)
        t_posbc = Tok()
        selT = _mk_ring(st, nc, "p7selT", 8, [128, 512], BF16)
        accr = _mk_ring(st, nc, "p7acc", 4, [128, D], F32)
        sqr = _mk_ring(st, nc, "p7sq", 2, [128, D], BF16)
        ssr = _mk_ring(st, nc, "p7ss", 4, [128, 2], F32)
        psm = _mk_ring(st, nc, "p7psm", 6, [128, 512], F32, psum=True)
        for g in range(NG):
            gs = slice(g * 512, (g + 1) * 512)
            S.dma("sp", lambda e, gs=gs: e.dma_start(out=posbc[:], in_=c.pos_d[:, gs].unsqueeze(0).broadcast_to([128, NE, 512])),
                  reads=[c.t_pos], writes=[t_posbc])
            accs = []
            for tl in range(4):
                i = g * 4 + tl
                acc, t_acc = accr.next()
                S.dma("sp", lambda e, acc=acc, i=i: e.dma_start(out=acc[:], in_=c.x1_d[i * 128:(i + 1) * 128, :]), reads=[c.t_x1], writes=[t_acc])
                accs.append((acc, t_acc))
            def build_sts(ex):
                sts = []
                for sc in range(4):
                    sT, t_sT = selT.next()
                    eng = "dve" if sc % 2 == 0 else "pool"
                    S.op(eng, lambda e, sT=sT, ex=ex, sc=sc: e.tensor_scalar(out=sT[:], in0=posbc[:, ex, :], scalar1=pidx[:, sc:sc + 1], scalar2=None,
                                                                            op0=ALU.is_equal), reads=[t_posbc, t_pidx], writes=[t_sT])
                    sts.append((sT, t_sT))
                return sts
            nxt_sts = build_sts(0)
            for ex in range(NE):
                sts = nxt_sts
                if ex + 1 < NE:
                    nxt_sts = build_sts(ex + 1)
                for tl in range(4):
                    i = g * 4 + tl
                    acc, t_acc = accs[tl]
                    for dh in range(2):
                        ps, t_ps = psm.next()
                        for sc in range(4):
                            sT, t_sT = sts[sc]
                            S.op("pe", lambda e, ps=ps, sT=sT, tl=tl, ex=ex, sc=sc, dh=dh: e.matmul(
                                ps[:], sT[:, tl * 128:(tl + 1) * 128], yall[:, ex, sc, dh * 512:(dh + 1) * 512], start=(sc == 0), stop=(sc == 3)),
                                reads=[t_sT], writes=[t_ps])
                        S.op("dve", lambda e, acc=acc, ps=ps, i=i, ex=ex, dh=dh: e.scalar_tensor_tensor(
                            out=acc[:, dh * 512:(dh + 1) * 512], in0=ps[:], scalar=aff[:, i, ex:ex + 1], in1=acc[:, dh * 512:(dh + 1) * 512],
                            op0=ALU.mult, op1=ALU.add), reads=[t_ps, t_aff, t_acc], writes=[t_acc])
            for tl in range(4):
                i = g * 4 + tl
                acc, t_acc = accs[tl]
                sq, t_sq = sqr.next()
                ss, t_ss = ssr.next()
                S.op("act", lambda e, acc=acc, sq=sq, ss=ss: e.activation(sq[:], acc[:], AF.Square, accum_out=ss[:, 0:1]),
                     reads=[t_acc], writes=[t_sq, t_ss])
                S.op("act", lambda e, ss=ss: e.activation(ss[:, 1:2], ss[:, 0:1], AF.Sqrt, bias=EPS, scale=1.0 / D), reads=[t_ss], writes=[t_ss])
                S.op("dve", lambda e, ss=ss: e.reciprocal(ss[:, 1:2], ss[:, 1:2]), reads=[t_ss], writes=[t_ss])
                S.op("dve", lambda e, acc=acc, ss=ss: e.scalar_tensor_tensor(
                    out=acc[:], in0=acc[:], scalar=ss[:, 1:2], in1=nfw[:], op0=ALU.mult, op1=ALU.mult),
                    reads=[t_acc, t_ss, t_nfw], writes=[t_acc])
                S.dma("sp", lambda e, acc=acc, i=i: e.dma_start(out=c.out[i * 128:(i + 1) * 128, :], in_=acc[:]), reads=[t_acc], writes=[c.t_out])
    S.barrier()


ALL_PHASES = ("p1", "p2", "p3", "p4", "p5", "p6a", "p6b")


def kernel(**inputs):
    n_cores = 8
    shared = host_prep(inputs)
    nc, c = build_program(debug=False, phases=ALL_PHASES)
    x = np.asarray(inputs["x"], dtype=np.float32)
    in_maps = []
    for b in range(n_cores):
        m = dict(shared)
        m["x"] = np.ascontiguousarray(x[b])
        in_maps.append({k: m[k] for k in c.input_names})
    res = run_bass_kernel_spmd(nc, in_maps, core_ids=list(range(n_cores)))
    out = np.stack([np.asarray(r["out"], dtype=np.float32) for r in res.results], axis=0)
    return out
```
